# Optimizing a Trainium2 kernel written in Bass

```python
import math
import jax, jax.numpy as jnp
from jax import lax
import numpy as np

D_MODEL = 2048
BATCH = 2
SEQ = 4096
DEPTH = 1

HEAD_DIM = 128
N_MOBA_HEADS = 8
N_FOX_HEADS = 8
MOBA_WIDTH = N_MOBA_HEADS * HEAD_DIM
FOX_WIDTH = N_FOX_HEADS * HEAD_DIM
MOBA_BLOCK = 256
MOBA_TOP_K = 3
MOBA_Q_CHUNK = 32
FOX_Q_BLOCK = 128
ROPE_THETA = 500000.0
ROPE_DIM = HEAD_DIM // 4
D_FF = 4 * D_MODEL
N_BRANCHES = 2
RMS_EPS = 1e-6
NEG_BIG = -1e30
IN_SPLITS = [MOBA_WIDTH, MOBA_WIDTH, MOBA_WIDTH, FOX_WIDTH, FOX_WIDTH, FOX_WIDTH, N_FOX_HEADS, D_MODEL, D_MODEL]
IN_COLS = sum(IN_SPLITS)

kernel_name = "hybrid_moba_fox_gated_block"


def rmsnorm(x, g):
    xf = x.astype(jnp.float32)
    y = xf * lax.rsqrt(jnp.mean(xf * xf, axis=-1, keepdims=True) + RMS_EPS)
    return (y * g.astype(jnp.float32)).astype(x.dtype)


def partial_rotary(t):
    S = t.shape[2]
    half = ROPE_DIM // 2
    inv_freq = ROPE_THETA ** (-jnp.arange(0, ROPE_DIM, 2, dtype=jnp.float32) / ROPE_DIM)
    ang = jnp.arange(S, dtype=jnp.float32)[:, None] * inv_freq[None, :]
    cos, sin = jnp.cos(ang), jnp.sin(ang)
    tf = t.astype(jnp.float32)
    x1, x2, rest = tf[..., :half], tf[..., half:ROPE_DIM], tf[..., ROPE_DIM:]
    out = jnp.concatenate([x1 * cos - x2 * sin, x2 * cos + x1 * sin, rest], axis=-1)
    return out.astype(t.dtype)


def split_heads(t, n_heads):
    B, S, _ = t.shape
    return t.reshape(B, S, n_heads, HEAD_DIM).transpose(0, 2, 1, 3)


def merge_heads(t):
    B, H, S, D = t.shape
    return t.transpose(0, 2, 1, 3).reshape(B, S, H * D)


def moba_attention(q, k, v):
    B, H, S, dh = q.shape
    nb = -(-S // MOBA_BLOCK)
    pad = nb * MOBA_BLOCK - S
    kp = jnp.pad(k, ((0, 0), (0, 0), (0, pad), (0, 0)))
    vp = jnp.pad(v, ((0, 0), (0, 0), (0, pad), (0, 0)))
    kb = kp.reshape(B, H, nb, MOBA_BLOCK, dh)
    vb = vp.reshape(B, H, nb, MOBA_BLOCK, dh)
    kmean = jnp.mean(kb.astype(jnp.float32), axis=3)
    n_sel = min(MOBA_TOP_K, nb)
    scale = dh ** -0.5
    n_chunks = S // MOBA_Q_CHUNK
    qc = q.reshape(B, H, n_chunks, MOBA_Q_CHUNK, dh).transpose(2, 0, 1, 3, 4)
    bi = jnp.arange(B)[:, None, None, None]
    hi = jnp.arange(H)[None, :, None, None]
    block_ids = jnp.arange(nb)
    own_offsets = jnp.arange(MOBA_BLOCK)

    def chunk_fn(args):
        c, qi = args
        q_pos = c * MOBA_Q_CHUNK + jnp.arange(MOBA_Q_CHUNK)
        blk = (c * MOBA_Q_CHUNK) // MOBA_BLOCK
        gate = jnp.einsum('bhqd,bhnd->bhqn', qi.astype(jnp.float32), kmean)
        gate = jnp.where(block_ids < blk, gate, -jnp.inf)
        _, idx = lax.top_k(gate, n_sel)
        sel_valid = idx < blk
        k_sel = kb[bi, hi, idx]
        v_sel = vb[bi, hi, idx]
        s_sel = jnp.einsum('bhqd,bhqnkd->bhqnk', qi, k_sel, preferred_element_type=jnp.float32) * scale
        s_sel = jnp.where(sel_valid[..., None], s_sel, NEG_BIG)
        k_own = lax.dynamic_index_in_dim(kb, blk, axis=2, keepdims=False)
        v_own = lax.dynamic_index_in_dim(vb, blk, axis=2, keepdims=False)
        s_own = jnp.einsum('bhqd,bhkd->bhqk', qi, k_own, preferred_element_type=jnp.float32) * scale
        key_pos = blk * MOBA_BLOCK + own_offsets
        s_own = jnp.where(key_pos[None, :] <= q_pos[:, None], s_own, NEG_BIG)
        logits = jnp.concatenate([s_sel.reshape(B, H, MOBA_Q_CHUNK, n_sel * MOBA_BLOCK), s_own], axis=-1)
        p = jax.nn.softmax(logits, axis=-1).astype(v.dtype)
        p_sel = p[..., :n_sel * MOBA_BLOCK].reshape(B, H, MOBA_Q_CHUNK, n_sel, MOBA_BLOCK)
        p_own = p[..., n_sel * MOBA_BLOCK:]
        return (jnp.einsum('bhqnk,bhqnkd->bhqd', p_sel, v_sel)
                + jnp.einsum('bhqk,bhkd->bhqd', p_own, v_own))

    out = lax.map(chunk_fn, (jnp.arange(n_chunks), qc))
    return out.transpose(1, 2, 0, 3, 4).reshape(B, H, S, dh)


def fox_attention(q, k, v, log_f):
    B, H, S, dh = q.shape
    F = jnp.cumsum(log_f, axis=-1)
    scale = dh ** -0.5
    n_blocks = S // FOX_Q_BLOCK
    qb = q.reshape(B, H, n_blocks, FOX_Q_BLOCK, dh).transpose(2, 0, 1, 3, 4)
    Fq = F.reshape(B, H, n_blocks, FOX_Q_BLOCK).transpose(2, 0, 1, 3)
    key_pos = jnp.arange(S)

    def block_fn(args):
        c, qi, fi = args
        q_pos = c * FOX_Q_BLOCK + jnp.arange(FOX_Q_BLOCK)
        s = jnp.einsum('bhqd,bhkd->bhqk', qi, k, preferred_element_type=jnp.float32) * scale
        s = s + fi[..., :, None] - F[:, :, None, :]
        s = jnp.where(key_pos[None, :] <= q_pos[:, None], s, NEG_BIG)
        p = jax.nn.softmax(s, axis=-1).astype(v.dtype)
        return jnp.einsum('bhqk,bhkd->bhqd', p, v)

    out = lax.map(block_fn, (jnp.arange(n_blocks), qb, Fq))
    return out.transpose(1, 2, 0, 3, 4).reshape(B, H, S, dh)


def setup_inputs(seed: int = 0) -> dict:
    key = jax.random.key(seed)
    ks = jax.random.split(key, 13)
    f32 = jnp.float32
    x = jax.random.normal(ks[0], (BATCH, SEQ, D_MODEL), f32)
    w_in = jax.random.normal(ks[1], (DEPTH, D_MODEL, IN_COLS), f32) * D_MODEL ** -0.5
    b_forget = 3.0 + 0.1 * jax.random.normal(ks[2], (DEPTH, N_FOX_HEADS), f32)
    w_branch_moba = jax.random.normal(ks[3], (DEPTH, MOBA_WIDTH, D_MODEL), f32) * MOBA_WIDTH ** -0.5
    w_branch_fox = jax.random.normal(ks[4], (DEPTH, FOX_WIDTH, D_MODEL), f32) * FOX_WIDTH ** -0.5
    w_out = jax.random.normal(ks[5], (DEPTH, D_MODEL, D_MODEL), f32) * D_MODEL ** -0.5
    g_mix_pre = 1.0 + 0.05 * jax.random.normal(ks[6], (DEPTH, D_MODEL), f32)
    g_mix_post = 1.0 + 0.05 * jax.random.normal(ks[7], (DEPTH, D_MODEL), f32)
    w_up = jax.random.normal(ks[8], (DEPTH, D_MODEL, D_FF), f32) * D_MODEL ** -0.5
    w_down = jax.random.normal(ks[9], (DEPTH, D_FF, D_MODEL), f32) * D_FF ** -0.5
    g_mlp_pre = 1.0 + 0.05 * jax.random.normal(ks[10], (DEPTH, D_MODEL), f32)
    g_mlp_post = 1.0 + 0.05 * jax.random.normal(ks[11], (DEPTH, D_MODEL), f32)
    return {"x": x, "w_in": w_in, "b_forget": b_forget, "w_branch_moba": w_branch_moba,
            "w_branch_fox": w_branch_fox, "w_out": w_out, "g_mix_pre": g_mix_pre,
            "g_mix_post": g_mix_post, "w_up": w_up, "w_down": w_down,
            "g_mlp_pre": g_mlp_pre, "g_mlp_post": g_mlp_post}


def reference(x, w_in, b_forget, w_branch_moba, w_branch_fox, w_out, g_mix_pre, g_mix_post,
              w_up, w_down, g_mlp_pre, g_mlp_post):
    split_points = list(np.cumsum(IN_SPLITS)[:-1])
    for l in range(DEPTH):
        h = rmsnorm(x, g_mix_pre[l])
        proj = jnp.einsum('bsd,dc->bsc', h, w_in[l])
        mq, mk, mv, fq, fk, fv, ff, ga, gb = jnp.split(proj, split_points, axis=-1)
        mq = partial_rotary(split_heads(mq, N_MOBA_HEADS))
        mk = partial_rotary(split_heads(mk, N_MOBA_HEADS))
        o_moba = merge_heads(moba_attention(mq, mk, split_heads(mv, N_MOBA_HEADS)))
        log_f = jax.nn.log_sigmoid((ff + b_forget[l]).astype(jnp.float32)).transpose(0, 2, 1)
        o_fox = merge_heads(fox_attention(split_heads(fq, N_FOX_HEADS), split_heads(fk, N_FOX_HEADS),
                                          split_heads(fv, N_FOX_HEADS), log_f))
        y_moba = jnp.einsum('bsc,cd->bsd', o_moba, w_branch_moba[l])
        y_fox = jnp.einsum('bsc,cd->bsd', o_fox, w_branch_fox[l])
        merged = jax.nn.sigmoid(ga) * y_moba + jax.nn.sigmoid(gb) * y_fox
        mixed = jnp.einsum('bsd,de->bse', merged, w_out[l])
        x = x + rmsnorm(mixed, g_mix_post[l])
        h = rmsnorm(x, g_mlp_pre[l])
        u = jnp.einsum('bsd,df->bsf', h, w_up[l])
        m = jnp.einsum('bsf,fd->bsd', jnp.square(jax.nn.relu(u)), w_down[l])
        x = x + rmsnorm(m, g_mlp_post[l])
    return x
```

```python
import contextlib
import numpy as np
import concourse.bass as bass
import concourse.mybir as mybir
from concourse.bass_utils import run_bass_kernel_spmd

F32 = mybir.dt.float32
BF16 = mybir.dt.bfloat16
AF = mybir.ActivationFunctionType
ALU = mybir.AluOpType
AX = mybir.AxisListType

ENGS = ("pe", "act", "dve", "pool", "sp")
N_DMA_SEMS = 24
N_HW_SEMS = 16
OP_LIMIT = None
RMS_EPS = 1e-6
ROPE_THETA = 500000.0
NEG_MASK = -30000.0


class _Op:
    __slots__ = ("eng", "fn", "is_dma", "signal", "sigval", "dsem", "dval", "deps", "waits")


class SemState:
    def __init__(self, nc, stack):
        self.sems = {}
        for e in ("pe", "act", "dve", "pool"):
            self.sems[("eng", e)] = stack.enter_context(nc.semaphore("s_" + e))
        for i in range(N_DMA_SEMS):
            self.sems[("dma", i)] = stack.enter_context(nc.semaphore("s_dma%d" % i))
        self.val = {k: 0 for k in self.sems}
        self.dma_count = {"hw": 0, "sw": 0}


def _is_psum(k):
    n = k[0] if isinstance(k, tuple) else k
    return isinstance(n, str) and (n.startswith("ps") or n == "oacc")


class Sched:
    def __init__(self, nc, ss):
        self.nc = nc
        self.ss = ss
        self.ops = []
        self.streams = {e: [] for e in ENGS}
        self.last_w = {}
        self.readers = {}
        self.dma_last = [None] * N_DMA_SEMS
        self.dma_val = [ss.val[("dma", i)] for i in range(N_DMA_SEMS)]

    def add(self, eng, fn, reads=(), writes=(), dma=False):
        if OP_LIMIT is not None and len(self.ops) >= OP_LIMIT:
            return None
        pr = [k for k in reads if _is_psum(k)]
        if pr:
            reads = [k for k in reads if not _is_psum(k)]
            writes = list(writes) + [k for k in pr if k not in writes]
        op = _Op()
        op.eng, op.fn, op.is_dma = eng, fn, dma
        op.signal, op.sigval, op.dsem, op.dval, op.waits = False, None, None, None, None
        deps = []
        for k in reads:
            w = self.last_w.get(k)
            if w is not None:
                deps.append(w)
        for k in writes:
            w = self.last_w.get(k)
            if w is not None:
                deps.append(w)
            rd = self.readers.get(k)
            if rd:
                deps.extend(rd.values())
        if dma:
            if eng == "pool":
                s = N_HW_SEMS + self.ss.dma_count["sw"] % (N_DMA_SEMS - N_HW_SEMS)
                self.ss.dma_count["sw"] += 1
            else:
                s = self.ss.dma_count["hw"] % N_HW_SEMS
                self.ss.dma_count["hw"] += 1
            if self.dma_last[s] is not None:
                deps.append(self.dma_last[s])
            self.dma_val[s] += 16
            op.dsem, op.dval = s, self.dma_val[s]
            self.dma_last[s] = op
        op.deps = deps
        gid = len(self.ops)
        for k in reads:
            self.readers.setdefault(k, {})[("dma", gid) if dma else eng] = op
        for k in writes:
            self.last_w[k] = op
            self.readers[k] = {}
        self.streams[eng].append(op)
        self.ops.append(op)
        return op

    @staticmethod
    def _skip(op, d):
        return (not d.is_dma) and d.eng == op.eng and op.eng == "pe" and not op.is_dma

    def emit(self, name):
        nc, ss = self.nc, self.ss
        for op in self.ops:
            for d in op.deps:
                if not d.is_dma and not self._skip(op, d):
                    d.signal = True
        for e in ("pe", "act", "dve", "pool"):
            c = ss.val[("eng", e)]
            for op in self.streams[e]:
                if op.signal and not op.is_dma:
                    c += 1
                    op.sigval = c
            ss.val[("eng", e)] = c
        for i in range(N_DMA_SEMS):
            ss.val[("dma", i)] = self.dma_val[i]
        for e in ENGS:
            known = {}
            for op in self.streams[e]:
                need = {}
                for d in op.deps:
                    if d.is_dma:
                        key, val = ("dma", d.dsem), d.dval
                    elif self._skip(op, d):
                        continue
                    else:
                        key, val = ("eng", d.eng), d.sigval
                    if known.get(key, 0) >= val:
                        continue
                    if need.get(key, 0) < val:
                        need[key] = val
                known.update(need)
                op.waits = need
        sems = ss.sems
        with nc.Block() as block:
            def run(e, eng):
                for op in self.streams[e]:
                    for k, v in op.waits.items():
                        eng.wait_ge(sems[k], v)
                    ins = op.fn(eng)
                    if op.is_dma:
                        ins.then_inc(sems[("dma", op.dsem)], 16)
                    elif op.signal:
                        ins.then_inc(sems[("eng", e)], 1)

            @block.tensor
            def _(eng):
                run("pe", eng)

            @block.scalar
            def _(eng):
                run("act", eng)

            @block.vector
            def _(eng):
                run("dve", eng)

            @block.gpsimd
            def _(eng):
                run("pool", eng)

            @block.sync
            def _(eng):
                run("sp", eng)
                for i in range(N_DMA_SEMS):
                    if self.dma_val[i] > 0:
                        eng.wait_ge(sems[("dma", i)], self.dma_val[i])
                for e in ("pe", "act", "dve", "pool"):
                    if ss.val[("eng", e)] > 0:
                        eng.wait_ge(sems[("eng", e)], ss.val[("eng", e)])


class Cfg:
    def __init__(self, D=2048, H=8, NCH=4, CH=1024, DFF=8192):
        self.D, self.H, self.NCH, self.CH, self.DFF = D, H, NCH, CH, DFF
        self.KC = D // 128
        self.NST = CH // 128
        self.NT = NCH * self.NST
        self.TCTX = NCH * CH
        self.NB = self.TCTX // 256
        self.NG = CH // 512
        self.HG = min(4, H)
        self.GW = self.HG * 128
        self.HW = H * 128
        self.o_mq, self.o_mk, self.o_mv = 0, self.HW, 2 * self.HW
        self.o_fq, self.o_fk, self.o_fv = 3 * self.HW, 4 * self.HW, 5 * self.HW
        self.o_ff = 6 * self.HW
        self.o_ga = self.o_ff + H
        self.o_gb = self.o_ga + D
        self.WIN = self.o_gb + D
        self.GD = min(512, D)
        self.FC = DFF // 128
        self.FB = min(1024, DFF)
        self.NFB = DFF // self.FB
        self.UW = min(256, DFF)
        self.TOPK = 3
        assert H >= 2 and self.NB >= 8


FULL = Cfg()


def _cp(e, out, in_):
    if hasattr(e, "tensor_copy"):
        return e.tensor_copy(out=out, in_=in_)
    return e.copy(out=out, in_=in_)


def MARK(name, S):
    if MARKS is not None:
        MARKS.append((name, len(S.ops)))


MARKS = None


def build(cfg=FULL, upto=4, debug=False):
    c = cfg
    D, H, KC, NCH, CH, NST, NT, TCTX = c.D, c.H, c.KC, c.NCH, c.CH, c.NST, c.NT, c.TCTX
    NB, NG, HG, GW, HW, GD = c.NB, c.NG, c.HG, c.GW, c.HW, c.GD
    H2 = 2 * H
    NN = CH // 512
    nc = bass.Bass("TRN2", target_bir_lowering=False)
    din = lambda n, s, dt=F32: nc.dram_tensor(n, s, dt, kind="ExternalInput").ap()
    dscr = lambda n, s, dt: nc.dram_tensor(n, s, dt, kind="Internal").ap()
    xctx = din("xctx", [TCTX, D])
    w_in = din("w_in", [D, c.WIN])
    w_bm = din("w_bm", [HW, D])
    w_bf = din("w_bf", [HW, D])
    w_out = din("w_out", [D, D])
    w_up = din("w_up", [D, c.DFF])
    w_down = din("w_down", [c.DFF, D])
    gvec = din("gvec", [4, D])
    bfg = din("bfg", [1, H])
    rcos = din("rope_cos", [TCTX, 16])
    rsin = din("rope_sin", [TCTX, 16])
    gbias_d = din("gbias", [1, NST * NB])
    vflag_d = din("vflag", [1, NCH])
    cident = din("cident", [128, 128])
    ctri = din("ctri", [128, 128])
    cE = din("cE", [NB, NB * 128])
    out = nc.dram_tensor("out", [CH, D], F32, kind="ExternalOutput").ap()
    KTd = dscr("KTd", [H2, 128, TCTX], BF16)
    QTd = dscr("QTd", [H2, 128, CH], BF16)
    Vd = dscr("Vd", [NT, 128, H2 * 128], BF16)
    sgd = dscr("sgd", [2, KC, 128, CH], BF16)
    x1d = dscr("x1d", [CH, D], F32)
    if debug:
        dbg_logf = nc.dram_tensor("dbg_logf", [128, NT * H], F32, kind="ExternalOutput").ap()
        dbg_kmT = nc.dram_tensor("dbg_kmT", [128, H * NB], F32, kind="ExternalOutput").ap()
        dbg_oT = nc.dram_tensor("dbg_oT", [128, H2 * CH], F32, kind="ExternalOutput").ap()
        dbg_mg = nc.dram_tensor("dbg_mg", [128, KC * CH], F32, kind="ExternalOutput").ap()

    scale = 128.0 ** -0.5

    with contextlib.ExitStack() as top:
        ss = SemState(nc, top)
        T = lambda st, n, s, dt: st.enter_context(nc.sbuf_tensor(n, s, dt))
        P = lambda st, n, s, dt: st.enter_context(nc.psum_tensor(n, s, dt))
        ident_b = T(top, "ident_b", [128, 128], BF16)
        tri_b = T(top, "tri_b", [128, 128], BF16)
        tri_f = T(top, "tri_f", [128, 128], F32)
        ones_f = T(top, "ones_f", [128, 128], F32)
        nones_f = T(top, "nones_f", [128, 128], F32)
        ones_b = T(top, "ones_b", [128, 128], BF16)
        c256 = T(top, "c256", [128, 1], BF16)
        epsc = T(top, "epsc", [128, 1], F32)
        E_b = T(top, "E_b", [NB, NB * 128], BF16)
        zff = T(top, "zff", [128, NT, H], F32)
        logf = T(top, "logf", [128, NT, H], F32)
        kmT = T(top, "kmT", [128, H, NB], BF16)
        stat = T(top, "stat", [128, NT, 8], F32)
        junk = T(top, "junk", [128, D], BF16)

        with contextlib.ExitStack() as ph:
            S = Sched(nc, ss)
            g_rep = T(ph, "g_rep", [128, D], F32)
            bfg_rep = T(ph, "bfg_rep", [128, H], F32)
            cos_sb = T(ph, "cos_sb", [128, NT, 16], F32)
            sin_sb = T(ph, "sin_sb", [128, NT, 16], F32)
            xs = [T(ph, "xs%d" % i, [128, D], F32) for i in range(2)]
            hb = [T(ph, "hb%d" % i, [128, D], BF16) for i in range(2)]
            hT = [T(ph, "hT%d" % i, [128, KC, CH], BF16) for i in range(2)]
            wb = [T(ph, "wb%d" % i, [128, KC, max(GD, GW)], BF16) for i in range(2)]
            vst = [T(ph, "vst%d" % i, [128, NST, GW], BF16) for i in range(2)]
            kst = [T(ph, "kst%d" % i, [128, HG, CH], BF16) for i in range(2)]
            kb = [T(ph, "kb%d" % i, [128, GW], BF16) for i in range(2)]
            rt = [T(ph, "rt%d" % i, [128, HG, 16], F32) for i in range(4)]
            wff = T(ph, "wff", [128, KC, H], BF16)
            lt = T(ph, "lt", [128, NT * H], F32)
            pst = [P(ph, "pst%d" % i, [128, 8, 128], BF16) for i in range(2)]
            psmm = [P(ph, "psmm%d" % i, [128, 512], F32) for i in range(3)]
            pstk = [P(ph, "pstk%d" % i, [128, 8, 128], BF16) for i in range(2)]
            pskm = P(ph, "pskm", [128, 512], F32)

            S.add("pool", lambda e: e.dma_start(out=ident_b[:], in_=cident), writes=["ident_b"], dma=True)
            S.add("pool", lambda e: e.dma_start(out=tri_b[:], in_=ctri), writes=["tri_b"], dma=True)
            S.add("sp", lambda e: e.dma_start(out=tri_f[:], in_=ctri), writes=["tri_f"], dma=True)
            S.add("pool", lambda e: e.dma_start(out=E_b[:], in_=cE), writes=["E_b"], dma=True)
            S.add("dve", lambda e: e.memset(ones_f[:], 1.0), writes=["ones_f"])
            S.add("dve", lambda e: e.memset(nones_f[:], -1.0), writes=["nones_f"])
            S.add("dve", lambda e: e.memset(ones_b[:], 1.0), writes=["ones_b"])
            S.add("dve", lambda e: e.memset(c256[:], 1.0 / 256.0), writes=["c256"])
            S.add("dve", lambda e: e.memset(epsc[:], RMS_EPS), writes=["epsc"])
            S.add("sp", lambda e: e.dma_start(out=g_rep[:], in_=gvec[0, :].partition_broadcast(128)),
                  writes=["g_rep"], dma=True)
            S.add("sp", lambda e: e.dma_start(out=bfg_rep[:], in_=bfg[0, :].partition_broadcast(128)),
                  writes=["bfg_rep"], dma=True)
            S.add("sp", lambda e: e.dma_start(out=cos_sb[:], in_=rcos.rearrange("(t p) f -> p t f", p=128)),
                  writes=["cos_sb"], dma=True)
            S.add("sp", lambda e: e.dma_start(out=sin_sb[:], in_=rsin.rearrange("(t p) f -> p t f", p=128)),
                  writes=["sin_sb"], dma=True)
            S.add("pool", lambda e: e.dma_start(
                out=wff[:], in_=w_in[:, c.o_ff:c.o_ff + H].rearrange("(kc p) c -> p kc c", p=128)),
                writes=["wff"], dma=True)

            cnt = {"mm": 0, "w": 0, "v": 0, "k": 0, "kb": 0, "tk": 0, "tk2": 0}

            def load_w(src2d, col0, width):
                i = cnt["w"] % 2
                cnt["w"] += 1
                buf = wb[i]
                for k0 in range(0, KC, 8):
                    k1 = min(KC, k0 + 8)
                    S.add("pool", lambda e, k0=k0, k1=k1: e.dma_start(
                        out=buf[:, k0:k1, 0:width],
                        in_=src2d[k0 * 128:k1 * 128, col0:col0 + width].rearrange("(kc p) c -> p kc c", p=128)),
                        writes=[("wb", i)], dma=True)
                return buf, ("wb", i)

            def hT_subtile(d, s):
                hTd = hT[d % 2]
                t = d * NST + s
                xi = t % 2
                S.add("sp", lambda e: e.dma_start(out=xs[xi][:], in_=xctx[t * 128:(t + 1) * 128, :]),
                      writes=[("xs", xi)], dma=True)
                S.add("dve", lambda e: e.scalar_tensor_tensor(
                    out=hb[xi][:], in0=xs[xi][:], scalar=1.0, in1=xs[xi][:], op0=ALU.mult, op1=ALU.mult,
                    accum_out=stat[:, t, 0:1]),
                    reads=[("xs", xi)], writes=[("hb", xi), ("stat", t)])
                S.add("act", lambda e: e.activation(out=stat[:, t, 1:2], in_=stat[:, t, 0:1], func=AF.Sqrt,
                                                    bias=epsc[:], scale=1.0 / D),
                      reads=[("stat", t), "epsc"], writes=[("stat", t)])
                S.add("dve", lambda e: e.reciprocal(out=stat[:, t, 2:3], in_=stat[:, t, 1:2]),
                      reads=[("stat", t)], writes=[("stat", t)])
                S.add("dve", lambda e: e.scalar_tensor_tensor(
                    out=hb[xi][:], in0=xs[xi][:], scalar=stat[:, t, 2:3], in1=g_rep[:], op0=ALU.mult,
                    op1=ALU.mult),
                    reads=[("xs", xi), ("stat", t), "g_rep"], writes=[("hb", xi)])
                nq = min(4, KC)
                for q0 in range(0, KC, nq):
                    pi = cnt["tk"] % 2
                    cnt["tk"] += 1
                    for kk in range(nq):
                        S.add("pe", lambda e, pi=pi, kk=kk, q0=q0: e.transpose(
                            out=pst[pi][:, kk, :], in_=hb[xi][:, (q0 + kk) * 128:(q0 + kk + 1) * 128],
                            identity=ident_b[:]),
                            reads=[("hb", xi), "ident_b"], writes=[("pst", pi)])
                    S.add("act", lambda e, pi=pi, q0=q0: e.copy(
                        out=hTd[:, q0:q0 + nq, s * 128:(s + 1) * 128], in_=pst[pi][:, 0:nq, :]),
                        reads=[("pst", pi)], writes=[("hT", d % 2)])

            pending = []

            def drip(n=1):
                for _ in range(n):
                    if pending:
                        hT_subtile(*pending.pop(0))

            def next_ps():
                i = cnt["mm"] % 3
                cnt["mm"] += 1
                return psmm[i], ("psmm", i)

            def tok_group(d, col0, width, mode, hbase):
                hTd = hT[d % 2]
                wbuf, wkey = load_w(w_in, col0, width)
                nh = width // 128
                if mode == "v":
                    vi = cnt["v"] % 2
                    cnt["v"] += 1
                else:
                    ki = cnt["k"] % 2
                    cnt["k"] += 1
                for s in range(NST):
                    t = d * NST + s
                    ps, pk = next_ps()
                    for kc in range(KC):
                        S.add("pe", lambda e, ps=ps, kc=kc, s=s: e.matmul(
                            ps[:, 0:width], lhsT=hTd[:, kc, s * 128:(s + 1) * 128], rhs=wbuf[:, kc, 0:width],
                            start=(kc == 0), stop=(kc == KC - 1)),
                            reads=[("hT", d % 2), wkey], writes=[pk])
                    if mode == "v":
                        S.add("act", lambda e, ps=ps, s=s: e.copy(out=vst[vi][:, s, 0:width], in_=ps[:, 0:width]),
                              reads=[pk], writes=[("vst", vi)])
                        continue
                    bi = cnt["kb"] % 2
                    cnt["kb"] += 1
                    kbt = kb[bi]
                    ps3 = ps[:, 0:width].rearrange("p (h c) -> p h c", c=128)
                    kb3 = kbt[:, 0:width].rearrange("p (h c) -> p h c", c=128)
                    cosb = cos_sb[:, t:t + 1, :].broadcast_to([128, nh, 16])
                    sinb = sin_sb[:, t:t + 1, :].broadcast_to([128, nh, 16])
                    S.add("act", lambda e, ps=ps, kbt=kbt: e.copy(out=kbt[:, 0:width], in_=ps[:, 0:width]),
                          reads=[pk], writes=[("kb", bi)])
                    x1, x2 = ps3[:, :, 0:16], ps3[:, :, 16:32]
                    r = [rt[i][:, 0:nh, :] for i in range(4)]
                    S.add("dve", lambda e, x1=x1, cosb=cosb, r=r: e.tensor_tensor(out=r[0], in0=x1, in1=cosb, op=ALU.mult),
                          reads=[pk, "cos_sb"], writes=[("rt", 0)])
                    S.add("dve", lambda e, x2=x2, sinb=sinb, r=r: e.tensor_tensor(out=r[1], in0=x2, in1=sinb, op=ALU.mult),
                          reads=[pk, "sin_sb"], writes=[("rt", 1)])
                    S.add("dve", lambda e, x2=x2, cosb=cosb, r=r: e.tensor_tensor(out=r[2], in0=x2, in1=cosb, op=ALU.mult),
                          reads=[pk, "cos_sb"], writes=[("rt", 2)])
                    S.add("dve", lambda e, x1=x1, sinb=sinb, r=r: e.tensor_tensor(out=r[3], in0=x1, in1=sinb, op=ALU.mult),
                          reads=[pk, "sin_sb"], writes=[("rt", 3)])
                    S.add("dve", lambda e, kb3=kb3, r=r: e.tensor_tensor(out=kb3[:, :, 0:16], in0=r[0], in1=r[1],
                                                                       op=ALU.subtract),
                          reads=[("rt", 0), ("rt", 1), ("kb", bi)], writes=[("kb", bi)])
                    S.add("dve", lambda e, kb3=kb3, r=r: e.tensor_tensor(out=kb3[:, :, 16:32], in0=r[2], in1=r[3],
                                                                       op=ALU.add),
                          reads=[("rt", 2), ("rt", 3), ("kb", bi)], writes=[("kb", bi)])
                    pi = cnt["tk2"] % 2
                    cnt["tk2"] += 1
                    for hh in range(nh):
                        S.add("pe", lambda e, pi=pi, hh=hh, kbt=kbt: e.transpose(
                            out=pstk[pi][:, hh, :], in_=kbt[:, hh * 128:(hh + 1) * 128], identity=ident_b[:]),
                            reads=[("kb", bi), "ident_b"], writes=[("pstk", pi)])
                    S.add("dve", lambda e, pi=pi, s=s: e.tensor_copy(
                        out=kst[ki][:, 0:nh, s * 128:(s + 1) * 128], in_=pstk[pi][:, 0:nh, :]),
                        reads=[("pstk", pi)], writes=[("kst", ki)])
                    if mode == "k_rope" and s % 2 == 1:
                        kprev = kb[1 - bi]
                        for hh in range(nh):
                            S.add("pe", lambda e, hh=hh, kprev=kprev: e.matmul(
                                pskm[:, hh:hh + 1], lhsT=kprev[:, hh * 128:(hh + 1) * 128], rhs=c256[:],
                                start=True, stop=False),
                                reads=[("kb", 1 - bi), "c256"], writes=["pskm"])
                            S.add("pe", lambda e, hh=hh, kbt=kbt: e.matmul(
                                pskm[:, hh:hh + 1], lhsT=kbt[:, hh * 128:(hh + 1) * 128], rhs=c256[:],
                                start=False, stop=True),
                                reads=[("kb", bi), "c256"], writes=["pskm"])
                        if True:
                            n = t // 2
                            S.add("dve", lambda e, n=n: e.tensor_copy(out=kmT[:, hbase:hbase + nh, n], in_=pskm[:, 0:nh]),
                                  reads=["pskm"], writes=["kmT"])
                if mode == "v":
                    S.add("sp", lambda e: e.dma_start(
                        out=Vd[d * NST:(d + 1) * NST, :, hbase * 128:hbase * 128 + width].rearrange("t p c -> p t c"),
                        in_=vst[vi][:, :, 0:width]),
                        reads=[("vst", vi)], dma=True)
                else:
                    dst = KTd[hbase:hbase + nh, :, d * CH:(d + 1) * CH] if mode == "k_rope" else QTd[hbase:hbase + nh, :, :]
                    S.add("sp", lambda e: e.dma_start(out=dst.rearrange("h p t -> p h t"), in_=kst[ki][:, 0:nh, :]),
                          reads=[("kst", ki)], dma=True)
                drip()

            def feat_group(d, col0, width, mode, hbase, kc0=0):
                hTd = hT[d % 2]
                wbuf, wkey = load_w(w_in, col0, width)
                nh = width // 128
                for h0 in range(0, nh, HG):
                    h1 = min(nh, h0 + HG)
                    ki = cnt["k"] % 2
                    cnt["k"] += 1
                    for hh in range(h0, h1):
                        for n0 in range(0, CH, 512):
                            ps, pk = next_ps()
                            for kc in range(KC):
                                S.add("pe", lambda e, ps=ps, kc=kc, hh=hh, n0=n0: e.matmul(
                                    ps[:, 0:512], lhsT=wbuf[:, kc, hh * 128:(hh + 1) * 128],
                                    rhs=hTd[:, kc, n0:n0 + 512], start=(kc == 0), stop=(kc == KC - 1)),
                                    reads=[("hT", d % 2), wkey], writes=[pk])
                            if mode == "gate":
                                S.add("act", lambda e, ps=ps, hh=hh, n0=n0, ki=ki, h0=h0: e.activation(
                                    out=kst[ki][:, hh - h0, n0:n0 + 512], in_=ps[:, 0:512], func=AF.Sigmoid),
                                    reads=[pk], writes=[("kst", ki)])
                            else:
                                S.add("dve", lambda e, ps=ps, hh=hh, n0=n0, ki=ki, h0=h0: e.tensor_copy(
                                    out=kst[ki][:, hh - h0, n0:n0 + 512], in_=ps[:, 0:512]),
                                    reads=[pk], writes=[("kst", ki)])
                    if mode == "kT":
                        dst = KTd[hbase + h0:hbase + h1, :, d * CH:(d + 1) * CH]
                    elif mode == "qT":
                        dst = QTd[hbase + h0:hbase + h1, :, :]
                    else:
                        dst = sgd[hbase, kc0 + h0:kc0 + h1, :, :]
                    S.add("sp", lambda e, ki=ki, dst=dst, h0=h0, h1=h1: e.dma_start(
                        out=dst.rearrange("h p t -> p h t"), in_=kst[ki][:, 0:h1 - h0, :]),
                        reads=[("kst", ki)], dma=True)
                drip()

            def ff_group(d):
                hTd = hT[d % 2]
                for s in range(NST):
                    t = d * NST + s
                    ps, pk = next_ps()
                    for kc in range(KC):
                        S.add("pe", lambda e, ps=ps, kc=kc, s=s: e.matmul(
                            ps[:, 0:H], lhsT=hTd[:, kc, s * 128:(s + 1) * 128], rhs=wff[:, kc, :],
                            start=(kc == 0), stop=(kc == KC - 1)),
                            reads=[("hT", d % 2), "wff"], writes=[pk])
                    S.add("dve", lambda e, ps=ps, t=t: e.tensor_tensor(out=zff[:, t, :], in0=ps[:, 0:H], in1=bfg_rep[:],
                                                                     op=ALU.add),
                          reads=[pk, "bfg_rep"], writes=["zff"])
                drip()

            MARK("consts", S)
            for s in range(NST):
                hT_subtile(0, s)
            MARK("hT0", S)
            for d in range(NCH):
                if d + 1 < NCH:
                    pending.extend((d + 1, s) for s in range(NST))
                for g0 in range(0, H, HG):
                    tok_group(d, c.o_mk + g0 * 128, GW, "k_rope", g0)
                MARK("k_rope%d" % d, S)
                for g0 in range(0, H, HG):
                    tok_group(d, c.o_mv + g0 * 128, GW, "v", g0)
                MARK("v%d" % d, S)
                for g0 in range(0, H, HG):
                    feat_group(d, c.o_fk + g0 * 128, GW, "kT", H + g0)
                for g0 in range(0, H, HG):
                    tok_group(d, c.o_fv + g0 * 128, GW, "v", H + g0)
                MARK("fkv%d" % d, S)
                ff_group(d)
                MARK("ff%d" % d, S)
                drip(NST)
                if d == 0:
                    for g0 in range(0, H, HG):
                        tok_group(d, c.o_mq + g0 * 128, GW, "q_rope", g0)
                    for g0 in range(0, H, HG):
                        feat_group(d, c.o_fq + g0 * 128, GW, "qT", H + g0)
                    for c0 in range(0, D, GD):
                        feat_group(d, c.o_ga + c0, GD, "gate", 0, kc0=c0 // 128)
                    for c0 in range(0, D, GD):
                        feat_group(d, c.o_gb + c0, GD, "gate", 1, kc0=c0 // 128)
            MARK("proj_done", S)
            zf = zff[:].rearrange("p t h -> p (t h)")
            lf = logf[:].rearrange("p t h -> p (t h)")
            S.add("act", lambda e: e.activation(out=lt[:], in_=zf, func=AF.Exp, scale=-1.0), reads=["zff"], writes=["lt"])
            S.add("act", lambda e: e.activation(out=lt[:], in_=lt[:], func=AF.Ln, bias=1.0), reads=["lt"], writes=["lt"])
            S.add("dve", lambda e: e.tensor_scalar(out=lf, in0=lt[:], scalar1=-1.0, scalar2=None, op0=ALU.mult),
                  reads=["lt"], writes=["logf"])
            if debug:
                S.add("sp", lambda e: e.dma_start(out=dbg_logf, in_=lf), reads=["logf"], dma=True)
                S.add("pool", lambda e: e.dma_start(out=dbg_kmT, in_=kmT[:].rearrange("p h n -> p (h n)")),
                      reads=["kmT"], dma=True)
            S.emit("ph1")
        if upto < 2:
            return nc

        with contextlib.ExitStack() as mid:
            R1 = T(mid, "R1", [128, max(H2, KC), CH], BF16)
            R2 = T(mid, "R2", [128, max(KC, 16), CH], BF16)
            oT, h2T, mergedT = R1, R1, R2

            with contextlib.ExitStack() as ph:
                S = Sched(nc, ss)
                KTb = [T(ph, "KTb%d" % i, [128, TCTX], BF16) for i in range(2)]
                Vb = [T(ph, "Vb%d" % i, [128, NT, 130], BF16) for i in range(2)]
                QTb = [T(ph, "QTb%d" % i, [128, CH], BF16) for i in range(2)]
                pT = [T(ph, "pT%d" % i, [128, 512], BF16) for i in range(4)]
                sqb = [T(ph, "sqb%d" % i, [128, 512], BF16) for i in range(2)]
                nmx = T(ph, "nmx", [128, 2, 32], F32)
                negC = T(ph, "negC", [128, H2], F32)
                gbias_rep = T(ph, "gbias_rep", [128, NST * NB], F32)
                vfl_rep = T(ph, "vfl_rep", [128, NCH], F32)
                Fsb = T(ph, "Fsb", [128, NT, H], F32)
                FrefB = T(ph, "FrefB", [128, NG, H], F32)
                biasF = T(ph, "biasF", [128, NG, NT, H], F32)
                bfh = [T(ph, "bfh%d" % i, [128, NG, NT], F32) for i in range(2)]
                gmb = [T(ph, "gmb%d" % i, [128, NB], F32) for i in range(2)]
                m8 = [T(ph, "m8_%d" % i, [128, 8], F32) for i in range(2)]
                b1 = [T(ph, "b1_%d" % i, [128, NB], F32) for i in range(2)]
                bb = [T(ph, "bb%d" % i, [128, NB], BF16) for i in range(2)]
                biasT = [T(ph, "biasT%d" % i, [NB, CH], BF16) for i in range(2)]
                otok = [T(ph, "otok%d" % i, [128, 4, 128], BF16) for i in range(2)]
                rc = T(ph, "rc", [128, 8], F32)
                psS = [P(ph, "psS%d" % i, [128, 512], F32) for i in range(2)]
                oacc_t = [[P(ph, "oacc%d_%d" % (i, k), [128, 512], F32) for k in range(2)] for i in range(2)]
                oacc = [[t[:, 0:260].rearrange("p (r c) -> p r c", c=130) for t in row] for row in oacc_t]
                psX = [P(ph, "psX%d" % i, [128, 512], F32) for i in range(2)]
                psXb = [p[:].bitcast(BF16) for p in psX]
                cn = {"x": 0, "s": 0, "p": 0, "a": 0, "sq": 0, "g": 0}

                def next_x():
                    i = cn["x"] % 2
                    cn["x"] += 1
                    return i, ("psX", i)

                S.add("sp", lambda e: e.dma_start(out=gbias_rep[:], in_=gbias_d[0, :].partition_broadcast(128)),
                      writes=["gbias_rep"], dma=True)
                S.add("sp", lambda e: e.dma_start(out=vfl_rep[:], in_=vflag_d[0, :].partition_broadcast(128)),
                      writes=["vfl_rep"], dma=True)
                for i in range(2):
                    S.add("pool", lambda e, i=i: e.memset(Vb[i][:, :, 128:129], 1.0), writes=[("V", i)])

                def load_head(h, par):
                    S.add("sp", lambda e: e.dma_start(out=KTb[par][:], in_=KTd[h]), writes=[("KT", par)], dma=True)
                    S.add("sp", lambda e: e.dma_start(out=QTb[par][:], in_=QTd[h]), writes=[("QT", par)], dma=True)
                    for t0 in range(0, NT, 8):
                        S.add("sp", lambda e, t0=t0: e.dma_start(
                            out=Vb[par][:, t0:t0 + 8, 0:128],
                            in_=Vd[t0:t0 + 8, :, h * 128:(h + 1) * 128].rearrange("t p c -> p t c")),
                            writes=[("V", par)], dma=True)

                load_head(0, 0)

                xi, xk = next_x()
                psF = psX[xi][:, 0:NT * H].rearrange("p (t h) -> p t h", h=H)
                for d in range(NCH):
                    for j in range(NST):
                        t = d * NST + j
                        terms = [(tri_f, t)] + [(ones_f, d * NST + j2) for j2 in range(j)]
                        if d >= 1:
                            terms += [(nones_f, d2 * NST + j2) for d2 in range(1, d + 1) for j2 in range(NST)]
                        for i, (mm, tt) in enumerate(terms):
                            S.add("pe", lambda e, t=t, mm=mm, tt=tt, i=i, n=len(terms): e.matmul(
                                psF[:, t, :], lhsT=mm[:], rhs=logf[:, tt, :], start=(i == 0), stop=(i == n - 1)),
                                reads=["logf", "tri_f", "ones_f", "nones_f"], writes=[xk])
                S.add("dve", lambda e: e.tensor_copy(out=Fsb[:], in_=psF), reads=[xk], writes=["Fsb"])
                xi2, xk2 = next_x()
                psR = psX[xi2][:, 0:NG * H].rearrange("p (g h) -> p g h", h=H)
                for g in range(NG):
                    n = 4 * g + 2
                    for j2 in range(n):
                        S.add("pe", lambda e, g=g, j2=j2, n=n: e.matmul(
                            psR[:, g, :], lhsT=ones_f[:], rhs=logf[:, j2, :], start=(j2 == 0), stop=(j2 == n - 1)),
                            reads=["logf", "ones_f"], writes=[xk2])
                S.add("dve", lambda e: e.tensor_copy(out=FrefB[:], in_=psR), reads=[xk2], writes=["FrefB"])
                for g in range(NG):
                    S.add("dve", lambda e, g=g: e.tensor_tensor(
                        out=biasF[:, g], in0=FrefB[:, g:g + 1, :].broadcast_to([128, NT, H]), in1=Fsb[:],
                        op=ALU.subtract), reads=["FrefB", "Fsb"], writes=["biasF"])
                    S.add("dve", lambda e, g=g: e.tensor_tensor(
                        out=biasF[:, g].rearrange("p (d j) h -> p d (j h)", d=NCH),
                        in0=biasF[:, g].rearrange("p (d j) h -> p d (j h)", d=NCH),
                        in1=vfl_rep[:].unsqueeze(2).broadcast_to([128, NCH, NST * H]), op=ALU.add),
                        reads=["biasF", "vfl_rep"], writes=["biasF"])

                def head_norm(par, h):
                    tiles = [(KTb[par], ("KT", par), n0) for n0 in range(0, TCTX, 512)]
                    tiles += [(QTb[par], ("QT", par), n0) for n0 in range(0, CH, 512)]
                    for i, (buf, bkey, n0) in enumerate(tiles):
                        k = cn["sq"] % 2
                        cn["sq"] += 1
                        S.add("pool", lambda e, buf=buf, n0=n0, k=k: e.tensor_tensor(
                            out=sqb[k][:], in0=buf[:, n0:n0 + 512], in1=buf[:, n0:n0 + 512], op=ALU.mult),
                            reads=[bkey], writes=[("sqb", k)])
                        xi, xk = next_x()
                        S.add("pe", lambda e, xi=xi, k=k: e.matmul(psX[xi][:, 0:512], lhsT=ones_b[:], rhs=sqb[k][:],
                                                                  start=True, stop=True),
                              reads=[("sqb", k), "ones_b"], writes=[xk])
                        S.add("dve", lambda e, xi=xi, i=i: e.tensor_reduce(
                            out=nmx[:, par, i:i + 1], in_=psX[xi][:, 0:512], axis=AX.X, op=ALU.max),
                            reads=[xk], writes=[("nmx", par)])
                    nt_ = len(tiles)
                    S.add("dve", lambda e: e.tensor_reduce(out=negC[:, h:h + 1], in_=nmx[:, par, 0:nt_], axis=AX.X,
                                                           op=ALU.max),
                          reads=[("nmx", par)], writes=[("negC", h)])
                    S.add("dve", lambda e: e.tensor_scalar(out=negC[:, h:h + 1], in0=negC[:, h:h + 1],
                                                           scalar1=-1.02 * scale, scalar2=None, op0=ALU.mult),
                          reads=[("negC", h)], writes=[("negC", h)])

                def moba_gate(par, hm):
                    for qt in range(NST):
                        k = cn["g"] % 2
                        cn["g"] += 1
                        xi, xk = next_x()
                        gsl = gbias_rep[:, qt * NB:(qt + 1) * NB]
                        S.add("pe", lambda e, xi=xi, qt=qt: e.matmul(
                            psX[xi][:, 0:NB], lhsT=QTb[par][:, qt * 128:(qt + 1) * 128], rhs=kmT[:, hm, :],
                            start=True, stop=True), reads=[("QT", par), "kmT"], writes=[xk])
                        S.add("dve", lambda e, xi=xi, k=k, gsl=gsl: e.tensor_tensor(
                            out=gmb[k][:], in0=psX[xi][:, 0:NB], in1=gsl, op=ALU.add),
                            reads=[xk, "gbias_rep"], writes=[("gmb", k)])
                        S.add("dve", lambda e, k=k: e.max(out=m8[k][:], in_=gmb[k][:]),
                              reads=[("gmb", k)], writes=[("m8", k)])
                        S.add("dve", lambda e, k=k: e.tensor_scalar(
                            out=b1[k][:], in0=gmb[k][:], scalar1=m8[k][:, c.TOPK - 1:c.TOPK], scalar2=-NEG_MASK,
                            op0=ALU.is_ge, op1=ALU.mult), reads=[("gmb", k), ("m8", k)], writes=[("b1", k)])
                        S.add("dve", lambda e, k=k, gsl=gsl: e.scalar_tensor_tensor(
                            out=bb[k][:], in0=b1[k][:], scalar=NEG_MASK, in1=gsl, op0=ALU.add, op1=ALU.min),
                            reads=[("b1", k), "gbias_rep"], writes=[("bb", k)])
                        S.add("dve", lambda e, k=k, qt=qt: e.memset(bb[k][:, qt // 2:qt // 2 + 1], 0.0),
                              reads=[("bb", k)], writes=[("bb", k)])
                        xi2, xk2 = next_x()
                        S.add("pe", lambda e, xi2=xi2, k=k: e.transpose(out=psXb[xi2][0:NB, 0:128], in_=bb[k][:],
                                                                        identity=ident_b[:]),
                              reads=[("bb", k), "ident_b"], writes=[xk2])
                        S.add("dve", lambda e, xi2=xi2, qt=qt: e.tensor_copy(
                            out=biasT[par][0:NB, qt * 128:(qt + 1) * 128], in_=psXb[xi2][0:NB, 0:128]),
                            reads=[xk2], writes=[("biasT", par)])

                def attention(par, h, moba):
                    for g in range(NG):
                        a = cn["a"] % 2
                        cn["a"] += 1
                        tiles = [(d, j) for d in range(NCH - 1, 0, -1) for j in range(NST)]
                        tiles += [(0, j) for j in range(4 * g + 4)]
                        for idx, (d, j) in enumerate(tiles):
                            t = d * NST + j
                            c0 = 0 if (d > 0 or j < 4 * g) else (j - 4 * g) * 128
                            N = 512 - c0
                            q0 = g * 512 + c0
                            si = cn["s"] % 2
                            cn["s"] += 1
                            ps, pk = psS[si], ("psS", si)
                            S.add("pe", lambda e, ps=ps, t=t, q0=q0, N=N: e.matmul(
                                ps[:, 0:N], lhsT=KTb[par][:, t * 128:(t + 1) * 128], rhs=QTb[par][:, q0:q0 + N],
                                start=True, stop=not moba), reads=[("KT", par), ("QT", par)], writes=[pk])
                            if moba:
                                n = t // 2
                                S.add("pe", lambda e, ps=ps, n=n, q0=q0, N=N: e.matmul(
                                    ps[:, 0:N], lhsT=E_b[0:NB, n * 128:(n + 1) * 128], rhs=biasT[par][0:NB, q0:q0 + N],
                                    start=False, stop=True), reads=["E_b", ("biasT", par)], writes=[pk])
                            pi = cn["p"] % 4
                            cn["p"] += 1
                            pt, ptk = pT[pi], ("pT", pi)
                            if moba:
                                bias_ap, bkey = negC[:, h:h + 1], ("negC", h)
                            else:
                                bias_ap, bkey = bfh[par][:, g, t:t + 1], ("bfh", par)
                            S.add("act", lambda e, ps=ps, pt=pt, N=N, bias_ap=bias_ap: e.activation(
                                out=pt[:, 0:N], in_=ps[:, 0:N], func=AF.Exp, bias=bias_ap, scale=scale),
                                reads=[pk, bkey], writes=[ptk])
                            if d == 0 and j >= 4 * g:
                                S.add("pool", lambda e, pt=pt: e.tensor_tensor(out=pt[:, 0:128], in0=pt[:, 0:128],
                                                                               in1=tri_b[:], op=ALU.mult),
                                      reads=[ptk, "tri_b"], writes=[ptk])
                            for qs in range(c0 // 128, 4):
                                lo = qs * 128 - c0
                                S.add("pe", lambda e, pt=pt, qs=qs, lo=lo, t=t, a=a, first=(idx == 0 and qs % 2 == 0),
                                      last=(d == 0 and j == 4 * g + qs): e.matmul(
                                    oacc[a][qs // 2][:, qs % 2, 0:129], lhsT=pt[:, lo:lo + 128],
                                    rhs=Vb[par][:, t, 0:129], start=first, stop=last, skip_group_check=True),
                                    reads=[ptk, ("V", par)], writes=[("oacc", a, qs // 2)])
                        ot = otok[a]
                        for qs in range(4):
                            oa = oacc[a][qs // 2]
                            S.add("dve", lambda e, oa=oa, qs=qs, a=a: e.reciprocal(out=rc[:, a * 4 + qs:a * 4 + qs + 1],
                                                                            in_=oa[:, qs % 2, 128:129]),
                                  reads=[("oacc", a, qs // 2)], writes=[("rc", a, qs)])
                            S.add("dve", lambda e, oa=oa, qs=qs, ot=ot, a=a: e.tensor_scalar(
                                out=ot[:, qs, :], in0=oa[:, qs % 2, 0:128], scalar1=rc[:, a * 4 + qs:a * 4 + qs + 1],
                                scalar2=None, op0=ALU.mult),
                                reads=[("oacc", a, qs // 2), ("rc", a, qs)], writes=[("otok", a)])
                        xi, xk = next_x()
                        for qs in range(4):
                            S.add("pe", lambda e, xi=xi, qs=qs, ot=ot: e.transpose(
                                out=psXb[xi][:, qs * 128:(qs + 1) * 128], in_=ot[:, qs, :], identity=ident_b[:]),
                                reads=[("otok", a), "ident_b"], writes=[xk])
                        S.add("act", lambda e, xi=xi, g=g: e.copy(out=oT[:, h, g * 512:(g + 1) * 512],
                                                                  in_=psXb[xi][:, 0:512]),
                              reads=[xk], writes=["oT"])

                for h in range(H2):
                    par = h % 2
                    if h + 1 < H2:
                        load_head(h + 1, (h + 1) % 2)
                    head_norm(par, h)
                    if h < H:
                        moba_gate(par, h)
                    else:
                        S.add("dve", lambda e, h=h, par=par: e.tensor_scalar(
                            out=bfh[par][:], in0=biasF[:, :, :, h - H], scalar1=negC[:, h:h + 1], scalar2=None,
                            op0=ALU.add), reads=["biasF", ("negC", h)], writes=[("bfh", par)])
                    attention(par, h, h < H)
                if debug:
                    S.add("pool", lambda e: e.dma_start(out=dbg_oT, in_=oT[:, 0:H2, :].rearrange("p h t -> p (h t)")),
                          reads=["oT"], dma=True)
                S.emit("ph2")
            if upto < 3:
                return nc

            with contextlib.ExitStack() as ph:
                S = Sched(nc, ss)
                Wbm = T(ph, "Wbm", [128, H, D], BF16)
                Wbf = T(ph, "Wbf", [128, H, D], BF16)
                sga = [T(ph, "sga%d" % i, [128, CH], BF16) for i in range(2)]
                sgb = [T(ph, "sgb%d" % i, [128, CH], BF16) for i in range(2)]
                t1 = [T(ph, "t1_%d" % i, [128, 512], F32) for i in range(2)]
                t2 = [T(ph, "t2_%d" % i, [128, 512], F32) for i in range(2)]
                psm = [[P(ph, "psm%d_%d" % (i, n), [128, 512], F32) for n in range(NN)] for i in range(2)]
                psf = [[P(ph, "psf%d_%d" % (i, n), [128, 512], F32) for n in range(NN)] for i in range(2)]
                for h0 in range(0, H, 4):
                    h1 = min(H, h0 + 4)
                    for (wsb, wdr, nm) in ((Wbm, w_bm, "Wbm"), (Wbf, w_bf, "Wbf")):
                        S.add("pool", lambda e, wsb=wsb, wdr=wdr, h0=h0, h1=h1: e.dma_start(
                            out=wsb[:, h0:h1, :], in_=wdr[h0 * 128:h1 * 128, :].rearrange("(h p) c -> p h c", p=128)),
                            writes=[nm], dma=True)
                tc_ = 0
                for cc in range(KC):
                    a = cc % 2
                    S.add("sp", lambda e, a=a, cc=cc: e.dma_start(out=sga[a][:], in_=sgd[0, cc]), writes=[("sga", a)], dma=True)
                    S.add("sp", lambda e, a=a, cc=cc: e.dma_start(out=sgb[a][:], in_=sgd[1, cc]), writes=[("sgb", a)], dma=True)
                    for n in range(NN):
                        for hh in range(H):
                            S.add("pe", lambda e, a=a, n=n, hh=hh, cc=cc: e.matmul(
                                psm[a][n][:, 0:512], lhsT=Wbm[:, hh, cc * 128:(cc + 1) * 128],
                                rhs=oT[:, hh, n * 512:(n + 1) * 512], start=(hh == 0), stop=(hh == H - 1)),
                                reads=["Wbm", "oT"], writes=[("psm", a, n)])
                        for hh in range(H):
                            S.add("pe", lambda e, a=a, n=n, hh=hh, cc=cc: e.matmul(
                                psf[a][n][:, 0:512], lhsT=Wbf[:, hh, cc * 128:(cc + 1) * 128],
                                rhs=oT[:, H + hh, n * 512:(n + 1) * 512], start=(hh == 0), stop=(hh == H - 1)),
                                reads=["Wbf", "oT"], writes=[("psf", a, n)])
                        k = tc_ % 2
                        tc_ += 1
                        S.add("dve", lambda e, a=a, n=n, k=k: e.tensor_tensor(
                            out=t1[k][:], in0=psm[a][n][:, 0:512], in1=sga[a][:, n * 512:(n + 1) * 512], op=ALU.mult),
                            reads=[("psm", a, n), ("sga", a)], writes=[("t1", k)])
                        S.add("dve", lambda e, a=a, n=n, k=k: e.tensor_tensor(
                            out=t2[k][:], in0=psf[a][n][:, 0:512], in1=sgb[a][:, n * 512:(n + 1) * 512], op=ALU.mult),
                            reads=[("psf", a, n), ("sgb", a)], writes=[("t2", k)])
                        S.add("pool", lambda e, cc=cc, n=n, k=k: e.tensor_tensor(
                            out=mergedT[:, cc, n * 512:(n + 1) * 512], in0=t1[k][:], in1=t2[k][:], op=ALU.add),
                            reads=[("t1", k), ("t2", k)], writes=["mergedT"])
                if debug:
                    S.add("pool", lambda e: e.dma_start(out=dbg_mg, in_=mergedT[:, 0:KC, :].rearrange("p h t -> p (h t)")),
                          reads=["mergedT"], dma=True)
                S.emit("ph3a")

            with contextlib.ExitStack() as ph:
                S = Sched(nc, ss)
                Wo = T(ph, "Wo", [128, KC, D], BF16)
                gpost = T(ph, "gpost", [128, D], F32)
                gpre2 = T(ph, "gpre2", [128, D], F32)
                xs2 = [T(ph, "xs2_%d" % i, [128, D], F32) for i in range(2)]
                tt_ = T(ph, "tt_", [128, D], F32)
                hb2 = [T(ph, "hb2_%d" % i, [128, D], BF16) for i in range(2)]
                st3 = T(ph, "st3", [128, NST, 16], F32)
                ncg = D // GD
                psM = [P(ph, "psM%d" % i, [128, 512], F32) for i in range(6)]
                pst2 = [P(ph, "pst2_%d" % i, [128, 8, 128], BF16) for i in range(2)]
                S.add("sp", lambda e: e.dma_start(out=gpost[:], in_=gvec[1, :].partition_broadcast(128)),
                      writes=["gpost"], dma=True)
                S.add("sp", lambda e: e.dma_start(out=gpre2[:], in_=gvec[2, :].partition_broadcast(128)),
                      writes=["gpre2"], dma=True)
                for c0 in range(0, D, GD):
                    for k0 in range(0, KC, 8):
                        k1 = min(KC, k0 + 8)
                        S.add("pool", lambda e, c0=c0, k0=k0, k1=k1: e.dma_start(
                            out=Wo[:, k0:k1, c0:c0 + GD],
                            in_=w_out[k0 * 128:k1 * 128, c0:c0 + GD].rearrange("(kc p) c -> p kc c", p=128)),
                            writes=[("Wo", c0)], dma=True)
                mc = 0
                tk = 0
                for s in range(NST):
                    xi = s % 2
                    S.add("sp", lambda e, s=s, xi=xi: e.dma_start(out=xs2[xi][:], in_=xctx[s * 128:(s + 1) * 128, :]),
                          writes=[("xs2", xi)], dma=True)
                    pss = []
                    for cg in range(ncg):
                        pi = mc % 6
                        mc += 1
                        pss.append(pi)
                        for kc in range(KC):
                            S.add("pe", lambda e, pi=pi, kc=kc, s=s, cg=cg: e.matmul(
                                psM[pi][:, 0:GD], lhsT=mergedT[:, kc, s * 128:(s + 1) * 128],
                                rhs=Wo[:, kc, cg * GD:(cg + 1) * GD], start=(kc == 0), stop=(kc == KC - 1)),
                                reads=["mergedT", ("Wo", cg * GD)], writes=[("psM", pi)])
                        S.add("act", lambda e, pi=pi, s=s, cg=cg: e.activation(
                            out=junk[:, 0:GD], in_=psM[pi][:, 0:GD], func=AF.Square, accum_out=st3[:, s, cg:cg + 1]),
                            reads=[("psM", pi)], writes=["junk", ("st3", s)])
                    S.add("dve", lambda e, s=s: e.tensor_reduce(out=st3[:, s, 8:9], in_=st3[:, s, 0:ncg], axis=AX.X,
                                                                op=ALU.add), reads=[("st3", s)], writes=[("st3", s)])
                    S.add("act", lambda e, s=s: e.activation(out=st3[:, s, 9:10], in_=st3[:, s, 8:9], func=AF.Sqrt,
                                                             bias=epsc[:], scale=1.0 / D),
                          reads=[("st3", s), "epsc"], writes=[("st3", s)])
                    S.add("dve", lambda e, s=s: e.reciprocal(out=st3[:, s, 10:11], in_=st3[:, s, 9:10]),
                          reads=[("st3", s)], writes=[("st3", s)])
                    for cg in range(ncg):
                        pi = pss[cg]
                        S.add("dve", lambda e, pi=pi, s=s, cg=cg: e.scalar_tensor_tensor(
                            out=tt_[:, cg * GD:(cg + 1) * GD], in0=psM[pi][:, 0:GD], scalar=st3[:, s, 10:11],
                            in1=gpost[:, cg * GD:(cg + 1) * GD], op0=ALU.mult, op1=ALU.mult),
                            reads=[("psM", pi), ("st3", s), "gpost"], writes=["tt_"])
                    S.add("dve", lambda e, xi=xi: e.tensor_tensor(out=xs2[xi][:], in0=tt_[:], in1=xs2[xi][:], op=ALU.add),
                          reads=["tt_", ("xs2", xi)], writes=[("xs2", xi)])
                    S.add("sp", lambda e, s=s, xi=xi: e.dma_start(out=x1d[s * 128:(s + 1) * 128, :], in_=xs2[xi][:]),
                          reads=[("xs2", xi)], dma=True)
                    S.add("dve", lambda e, s=s, xi=xi: e.scalar_tensor_tensor(
                        out=hb2[xi][:], in0=xs2[xi][:], scalar=1.0, in1=xs2[xi][:], op0=ALU.mult, op1=ALU.mult,
                        accum_out=st3[:, s, 11:12]), reads=[("xs2", xi)], writes=[("hb2", xi), ("st3", s)])
                    S.add("act", lambda e, s=s: e.activation(out=st3[:, s, 12:13], in_=st3[:, s, 11:12], func=AF.Sqrt,
                                                             bias=epsc[:], scale=1.0 / D),
                          reads=[("st3", s), "epsc"], writes=[("st3", s)])
                    S.add("dve", lambda e, s=s: e.reciprocal(out=st3[:, s, 13:14], in_=st3[:, s, 12:13]),
                          reads=[("st3", s)], writes=[("st3", s)])
                    S.add("dve", lambda e, s=s, xi=xi: e.scalar_tensor_tensor(
                        out=hb2[xi][:], in0=xs2[xi][:], scalar=st3[:, s, 13:14], in1=gpre2[:], op0=ALU.mult,
                        op1=ALU.mult), reads=[("xs2", xi), ("st3", s), "gpre2"], writes=[("hb2", xi)])
                    nq = min(4, KC)
                    for q0 in range(0, KC, nq):
                        pi2 = tk % 2
                        tk += 1
                        for kk in range(nq):
                            S.add("pe", lambda e, pi2=pi2, kk=kk, q0=q0, xi=xi: e.transpose(
                                out=pst2[pi2][:, kk, :], in_=hb2[xi][:, (q0 + kk) * 128:(q0 + kk + 1) * 128],
                                identity=ident_b[:]), reads=[("hb2", xi), "ident_b"], writes=[("pst2", pi2)])
                        S.add("act", lambda e, pi2=pi2, q0=q0, s=s: e.copy(
                            out=h2T[:, q0:q0 + nq, s * 128:(s + 1) * 128], in_=pst2[pi2][:, 0:nq, :]),
                            reads=[("pst2", pi2)], writes=["h2T"])
                S.emit("ph3b")
            if upto < 4:
                return nc

            with contextlib.ExitStack() as ph:
                S = Sched(nc, ss)
                FB, UW = c.FB, c.UW
                FK = FB // 128
                Wu = [T(ph, "Wu%d" % i, [128, KC, UW], BF16) for i in range(2)]
                Wd = [T(ph, "Wd%d" % i, [128, FK, GD], BF16) for i in range(2)]
                m = T(ph, "m", [128, NST, D], F32)
                rl = [T(ph, "rl%d" % i, [128, 512], F32) for i in range(2)]
                gpost2 = T(ph, "gpost2", [128, D], F32)
                xs3 = [T(ph, "xs3_%d" % i, [128, D], F32) for i in range(2)]
                st4 = T(ph, "st4", [128, NST, 4], F32)
                psU = [P(ph, "psU%d" % i, [128, 512], F32) for i in range(3)]
                psD = [P(ph, "psD%d" % i, [128, 512], F32) for i in range(3)]
                aTv = lambda par, k: R2[:, par * FK + k, :]
                ncg = D // GD
                S.add("sp", lambda e: e.dma_start(out=gpost2[:], in_=gvec[3, :].partition_broadcast(128)),
                      writes=["gpost2"], dma=True)
                uc = dc = wu = wd_ = rk = 0
                for fb in range(c.NFB):
                    par = fb % 2
                    for u0 in range(0, FB, UW):
                        col0 = fb * FB + u0
                        wi = wu % 2
                        wu += 1
                        for k0 in range(0, KC, 8):
                            k1 = min(KC, k0 + 8)
                            S.add("pool", lambda e, wi=wi, k0=k0, k1=k1, col0=col0: e.dma_start(
                                out=Wu[wi][:, k0:k1, :],
                                in_=w_up[k0 * 128:k1 * 128, col0:col0 + UW].rearrange("(kc p) c -> p kc c", p=128)),
                                writes=[("Wu", wi)], dma=True)
                        for f in range(UW // 128):
                            fcl = (u0 + f * 128) // 128
                            for n in range(NN):
                                pi = uc % 3
                                uc += 1
                                for kc in range(KC):
                                    S.add("pe", lambda e, pi=pi, kc=kc, f=f, n=n, wi=wi: e.matmul(
                                        psU[pi][:, 0:512], lhsT=Wu[wi][:, kc, f * 128:(f + 1) * 128],
                                        rhs=h2T[:, kc, n * 512:(n + 1) * 512], start=(kc == 0), stop=(kc == KC - 1)),
                                        reads=[("Wu", wi), "h2T"], writes=[("psU", pi)])
                                k = rk % 2
                                rk += 1
                                S.add("act", lambda e, pi=pi, k=k: e.activation(out=rl[k][:], in_=psU[pi][:, 0:512],
                                                                                func=AF.Relu),
                                      reads=[("psU", pi)], writes=[("rl", k)])
                                S.add("pool", lambda e, k=k, par=par, fcl=fcl, n=n: e.tensor_tensor(
                                    out=aTv(par, fcl)[:, n * 512:(n + 1) * 512], in0=rl[k][:], in1=rl[k][:], op=ALU.mult),
                                    reads=[("rl", k)], writes=[("aT", par)])
                    for cg in range(ncg):
                        wi = wd_ % 2
                        wd_ += 1
                        S.add("pool", lambda e, wi=wi, fb=fb, cg=cg: e.dma_start(
                            out=Wd[wi][:],
                            in_=w_down[fb * FB:(fb + 1) * FB, cg * GD:(cg + 1) * GD].rearrange("(kc p) c -> p kc c", p=128)),
                            writes=[("Wd", wi)], dma=True)
                        for s in range(NST):
                            pi = dc % 3
                            dc += 1
                            for kc in range(FK):
                                S.add("pe", lambda e, pi=pi, kc=kc, s=s, wi=wi, par=par: e.matmul(
                                    psD[pi][:, 0:GD], lhsT=aTv(par, kc)[:, s * 128:(s + 1) * 128], rhs=Wd[wi][:, kc, :],
                                    start=(kc == 0), stop=(kc == FK - 1)),
                                    reads=[("aT", par), ("Wd", wi)], writes=[("psD", pi)])
                            msl = m[:, s, cg * GD:(cg + 1) * GD]
                            if fb == 0:
                                S.add("act", lambda e, pi=pi, msl=msl: e.copy(out=msl, in_=psD[pi][:, 0:GD]),
                                      reads=[("psD", pi)], writes=[("m", s, cg)])
                            else:
                                S.add("dve", lambda e, pi=pi, msl=msl: e.tensor_tensor(out=msl, in0=psD[pi][:, 0:GD],
                                                                                     in1=msl, op=ALU.add),
                                      reads=[("psD", pi), ("m", s, cg)], writes=[("m", s, cg)])
                for s in range(NST):
                    xi = s % 2
                    mk = [("m", s, cg) for cg in range(ncg)]
                    S.add("sp", lambda e, s=s, xi=xi: e.dma_start(out=xs3[xi][:], in_=x1d[s * 128:(s + 1) * 128, :]),
                          writes=[("xs3", xi)], dma=True)
                    S.add("dve", lambda e, s=s: e.scalar_tensor_tensor(
                        out=junk[:], in0=m[:, s, :], scalar=1.0, in1=m[:, s, :], op0=ALU.mult, op1=ALU.mult,
                        accum_out=st4[:, s, 0:1]), reads=mk, writes=["junk", ("st4", s)])
                    S.add("act", lambda e, s=s: e.activation(out=st4[:, s, 1:2], in_=st4[:, s, 0:1], func=AF.Sqrt,
                                                             bias=epsc[:], scale=1.0 / D),
                          reads=[("st4", s), "epsc"], writes=[("st4", s)])
                    S.add("dve", lambda e, s=s: e.reciprocal(out=st4[:, s, 2:3], in_=st4[:, s, 1:2]),
                          reads=[("st4", s)], writes=[("st4", s)])
                    S.add("dve", lambda e, s=s: e.scalar_tensor_tensor(
                        out=m[:, s, :], in0=m[:, s, :], scalar=st4[:, s, 2:3], in1=gpost2[:], op0=ALU.mult,
                        op1=ALU.mult), reads=mk + [("st4", s), "gpost2"], writes=mk)
                    S.add("dve", lambda e, s=s, xi=xi: e.tensor_tensor(out=xs3[xi][:], in0=m[:, s, :], in1=xs3[xi][:],
                                                                     op=ALU.add),
                          reads=mk + [("xs3", xi)], writes=[("xs3", xi)])
                    S.add("sp", lambda e, s=s, xi=xi: e.dma_start(out=out[s * 128:(s + 1) * 128, :], in_=xs3[xi][:]),
                          reads=[("xs3", xi)], dma=True)
                S.emit("ph4")
    return nc


def make_core_inputs(inp, core, cfg=FULL):
    c = cfg
    b, j = divmod(core, c.NCH)
    x = np.asarray(inp["x"])[b]
    CH, NCH = c.CH, c.NCH
    order = [(j - d) % NCH for d in range(NCH)]
    xctx = np.ascontiguousarray(np.concatenate([x[o * CH:(o + 1) * CH] for o in order], 0), dtype=np.float32)
    pos = np.concatenate([np.arange(o * CH, (o + 1) * CH) for o in order]).astype(np.float32)
    inv_freq = (np.float32(ROPE_THETA) ** (-np.arange(0, 32, 2, dtype=np.float32) / np.float32(32))).astype(np.float32)
    ang = (pos[:, None] * inv_freq[None, :]).astype(np.float32)
    valid = [j - d >= 0 for d in range(NCH)]
    vflag = np.array([[0.0 if v else NEG_MASK for v in valid]], np.float32)
    bpc = CH // 256
    gb = np.full((c.NST, c.NB), -1e30, np.float32)
    for qt in range(c.NST):
        bq = qt // 2
        for n in range(c.NB):
            d, i = divmod(n, bpc)
            if (d == 0 and i < bq) or (d >= 1 and valid[d]):
                gb[qt, n] = 0.0
    k = np.arange(128)
    cE = np.zeros((c.NB, c.NB * 128), np.float32)
    for n in range(c.NB):
        cE[n, n * 128:(n + 1) * 128] = 1.0
    sq = lambda a: np.ascontiguousarray(np.asarray(a)[0], dtype=np.float32)
    return {
        "xctx": xctx,
        "w_in": sq(inp["w_in"]), "w_bm": sq(inp["w_branch_moba"]), "w_bf": sq(inp["w_branch_fox"]),
        "w_out": sq(inp["w_out"]), "w_up": sq(inp["w_up"]), "w_down": sq(inp["w_down"]),
        "gvec": np.stack([sq(inp["g_mix_pre"]), sq(inp["g_mix_post"]), sq(inp["g_mlp_pre"]), sq(inp["g_mlp_post"])], 0),
        "bfg": np.asarray(inp["b_forget"], np.float32).reshape(1, c.H),
        "rope_cos": np.cos(ang).astype(np.float32), "rope_sin": np.sin(ang).astype(np.float32),
        "gbias": gb.reshape(1, -1), "vflag": vflag,
        "cident": np.eye(128, dtype=np.float32),
        "ctri": (k[:, None] <= k[None, :]).astype(np.float32),
        "cE": cE,
    }


_NC_CACHE = {}


def kernel(**inputs):
    cfg = FULL
    if "nc" not in _NC_CACHE:
        _NC_CACHE["nc"] = build(cfg)
    nc = _NC_CACHE["nc"]
    n_cores = 2 * cfg.NCH
    shared = None
    in_maps = []
    for core in range(n_cores):
        m = make_core_inputs(inputs, core, cfg)
        if shared is None:
            shared = m
        else:
            for key in m:
                if key not in ("xctx", "rope_cos", "rope_sin", "gbias", "vflag"):
                    m[key] = shared[key]
        in_maps.append(m)
    res = run_bass_kernel_spmd(nc, in_maps, core_ids=list(range(n_cores)))
    B = n_cores // cfg.NCH
    outp = np.empty((B, cfg.TCTX, cfg.D), np.float32)
    for core in range(n_cores):
        b, j = divmod(core, cfg.NCH)
        outp[b, j * cfg.CH:(j + 1) * cfg.CH] = np.asarray(res.results[core]["out"], np.float32)
    return outp
```

```python
import contextlib
import numpy as np
import concourse.bass as bass
import concourse.mybir as mybir
from concourse.bass_utils import run_bass_kernel_spmd

F32 = mybir.dt.float32
BF16 = mybir.dt.bfloat16
AF = mybir.ActivationFunctionType
ALU = mybir.AluOpType
AX = mybir.AxisListType

ENGS = ("pe", "act", "dve", "pool", "sp")
N_DMA_SEMS = 24
N_HW_SEMS = 16
OP_LIMIT = None
RMS_EPS = 1e-6
ROPE_THETA = 500000.0
NEG_MASK = -30000.0


class _Op:
    __slots__ = ("eng", "fn", "is_dma", "signal", "sigval", "dsem", "dval", "deps", "waits")


class SemState:
    def __init__(self, nc, stack):
        self.sems = {}
        for e in ("pe", "act", "dve", "pool"):
            self.sems[("eng", e)] = stack.enter_context(nc.semaphore("s_" + e))
        for i in range(N_DMA_SEMS):
            self.sems[("dma", i)] = stack.enter_context(nc.semaphore("s_dma%d" % i))
        self.val = {k: 0 for k in self.sems}
        self.dma_count = {"hw": 0, "sw": 0}


def _is_psum(k):
    n = k[0] if isinstance(k, tuple) else k
    return isinstance(n, str) and (n.startswith("ps") or n == "oacc")


class Sched:
    def __init__(self, nc, ss):
        self.nc = nc
        self.ss = ss
        self.ops = []
        self.streams = {e: [] for e in ENGS}
        self.last_w = {}
        self.readers = {}
        self.dma_last = [None] * N_DMA_SEMS
        self.dma_val = [ss.val[("dma", i)] for i in range(N_DMA_SEMS)]

    def add(self, eng, fn, reads=(), writes=(), dma=False):
        if OP_LIMIT is not None and len(self.ops) >= OP_LIMIT:
            return None
        pr = [k for k in reads if _is_psum(k)]
        if pr:
            reads = [k for k in reads if not _is_psum(k)]
            writes = list(writes) + [k for k in pr if k not in writes]
        op = _Op()
        op.eng, op.fn, op.is_dma = eng, fn, dma
        op.signal, op.sigval, op.dsem, op.dval, op.waits = False, None, None, None, None
        deps = []
        for k in reads:
            w = self.last_w.get(k)
            if w is not None:
                deps.append(w)
        for k in writes:
            w = self.last_w.get(k)
            if w is not None:
                deps.append(w)
            rd = self.readers.get(k)
            if rd:
                deps.extend(rd.values())
        if dma:
            if eng == "pool":
                s = N_HW_SEMS + self.ss.dma_count["sw"] % (N_DMA_SEMS - N_HW_SEMS)
                self.ss.dma_count["sw"] += 1
            else:
                s = self.ss.dma_count["hw"] % N_HW_SEMS
                self.ss.dma_count["hw"] += 1
            if self.dma_last[s] is not None:
                deps.append(self.dma_last[s])
            self.dma_val[s] += 16
            op.dsem, op.dval = s, self.dma_val[s]
            self.dma_last[s] = op
        op.deps = deps
        gid = len(self.ops)
        for k in reads:
            self.readers.setdefault(k, {})[("dma", gid) if dma else eng] = op
        for k in writes:
            self.last_w[k] = op
            self.readers[k] = {}
        self.streams[eng].append(op)
        self.ops.append(op)
        return op

    @staticmethod
    def _skip(op, d):
        return (not d.is_dma) and d.eng == op.eng and op.eng == "pe" and not op.is_dma

    def emit(self, name):
        nc, ss = self.nc, self.ss
        for op in self.ops:
            for d in op.deps:
                if not d.is_dma and not self._skip(op, d):
                    d.signal = True
        for e in ("pe", "act", "dve", "pool"):
            c = ss.val[("eng", e)]
            for op in self.streams[e]:
                if op.signal and not op.is_dma:
                    c += 1
                    op.sigval = c
            ss.val[("eng", e)] = c
        for i in range(N_DMA_SEMS):
            ss.val[("dma", i)] = self.dma_val[i]
        for e in ENGS:
            known = {}
            for op in self.streams[e]:
                need = {}
                for d in op.deps:
                    if d.is_dma:
                        key, val = ("dma", d.dsem), d.dval
                    elif self._skip(op, d):
                        continue
                    else:
                        key, val = ("eng", d.eng), d.sigval
                    if known.get(key, 0) >= val:
                        continue
                    if need.get(key, 0) < val:
                        need[key] = val
                known.update(need)
                op.waits = need
        sems = ss.sems
        with nc.Block() as block:
            def run(e, eng):
                for op in self.streams[e]:
                    for k, v in op.waits.items():
                        eng.wait_ge(sems[k], v)
                    ins = op.fn(eng)
                    if op.is_dma:
                        ins.then_inc(sems[("dma", op.dsem)], 16)
                    elif op.signal:
                        ins.then_inc(sems[("eng", e)], 1)

            @block.tensor
            def _(eng):
                run("pe", eng)

            @block.scalar
            def _(eng):
                run("act", eng)

            @block.vector
            def _(eng):
                run("dve", eng)

            @block.gpsimd
            def _(eng):
                run("pool", eng)

            @block.sync
            def _(eng):
                run("sp", eng)
                for i in range(N_DMA_SEMS):
                    if self.dma_val[i] > 0:
                        eng.wait_ge(sems[("dma", i)], self.dma_val[i])
                for e in ("pe", "act", "dve", "pool"):
                    if ss.val[("eng", e)] > 0:
                        eng.wait_ge(sems[("eng", e)], ss.val[("eng", e)])


class Cfg:
    def __init__(self, D=2048, H=8, NCH=4, CH=1024, DFF=8192):
        self.D, self.H, self.NCH, self.CH, self.DFF = D, H, NCH, CH, DFF
        self.KC = D // 128
        self.NST = CH // 128
        self.NT = NCH * self.NST
        self.TCTX = NCH * CH
        self.NB = self.TCTX // 256
        self.NG = CH // 512
        self.HG = min(4, H)
        self.GW = self.HG * 128
        self.HW = H * 128
        self.o_mq, self.o_mk, self.o_mv = 0, self.HW, 2 * self.HW
        self.o_fq, self.o_fk, self.o_fv = 3 * self.HW, 4 * self.HW, 5 * self.HW
        self.o_ff = 6 * self.HW
        self.o_ga = self.o_ff + H
        self.o_gb = self.o_ga + D
        self.WIN = self.o_gb + D
        self.GD = min(512, D)
        self.FC = DFF // 128
        self.FB = min(1024, DFF)
        self.NFB = DFF // self.FB
        self.UW = min(256, DFF)
        self.TOPK = 3
        assert H >= 2 and self.NB >= 8


FULL = Cfg()


def _cp(e, out, in_):
    if hasattr(e, "tensor_copy"):
        return e.tensor_copy(out=out, in_=in_)
    return e.copy(out=out, in_=in_)


def MARK(name, S):
    if MARKS is not None:
        MARKS.append((name, len(S.ops)))


MARKS = None


def build(cfg=FULL, upto=4, debug=False):
    c = cfg
    D, H, KC, NCH, CH, NST, NT, TCTX = c.D, c.H, c.KC, c.NCH, c.CH, c.NST, c.NT, c.TCTX
    NB, NG, HG, GW, HW, GD = c.NB, c.NG, c.HG, c.GW, c.HW, c.GD
    H2 = 2 * H
    NN = CH // 512
    nc = bass.Bass("TRN2", target_bir_lowering=False)
    din = lambda n, s, dt=F32: nc.dram_tensor(n, s, dt, kind="ExternalInput").ap()
    dscr = lambda n, s, dt: nc.dram_tensor(n, s, dt, kind="Internal").ap()
    xctx = din("xctx", [TCTX, D])
    w_in = din("w_in", [D, c.WIN])
    w_bm = din("w_bm", [HW, D])
    w_bf = din("w_bf", [HW, D])
    w_out = din("w_out", [D, D])
    w_up = din("w_up", [D, c.DFF])
    w_down = din("w_down", [c.DFF, D])
    gvec = din("gvec", [4, D])
    bfg = din("bfg", [1, H])
    rcos = din("rope_cos", [TCTX, 16])
    rsin = din("rope_sin", [TCTX, 16])
    gbias_d = din("gbias", [1, NST * NB])
    vflag_d = din("vflag", [1, NCH])
    cident = din("cident", [128, 128])
    ctri = din("ctri", [128, 128])
    cE = din("cE", [NB, NB * 128])
    out = nc.dram_tensor("out", [CH, D], F32, kind="ExternalOutput").ap()
    KTd = dscr("KTd", [H2, 128, TCTX], BF16)
    QTd = dscr("QTd", [H2, 128, CH], BF16)
    Vd = dscr("Vd", [NT, 128, H2 * 128], BF16)
    sgd = dscr("sgd", [2, KC, 128, CH], BF16)
    x1d = dscr("x1d", [CH, D], F32)
    if debug:
        dbg_logf = nc.dram_tensor("dbg_logf", [128, NT * H], F32, kind="ExternalOutput").ap()
        dbg_kmT = nc.dram_tensor("dbg_kmT", [128, H * NB], F32, kind="ExternalOutput").ap()
        dbg_oT = nc.dram_tensor("dbg_oT", [128, H2 * CH], F32, kind="ExternalOutput").ap()
        dbg_mg = nc.dram_tensor("dbg_mg", [128, KC * CH], F32, kind="ExternalOutput").ap()

    scale = 128.0 ** -0.5

    with contextlib.ExitStack() as top:
        ss = SemState(nc, top)
        T = lambda st, n, s, dt: st.enter_context(nc.sbuf_tensor(n, s, dt))
        P = lambda st, n, s, dt: st.enter_context(nc.psum_tensor(n, s, dt))
        ident_b = T(top, "ident_b", [128, 128], BF16)
        tri_b = T(top, "tri_b", [128, 128], BF16)
        tri_f = T(top, "tri_f", [128, 128], F32)
        ones_f = T(top, "ones_f", [128, 128], F32)
        nones_f = T(top, "nones_f", [128, 128], F32)
        ones_b = T(top, "ones_b", [128, 128], BF16)
        c256 = T(top, "c256", [128, 1], BF16)
        epsc = T(top, "epsc", [128, 1], F32)
        E_b = T(top, "E_b", [NB, NB * 128], BF16)
        zff = T(top, "zff", [128, NT, H], F32)
        logf = T(top, "logf", [128, NT, H], F32)
        kmT = T(top, "kmT", [128, H, NB], BF16)
        stat = T(top, "stat", [128, NT, 8], F32)
        junk = T(top, "junk", [128, D], BF16)

        with contextlib.ExitStack() as ph:
            S = Sched(nc, ss)
            g_rep = T(ph, "g_rep", [128, D], F32)
            bfg_rep = T(ph, "bfg_rep", [128, H], F32)
            cos_sb = T(ph, "cos_sb", [128, NT, 16], F32)
            sin_sb = T(ph, "sin_sb", [128, NT, 16], F32)
            xs = [T(ph, "xs%d" % i, [128, D], F32) for i in range(2)]
            hb = [T(ph, "hb%d" % i, [128, D], BF16) for i in range(3)]
            hT = [T(ph, "hT%d" % i, [128, KC, CH], BF16) for i in range(2)]
            NWB = 3
            wb = [T(ph, "wb%d" % i, [128, KC, max(GD, GW)], BF16) for i in range(NWB)]
            vst = [T(ph, "vst%d" % i, [128, NST, GW], BF16) for i in range(2)]
            kst = [T(ph, "kst%d" % i, [128, HG, CH], BF16) for i in range(2)]
            kb = [T(ph, "kb%d" % i, [128, GW], BF16) for i in range(4)]
            rt = [T(ph, "rt%d" % i, [128, HG, 16], F32) for i in range(4)]
            wff = T(ph, "wff", [128, KC, H], BF16)
            lt = T(ph, "lt", [128, NT * H], F32)
            pst = [P(ph, "pst%d" % i, [128, 8, 128], BF16) for i in range(2)]
            psmm = [P(ph, "psmm%d" % i, [128, 512], F32) for i in range(3)]
            pstk = [P(ph, "pstk%d" % i, [128, 8, 128], BF16) for i in range(2)]
            pskm = P(ph, "pskm", [128, 512], F32)

            S.add("pool", lambda e: e.dma_start(out=ident_b[:], in_=cident), writes=["ident_b"], dma=True)
            S.add("pool", lambda e: e.dma_start(out=tri_b[:], in_=ctri), writes=["tri_b"], dma=True)
            S.add("sp", lambda e: e.dma_start(out=tri_f[:], in_=ctri), writes=["tri_f"], dma=True)
            S.add("pool", lambda e: e.dma_start(out=E_b[:], in_=cE), writes=["E_b"], dma=True)
            S.add("dve", lambda e: e.memset(ones_f[:], 1.0), writes=["ones_f"])
            S.add("dve", lambda e: e.memset(nones_f[:], -1.0), writes=["nones_f"])
            S.add("dve", lambda e: e.memset(ones_b[:], 1.0), writes=["ones_b"])
            S.add("dve", lambda e: e.memset(c256[:], 1.0 / 256.0), writes=["c256"])
            S.add("dve", lambda e: e.memset(epsc[:], RMS_EPS), writes=["epsc"])
            S.add("sp", lambda e: e.dma_start(out=g_rep[:], in_=gvec[0, :].partition_broadcast(128)),
                  writes=["g_rep"], dma=True)
            S.add("sp", lambda e: e.dma_start(out=bfg_rep[:], in_=bfg[0, :].partition_broadcast(128)),
                  writes=["bfg_rep"], dma=True)
            S.add("sp", lambda e: e.dma_start(out=cos_sb[:], in_=rcos.rearrange("(t p) f -> p t f", p=128)),
                  writes=["cos_sb"], dma=True)
            S.add("sp", lambda e: e.dma_start(out=sin_sb[:], in_=rsin.rearrange("(t p) f -> p t f", p=128)),
                  writes=["sin_sb"], dma=True)
            S.add("pool", lambda e: e.dma_start(
                out=wff[:], in_=w_in[:, c.o_ff:c.o_ff + H].rearrange("(kc p) c -> p kc c", p=128)),
                writes=["wff"], dma=True)

            cnt = {"mm": 0, "w": 0, "v": 0, "k": 0, "kb": 0, "tk": 0, "tk2": 0}

            def load_w(src2d, col0, width):
                i = cnt["w"] % NWB
                cnt["w"] += 1
                buf = wb[i]
                for k0 in range(0, KC, 8):
                    k1 = min(KC, k0 + 8)
                    S.add("pool", lambda e, k0=k0, k1=k1: e.dma_start(
                        out=buf[:, k0:k1, 0:width],
                        in_=src2d[k0 * 128:k1 * 128, col0:col0 + width].rearrange("(kc p) c -> p kc c", p=128)),
                        writes=[("wb", i)], dma=True)
                return buf, ("wb", i)

            def hT_stageA(d, s):
                t = d * NST + s
                xi = t % 2
                hi = t % 3
                S.add("sp", lambda e: e.dma_start(out=xs[xi][:], in_=xctx[t * 128:(t + 1) * 128, :]),
                      writes=[("xs", xi)], dma=True)
                S.add("dve", lambda e: e.scalar_tensor_tensor(
                    out=hb[hi][:], in0=xs[xi][:], scalar=1.0, in1=xs[xi][:], op0=ALU.mult, op1=ALU.mult,
                    accum_out=stat[:, t, 0:1]),
                    reads=[("xs", xi)], writes=[("hb", hi), ("stat", t)])
                S.add("act", lambda e: e.activation(out=stat[:, t, 1:2], in_=stat[:, t, 0:1], func=AF.Sqrt,
                                                    bias=epsc[:], scale=1.0 / D),
                      reads=[("stat", t), "epsc"], writes=[("stat", t)])
                S.add("dve", lambda e: e.reciprocal(out=stat[:, t, 2:3], in_=stat[:, t, 1:2]),
                      reads=[("stat", t)], writes=[("stat", t)])
                S.add("dve", lambda e: e.scalar_tensor_tensor(
                    out=hb[hi][:], in0=xs[xi][:], scalar=stat[:, t, 2:3], in1=g_rep[:], op0=ALU.mult,
                    op1=ALU.mult),
                    reads=[("xs", xi), ("stat", t), "g_rep"], writes=[("hb", hi)])

            def hT_stageB(d, s):
                hTd = hT[d % 2]
                t = d * NST + s
                hi = t % 3
                nq = min(4, KC)
                for q0 in range(0, KC, nq):
                    pi = cnt["tk"] % 2
                    cnt["tk"] += 1
                    for kk in range(nq):
                        S.add("pe", lambda e, pi=pi, kk=kk, q0=q0: e.transpose(
                            out=pst[pi][:, kk, :], in_=hb[hi][:, (q0 + kk) * 128:(q0 + kk + 1) * 128],
                            identity=ident_b[:]),
                            reads=[("hb", hi), "ident_b"], writes=[("pst", pi)])
                    S.add("act", lambda e, pi=pi, q0=q0: e.copy(
                        out=hTd[:, q0:q0 + nq, s * 128:(s + 1) * 128], in_=pst[pi][:, 0:nq, :]),
                        reads=[("pst", pi)], writes=[("hT", d % 2)])

            pendA = []
            pendB = []

            def drip(n=1):
                for _ in range(n):
                    if pendA:
                        a = pendA.pop(0)
                        hT_stageA(*a)
                        pendB.append(a)
                    if pendB and (len(pendB) > 1 or not pendA):
                        hT_stageB(*pendB.pop(0))

            def next_ps():
                i = cnt["mm"] % 3
                cnt["mm"] += 1
                return psmm[i], ("psmm", i)

            def rope_tail(s, d, mode, hbase, nh, ki):
                t = d * NST + s
                bi = s % 4
                kbt = kb[bi]
                pi = cnt["tk2"] % 2
                cnt["tk2"] += 1
                for hh in range(nh):
                    S.add("pe", lambda e, pi=pi, hh=hh: e.transpose(
                        out=pstk[pi][:, hh, :], in_=kbt[:, hh * 128:(hh + 1) * 128], identity=ident_b[:]),
                        reads=[("kb", bi), "ident_b"], writes=[("pstk", pi)])
                S.add("dve", lambda e, pi=pi: e.tensor_copy(
                    out=kst[ki][:, 0:nh, s * 128:(s + 1) * 128], in_=pstk[pi][:, 0:nh, :]),
                    reads=[("pstk", pi)], writes=[("kst", ki)])
                if mode == "k_rope" and s % 2 == 1:
                    kprev = kb[(s - 1) % 4]
                    for hh in range(nh):
                        S.add("pe", lambda e, hh=hh: e.matmul(
                            pskm[:, hh:hh + 1], lhsT=kprev[:, hh * 128:(hh + 1) * 128], rhs=c256[:],
                            start=True, stop=False),
                            reads=[("kb", (s - 1) % 4), "c256"], writes=["pskm"])
                        S.add("pe", lambda e, hh=hh: e.matmul(
                            pskm[:, hh:hh + 1], lhsT=kbt[:, hh * 128:(hh + 1) * 128], rhs=c256[:],
                            start=False, stop=True),
                            reads=[("kb", bi), "c256"], writes=["pskm"])
                    n = t // 2
                    S.add("dve", lambda e: e.tensor_copy(out=kmT[:, hbase:hbase + nh, n], in_=pskm[:, 0:nh]),
                          reads=["pskm"], writes=["kmT"])

            def tok_group(d, col0, width, mode, hbase, wbuf, wkey):
                hTd = hT[d % 2]
                nh = width // 128
                if mode == "v":
                    vi = cnt["v"] % 2
                    cnt["v"] += 1
                else:
                    ki = cnt["k"] % 2
                    cnt["k"] += 1
                for s in range(NST):
                    t = d * NST + s
                    ps, pk = next_ps()
                    for kc in range(KC):
                        S.add("pe", lambda e, ps=ps, kc=kc, s=s: e.matmul(
                            ps[:, 0:width], lhsT=hTd[:, kc, s * 128:(s + 1) * 128], rhs=wbuf[:, kc, 0:width],
                            start=(kc == 0), stop=(kc == KC - 1)),
                            reads=[("hT", d % 2), wkey], writes=[pk])
                    if mode == "v":
                        S.add("act", lambda e, ps=ps, s=s: e.copy(out=vst[vi][:, s, 0:width], in_=ps[:, 0:width]),
                              reads=[pk], writes=[("vst", vi)])
                        continue
                    bi = s % 4
                    kbt = kb[bi]
                    ps3 = ps[:, 0:width].rearrange("p (h c) -> p h c", c=128)
                    kb3 = kbt[:, 0:width].rearrange("p (h c) -> p h c", c=128)
                    cosb = cos_sb[:, t:t + 1, :].broadcast_to([128, nh, 16])
                    sinb = sin_sb[:, t:t + 1, :].broadcast_to([128, nh, 16])
                    S.add("act", lambda e, ps=ps, kbt=kbt: e.copy(out=kbt[:, 0:width], in_=ps[:, 0:width]),
                          reads=[pk], writes=[("kb", bi)])
                    x1, x2 = ps3[:, :, 0:16], ps3[:, :, 16:32]
                    r = [rt[i][:, 0:nh, :] for i in range(4)]
                    S.add("dve", lambda e, x1=x1, cosb=cosb, r=r: e.tensor_tensor(out=r[0], in0=x1, in1=cosb, op=ALU.mult),
                          reads=[pk, "cos_sb"], writes=[("rt", 0)])
                    S.add("dve", lambda e, x2=x2, sinb=sinb, r=r: e.tensor_tensor(out=r[1], in0=x2, in1=sinb, op=ALU.mult),
                          reads=[pk, "sin_sb"], writes=[("rt", 1)])
                    S.add("dve", lambda e, x2=x2, cosb=cosb, r=r: e.tensor_tensor(out=r[2], in0=x2, in1=cosb, op=ALU.mult),
                          reads=[pk, "cos_sb"], writes=[("rt", 2)])
                    S.add("dve", lambda e, x1=x1, sinb=sinb, r=r: e.tensor_tensor(out=r[3], in0=x1, in1=sinb, op=ALU.mult),
                          reads=[pk, "sin_sb"], writes=[("rt", 3)])
                    S.add("dve", lambda e, kb3=kb3, r=r: e.tensor_tensor(out=kb3[:, :, 0:16], in0=r[0], in1=r[1],
                                                                       op=ALU.subtract),
                          reads=[("rt", 0), ("rt", 1), ("kb", bi)], writes=[("kb", bi)])
                    S.add("dve", lambda e, kb3=kb3, r=r: e.tensor_tensor(out=kb3[:, :, 16:32], in0=r[2], in1=r[3],
                                                                       op=ALU.add),
                          reads=[("rt", 2), ("rt", 3), ("kb", bi)], writes=[("kb", bi)])
                    if s >= 1:
                        rope_tail(s - 1, d, mode, hbase, nh, ki)
                if mode != "v":
                    rope_tail(NST - 1, d, mode, hbase, nh, ki)
                if mode == "v":
                    S.add("sp", lambda e: e.dma_start(
                        out=Vd[d * NST:(d + 1) * NST, :, hbase * 128:hbase * 128 + width].rearrange("t p c -> p t c"),
                        in_=vst[vi][:, :, 0:width]),
                        reads=[("vst", vi)], dma=True)
                else:
                    dst = KTd[hbase:hbase + nh, :, d * CH:(d + 1) * CH] if mode == "k_rope" else QTd[hbase:hbase + nh, :, :]
                    S.add("sp", lambda e: e.dma_start(out=dst.rearrange("h p t -> p h t"), in_=kst[ki][:, 0:nh, :]),
                          reads=[("kst", ki)], dma=True)
                drip()

            def feat_group(d, col0, width, mode, hbase, wbuf, wkey, kc0=0):
                hTd = hT[d % 2]
                nh = width // 128
                for h0 in range(0, nh, HG):
                    h1 = min(nh, h0 + HG)
                    ki = cnt["k"] % 2
                    cnt["k"] += 1
                    for hh in range(h0, h1):
                        for n0 in range(0, CH, 512):
                            ps, pk = next_ps()
                            for kc in range(KC):
                                S.add("pe", lambda e, ps=ps, kc=kc, hh=hh, n0=n0: e.matmul(
                                    ps[:, 0:512], lhsT=wbuf[:, kc, hh * 128:(hh + 1) * 128],
                                    rhs=hTd[:, kc, n0:n0 + 512], start=(kc == 0), stop=(kc == KC - 1)),
                                    reads=[("hT", d % 2), wkey], writes=[pk])
                            if mode == "gate":
                                S.add("act", lambda e, ps=ps, hh=hh, n0=n0, ki=ki, h0=h0: e.activation(
                                    out=kst[ki][:, hh - h0, n0:n0 + 512], in_=ps[:, 0:512], func=AF.Sigmoid),
                                    reads=[pk], writes=[("kst", ki)])
                            else:
                                S.add("dve", lambda e, ps=ps, hh=hh, n0=n0, ki=ki, h0=h0: e.tensor_copy(
                                    out=kst[ki][:, hh - h0, n0:n0 + 512], in_=ps[:, 0:512]),
                                    reads=[pk], writes=[("kst", ki)])
                    if mode == "kT":
                        dst = KTd[hbase + h0:hbase + h1, :, d * CH:(d + 1) * CH]
                    elif mode == "qT":
                        dst = QTd[hbase + h0:hbase + h1, :, :]
                    else:
                        dst = sgd[hbase, kc0 + h0:kc0 + h1, :, :]
                    S.add("sp", lambda e, ki=ki, dst=dst, h0=h0, h1=h1: e.dma_start(
                        out=dst.rearrange("h p t -> p h t"), in_=kst[ki][:, 0:h1 - h0, :]),
                        reads=[("kst", ki)], dma=True)
                drip()

            def ff_group(d):
                hTd = hT[d % 2]
                for s in range(NST):
                    t = d * NST + s
                    ps, pk = next_ps()
                    for kc in range(KC):
                        S.add("pe", lambda e, ps=ps, kc=kc, s=s: e.matmul(
                            ps[:, 0:H], lhsT=hTd[:, kc, s * 128:(s + 1) * 128], rhs=wff[:, kc, :],
                            start=(kc == 0), stop=(kc == KC - 1)),
                            reads=[("hT", d % 2), "wff"], writes=[pk])
                    S.add("dve", lambda e, ps=ps, t=t: e.tensor_tensor(out=zff[:, t, :], in0=ps[:, 0:H], in1=bfg_rep[:],
                                                                     op=ALU.add),
                          reads=[pk, "bfg_rep"], writes=["zff"])
                drip()

            MARK("consts", S)
            for s in range(NST):
                hT_stageA(0, s)
                if s >= 1:
                    hT_stageB(0, s - 1)
            hT_stageB(0, NST - 1)
            tasks = []
            for d in range(NCH):
                if d + 1 < NCH:
                    tasks.append((None, None, lambda d=d: pendA.extend((d + 1, s) for s in range(NST))))
                for g0 in range(0, H, HG):
                    tasks.append((c.o_mk + g0 * 128, GW, lambda wb_, wk_, d=d, g0=g0: tok_group(
                        d, c.o_mk + g0 * 128, GW, "k_rope", g0, wb_, wk_)))
                for g0 in range(0, H, HG):
                    tasks.append((c.o_mv + g0 * 128, GW, lambda wb_, wk_, d=d, g0=g0: tok_group(
                        d, c.o_mv + g0 * 128, GW, "v", g0, wb_, wk_)))
                for g0 in range(0, H, HG):
                    tasks.append((c.o_fk + g0 * 128, GW, lambda wb_, wk_, d=d, g0=g0: feat_group(
                        d, c.o_fk + g0 * 128, GW, "kT", H + g0, wb_, wk_)))
                for g0 in range(0, H, HG):
                    tasks.append((c.o_fv + g0 * 128, GW, lambda wb_, wk_, d=d, g0=g0: tok_group(
                        d, c.o_fv + g0 * 128, GW, "v", H + g0, wb_, wk_)))
                tasks.append((None, None, lambda d=d: ff_group(d)))
                tasks.append((None, None, lambda: drip(2 * NST + 2)))
                if d == 0:
                    for g0 in range(0, H, HG):
                        tasks.append((c.o_mq + g0 * 128, GW, lambda wb_, wk_, g0=g0: tok_group(
                            0, c.o_mq + g0 * 128, GW, "q_rope", g0, wb_, wk_)))
                    for g0 in range(0, H, HG):
                        tasks.append((c.o_fq + g0 * 128, GW, lambda wb_, wk_, g0=g0: feat_group(
                            0, c.o_fq + g0 * 128, GW, "qT", H + g0, wb_, wk_)))
                    for c0 in range(0, D, GD):
                        tasks.append((c.o_ga + c0, GD, lambda wb_, wk_, c0=c0: feat_group(
                            0, c.o_ga + c0, GD, "gate", 0, wb_, wk_, kc0=c0 // 128)))
                    for c0 in range(0, D, GD):
                        tasks.append((c.o_gb + c0, GD, lambda wb_, wk_, c0=c0: feat_group(
                            0, c.o_gb + c0, GD, "gate", 1, wb_, wk_, kc0=c0 // 128)))
            PREF = NWB - 1
            wtasks = [i for i, tk_ in enumerate(tasks) if tk_[0] is not None]
            loaded = {}
            nxt = 0
            for i, (col0, width, fn) in enumerate(tasks):
                if col0 is None:
                    fn()
                    continue
                pos = wtasks.index(i)
                while nxt < len(wtasks) and nxt <= pos + PREF:
                    ti = wtasks[nxt]
                    loaded[ti] = load_w(w_in, tasks[ti][0], tasks[ti][1])
                    nxt += 1
                fn(*loaded.pop(i))
            MARK("proj_done", S)
            zf = zff[:].rearrange("p t h -> p (t h)")
            lf = logf[:].rearrange("p t h -> p (t h)")
            S.add("act", lambda e: e.activation(out=lt[:], in_=zf, func=AF.Exp, scale=-1.0), reads=["zff"], writes=["lt"])
            S.add("act", lambda e: e.activation(out=lt[:], in_=lt[:], func=AF.Ln, bias=1.0), reads=["lt"], writes=["lt"])
            S.add("dve", lambda e: e.tensor_scalar(out=lf, in0=lt[:], scalar1=-1.0, scalar2=None, op0=ALU.mult),
                  reads=["lt"], writes=["logf"])
            if debug:
                S.add("sp", lambda e: e.dma_start(out=dbg_logf, in_=lf), reads=["logf"], dma=True)
                S.add("pool", lambda e: e.dma_start(out=dbg_kmT, in_=kmT[:].rearrange("p h n -> p (h n)")),
                      reads=["kmT"], dma=True)
            S.emit("ph1")
        if upto < 2:
            return nc

        with contextlib.ExitStack() as mid:
            R1 = T(mid, "R1", [128, max(H2, KC), CH], BF16)
            R2 = T(mid, "R2", [128, max(KC, 16), CH], BF16)
            oT, h2T, mergedT = R1, R1, R2

            mid2 = contextlib.ExitStack()
            mid2.__enter__()
            Wbm = T(mid2, "Wbm", [128, H, D], BF16)
            Wbf = T(mid2, "Wbf", [128, H, D], BF16)
            with contextlib.ExitStack() as ph:
                S = Sched(nc, ss)
                KTb = [T(ph, "KTb%d" % i, [128, TCTX], BF16) for i in range(2)]
                Vb = [T(ph, "Vb%d" % i, [128, NT, 130], BF16) for i in range(2)]
                QTb = [T(ph, "QTb%d" % i, [128, CH], BF16) for i in range(2)]
                pT = [T(ph, "pT%d" % i, [128, 512], BF16) for i in range(4)]
                sqb = [T(ph, "sqb%d" % i, [128, 512], BF16) for i in range(2)]
                nmx = T(ph, "nmx", [128, 2, 32], F32)
                negC = T(ph, "negC", [128, H2], F32)
                gbias_rep = T(ph, "gbias_rep", [128, NST * NB], F32)
                vfl_rep = T(ph, "vfl_rep", [128, NCH], F32)
                Fsb = T(ph, "Fsb", [128, NT, H], F32)
                FrefB = T(ph, "FrefB", [128, NG, H], F32)
                biasF = T(ph, "biasF", [128, NG, NT, H], F32)
                bfh = [T(ph, "bfh%d" % i, [128, NG, NT], F32) for i in range(2)]
                gmb = [T(ph, "gmb%d" % i, [128, NB], F32) for i in range(2)]
                m8 = [T(ph, "m8_%d" % i, [128, 8], F32) for i in range(2)]
                b1 = [T(ph, "b1_%d" % i, [128, NB], F32) for i in range(2)]
                bb = [T(ph, "bb%d" % i, [128, NB], BF16) for i in range(2)]
                biasT = [T(ph, "biasT%d" % i, [NB, CH], BF16) for i in range(2)]
                otok = [T(ph, "otok%d" % i, [128, 4, 128], BF16) for i in range(2)]
                rc = T(ph, "rc", [128, 8], F32)
                psS = [P(ph, "psS%d" % i, [128, 512], F32) for i in range(2)]
                oacc_t = [[P(ph, "oacc%d_%d" % (i, k), [128, 512], F32) for k in range(2)] for i in range(2)]
                oacc = [[t[:, 0:260].rearrange("p (r c) -> p r c", c=130) for t in row] for row in oacc_t]
                psX = [P(ph, "psX%d" % i, [128, 512], F32) for i in range(2)]
                psXb = [p[:].bitcast(BF16) for p in psX]
                cn = {"x": 0, "s": 0, "p": 0, "a": 0, "sq": 0, "g": 0}

                def next_x():
                    i = cn["x"] % 2
                    cn["x"] += 1
                    return i, ("psX", i)

                S.add("sp", lambda e: e.dma_start(out=gbias_rep[:], in_=gbias_d[0, :].partition_broadcast(128)),
                      writes=["gbias_rep"], dma=True)
                S.add("sp", lambda e: e.dma_start(out=vfl_rep[:], in_=vflag_d[0, :].partition_broadcast(128)),
                      writes=["vfl_rep"], dma=True)
                for i in range(2):
                    S.add("pool", lambda e, i=i: e.memset(Vb[i][:, :, 128:129], 1.0), writes=[("V", i)])

                def load_head(h, par):
                    S.add("sp", lambda e: e.dma_start(out=KTb[par][:], in_=KTd[h]), writes=[("KT", par)], dma=True)
                    S.add("sp", lambda e: e.dma_start(out=QTb[par][:], in_=QTd[h]), writes=[("QT", par)], dma=True)
                    for t0 in range(0, NT, 8):
                        S.add("sp", lambda e, t0=t0: e.dma_start(
                            out=Vb[par][:, t0:t0 + 8, 0:128],
                            in_=Vd[t0:t0 + 8, :, h * 128:(h + 1) * 128].rearrange("t p c -> p t c")),
                            writes=[("V", par)], dma=True)

                load_head(0, 0)

                xi, xk = next_x()
                psF = psX[xi][:, 0:NT * H].rearrange("p (t h) -> p t h", h=H)
                for d in range(NCH):
                    for j in range(NST):
                        t = d * NST + j
                        terms = [(tri_f, t)] + [(ones_f, d * NST + j2) for j2 in range(j)]
                        if d >= 1:
                            terms += [(nones_f, d2 * NST + j2) for d2 in range(1, d + 1) for j2 in range(NST)]
                        for i, (mm, tt) in enumerate(terms):
                            S.add("pe", lambda e, t=t, mm=mm, tt=tt, i=i, n=len(terms): e.matmul(
                                psF[:, t, :], lhsT=mm[:], rhs=logf[:, tt, :], start=(i == 0), stop=(i == n - 1)),
                                reads=["logf", "tri_f", "ones_f", "nones_f"], writes=[xk])
                S.add("dve", lambda e: e.tensor_copy(out=Fsb[:], in_=psF), reads=[xk], writes=["Fsb"])
                xi2, xk2 = next_x()
                psR = psX[xi2][:, 0:NG * H].rearrange("p (g h) -> p g h", h=H)
                for g in range(NG):
                    n = 4 * g + 2
                    for j2 in range(n):
                        S.add("pe", lambda e, g=g, j2=j2, n=n: e.matmul(
                            psR[:, g, :], lhsT=ones_f[:], rhs=logf[:, j2, :], start=(j2 == 0), stop=(j2 == n - 1)),
                            reads=["logf", "ones_f"], writes=[xk2])
                S.add("dve", lambda e: e.tensor_copy(out=FrefB[:], in_=psR), reads=[xk2], writes=["FrefB"])
                for g in range(NG):
                    S.add("dve", lambda e, g=g: e.tensor_tensor(
                        out=biasF[:, g], in0=FrefB[:, g:g + 1, :].broadcast_to([128, NT, H]), in1=Fsb[:],
                        op=ALU.subtract), reads=["FrefB", "Fsb"], writes=["biasF"])
                    S.add("dve", lambda e, g=g: e.tensor_tensor(
                        out=biasF[:, g].rearrange("p (d j) h -> p d (j h)", d=NCH),
                        in0=biasF[:, g].rearrange("p (d j) h -> p d (j h)", d=NCH),
                        in1=vfl_rep[:].unsqueeze(2).broadcast_to([128, NCH, NST * H]), op=ALU.add),
                        reads=["biasF", "vfl_rep"], writes=["biasF"])

                def head_norm(par, h):
                    tiles = [(KTb[par], ("KT", par), n0) for n0 in range(0, TCTX, 512)]
                    tiles += [(QTb[par], ("QT", par), n0) for n0 in range(0, CH, 512)]
                    for i, (buf, bkey, n0) in enumerate(tiles):
                        k = cn["sq"] % 2
                        cn["sq"] += 1
                        S.add("pool", lambda e, buf=buf, n0=n0, k=k: e.tensor_tensor(
                            out=sqb[k][:], in0=buf[:, n0:n0 + 512], in1=buf[:, n0:n0 + 512], op=ALU.mult),
                            reads=[bkey], writes=[("sqb", k)])
                        xi, xk = next_x()
                        S.add("pe", lambda e, xi=xi, k=k: e.matmul(psX[xi][:, 0:512], lhsT=ones_b[:], rhs=sqb[k][:],
                                                                  start=True, stop=True),
                              reads=[("sqb", k), "ones_b"], writes=[xk])
                        S.add("dve", lambda e, xi=xi, i=i: e.tensor_reduce(
                            out=nmx[:, par, i:i + 1], in_=psX[xi][:, 0:512], axis=AX.X, op=ALU.max),
                            reads=[xk], writes=[("nmx", par)])
                    nt_ = len(tiles)
                    S.add("dve", lambda e: e.tensor_reduce(out=negC[:, h:h + 1], in_=nmx[:, par, 0:nt_], axis=AX.X,
                                                           op=ALU.max),
                          reads=[("nmx", par)], writes=[("negC", h)])
                    S.add("dve", lambda e: e.tensor_scalar(out=negC[:, h:h + 1], in0=negC[:, h:h + 1],
                                                           scalar1=-1.02 * scale, scalar2=None, op0=ALU.mult),
                          reads=[("negC", h)], writes=[("negC", h)])

                def moba_gate(par, hm):
                    for qt in range(NST):
                        k = cn["g"] % 2
                        cn["g"] += 1
                        xi, xk = next_x()
                        gsl = gbias_rep[:, qt * NB:(qt + 1) * NB]
                        S.add("pe", lambda e, xi=xi, qt=qt: e.matmul(
                            psX[xi][:, 0:NB], lhsT=QTb[par][:, qt * 128:(qt + 1) * 128], rhs=kmT[:, hm, :],
                            start=True, stop=True), reads=[("QT", par), "kmT"], writes=[xk])
                        S.add("dve", lambda e, xi=xi, k=k, gsl=gsl: e.tensor_tensor(
                            out=gmb[k][:], in0=psX[xi][:, 0:NB], in1=gsl, op=ALU.add),
                            reads=[xk, "gbias_rep"], writes=[("gmb", k)])
                        S.add("dve", lambda e, k=k: e.max(out=m8[k][:], in_=gmb[k][:]),
                              reads=[("gmb", k)], writes=[("m8", k)])
                        S.add("dve", lambda e, k=k: e.tensor_scalar(
                            out=b1[k][:], in0=gmb[k][:], scalar1=m8[k][:, c.TOPK - 1:c.TOPK], scalar2=-NEG_MASK,
                            op0=ALU.is_ge, op1=ALU.mult), reads=[("gmb", k), ("m8", k)], writes=[("b1", k)])
                        S.add("dve", lambda e, k=k, gsl=gsl: e.scalar_tensor_tensor(
                            out=bb[k][:], in0=b1[k][:], scalar=NEG_MASK, in1=gsl, op0=ALU.add, op1=ALU.min),
                            reads=[("b1", k), "gbias_rep"], writes=[("bb", k)])
                        S.add("dve", lambda e, k=k, qt=qt: e.memset(bb[k][:, qt // 2:qt // 2 + 1], 0.0),
                              reads=[("bb", k)], writes=[("bb", k)])
                        xi2, xk2 = next_x()
                        S.add("pe", lambda e, xi2=xi2, k=k: e.transpose(out=psXb[xi2][0:NB, 0:128], in_=bb[k][:],
                                                                        identity=ident_b[:]),
                              reads=[("bb", k), "ident_b"], writes=[xk2])
                        S.add("dve", lambda e, xi2=xi2, qt=qt: e.tensor_copy(
                            out=biasT[par][0:NB, qt * 128:(qt + 1) * 128], in_=psXb[xi2][0:NB, 0:128]),
                            reads=[xk2], writes=[("biasT", par)])

                deferred = []

                def flush_deferred():
                    while deferred:
                        deferred.pop(0)()

                def attention(par, h, moba, mid_hook=None):
                    for g in range(NG):
                        a = cn["a"] % 2
                        cn["a"] += 1
                        tiles = [(d, j) for d in range(NCH - 1, 0, -1) for j in range(NST)]
                        tiles += [(0, j) for j in range(4 * g + 4)]

                        def qk_step(idx, d, j, g=g, a=a):
                            t = d * NST + j
                            c0 = 0 if (d > 0 or j < 4 * g) else (j - 4 * g) * 128
                            N = 512 - c0
                            q0 = g * 512 + c0
                            si = cn["s"] % 2
                            cn["s"] += 1
                            ps, pk = psS[si], ("psS", si)
                            S.add("pe", lambda e: e.matmul(
                                ps[:, 0:N], lhsT=KTb[par][:, t * 128:(t + 1) * 128], rhs=QTb[par][:, q0:q0 + N],
                                start=True, stop=not moba), reads=[("KT", par), ("QT", par)], writes=[pk])
                            if moba:
                                n = t // 2
                                S.add("pe", lambda e: e.matmul(
                                    ps[:, 0:N], lhsT=E_b[0:NB, n * 128:(n + 1) * 128], rhs=biasT[par][0:NB, q0:q0 + N],
                                    start=False, stop=True), reads=["E_b", ("biasT", par)], writes=[pk])
                            pi = cn["p"] % 4
                            cn["p"] += 1
                            pt, ptk = pT[pi], ("pT", pi)
                            if moba:
                                bias_ap, bkey = negC[:, h:h + 1], ("negC", h)
                            else:
                                bias_ap, bkey = bfh[par][:, g, t:t + 1], ("bfh", par)
                            S.add("act", lambda e: e.activation(
                                out=pt[:, 0:N], in_=ps[:, 0:N], func=AF.Exp, bias=bias_ap, scale=scale),
                                reads=[pk, bkey], writes=[ptk])
                            if d == 0 and j >= 4 * g:
                                S.add("pool", lambda e: e.tensor_tensor(out=pt[:, 0:128], in0=pt[:, 0:128],
                                                                        in1=tri_b[:], op=ALU.mult),
                                      reads=[ptk, "tri_b"], writes=[ptk])
                            return (idx, d, j, t, c0, pt, ptk)

                        def pv_step(st, g=g, a=a):
                            idx, d, j, t, c0, pt, ptk = st
                            for qs in range(c0 // 128, 4):
                                lo = qs * 128 - c0
                                S.add("pe", lambda e, qs=qs, lo=lo, first=(idx == 0 and qs % 2 == 0),
                                      last=(d == 0 and j == 4 * g + qs): e.matmul(
                                    oacc[a][qs // 2][:, qs % 2, 0:129], lhsT=pt[:, lo:lo + 128],
                                    rhs=Vb[par][:, t, 0:129], start=first, stop=last, skip_group_check=True),
                                    reads=[ptk, ("V", par)], writes=[("oacc", a, qs // 2)])

                        prev = None
                        for idx, (d, j) in enumerate(tiles):
                            cur = qk_step(idx, d, j)
                            if prev is not None:
                                pv_step(prev)
                            prev = cur
                            if idx == 2:
                                flush_deferred()
                        pv_step(prev)
                        ot = otok[a]
                        for qs in range(4):
                            oa = oacc[a][qs // 2]
                            S.add("dve", lambda e, oa=oa, qs=qs, a=a: e.reciprocal(out=rc[:, a * 4 + qs:a * 4 + qs + 1],
                                                                                 in_=oa[:, qs % 2, 128:129]),
                                  reads=[("oacc", a, qs // 2)], writes=[("rc", a, qs)])
                            S.add("dve", lambda e, oa=oa, qs=qs, ot=ot, a=a: e.tensor_scalar(
                                out=ot[:, qs, :], in0=oa[:, qs % 2, 0:128], scalar1=rc[:, a * 4 + qs:a * 4 + qs + 1],
                                scalar2=None, op0=ALU.mult),
                                reads=[("oacc", a, qs // 2), ("rc", a, qs)], writes=[("otok", a)])

                        def tail(g=g, a=a, ot=ot):
                            xi, xk = next_x()
                            for qs in range(4):
                                S.add("pe", lambda e, xi=xi, qs=qs: e.transpose(
                                    out=psXb[xi][:, qs * 128:(qs + 1) * 128], in_=ot[:, qs, :], identity=ident_b[:]),
                                    reads=[("otok", a), "ident_b"], writes=[xk])
                            S.add("act", lambda e, xi=xi: e.copy(out=oT[:, h, g * 512:(g + 1) * 512],
                                                                 in_=psXb[xi][:, 0:512]),
                                  reads=[xk], writes=["oT"])
                        deferred.append(tail)
                        if g == 0 and mid_hook is not None:
                            mid_hook()
                    if NG == 1 and mid_hook is not None:
                        pass

                def prep(h):
                    par = h % 2
                    head_norm(par, h)
                    if h < H:
                        moba_gate(par, h)
                    else:
                        S.add("dve", lambda e: e.tensor_scalar(
                            out=bfh[par][:], in0=biasF[:, :, :, h - H], scalar1=negC[:, h:h + 1], scalar2=None,
                            op0=ALU.add), reads=["biasF", ("negC", h)], writes=[("bfh", par)])

                prep(0)
                for h in range(H2):
                    par = h % 2

                    def hook(h=h):
                        if h + 1 < H2:
                            prep(h + 1)
                        if h == H2 - 3:
                            for h0 in range(0, H, 4):
                                h1 = min(H, h0 + 4)
                                for (wsb, wdr, nm) in ((Wbm, w_bm, "Wbm"), (Wbf, w_bf, "Wbf")):
                                    S.add("pool", lambda e, wsb=wsb, wdr=wdr, h0=h0, h1=h1: e.dma_start(
                                        out=wsb[:, h0:h1, :],
                                        in_=wdr[h0 * 128:h1 * 128, :].rearrange("(h p) c -> p h c", p=128)),
                                        writes=[nm], dma=True)
                    if h + 1 < H2:
                        load_head(h + 1, (h + 1) % 2)
                    attention(par, h, h < H, mid_hook=hook)
                flush_deferred()
                if debug:
                    S.add("pool", lambda e: e.dma_start(out=dbg_oT, in_=oT[:, 0:H2, :].rearrange("p h t -> p (h t)")),
                          reads=["oT"], dma=True)
                S.emit("ph2")
            if upto < 3:
                return nc

            with contextlib.ExitStack() as ph:
                S = Sched(nc, ss)
                sga = [T(ph, "sga%d" % i, [128, CH], BF16) for i in range(2)]
                sgb = [T(ph, "sgb%d" % i, [128, CH], BF16) for i in range(2)]
                t1 = [T(ph, "t1_%d" % i, [128, 512], F32) for i in range(2)]
                t2 = [T(ph, "t2_%d" % i, [128, 512], F32) for i in range(2)]
                psm = [[P(ph, "psm%d_%d" % (i, n), [128, 512], F32) for n in range(NN)] for i in range(2)]
                psf = [[P(ph, "psf%d_%d" % (i, n), [128, 512], F32) for n in range(NN)] for i in range(2)]
                tc_ = 0
                for cc in range(KC):
                    a = cc % 2
                    S.add("sp", lambda e, a=a, cc=cc: e.dma_start(out=sga[a][:], in_=sgd[0, cc]), writes=[("sga", a)], dma=True)
                    S.add("sp", lambda e, a=a, cc=cc: e.dma_start(out=sgb[a][:], in_=sgd[1, cc]), writes=[("sgb", a)], dma=True)
                    for n in range(NN):
                        for hh in range(H):
                            S.add("pe", lambda e, a=a, n=n, hh=hh, cc=cc: e.matmul(
                                psm[a][n][:, 0:512], lhsT=Wbm[:, hh, cc * 128:(cc + 1) * 128],
                                rhs=oT[:, hh, n * 512:(n + 1) * 512], start=(hh == 0), stop=(hh == H - 1)),
                                reads=["Wbm", "oT"], writes=[("psm", a, n)])
                        for hh in range(H):
                            S.add("pe", lambda e, a=a, n=n, hh=hh, cc=cc: e.matmul(
                                psf[a][n][:, 0:512], lhsT=Wbf[:, hh, cc * 128:(cc + 1) * 128],
                                rhs=oT[:, H + hh, n * 512:(n + 1) * 512], start=(hh == 0), stop=(hh == H - 1)),
                                reads=["Wbf", "oT"], writes=[("psf", a, n)])
                        k = tc_ % 2
                        tc_ += 1
                        S.add("dve", lambda e, a=a, n=n, k=k: e.tensor_tensor(
                            out=t1[k][:], in0=psm[a][n][:, 0:512], in1=sga[a][:, n * 512:(n + 1) * 512], op=ALU.mult),
                            reads=[("psm", a, n), ("sga", a)], writes=[("t1", k)])
                        S.add("dve", lambda e, a=a, n=n, k=k: e.tensor_tensor(
                            out=t2[k][:], in0=psf[a][n][:, 0:512], in1=sgb[a][:, n * 512:(n + 1) * 512], op=ALU.mult),
                            reads=[("psf", a, n), ("sgb", a)], writes=[("t2", k)])
                        S.add("pool", lambda e, cc=cc, n=n, k=k: e.tensor_tensor(
                            out=mergedT[:, cc, n * 512:(n + 1) * 512], in0=t1[k][:], in1=t2[k][:], op=ALU.add),
                            reads=[("t1", k), ("t2", k)], writes=["mergedT"])
                if debug:
                    S.add("pool", lambda e: e.dma_start(out=dbg_mg, in_=mergedT[:, 0:KC, :].rearrange("p h t -> p (h t)")),
                          reads=["mergedT"], dma=True)
                S.emit("ph3a")
            mid2.close()

            with contextlib.ExitStack() as ph:
                S = Sched(nc, ss)
                Wo = T(ph, "Wo", [128, KC, D], BF16)
                gpost = T(ph, "gpost", [128, D], F32)
                gpre2 = T(ph, "gpre2", [128, D], F32)
                xs2 = [T(ph, "xs2_%d" % i, [128, D], F32) for i in range(2)]
                tt_ = T(ph, "tt_", [128, D], F32)
                hb2 = [T(ph, "hb2_%d" % i, [128, D], BF16) for i in range(2)]
                st3 = T(ph, "st3", [128, NST, 16], F32)
                ncg = D // GD
                psM = [P(ph, "psM%d" % i, [128, 512], F32) for i in range(6)]
                pst2 = [P(ph, "pst2_%d" % i, [128, 8, 128], BF16) for i in range(2)]
                S.add("sp", lambda e: e.dma_start(out=gpost[:], in_=gvec[1, :].partition_broadcast(128)),
                      writes=["gpost"], dma=True)
                S.add("sp", lambda e: e.dma_start(out=gpre2[:], in_=gvec[2, :].partition_broadcast(128)),
                      writes=["gpre2"], dma=True)
                for c0 in range(0, D, GD):
                    for k0 in range(0, KC, 8):
                        k1 = min(KC, k0 + 8)
                        S.add("pool", lambda e, c0=c0, k0=k0, k1=k1: e.dma_start(
                            out=Wo[:, k0:k1, c0:c0 + GD],
                            in_=w_out[k0 * 128:k1 * 128, c0:c0 + GD].rearrange("(kc p) c -> p kc c", p=128)),
                            writes=[("Wo", c0)], dma=True)
                mc = 0
                tk = 0
                for s in range(NST):
                    xi = s % 2
                    S.add("sp", lambda e, s=s, xi=xi: e.dma_start(out=xs2[xi][:], in_=xctx[s * 128:(s + 1) * 128, :]),
                          writes=[("xs2", xi)], dma=True)
                    pss = []
                    for cg in range(ncg):
                        pi = mc % 6
                        mc += 1
                        pss.append(pi)
                        for kc in range(KC):
                            S.add("pe", lambda e, pi=pi, kc=kc, s=s, cg=cg: e.matmul(
                                psM[pi][:, 0:GD], lhsT=mergedT[:, kc, s * 128:(s + 1) * 128],
                                rhs=Wo[:, kc, cg * GD:(cg + 1) * GD], start=(kc == 0), stop=(kc == KC - 1)),
                                reads=["mergedT", ("Wo", cg * GD)], writes=[("psM", pi)])
                        S.add("act", lambda e, pi=pi, s=s, cg=cg: e.activation(
                            out=junk[:, 0:GD], in_=psM[pi][:, 0:GD], func=AF.Square, accum_out=st3[:, s, cg:cg + 1]),
                            reads=[("psM", pi)], writes=["junk", ("st3", s)])
                    S.add("dve", lambda e, s=s: e.tensor_reduce(out=st3[:, s, 8:9], in_=st3[:, s, 0:ncg], axis=AX.X,
                                                                op=ALU.add), reads=[("st3", s)], writes=[("st3", s)])
                    S.add("act", lambda e, s=s: e.activation(out=st3[:, s, 9:10], in_=st3[:, s, 8:9], func=AF.Sqrt,
                                                             bias=epsc[:], scale=1.0 / D),
                          reads=[("st3", s), "epsc"], writes=[("st3", s)])
                    S.add("dve", lambda e, s=s: e.reciprocal(out=st3[:, s, 10:11], in_=st3[:, s, 9:10]),
                          reads=[("st3", s)], writes=[("st3", s)])
                    for cg in range(ncg):
                        pi = pss[cg]
                        S.add("dve", lambda e, pi=pi, s=s, cg=cg: e.scalar_tensor_tensor(
                            out=tt_[:, cg * GD:(cg + 1) * GD], in0=psM[pi][:, 0:GD], scalar=st3[:, s, 10:11],
                            in1=gpost[:, cg * GD:(cg + 1) * GD], op0=ALU.mult, op1=ALU.mult),
                            reads=[("psM", pi), ("st3", s), "gpost"], writes=["tt_"])
                    S.add("dve", lambda e, xi=xi: e.tensor_tensor(out=xs2[xi][:], in0=tt_[:], in1=xs2[xi][:], op=ALU.add),
                          reads=["tt_", ("xs2", xi)], writes=[("xs2", xi)])
                    S.add("sp", lambda e, s=s, xi=xi: e.dma_start(out=x1d[s * 128:(s + 1) * 128, :], in_=xs2[xi][:]),
                          reads=[("xs2", xi)], dma=True)
                    S.add("dve", lambda e, s=s, xi=xi: e.scalar_tensor_tensor(
                        out=hb2[xi][:], in0=xs2[xi][:], scalar=1.0, in1=xs2[xi][:], op0=ALU.mult, op1=ALU.mult,
                        accum_out=st3[:, s, 11:12]), reads=[("xs2", xi)], writes=[("hb2", xi), ("st3", s)])
                    S.add("act", lambda e, s=s: e.activation(out=st3[:, s, 12:13], in_=st3[:, s, 11:12], func=AF.Sqrt,
                                                             bias=epsc[:], scale=1.0 / D),
                          reads=[("st3", s), "epsc"], writes=[("st3", s)])
                    S.add("dve", lambda e, s=s: e.reciprocal(out=st3[:, s, 13:14], in_=st3[:, s, 12:13]),
                          reads=[("st3", s)], writes=[("st3", s)])
                    S.add("dve", lambda e, s=s, xi=xi: e.scalar_tensor_tensor(
                        out=hb2[xi][:], in0=xs2[xi][:], scalar=st3[:, s, 13:14], in1=gpre2[:], op0=ALU.mult,
                        op1=ALU.mult), reads=[("xs2", xi), ("st3", s), "gpre2"], writes=[("hb2", xi)])
                    nq = min(4, KC)
                    for q0 in range(0, KC, nq):
                        pi2 = tk % 2
                        tk += 1
                        for kk in range(nq):
                            S.add("pe", lambda e, pi2=pi2, kk=kk, q0=q0, xi=xi: e.transpose(
                                out=pst2[pi2][:, kk, :], in_=hb2[xi][:, (q0 + kk) * 128:(q0 + kk + 1) * 128],
                                identity=ident_b[:]), reads=[("hb2", xi), "ident_b"], writes=[("pst2", pi2)])
                        S.add("act", lambda e, pi2=pi2, q0=q0, s=s: e.copy(
                            out=h2T[:, q0:q0 + nq, s * 128:(s + 1) * 128], in_=pst2[pi2][:, 0:nq, :]),
                            reads=[("pst2", pi2)], writes=["h2T"])
                S.emit("ph3b")
            if upto < 4:
                return nc

            with contextlib.ExitStack() as ph:
                S = Sched(nc, ss)
                FB, UW = c.FB, c.UW
                FK = FB // 128
                Wu = [T(ph, "Wu%d" % i, [128, KC, UW], BF16) for i in range(2)]
                Wd = [T(ph, "Wd%d" % i, [128, FK, GD], BF16) for i in range(2)]
                m = T(ph, "m", [128, NST, D], F32)
                rl = [T(ph, "rl%d" % i, [128, 512], F32) for i in range(2)]
                gpost2 = T(ph, "gpost2", [128, D], F32)
                xs3 = [T(ph, "xs3_%d" % i, [128, D], F32) for i in range(2)]
                st4 = T(ph, "st4", [128, NST, 4], F32)
                psU = [P(ph, "psU%d" % i, [128, 512], F32) for i in range(3)]
                psD = [P(ph, "psD%d" % i, [128, 512], F32) for i in range(3)]
                aTv = lambda par, k: R2[:, par * FK + k, :]
                ncg = D // GD
                S.add("sp", lambda e: e.dma_start(out=gpost2[:], in_=gvec[3, :].partition_broadcast(128)),
                      writes=["gpost2"], dma=True)
                cn4 = {"uc": 0, "dc": 0, "wu": 0, "wd": 0, "rk": 0}

                def load_u(fb, u0):
                    col0 = fb * FB + u0
                    wi = cn4["wu"] % 2
                    cn4["wu"] += 1
                    for k0 in range(0, KC, 8):
                        k1 = min(KC, k0 + 8)
                        S.add("pool", lambda e, k0=k0, k1=k1: e.dma_start(
                            out=Wu[wi][:, k0:k1, :],
                            in_=w_up[k0 * 128:k1 * 128, col0:col0 + UW].rearrange("(kc p) c -> p kc c", p=128)),
                            writes=[("Wu", wi)], dma=True)
                    return wi

                def comp_u(fb, u0, wi):
                    par = fb % 2
                    for f in range(UW // 128):
                        fcl = (u0 + f * 128) // 128
                        for n in range(NN):
                            pi = cn4["uc"] % 3
                            cn4["uc"] += 1
                            for kc in range(KC):
                                S.add("pe", lambda e, pi=pi, kc=kc, f=f, n=n: e.matmul(
                                    psU[pi][:, 0:512], lhsT=Wu[wi][:, kc, f * 128:(f + 1) * 128],
                                    rhs=h2T[:, kc, n * 512:(n + 1) * 512], start=(kc == 0), stop=(kc == KC - 1)),
                                    reads=[("Wu", wi), "h2T"], writes=[("psU", pi)])
                            k = cn4["rk"] % 2
                            cn4["rk"] += 1
                            S.add("act", lambda e, pi=pi, k=k: e.activation(out=rl[k][:], in_=psU[pi][:, 0:512],
                                                                            func=AF.Relu),
                                  reads=[("psU", pi)], writes=[("rl", k)])
                            S.add("pool", lambda e, k=k, fcl=fcl, n=n: e.tensor_tensor(
                                out=aTv(par, fcl)[:, n * 512:(n + 1) * 512], in0=rl[k][:], in1=rl[k][:], op=ALU.mult),
                                reads=[("rl", k)], writes=[("aT", par)])

                def load_d(fb, cg):
                    wi = cn4["wd"] % 2
                    cn4["wd"] += 1
                    S.add("pool", lambda e: e.dma_start(
                        out=Wd[wi][:],
                        in_=w_down[fb * FB:(fb + 1) * FB, cg * GD:(cg + 1) * GD].rearrange("(kc p) c -> p kc c", p=128)),
                        writes=[("Wd", wi)], dma=True)
                    return wi

                def comp_d(fb, cg, wi):
                    par = fb % 2
                    for s in range(NST):
                        pi = cn4["dc"] % 3
                        cn4["dc"] += 1
                        for kc in range(FK):
                            S.add("pe", lambda e, pi=pi, kc=kc, s=s: e.matmul(
                                psD[pi][:, 0:GD], lhsT=aTv(par, kc)[:, s * 128:(s + 1) * 128], rhs=Wd[wi][:, kc, :],
                                start=(kc == 0), stop=(kc == FK - 1)),
                                reads=[("aT", par), ("Wd", wi)], writes=[("psD", pi)])
                        msl = m[:, s, cg * GD:(cg + 1) * GD]
                        if fb == 0:
                            S.add("act", lambda e, pi=pi, msl=msl: e.copy(out=msl, in_=psD[pi][:, 0:GD]),
                                  reads=[("psD", pi)], writes=[("m", s, cg)])
                        else:
                            S.add("dve", lambda e, pi=pi, msl=msl: e.tensor_tensor(out=msl, in0=psD[pi][:, 0:GD],
                                                                                 in1=msl, op=ALU.add),
                                  reads=[("psD", pi), ("m", s, cg)], writes=[("m", s, cg)])

                tasks4 = []
                for fb in range(c.NFB):
                    for u0 in range(0, FB, UW):
                        tasks4.append((load_u, comp_u, fb, u0))
                    for cg in range(ncg):
                        tasks4.append((load_d, comp_d, fb, cg))
                nxt_w = tasks4[0][0](tasks4[0][2], tasks4[0][3])
                for i, (lf_, cf_, a1, a2) in enumerate(tasks4):
                    cur_w = nxt_w
                    if i + 1 < len(tasks4):
                        t2 = tasks4[i + 1]
                        nxt_w = t2[0](t2[2], t2[3])
                    cf_(a1, a2, cur_w)
                for s in range(NST):
                    xi = s % 2
                    mk = [("m", s, cg) for cg in range(ncg)]
                    S.add("sp", lambda e, s=s, xi=xi: e.dma_start(out=xs3[xi][:], in_=x1d[s * 128:(s + 1) * 128, :]),
                          writes=[("xs3", xi)], dma=True)
                    S.add("dve", lambda e, s=s: e.scalar_tensor_tensor(
                        out=junk[:], in0=m[:, s, :], scalar=1.0, in1=m[:, s, :], op0=ALU.mult, op1=ALU.mult,
                        accum_out=st4[:, s, 0:1]), reads=mk, writes=["junk", ("st4", s)])
                    S.add("act", lambda e, s=s: e.activation(out=st4[:, s, 1:2], in_=st4[:, s, 0:1], func=AF.Sqrt,
                                                             bias=epsc[:], scale=1.0 / D),
                          reads=[("st4", s), "epsc"], writes=[("st4", s)])
                    S.add("dve", lambda e, s=s: e.reciprocal(out=st4[:, s, 2:3], in_=st4[:, s, 1:2]),
                          reads=[("st4", s)], writes=[("st4", s)])
                    S.add("dve", lambda e, s=s: e.scalar_tensor_tensor(
                        out=m[:, s, :], in0=m[:, s, :], scalar=st4[:, s, 2:3], in1=gpost2[:], op0=ALU.mult,
                        op1=ALU.mult), reads=mk + [("st4", s), "gpost2"], writes=mk)
                    S.add("dve", lambda e, s=s, xi=xi: e.tensor_tensor(out=xs3[xi][:], in0=m[:, s, :], in1=xs3[xi][:],
                                                                     op=ALU.add),
                          reads=mk + [("xs3", xi)], writes=[("xs3", xi)])
                    S.add("sp", lambda e, s=s, xi=xi: e.dma_start(out=out[s * 128:(s + 1) * 128, :], in_=xs3[xi][:]),
                          reads=[("xs3", xi)], dma=True)
                S.emit("ph4")
    return nc


def make_core_inputs(inp, core, cfg=FULL):
    c = cfg
    b, j = divmod(core, c.NCH)
    x = np.asarray(inp["x"])[b]
    CH, NCH = c.CH, c.NCH
    order = [(j - d) % NCH for d in range(NCH)]
    xctx = np.ascontiguousarray(np.concatenate([x[o * CH:(o + 1) * CH] for o in order], 0), dtype=np.float32)
    pos = np.concatenate([np.arange(o * CH, (o + 1) * CH) for o in order]).astype(np.float32)
    inv_freq = (np.float32(ROPE_THETA) ** (-np.arange(0, 32, 2, dtype=np.float32) / np.float32(32))).astype(np.float32)
    ang = (pos[:, None] * inv_freq[None, :]).astype(np.float32)
    valid = [j - d >= 0 for d in range(NCH)]
    vflag = np.array([[0.0 if v else NEG_MASK for v in valid]], np.float32)
    bpc = CH // 256
    gb = np.full((c.NST, c.NB), -1e30, np.float32)
    for qt in range(c.NST):
        bq = qt // 2
        for n in range(c.NB):
            d, i = divmod(n, bpc)
            if (d == 0 and i < bq) or (d >= 1 and valid[d]):
                gb[qt, n] = 0.0
    k = np.arange(128)
    cE = np.zeros((c.NB, c.NB * 128), np.float32)
    for n in range(c.NB):
        cE[n, n * 128:(n + 1) * 128] = 1.0
    sq = lambda a: np.ascontiguousarray(np.asarray(a)[0], dtype=np.float32)
    return {
        "xctx": xctx,
        "w_in": sq(inp["w_in"]), "w_bm": sq(inp["w_branch_moba"]), "w_bf": sq(inp["w_branch_fox"]),
        "w_out": sq(inp["w_out"]), "w_up": sq(inp["w_up"]), "w_down": sq(inp["w_down"]),
        "gvec": np.stack([sq(inp["g_mix_pre"]), sq(inp["g_mix_post"]), sq(inp["g_mlp_pre"]), sq(inp["g_mlp_post"])], 0),
        "bfg": np.asarray(inp["b_forget"], np.float32).reshape(1, c.H),
        "rope_cos": np.cos(ang).astype(np.float32), "rope_sin": np.sin(ang).astype(np.float32),
        "gbias": gb.reshape(1, -1), "vflag": vflag,
        "cident": np.eye(128, dtype=np.float32),
        "ctri": (k[:, None] <= k[None, :]).astype(np.float32),
        "cE": cE,
    }


_NC_CACHE = {}


def kernel(**inputs):
    cfg = FULL
    if "nc" not in _NC_CACHE:
        _NC_CACHE["nc"] = build(cfg)
    nc = _NC_CACHE["nc"]
    n_cores = 2 * cfg.NCH
    shared = None
    in_maps = []
    for core in range(n_cores):
        m = make_core_inputs(inputs, core, cfg)
        if shared is None:
            shared = m
        else:
            for key in m:
                if key not in ("xctx", "rope_cos", "rope_sin", "gbias", "vflag"):
                    m[key] = shared[key]
        in_maps.append(m)
    res = run_bass_kernel_spmd(nc, in_maps, core_ids=list(range(n_cores)))
    B = n_cores // cfg.NCH
    outp = np.empty((B, cfg.TCTX, cfg.D), np.float32)
    for core in range(n_cores):
        b, j = divmod(core, cfg.NCH)
        outp[b, j * cfg.CH:(j + 1) * cfg.CH] = np.asarray(res.results[core]["out"], np.float32)
    return outp
```

```python
import contextlib
import numpy as np
import concourse.bass as bass
import concourse.mybir as mybir
from concourse.bass_utils import run_bass_kernel_spmd

F32 = mybir.dt.float32
BF16 = mybir.dt.bfloat16
AF = mybir.ActivationFunctionType
ALU = mybir.AluOpType
AX = mybir.AxisListType

ENGS = ("pe", "act", "dve", "pool", "sp")
N_DMA_SEMS = 24
N_HW_SEMS = 16
OP_LIMIT = None
RMS_EPS = 1e-6
ROPE_THETA = 500000.0
NEG_MASK = -30000.0


class _Op:
    __slots__ = ("eng", "fn", "is_dma", "signal", "sigval", "dsem", "dval", "deps", "waits")


class SemState:
    def __init__(self, nc, stack):
        self.sems = {}
        for e in ("pe", "act", "dve", "pool"):
            self.sems[("eng", e)] = stack.enter_context(nc.semaphore("s_" + e))
        for i in range(N_DMA_SEMS):
            self.sems[("dma", i)] = stack.enter_context(nc.semaphore("s_dma%d" % i))
        self.val = {k: 0 for k in self.sems}
        self.dma_count = {"hw": 0, "sw": 0}


def _is_psum(k):
    n = k[0] if isinstance(k, tuple) else k
    return isinstance(n, str) and (n.startswith("ps") or n == "oacc")


class Sched:
    def __init__(self, nc, ss):
        self.nc = nc
        self.ss = ss
        self.ops = []
        self.streams = {e: [] for e in ENGS}
        self.last_w = {}
        self.readers = {}
        self.dma_last = [None] * N_DMA_SEMS
        self.dma_val = [ss.val[("dma", i)] for i in range(N_DMA_SEMS)]

    def add(self, eng, fn, reads=(), writes=(), dma=False):
        if OP_LIMIT is not None and len(self.ops) >= OP_LIMIT:
            return None
        pr = [k for k in reads if _is_psum(k)]
        if pr:
            reads = [k for k in reads if not _is_psum(k)]
            writes = list(writes) + [k for k in pr if k not in writes]
        op = _Op()
        op.eng, op.fn, op.is_dma = eng, fn, dma
        op.signal, op.sigval, op.dsem, op.dval, op.waits = False, None, None, None, None
        deps = []
        for k in reads:
            w = self.last_w.get(k)
            if w is not None:
                deps.append(w)
        for k in writes:
            w = self.last_w.get(k)
            if w is not None:
                deps.append(w)
            rd = self.readers.get(k)
            if rd:
                deps.extend(rd.values())
        if dma:
            if eng == "pool":
                s = N_HW_SEMS + self.ss.dma_count["sw"] % (N_DMA_SEMS - N_HW_SEMS)
                self.ss.dma_count["sw"] += 1
            else:
                s = self.ss.dma_count["hw"] % N_HW_SEMS
                self.ss.dma_count["hw"] += 1
            if self.dma_last[s] is not None:
                deps.append(self.dma_last[s])
            self.dma_val[s] += 16
            op.dsem, op.dval = s, self.dma_val[s]
            self.dma_last[s] = op
        op.deps = deps
        gid = len(self.ops)
        for k in reads:
            self.readers.setdefault(k, {})[("dma", gid) if dma else eng] = op
        for k in writes:
            self.last_w[k] = op
            self.readers[k] = {}
        self.streams[eng].append(op)
        self.ops.append(op)
        return op

    @staticmethod
    def _skip(op, d):
        return (not d.is_dma) and d.eng == op.eng and op.eng == "pe" and not op.is_dma

    def emit(self, name):
        nc, ss = self.nc, self.ss
        for op in self.ops:
            for d in op.deps:
                if not d.is_dma and not self._skip(op, d):
                    d.signal = True
        for e in ("pe", "act", "dve", "pool"):
            c = ss.val[("eng", e)]
            for op in self.streams[e]:
                if op.signal and not op.is_dma:
                    c += 1
                    op.sigval = c
            ss.val[("eng", e)] = c
        for i in range(N_DMA_SEMS):
            ss.val[("dma", i)] = self.dma_val[i]
        for e in ENGS:
            known = {}
            for op in self.streams[e]:
                need = {}
                for d in op.deps:
                    if d.is_dma:
                        key, val = ("dma", d.dsem), d.dval
                    elif self._skip(op, d):
                        continue
                    else:
                        key, val = ("eng", d.eng), d.sigval
                    if known.get(key, 0) >= val:
                        continue
                    if need.get(key, 0) < val:
                        need[key] = val
                known.update(need)
                op.waits = need
        sems = ss.sems
        with nc.Block() as block:
            def run(e, eng):
                for op in self.streams[e]:
                    for k, v in op.waits.items():
                        eng.wait_ge(sems[k], v)
                    ins = op.fn(eng)
                    if op.is_dma:
                        ins.then_inc(sems[("dma", op.dsem)], 16)
                    elif op.signal:
                        ins.then_inc(sems[("eng", e)], 1)

            @block.tensor
            def _(eng):
                run("pe", eng)

            @block.scalar
            def _(eng):
                run("act", eng)

            @block.vector
            def _(eng):
                run("dve", eng)

            @block.gpsimd
            def _(eng):
                run("pool", eng)

            @block.sync
            def _(eng):
                run("sp", eng)
                for i in range(N_DMA_SEMS):
                    if self.dma_val[i] > 0:
                        eng.wait_ge(sems[("dma", i)], self.dma_val[i])
                for e in ("pe", "act", "dve", "pool"):
                    if ss.val[("eng", e)] > 0:
                        eng.wait_ge(sems[("eng", e)], ss.val[("eng", e)])


class Cfg:
    def __init__(self, D=2048, H=8, NCH=4, CH=1024, DFF=8192):
        self.D, self.H, self.NCH, self.CH, self.DFF = D, H, NCH, CH, DFF
        self.KC = D // 128
        self.NST = CH // 128
        self.NT = NCH * self.NST
        self.TCTX = NCH * CH
        self.NB = self.TCTX // 256
        self.NG = CH // 512
        self.HG = min(4, H)
        self.GW = self.HG * 128
        self.HW = H * 128
        self.o_mq, self.o_mk, self.o_mv = 0, self.HW, 2 * self.HW
        self.o_fq, self.o_fk, self.o_fv = 3 * self.HW, 4 * self.HW, 5 * self.HW
        self.o_ff = 6 * self.HW
        self.o_ga = self.o_ff + H
        self.o_gb = self.o_ga + D
        self.WIN = self.o_gb + D
        self.GD = min(512, D)
        self.FC = DFF // 128
        self.FB = min(1024, DFF)
        self.NFB = DFF // self.FB
        self.UW = min(256, DFF)
        self.TOPK = 3
        assert H >= 2 and self.NB >= 8


FULL = Cfg()


def _cp(e, out, in_):
    if hasattr(e, "tensor_copy"):
        return e.tensor_copy(out=out, in_=in_)
    return e.copy(out=out, in_=in_)


def MARK(name, S):
    if MARKS is not None:
        MARKS.append((name, len(S.ops)))


MARKS = None


def build(cfg=FULL, upto=4, debug=False):
    c = cfg
    D, H, KC, NCH, CH, NST, NT, TCTX = c.D, c.H, c.KC, c.NCH, c.CH, c.NST, c.NT, c.TCTX
    NB, NG, HG, GW, HW, GD = c.NB, c.NG, c.HG, c.GW, c.HW, c.GD
    H2 = 2 * H
    NN = CH // 512
    nc = bass.Bass("TRN2", target_bir_lowering=False)
    din = lambda n, s, dt=F32: nc.dram_tensor(n, s, dt, kind="ExternalInput").ap()
    dscr = lambda n, s, dt: nc.dram_tensor(n, s, dt, kind="Internal").ap()
    xctx = din("xctx", [TCTX, D])
    w_in = din("w_in", [D, c.WIN])
    w_bm = din("w_bm", [HW, D])
    w_bf = din("w_bf", [HW, D])
    w_out = din("w_out", [D, D])
    w_up = din("w_up", [D, c.DFF])
    w_down = din("w_down", [c.DFF, D])
    gvec = din("gvec", [4, D])
    bfg = din("bfg", [1, H])
    rcos = din("rope_cos", [TCTX, 16])
    rsin = din("rope_sin", [TCTX, 16])
    gbias_d = din("gbias", [1, NST * NB])
    vflag_d = din("vflag", [1, NCH])
    cident = din("cident", [128, 128])
    ctri = din("ctri", [128, 128])
    cE = din("cE", [NB, NB * 128])
    out = nc.dram_tensor("out", [CH, D], F32, kind="ExternalOutput").ap()
    KTd = dscr("KTd", [H2, 128, TCTX], BF16)
    QTd = dscr("QTd", [H2, 128, CH], BF16)
    Vd = dscr("Vd", [NT, 128, H2 * 128], BF16)
    sgd = dscr("sgd", [2, KC, 128, CH], BF16)
    x1d = dscr("x1d", [CH, D], F32)
    if debug:
        dbg_logf = nc.dram_tensor("dbg_logf", [128, NT * H], F32, kind="ExternalOutput").ap()
        dbg_kmT = nc.dram_tensor("dbg_kmT", [128, H * NB], F32, kind="ExternalOutput").ap()
        dbg_oT = nc.dram_tensor("dbg_oT", [128, H2 * CH], F32, kind="ExternalOutput").ap()
        dbg_mg = nc.dram_tensor("dbg_mg", [128, KC * CH], F32, kind="ExternalOutput").ap()

    scale = 128.0 ** -0.5

    with contextlib.ExitStack() as top:
        ss = SemState(nc, top)
        T = lambda st, n, s, dt: st.enter_context(nc.sbuf_tensor(n, s, dt))
        P = lambda st, n, s, dt: st.enter_context(nc.psum_tensor(n, s, dt))
        ident_b = T(top, "ident_b", [128, 128], BF16)
        tri_b = T(top, "tri_b", [128, 128], BF16)
        tri_f = T(top, "tri_f", [128, 128], F32)
        ones_f = T(top, "ones_f", [128, 128], F32)
        nones_f = T(top, "nones_f", [128, 128], F32)
        ones_b = T(top, "ones_b", [128, 128], BF16)
        c256 = T(top, "c256", [128, 1], BF16)
        epsc = T(top, "epsc", [128, 1], F32)
        E_b = T(top, "E_b", [NB, NB * 128], BF16)
        zff = T(top, "zff", [128, NT, H], F32)
        logf = T(top, "logf", [128, NT, H], F32)
        kmT = T(top, "kmT", [128, H, NB], BF16)
        stat = T(top, "stat", [128, NT, 8], F32)
        junk = T(top, "junk", [128, D], BF16)

        with contextlib.ExitStack() as ph:
            S = Sched(nc, ss)
            g_rep = T(ph, "g_rep", [128, D], F32)
            bfg_rep = T(ph, "bfg_rep", [128, H], F32)
            cos_sb = T(ph, "cos_sb", [128, NT, 16], F32)
            sin_sb = T(ph, "sin_sb", [128, NT, 16], F32)
            xs = [T(ph, "xs%d" % i, [128, D], F32) for i in range(2)]
            hb = [T(ph, "hb%d" % i, [128, D], BF16) for i in range(3)]
            hT = [T(ph, "hT%d" % i, [128, KC, CH], BF16) for i in range(2)]
            NWB = 3
            wb = [T(ph, "wb%d" % i, [128, KC, max(GD, GW)], BF16) for i in range(NWB)]
            vst = [T(ph, "vst%d" % i, [128, NST, GW], BF16) for i in range(2)]
            kst = [T(ph, "kst%d" % i, [128, HG, CH], BF16) for i in range(2)]
            kb = [T(ph, "kb%d" % i, [128, GW], BF16) for i in range(4)]
            rt = [T(ph, "rt%d" % i, [128, HG, 16], F32) for i in range(4)]
            wff = T(ph, "wff", [128, KC, H], BF16)
            lt = T(ph, "lt", [128, NT * H], F32)
            pst = [P(ph, "pst%d" % i, [128, 8, 128], BF16) for i in range(2)]
            psmm = [P(ph, "psmm%d" % i, [128, 512], F32) for i in range(3)]
            pstk = [P(ph, "pstk%d" % i, [128, 8, 128], BF16) for i in range(2)]
            pskm = P(ph, "pskm", [128, 512], F32)

            S.add("pool", lambda e: e.dma_start(out=ident_b[:], in_=cident), writes=["ident_b"], dma=True)
            S.add("pool", lambda e: e.dma_start(out=tri_b[:], in_=ctri), writes=["tri_b"], dma=True)
            S.add("sp", lambda e: e.dma_start(out=tri_f[:], in_=ctri), writes=["tri_f"], dma=True)
            S.add("pool", lambda e: e.dma_start(out=E_b[:], in_=cE), writes=["E_b"], dma=True)
            S.add("dve", lambda e: e.memset(ones_f[:], 1.0), writes=["ones_f"])
            S.add("dve", lambda e: e.memset(nones_f[:], -1.0), writes=["nones_f"])
            S.add("dve", lambda e: e.memset(ones_b[:], 1.0), writes=["ones_b"])
            S.add("dve", lambda e: e.memset(c256[:], 1.0 / 256.0), writes=["c256"])
            S.add("dve", lambda e: e.memset(epsc[:], RMS_EPS), writes=["epsc"])
            S.add("sp", lambda e: e.dma_start(out=g_rep[:], in_=gvec[0, :].partition_broadcast(128)),
                  writes=["g_rep"], dma=True)
            S.add("sp", lambda e: e.dma_start(out=bfg_rep[:], in_=bfg[0, :].partition_broadcast(128)),
                  writes=["bfg_rep"], dma=True)
            S.add("sp", lambda e: e.dma_start(out=cos_sb[:], in_=rcos.rearrange("(t p) f -> p t f", p=128)),
                  writes=["cos_sb"], dma=True)
            S.add("sp", lambda e: e.dma_start(out=sin_sb[:], in_=rsin.rearrange("(t p) f -> p t f", p=128)),
                  writes=["sin_sb"], dma=True)
            S.add("pool", lambda e: e.dma_start(
                out=wff[:], in_=w_in[:, c.o_ff:c.o_ff + H].rearrange("(kc p) c -> p kc c", p=128)),
                writes=["wff"], dma=True)

            cnt = {"mm": 0, "w": 0, "v": 0, "k": 0, "kb": 0, "tk": 0, "tk2": 0}

            def load_w(src2d, col0, width):
                i = cnt["w"] % NWB
                cnt["w"] += 1
                buf = wb[i]
                for k0 in range(0, KC, 8):
                    k1 = min(KC, k0 + 8)
                    S.add("pool", lambda e, k0=k0, k1=k1: e.dma_start(
                        out=buf[:, k0:k1, 0:width],
                        in_=src2d[k0 * 128:k1 * 128, col0:col0 + width].rearrange("(kc p) c -> p kc c", p=128)),
                        writes=[("wb", i)], dma=True)
                return buf, ("wb", i)

            def hT_stageA(d, s):
                t = d * NST + s
                xi = t % 2
                hi = t % 3
                S.add("sp", lambda e: e.dma_start(out=xs[xi][:], in_=xctx[t * 128:(t + 1) * 128, :]),
                      writes=[("xs", xi)], dma=True)
                S.add("dve", lambda e: e.scalar_tensor_tensor(
                    out=hb[hi][:], in0=xs[xi][:], scalar=1.0, in1=xs[xi][:], op0=ALU.mult, op1=ALU.mult,
                    accum_out=stat[:, t, 0:1]),
                    reads=[("xs", xi)], writes=[("hb", hi), ("stat", t)])
                S.add("act", lambda e: e.activation(out=stat[:, t, 1:2], in_=stat[:, t, 0:1], func=AF.Sqrt,
                                                    bias=epsc[:], scale=1.0 / D),
                      reads=[("stat", t), "epsc"], writes=[("stat", t)])
                S.add("dve", lambda e: e.reciprocal(out=stat[:, t, 2:3], in_=stat[:, t, 1:2]),
                      reads=[("stat", t)], writes=[("stat", t)])
                S.add("dve", lambda e: e.scalar_tensor_tensor(
                    out=hb[hi][:], in0=xs[xi][:], scalar=stat[:, t, 2:3], in1=g_rep[:], op0=ALU.mult,
                    op1=ALU.mult),
                    reads=[("xs", xi), ("stat", t), "g_rep"], writes=[("hb", hi)])

            def hT_stageB(d, s):
                hTd = hT[d % 2]
                t = d * NST + s
                hi = t % 3
                nq = min(8, KC)
                for q0 in range(0, KC, nq):
                    pi = cnt["tk"] % 2
                    cnt["tk"] += 1
                    for kk in range(nq):
                        S.add("pe", lambda e, pi=pi, kk=kk, q0=q0: e.transpose(
                            out=pst[pi][:, kk, :], in_=hb[hi][:, (q0 + kk) * 128:(q0 + kk + 1) * 128],
                            identity=ident_b[:]),
                            reads=[("hb", hi), "ident_b"], writes=[("pst", pi)])
                    S.add("act", lambda e, pi=pi, q0=q0: e.copy(
                        out=hTd[:, q0:q0 + nq, s * 128:(s + 1) * 128], in_=pst[pi][:, 0:nq, :]),
                        reads=[("pst", pi)], writes=[("hT", d % 2)])

            pendA = []
            pendB = []

            def drip(n=1):
                for _ in range(n):
                    if pendA:
                        a = pendA.pop(0)
                        hT_stageA(*a)
                        pendB.append(a)
                    if pendB and (len(pendB) > 1 or not pendA):
                        hT_stageB(*pendB.pop(0))

            def next_ps():
                i = cnt["mm"] % 3
                cnt["mm"] += 1
                return psmm[i], ("psmm", i)

            def rope_tail(s, d, mode, hbase, nh, ki):
                t = d * NST + s
                bi = s % 4
                kbt = kb[bi]
                pi = cnt["tk2"] % 2
                cnt["tk2"] += 1
                for hh in range(nh):
                    S.add("pe", lambda e, pi=pi, hh=hh: e.transpose(
                        out=pstk[pi][:, hh, :], in_=kbt[:, hh * 128:(hh + 1) * 128], identity=ident_b[:]),
                        reads=[("kb", bi), "ident_b"], writes=[("pstk", pi)])
                S.add("dve", lambda e, pi=pi: e.tensor_copy(
                    out=kst[ki][:, 0:nh, s * 128:(s + 1) * 128], in_=pstk[pi][:, 0:nh, :]),
                    reads=[("pstk", pi)], writes=[("kst", ki)])
                if mode == "k_rope" and s % 2 == 1:
                    kprev = kb[(s - 1) % 4]
                    for hh in range(nh):
                        S.add("pe", lambda e, hh=hh: e.matmul(
                            pskm[:, hh:hh + 1], lhsT=kprev[:, hh * 128:(hh + 1) * 128], rhs=c256[:],
                            start=True, stop=False),
                            reads=[("kb", (s - 1) % 4), "c256"], writes=["pskm"])
                        S.add("pe", lambda e, hh=hh: e.matmul(
                            pskm[:, hh:hh + 1], lhsT=kbt[:, hh * 128:(hh + 1) * 128], rhs=c256[:],
                            start=False, stop=True),
                            reads=[("kb", bi), "c256"], writes=["pskm"])
                    n = t // 2
                    S.add("dve", lambda e: e.tensor_copy(out=kmT[:, hbase:hbase + nh, n], in_=pskm[:, 0:nh]),
                          reads=["pskm"], writes=["kmT"])

            def tok_group(d, col0, width, mode, hbase, wbuf, wkey):
                hTd = hT[d % 2]
                nh = width // 128
                if mode == "v":
                    vi = cnt["v"] % 2
                    cnt["v"] += 1
                else:
                    ki = cnt["k"] % 2
                    cnt["k"] += 1
                for s in range(NST):
                    t = d * NST + s
                    ps, pk = next_ps()
                    for kc in range(KC):
                        S.add("pe", lambda e, ps=ps, kc=kc, s=s: e.matmul(
                            ps[:, 0:width], lhsT=hTd[:, kc, s * 128:(s + 1) * 128], rhs=wbuf[:, kc, 0:width],
                            start=(kc == 0), stop=(kc == KC - 1)),
                            reads=[("hT", d % 2), wkey], writes=[pk])
                    if mode == "v":
                        S.add("act", lambda e, ps=ps, s=s: e.copy(out=vst[vi][:, s, 0:width], in_=ps[:, 0:width]),
                              reads=[pk], writes=[("vst", vi)])
                        continue
                    bi = s % 4
                    kbt = kb[bi]
                    ps3 = ps[:, 0:width].rearrange("p (h c) -> p h c", c=128)
                    kb3 = kbt[:, 0:width].rearrange("p (h c) -> p h c", c=128)
                    cosb = cos_sb[:, t:t + 1, :].broadcast_to([128, nh, 16])
                    sinb = sin_sb[:, t:t + 1, :].broadcast_to([128, nh, 16])
                    S.add("act", lambda e, ps=ps, kbt=kbt: e.copy(out=kbt[:, 0:width], in_=ps[:, 0:width]),
                          reads=[pk], writes=[("kb", bi)])
                    x1, x2 = ps3[:, :, 0:16], ps3[:, :, 16:32]
                    r = [rt[i][:, 0:nh, :] for i in range(4)]
                    S.add("dve", lambda e, x1=x1, cosb=cosb, r=r: e.tensor_tensor(out=r[0], in0=x1, in1=cosb, op=ALU.mult),
                          reads=[pk, "cos_sb"], writes=[("rt", 0)])
                    S.add("dve", lambda e, x2=x2, sinb=sinb, r=r: e.tensor_tensor(out=r[1], in0=x2, in1=sinb, op=ALU.mult),
                          reads=[pk, "sin_sb"], writes=[("rt", 1)])
                    S.add("dve", lambda e, x2=x2, cosb=cosb, r=r: e.tensor_tensor(out=r[2], in0=x2, in1=cosb, op=ALU.mult),
                          reads=[pk, "cos_sb"], writes=[("rt", 2)])
                    S.add("dve", lambda e, x1=x1, sinb=sinb, r=r: e.tensor_tensor(out=r[3], in0=x1, in1=sinb, op=ALU.mult),
                          reads=[pk, "sin_sb"], writes=[("rt", 3)])
                    S.add("dve", lambda e, kb3=kb3, r=r: e.tensor_tensor(out=kb3[:, :, 0:16], in0=r[0], in1=r[1],
                                                                       op=ALU.subtract),
                          reads=[("rt", 0), ("rt", 1), ("kb", bi)], writes=[("kb", bi)])
                    S.add("dve", lambda e, kb3=kb3, r=r: e.tensor_tensor(out=kb3[:, :, 16:32], in0=r[2], in1=r[3],
                                                                       op=ALU.add),
                          reads=[("rt", 2), ("rt", 3), ("kb", bi)], writes=[("kb", bi)])
                    if s >= 1:
                        rope_tail(s - 1, d, mode, hbase, nh, ki)
                if mode != "v":
                    rope_tail(NST - 1, d, mode, hbase, nh, ki)
                if mode == "v":
                    S.add("sp", lambda e: e.dma_start(
                        out=Vd[d * NST:(d + 1) * NST, :, hbase * 128:hbase * 128 + width].rearrange("t p c -> p t c"),
                        in_=vst[vi][:, :, 0:width]),
                        reads=[("vst", vi)], dma=True)
                else:
                    dst = KTd[hbase:hbase + nh, :, d * CH:(d + 1) * CH] if mode == "k_rope" else QTd[hbase:hbase + nh, :, :]
                    S.add("sp", lambda e: e.dma_start(out=dst.rearrange("h p t -> p h t"), in_=kst[ki][:, 0:nh, :]),
                          reads=[("kst", ki)], dma=True)
                drip()

            def feat_group(d, col0, width, mode, hbase, wbuf, wkey, kc0=0):
                hTd = hT[d % 2]
                nh = width // 128
                for h0 in range(0, nh, HG):
                    h1 = min(nh, h0 + HG)
                    ki = cnt["k"] % 2
                    cnt["k"] += 1
                    for hh in range(h0, h1):
                        for n0 in range(0, CH, 512):
                            ps, pk = next_ps()
                            for kc in range(KC):
                                S.add("pe", lambda e, ps=ps, kc=kc, hh=hh, n0=n0: e.matmul(
                                    ps[:, 0:512], lhsT=wbuf[:, kc, hh * 128:(hh + 1) * 128],
                                    rhs=hTd[:, kc, n0:n0 + 512], start=(kc == 0), stop=(kc == KC - 1)),
                                    reads=[("hT", d % 2), wkey], writes=[pk])
                            if mode == "gate":
                                S.add("act", lambda e, ps=ps, hh=hh, n0=n0, ki=ki, h0=h0: e.activation(
                                    out=kst[ki][:, hh - h0, n0:n0 + 512], in_=ps[:, 0:512], func=AF.Sigmoid),
                                    reads=[pk], writes=[("kst", ki)])
                            else:
                                S.add("dve", lambda e, ps=ps, hh=hh, n0=n0, ki=ki, h0=h0: e.tensor_copy(
                                    out=kst[ki][:, hh - h0, n0:n0 + 512], in_=ps[:, 0:512]),
                                    reads=[pk], writes=[("kst", ki)])
                    if mode == "kT":
                        dst = KTd[hbase + h0:hbase + h1, :, d * CH:(d + 1) * CH]
                    elif mode == "qT":
                        dst = QTd[hbase + h0:hbase + h1, :, :]
                    else:
                        dst = sgd[hbase, kc0 + h0:kc0 + h1, :, :]
                    S.add("sp", lambda e, ki=ki, dst=dst, h0=h0, h1=h1: e.dma_start(
                        out=dst.rearrange("h p t -> p h t"), in_=kst[ki][:, 0:h1 - h0, :]),
                        reads=[("kst", ki)], dma=True)
                drip()

            def ff_group(d):
                hTd = hT[d % 2]
                for s in range(NST):
                    t = d * NST + s
                    ps, pk = next_ps()
                    for kc in range(KC):
                        S.add("pe", lambda e, ps=ps, kc=kc, s=s: e.matmul(
                            ps[:, 0:H], lhsT=hTd[:, kc, s * 128:(s + 1) * 128], rhs=wff[:, kc, :],
                            start=(kc == 0), stop=(kc == KC - 1)),
                            reads=[("hT", d % 2), "wff"], writes=[pk])
                    S.add("dve", lambda e, ps=ps, t=t: e.tensor_tensor(out=zff[:, t, :], in0=ps[:, 0:H], in1=bfg_rep[:],
                                                                     op=ALU.add),
                          reads=[pk, "bfg_rep"], writes=["zff"])
                drip()

            MARK("consts", S)
            for s in range(NST):
                hT_stageA(0, s)
                if s >= 1:
                    hT_stageB(0, s - 1)
            hT_stageB(0, NST - 1)
            tasks = []
            for d in range(NCH):
                if d + 1 < NCH:
                    tasks.append((None, None, lambda d=d: pendA.extend((d + 1, s) for s in range(NST))))
                for g0 in range(0, H, HG):
                    tasks.append((c.o_mk + g0 * 128, GW, lambda wb_, wk_, d=d, g0=g0: tok_group(
                        d, c.o_mk + g0 * 128, GW, "k_rope", g0, wb_, wk_)))
                for g0 in range(0, H, HG):
                    tasks.append((c.o_mv + g0 * 128, GW, lambda wb_, wk_, d=d, g0=g0: tok_group(
                        d, c.o_mv + g0 * 128, GW, "v", g0, wb_, wk_)))
                for g0 in range(0, H, HG):
                    tasks.append((c.o_fk + g0 * 128, GW, lambda wb_, wk_, d=d, g0=g0: feat_group(
                        d, c.o_fk + g0 * 128, GW, "kT", H + g0, wb_, wk_)))
                for g0 in range(0, H, HG):
                    tasks.append((c.o_fv + g0 * 128, GW, lambda wb_, wk_, d=d, g0=g0: tok_group(
                        d, c.o_fv + g0 * 128, GW, "v", H + g0, wb_, wk_)))
                tasks.append((None, None, lambda d=d: ff_group(d)))
                tasks.append((None, None, lambda: drip(2 * NST + 2)))
                if d == 0:
                    for g0 in range(0, H, HG):
                        tasks.append((c.o_mq + g0 * 128, GW, lambda wb_, wk_, g0=g0: tok_group(
                            0, c.o_mq + g0 * 128, GW, "q_rope", g0, wb_, wk_)))
                    for g0 in range(0, H, HG):
                        tasks.append((c.o_fq + g0 * 128, GW, lambda wb_, wk_, g0=g0: feat_group(
                            0, c.o_fq + g0 * 128, GW, "qT", H + g0, wb_, wk_)))
                    for c0 in range(0, D, GD):
                        tasks.append((c.o_ga + c0, GD, lambda wb_, wk_, c0=c0: feat_group(
                            0, c.o_ga + c0, GD, "gate", 0, wb_, wk_, kc0=c0 // 128)))
                    for c0 in range(0, D, GD):
                        tasks.append((c.o_gb + c0, GD, lambda wb_, wk_, c0=c0: feat_group(
                            0, c.o_gb + c0, GD, "gate", 1, wb_, wk_, kc0=c0 // 128)))
            PREF = NWB - 1
            wtasks = [i for i, tk_ in enumerate(tasks) if tk_[0] is not None]
            loaded = {}
            nxt = 0
            for i, (col0, width, fn) in enumerate(tasks):
                if col0 is None:
                    fn()
                    continue
                pos = wtasks.index(i)
                while nxt < len(wtasks) and nxt <= pos + PREF:
                    ti = wtasks[nxt]
                    loaded[ti] = load_w(w_in, tasks[ti][0], tasks[ti][1])
                    nxt += 1
                fn(*loaded.pop(i))
            MARK("proj_done", S)
            zf = zff[:].rearrange("p t h -> p (t h)")
            lf = logf[:].rearrange("p t h -> p (t h)")
            S.add("act", lambda e: e.activation(out=lt[:], in_=zf, func=AF.Exp, scale=-1.0), reads=["zff"], writes=["lt"])
            S.add("act", lambda e: e.activation(out=lt[:], in_=lt[:], func=AF.Ln, bias=1.0), reads=["lt"], writes=["lt"])
            S.add("dve", lambda e: e.tensor_scalar(out=lf, in0=lt[:], scalar1=-1.0, scalar2=None, op0=ALU.mult),
                  reads=["lt"], writes=["logf"])
            if debug:
                S.add("sp", lambda e: e.dma_start(out=dbg_logf, in_=lf), reads=["logf"], dma=True)
                S.add("pool", lambda e: e.dma_start(out=dbg_kmT, in_=kmT[:].rearrange("p h n -> p (h n)")),
                      reads=["kmT"], dma=True)
            S.emit("ph1")
        if upto < 2:
            return nc

        with contextlib.ExitStack() as mid:
            R1 = T(mid, "R1", [128, max(H2, KC), CH], BF16)
            R2 = T(mid, "R2", [128, max(KC, 16), CH], BF16)
            oT, h2T, mergedT = R1, R1, R2

            mid2 = contextlib.ExitStack()
            mid2.__enter__()
            Wbm = T(mid2, "Wbm", [128, H, D], BF16)
            Wbf = T(mid2, "Wbf", [128, H, D], BF16)
            with contextlib.ExitStack() as ph:
                S = Sched(nc, ss)
                KTb = [T(ph, "KTb%d" % i, [128, TCTX], BF16) for i in range(2)]
                Vb = [T(ph, "Vb%d" % i, [128, NT, 130], BF16) for i in range(2)]
                QTb = [T(ph, "QTb%d" % i, [128, CH], BF16) for i in range(2)]
                pT = [T(ph, "pT%d" % i, [128, 512], BF16) for i in range(4)]
                NSQ = TCTX // 512 + CH // 512
                sqb1 = [T(ph, "sqb%d" % i, [128, 512], BF16) for i in range(NSQ)]
                sqb = [sqb1, sqb1]
                nmx = T(ph, "nmx", [128, 2, 32], F32)
                negC = T(ph, "negC", [128, H2], F32)
                gbias_rep = T(ph, "gbias_rep", [128, NST * NB], F32)
                vfl_rep = T(ph, "vfl_rep", [128, NCH], F32)
                Fsb = T(ph, "Fsb", [128, NT, H], F32)
                FrefB = T(ph, "FrefB", [128, NG, H], F32)
                biasF = T(ph, "biasF", [128, NG, NT, H], F32)
                bfh = [T(ph, "bfh%d" % i, [128, NG, NT], F32) for i in range(2)]
                gmb = T(ph, "gmb", [128, NST, NB], F32)
                m8 = T(ph, "m8", [128, NST, 8], F32)
                b1 = T(ph, "b1", [128, NST, NB], F32)
                bb = T(ph, "bb", [128, NST, NB], BF16)
                biasT = [T(ph, "biasT%d" % i, [NB, CH], BF16) for i in range(2)]
                otok = [T(ph, "otok%d" % i, [128, 4, 128], BF16) for i in range(2)]
                rc = T(ph, "rc", [128, 8], F32)
                psS = [P(ph, "psS%d" % i, [128, 512], F32) for i in range(2)]
                oacc_t = [[P(ph, "oacc%d_%d" % (i, k), [128, 512], F32) for k in range(2)] for i in range(2)]
                oacc = [[t[:, 0:260].rearrange("p (r c) -> p r c", c=130) for t in row] for row in oacc_t]
                psX = [P(ph, "psX%d" % i, [128, 512], F32) for i in range(2)]
                psXb = [p[:].bitcast(BF16) for p in psX]
                cn = {"x": 0, "s": 0, "p": 0, "a": 0, "sq": 0, "g": 0}

                def next_x():
                    i = cn["x"] % 2
                    cn["x"] += 1
                    return i, ("psX", i)

                S.add("sp", lambda e: e.dma_start(out=gbias_rep[:], in_=gbias_d[0, :].partition_broadcast(128)),
                      writes=["gbias_rep"], dma=True)
                S.add("sp", lambda e: e.dma_start(out=vfl_rep[:], in_=vflag_d[0, :].partition_broadcast(128)),
                      writes=["vfl_rep"], dma=True)
                for i in range(2):
                    S.add("pool", lambda e, i=i: e.memset(Vb[i][:, :, 128:129], 1.0), writes=[("V", i)])

                def load_head(h, par):
                    S.add("sp", lambda e: e.dma_start(out=KTb[par][:], in_=KTd[h]), writes=[("KT", par)], dma=True)
                    S.add("sp", lambda e: e.dma_start(out=QTb[par][:], in_=QTd[h]), writes=[("QT", par)], dma=True)
                    for t0 in range(0, NT, 8):
                        S.add("sp", lambda e, t0=t0: e.dma_start(
                            out=Vb[par][:, t0:t0 + 8, 0:128],
                            in_=Vd[t0:t0 + 8, :, h * 128:(h + 1) * 128].rearrange("t p c -> p t c")),
                            writes=[("V", par)], dma=True)

                load_head(0, 0)

                xi, xk = next_x()
                psF = psX[xi][:, 0:NT * H].rearrange("p (t h) -> p t h", h=H)
                for d in range(NCH):
                    for j in range(NST):
                        t = d * NST + j
                        terms = [(tri_f, t)] + [(ones_f, d * NST + j2) for j2 in range(j)]
                        if d >= 1:
                            terms += [(nones_f, d2 * NST + j2) for d2 in range(1, d + 1) for j2 in range(NST)]
                        for i, (mm, tt) in enumerate(terms):
                            S.add("pe", lambda e, t=t, mm=mm, tt=tt, i=i, n=len(terms): e.matmul(
                                psF[:, t, :], lhsT=mm[:], rhs=logf[:, tt, :], start=(i == 0), stop=(i == n - 1)),
                                reads=["logf", "tri_f", "ones_f", "nones_f"], writes=[xk])
                S.add("dve", lambda e: e.tensor_copy(out=Fsb[:], in_=psF), reads=[xk], writes=["Fsb"])
                xi2, xk2 = next_x()
                psR = psX[xi2][:, 0:NG * H].rearrange("p (g h) -> p g h", h=H)
                for g in range(NG):
                    n = 4 * g + 2
                    for j2 in range(n):
                        S.add("pe", lambda e, g=g, j2=j2, n=n: e.matmul(
                            psR[:, g, :], lhsT=ones_f[:], rhs=logf[:, j2, :], start=(j2 == 0), stop=(j2 == n - 1)),
                            reads=["logf", "ones_f"], writes=[xk2])
                S.add("dve", lambda e: e.tensor_copy(out=FrefB[:], in_=psR), reads=[xk2], writes=["FrefB"])
                for g in range(NG):
                    S.add("dve", lambda e, g=g: e.tensor_tensor(
                        out=biasF[:, g], in0=FrefB[:, g:g + 1, :].broadcast_to([128, NT, H]), in1=Fsb[:],
                        op=ALU.subtract), reads=["FrefB", "Fsb"], writes=["biasF"])
                    S.add("dve", lambda e, g=g: e.tensor_tensor(
                        out=biasF[:, g].rearrange("p (d j) h -> p d (j h)", d=NCH),
                        in0=biasF[:, g].rearrange("p (d j) h -> p d (j h)", d=NCH),
                        in1=vfl_rep[:].unsqueeze(2).broadcast_to([128, NCH, NST * H]), op=ALU.add),
                        reads=["biasF", "vfl_rep"], writes=["biasF"])

                def head_sq(par, h):
                    tiles = [(KTb[par], ("KT", par), n0) for n0 in range(0, TCTX, 512)]
                    tiles += [(QTb[par], ("QT", par), n0) for n0 in range(0, CH, 512)]
                    for i, (buf, bkey, n0) in enumerate(tiles):
                        S.add("pool", lambda e, buf=buf, n0=n0, i=i: e.tensor_tensor(
                            out=sqb[par][i][:], in0=buf[:, n0:n0 + 512], in1=buf[:, n0:n0 + 512], op=ALU.mult),
                            reads=[bkey], writes=[("sqb", i)])

                def head_norm(par, h):
                    for i in range(NSQ):
                        xi, xk = next_x()
                        S.add("pe", lambda e, xi=xi, i=i: e.matmul(psX[xi][:, 0:512], lhsT=ones_b[:], rhs=sqb[par][i][:],
                                                                  start=True, stop=True),
                              reads=[("sqb", i), "ones_b"], writes=[xk])
                        S.add("dve", lambda e, xi=xi, i=i: e.tensor_reduce(
                            out=nmx[:, par, i:i + 1], in_=psX[xi][:, 0:512], axis=AX.X, op=ALU.max),
                            reads=[xk], writes=[("nmx", par)])
                    S.add("dve", lambda e: e.tensor_reduce(out=negC[:, h:h + 1], in_=nmx[:, par, 0:NSQ], axis=AX.X,
                                                           op=ALU.max),
                          reads=[("nmx", par)], writes=[("negC", h)])
                    S.add("dve", lambda e: e.tensor_scalar(out=negC[:, h:h + 1], in0=negC[:, h:h + 1],
                                                           scalar1=-1.02 * scale, scalar2=None, op0=ALU.mult),
                          reads=[("negC", h)], writes=[("negC", h)])

                def moba_gate(par, hm):
                    xi, xk = next_x()
                    psg = psX[xi][:, 0:NST * NB].rearrange("p (q n) -> p q n", n=NB)
                    for qt in range(NST):
                        S.add("pe", lambda e, qt=qt: e.matmul(
                            psg[:, qt, :], lhsT=QTb[par][:, qt * 128:(qt + 1) * 128], rhs=kmT[:, hm, :],
                            start=True, stop=True), reads=[("QT", par), "kmT"], writes=[xk])
                    g3 = gbias_rep[:].rearrange("p (q n) -> p q n", n=NB)
                    S.add("dve", lambda e: e.tensor_tensor(out=gmb[:], in0=psg, in1=g3, op=ALU.add),
                          reads=[xk, "gbias_rep"], writes=["gmb"])
                    for qt in range(NST):
                        S.add("dve", lambda e, qt=qt: e.max(out=m8[:, qt, :], in_=gmb[:, qt, :]),
                              reads=["gmb"], writes=["m8"])
                    for qt in range(NST):
                        S.add("dve", lambda e, qt=qt: e.tensor_scalar(
                            out=b1[:, qt, :], in0=gmb[:, qt, :], scalar1=m8[:, qt, c.TOPK - 1:c.TOPK],
                            scalar2=-NEG_MASK, op0=ALU.is_ge, op1=ALU.mult), reads=["gmb", "m8"], writes=["b1"])
                    S.add("dve", lambda e: e.scalar_tensor_tensor(
                        out=bb[:].rearrange("p q n -> p (q n)"), in0=b1[:].rearrange("p q n -> p (q n)"),
                        scalar=NEG_MASK, in1=gbias_rep[:], op0=ALU.add, op1=ALU.min),
                        reads=["b1", "gbias_rep"], writes=["bb"])
                    for qt in range(NST):
                        S.add("dve", lambda e, qt=qt: e.memset(bb[:, qt, qt // 2:qt // 2 + 1], 0.0),
                              reads=["bb"], writes=["bb"])
                    xi2, xk2 = next_x()
                    for qt in range(NST):
                        S.add("pe", lambda e, qt=qt: e.transpose(out=psXb[xi2][0:NB, qt * 128:(qt + 1) * 128],
                                                                 in_=bb[:, qt, :], identity=ident_b[:]),
                              reads=["bb", "ident_b"], writes=[xk2])
                    S.add("dve", lambda e: e.tensor_copy(out=biasT[par][0:NB, 0:NST * 128],
                                                         in_=psXb[xi2][0:NB, 0:NST * 128]),
                          reads=[xk2], writes=[("biasT", par)])

                deferred = []

                def flush_deferred():
                    while deferred:
                        deferred.pop(0)()

                def attention(par, h, moba, mid_hook=None):
                    for g in range(NG):
                        a = cn["a"] % 2
                        cn["a"] += 1
                        tiles = [(d, j) for d in range(NCH - 1, 0, -1) for j in range(NST)]
                        tiles += [(0, j) for j in range(4 * g + 4)]

                        def qk_step(idx, d, j, g=g, a=a):
                            t = d * NST + j
                            c0 = 0 if (d > 0 or j < 4 * g) else (j - 4 * g) * 128
                            N = 512 - c0
                            q0 = g * 512 + c0
                            si = cn["s"] % 2
                            cn["s"] += 1
                            ps, pk = psS[si], ("psS", si)
                            S.add("pe", lambda e: e.matmul(
                                ps[:, 0:N], lhsT=KTb[par][:, t * 128:(t + 1) * 128], rhs=QTb[par][:, q0:q0 + N],
                                start=True, stop=not moba), reads=[("KT", par), ("QT", par)], writes=[pk])
                            if moba:
                                n = t // 2
                                S.add("pe", lambda e: e.matmul(
                                    ps[:, 0:N], lhsT=E_b[0:NB, n * 128:(n + 1) * 128], rhs=biasT[par][0:NB, q0:q0 + N],
                                    start=False, stop=True), reads=["E_b", ("biasT", par)], writes=[pk])
                            pi = cn["p"] % 4
                            cn["p"] += 1
                            pt, ptk = pT[pi], ("pT", pi)
                            if moba:
                                bias_ap, bkey = negC[:, h:h + 1], ("negC", h)
                            else:
                                bias_ap, bkey = bfh[par][:, g, t:t + 1], ("bfh", par)
                            S.add("act", lambda e: e.activation(
                                out=pt[:, 0:N], in_=ps[:, 0:N], func=AF.Exp, bias=bias_ap, scale=scale),
                                reads=[pk, bkey], writes=[ptk])
                            if d == 0 and j >= 4 * g:
                                S.add("pool", lambda e: e.tensor_tensor(out=pt[:, 0:128], in0=pt[:, 0:128],
                                                                        in1=tri_b[:], op=ALU.mult),
                                      reads=[ptk, "tri_b"], writes=[ptk])
                            return (idx, d, j, t, c0, pt, ptk)

                        def pv_step(st, g=g, a=a):
                            idx, d, j, t, c0, pt, ptk = st
                            for qs in range(c0 // 128, 4):
                                lo = qs * 128 - c0
                                S.add("pe", lambda e, qs=qs, lo=lo, first=(idx == 0 and qs % 2 == 0),
                                      last=(d == 0 and j == 4 * g + qs): e.matmul(
                                    oacc[a][qs // 2][:, qs % 2, 0:129], lhsT=pt[:, lo:lo + 128],
                                    rhs=Vb[par][:, t, 0:129], start=first, stop=last, skip_group_check=True),
                                    reads=[ptk, ("V", par)], writes=[("oacc", a, qs // 2)])

                        prev = None
                        for idx, (d, j) in enumerate(tiles):
                            cur = qk_step(idx, d, j)
                            if prev is not None:
                                pv_step(prev)
                            prev = cur
                            if idx == 2:
                                flush_deferred()
                        pv_step(prev)
                        ot = otok[a]
                        for qs in range(4):
                            oa = oacc[a][qs // 2]
                            S.add("dve", lambda e, oa=oa, qs=qs, a=a: e.reciprocal(out=rc[:, a * 4 + qs:a * 4 + qs + 1],
                                                                                 in_=oa[:, qs % 2, 128:129]),
                                  reads=[("oacc", a, qs // 2)], writes=[("rc", a, qs)])
                            S.add("dve", lambda e, oa=oa, qs=qs, ot=ot, a=a: e.tensor_scalar(
                                out=ot[:, qs, :], in0=oa[:, qs % 2, 0:128], scalar1=rc[:, a * 4 + qs:a * 4 + qs + 1],
                                scalar2=None, op0=ALU.mult),
                                reads=[("oacc", a, qs // 2), ("rc", a, qs)], writes=[("otok", a)])

                        def tail(g=g, a=a, ot=ot):
                            xi, xk = next_x()
                            for qs in range(4):
                                S.add("pe", lambda e, xi=xi, qs=qs: e.transpose(
                                    out=psXb[xi][:, qs * 128:(qs + 1) * 128], in_=ot[:, qs, :], identity=ident_b[:]),
                                    reads=[("otok", a), "ident_b"], writes=[xk])
                            S.add("act", lambda e, xi=xi: e.copy(out=oT[:, h, g * 512:(g + 1) * 512],
                                                                 in_=psXb[xi][:, 0:512]),
                                  reads=[xk], writes=["oT"])
                        deferred.append(tail)
                        if g == 0 and mid_hook is not None:
                            mid_hook()
                    if NG == 1 and mid_hook is not None:
                        pass

                def prep(h):
                    par = h % 2
                    head_norm(par, h)
                    if h < H:
                        moba_gate(par, h)
                    else:
                        S.add("dve", lambda e: e.tensor_scalar(
                            out=bfh[par][:], in0=biasF[:, :, :, h - H], scalar1=negC[:, h:h + 1], scalar2=None,
                            op0=ALU.add), reads=["biasF", ("negC", h)], writes=[("bfh", par)])

                head_sq(0, 0)
                prep(0)
                for h in range(H2):
                    par = h % 2

                    def hook(h=h):
                        if h + 1 < H2:
                            prep(h + 1)
                        if h == H2 - 3:
                            for h0 in range(0, H, 4):
                                h1 = min(H, h0 + 4)
                                for (wsb, wdr, nm) in ((Wbm, w_bm, "Wbm"), (Wbf, w_bf, "Wbf")):
                                    S.add("pool", lambda e, wsb=wsb, wdr=wdr, h0=h0, h1=h1: e.dma_start(
                                        out=wsb[:, h0:h1, :],
                                        in_=wdr[h0 * 128:h1 * 128, :].rearrange("(h p) c -> p h c", p=128)),
                                        writes=[nm], dma=True)
                    if h + 1 < H2:
                        load_head(h + 1, (h + 1) % 2)
                        head_sq((h + 1) % 2, h + 1)
                    attention(par, h, h < H, mid_hook=hook)
                flush_deferred()
                if debug:
                    S.add("pool", lambda e: e.dma_start(out=dbg_oT, in_=oT[:, 0:H2, :].rearrange("p h t -> p (h t)")),
                          reads=["oT"], dma=True)
                S.emit("ph2")
            if upto < 3:
                return nc

            with contextlib.ExitStack() as ph:
                S = Sched(nc, ss)
                sga = [T(ph, "sga%d" % i, [128, CH], BF16) for i in range(2)]
                sgb = [T(ph, "sgb%d" % i, [128, CH], BF16) for i in range(2)]
                t1 = [T(ph, "t1_%d" % i, [128, 512], F32) for i in range(2)]
                t2 = [T(ph, "t2_%d" % i, [128, 512], F32) for i in range(2)]
                psm = [[P(ph, "psm%d_%d" % (i, n), [128, 512], F32) for n in range(NN)] for i in range(2)]
                psf = [[P(ph, "psf%d_%d" % (i, n), [128, 512], F32) for n in range(NN)] for i in range(2)]
                tc_ = 0
                for cc in range(KC):
                    a = cc % 2
                    S.add("sp", lambda e, a=a, cc=cc: e.dma_start(out=sga[a][:], in_=sgd[0, cc]), writes=[("sga", a)], dma=True)
                    S.add("sp", lambda e, a=a, cc=cc: e.dma_start(out=sgb[a][:], in_=sgd[1, cc]), writes=[("sgb", a)], dma=True)
                    for n in range(NN):
                        for hh in range(H):
                            S.add("pe", lambda e, a=a, n=n, hh=hh, cc=cc: e.matmul(
                                psm[a][n][:, 0:512], lhsT=Wbm[:, hh, cc * 128:(cc + 1) * 128],
                                rhs=oT[:, hh, n * 512:(n + 1) * 512], start=(hh == 0), stop=(hh == H - 1)),
                                reads=["Wbm", "oT"], writes=[("psm", a, n)])
                        for hh in range(H):
                            S.add("pe", lambda e, a=a, n=n, hh=hh, cc=cc: e.matmul(
                                psf[a][n][:, 0:512], lhsT=Wbf[:, hh, cc * 128:(cc + 1) * 128],
                                rhs=oT[:, H + hh, n * 512:(n + 1) * 512], start=(hh == 0), stop=(hh == H - 1)),
                                reads=["Wbf", "oT"], writes=[("psf", a, n)])
                        k = tc_ % 2
                        tc_ += 1
                        S.add("dve", lambda e, a=a, n=n, k=k: e.tensor_tensor(
                            out=t1[k][:], in0=psm[a][n][:, 0:512], in1=sga[a][:, n * 512:(n + 1) * 512], op=ALU.mult),
                            reads=[("psm", a, n), ("sga", a)], writes=[("t1", k)])
                        S.add("dve", lambda e, a=a, n=n, k=k: e.tensor_tensor(
                            out=t2[k][:], in0=psf[a][n][:, 0:512], in1=sgb[a][:, n * 512:(n + 1) * 512], op=ALU.mult),
                            reads=[("psf", a, n), ("sgb", a)], writes=[("t2", k)])
                        S.add("pool", lambda e, cc=cc, n=n, k=k: e.tensor_tensor(
                            out=mergedT[:, cc, n * 512:(n + 1) * 512], in0=t1[k][:], in1=t2[k][:], op=ALU.add),
                            reads=[("t1", k), ("t2", k)], writes=["mergedT"])
                if debug:
                    S.add("pool", lambda e: e.dma_start(out=dbg_mg, in_=mergedT[:, 0:KC, :].rearrange("p h t -> p (h t)")),
                          reads=["mergedT"], dma=True)
                S.emit("ph3a")
            mid2.close()

            with contextlib.ExitStack() as ph:
                S = Sched(nc, ss)
                Wo = T(ph, "Wo", [128, KC, D], BF16)
                gpost = T(ph, "gpost", [128, D], F32)
                gpre2 = T(ph, "gpre2", [128, D], F32)
                xs2 = [T(ph, "xs2_%d" % i, [128, D], F32) for i in range(2)]
                tt_ = T(ph, "tt_", [128, D], F32)
                hb2 = [T(ph, "hb2_%d" % i, [128, D], BF16) for i in range(2)]
                st3 = T(ph, "st3", [128, NST, 16], F32)
                ncg = D // GD
                psM = [P(ph, "psM%d" % i, [128, 512], F32) for i in range(6)]
                pst2 = [P(ph, "pst2_%d" % i, [128, 8, 128], BF16) for i in range(2)]
                S.add("sp", lambda e: e.dma_start(out=gpost[:], in_=gvec[1, :].partition_broadcast(128)),
                      writes=["gpost"], dma=True)
                S.add("sp", lambda e: e.dma_start(out=gpre2[:], in_=gvec[2, :].partition_broadcast(128)),
                      writes=["gpre2"], dma=True)
                for c0 in range(0, D, GD):
                    for k0 in range(0, KC, 8):
                        k1 = min(KC, k0 + 8)
                        S.add("pool", lambda e, c0=c0, k0=k0, k1=k1: e.dma_start(
                            out=Wo[:, k0:k1, c0:c0 + GD],
                            in_=w_out[k0 * 128:k1 * 128, c0:c0 + GD].rearrange("(kc p) c -> p kc c", p=128)),
                            writes=[("Wo", c0)], dma=True)
                tkc = [0]

                def h2_tail(s):
                    xi = s % 2
                    nq = min(8, KC)
                    for q0 in range(0, KC, nq):
                        pi2 = tkc[0] % 2
                        tkc[0] += 1
                        for kk in range(nq):
                            S.add("pe", lambda e, pi2=pi2, kk=kk, q0=q0: e.transpose(
                                out=pst2[pi2][:, kk, :], in_=hb2[xi][:, (q0 + kk) * 128:(q0 + kk + 1) * 128],
                                identity=ident_b[:]), reads=[("hb2", xi), "ident_b"], writes=[("pst2", pi2)])
                        S.add("act", lambda e, pi2=pi2, q0=q0: e.copy(
                            out=h2T[:, q0:q0 + nq, s * 128:(s + 1) * 128], in_=pst2[pi2][:, 0:nq, :]),
                            reads=[("pst2", pi2)], writes=["h2T"])

                mc = 0
                tk = 0
                for s in range(NST):
                    xi = s % 2
                    S.add("sp", lambda e, s=s, xi=xi: e.dma_start(out=xs2[xi][:], in_=xctx[s * 128:(s + 1) * 128, :]),
                          writes=[("xs2", xi)], dma=True)
                    pss = []
                    for cg in range(ncg):
                        pi = mc % 6
                        mc += 1
                        pss.append(pi)
                        for kc in range(KC):
                            S.add("pe", lambda e, pi=pi, kc=kc, s=s, cg=cg: e.matmul(
                                psM[pi][:, 0:GD], lhsT=mergedT[:, kc, s * 128:(s + 1) * 128],
                                rhs=Wo[:, kc, cg * GD:(cg + 1) * GD], start=(kc == 0), stop=(kc == KC - 1)),
                                reads=["mergedT", ("Wo", cg * GD)], writes=[("psM", pi)])
                        S.add("act", lambda e, pi=pi, s=s, cg=cg: e.activation(
                            out=junk[:, 0:GD], in_=psM[pi][:, 0:GD], func=AF.Square, accum_out=st3[:, s, cg:cg + 1]),
                            reads=[("psM", pi)], writes=["junk", ("st3", s)])
                    S.add("dve", lambda e, s=s: e.tensor_reduce(out=st3[:, s, 8:9], in_=st3[:, s, 0:ncg], axis=AX.X,
                                                                op=ALU.add), reads=[("st3", s)], writes=[("st3", s)])
                    S.add("act", lambda e, s=s: e.activation(out=st3[:, s, 9:10], in_=st3[:, s, 8:9], func=AF.Sqrt,
                                                             bias=epsc[:], scale=1.0 / D),
                          reads=[("st3", s), "epsc"], writes=[("st3", s)])
                    S.add("dve", lambda e, s=s: e.reciprocal(out=st3[:, s, 10:11], in_=st3[:, s, 9:10]),
                          reads=[("st3", s)], writes=[("st3", s)])
                    for cg in range(ncg):
                        pi = pss[cg]
                        S.add("dve", lambda e, pi=pi, s=s, cg=cg: e.scalar_tensor_tensor(
                            out=tt_[:, cg * GD:(cg + 1) * GD], in0=psM[pi][:, 0:GD], scalar=st3[:, s, 10:11],
                            in1=gpost[:, cg * GD:(cg + 1) * GD], op0=ALU.mult, op1=ALU.mult),
                            reads=[("psM", pi), ("st3", s), "gpost"], writes=["tt_"])
                    S.add("dve", lambda e, xi=xi: e.tensor_tensor(out=xs2[xi][:], in0=tt_[:], in1=xs2[xi][:], op=ALU.add),
                          reads=["tt_", ("xs2", xi)], writes=[("xs2", xi)])
                    S.add("sp", lambda e, s=s, xi=xi: e.dma_start(out=x1d[s * 128:(s + 1) * 128, :], in_=xs2[xi][:]),
                          reads=[("xs2", xi)], dma=True)
                    S.add("dve", lambda e, s=s, xi=xi: e.scalar_tensor_tensor(
                        out=hb2[xi][:], in0=xs2[xi][:], scalar=1.0, in1=xs2[xi][:], op0=ALU.mult, op1=ALU.mult,
                        accum_out=st3[:, s, 11:12]), reads=[("xs2", xi)], writes=[("hb2", xi), ("st3", s)])
                    S.add("act", lambda e, s=s: e.activation(out=st3[:, s, 12:13], in_=st3[:, s, 11:12], func=AF.Sqrt,
                                                             bias=epsc[:], scale=1.0 / D),
                          reads=[("st3", s), "epsc"], writes=[("st3", s)])
                    S.add("dve", lambda e, s=s: e.reciprocal(out=st3[:, s, 13:14], in_=st3[:, s, 12:13]),
                          reads=[("st3", s)], writes=[("st3", s)])
                    S.add("dve", lambda e, s=s, xi=xi: e.scalar_tensor_tensor(
                        out=hb2[xi][:], in0=xs2[xi][:], scalar=st3[:, s, 13:14], in1=gpre2[:], op0=ALU.mult,
                        op1=ALU.mult), reads=[("xs2", xi), ("st3", s), "gpre2"], writes=[("hb2", xi)])
                    if s >= 1:
                        h2_tail(s - 1)
                h2_tail(NST - 1)
                S.emit("ph3b")
            if upto < 4:
                return nc

            with contextlib.ExitStack() as ph:
                S = Sched(nc, ss)
                FB, UW = c.FB, c.UW
                FK = FB // 128
                Wu = [T(ph, "Wu%d" % i, [128, KC, UW], BF16) for i in range(2)]
                Wd = [T(ph, "Wd%d" % i, [128, FK, GD], BF16) for i in range(2)]
                m = T(ph, "m", [128, NST, D], F32)
                rl = [T(ph, "rl%d" % i, [128, 512], F32) for i in range(2)]
                gpost2 = T(ph, "gpost2", [128, D], F32)
                xs3 = [T(ph, "xs3_%d" % i, [128, D], F32) for i in range(2)]
                st4 = T(ph, "st4", [128, NST, 4], F32)
                psU = [P(ph, "psU%d" % i, [128, 512], F32) for i in range(3)]
                psD = [P(ph, "psD%d" % i, [128, 512], F32) for i in range(3)]
                aTv = lambda par, k: R2[:, par * FK + k, :]
                ncg = D // GD
                S.add("sp", lambda e: e.dma_start(out=gpost2[:], in_=gvec[3, :].partition_broadcast(128)),
                      writes=["gpost2"], dma=True)
                cn4 = {"uc": 0, "dc": 0, "wu": 0, "wd": 0, "rk": 0}

                def load_u(fb, u0):
                    col0 = fb * FB + u0
                    wi = cn4["wu"] % 2
                    cn4["wu"] += 1
                    for k0 in range(0, KC, 8):
                        k1 = min(KC, k0 + 8)
                        S.add("pool", lambda e, k0=k0, k1=k1: e.dma_start(
                            out=Wu[wi][:, k0:k1, :],
                            in_=w_up[k0 * 128:k1 * 128, col0:col0 + UW].rearrange("(kc p) c -> p kc c", p=128)),
                            writes=[("Wu", wi)], dma=True)
                    return wi

                def comp_u(fb, u0, wi):
                    par = fb % 2
                    for f in range(UW // 128):
                        fcl = (u0 + f * 128) // 128
                        for n in range(NN):
                            pi = cn4["uc"] % 3
                            cn4["uc"] += 1
                            for kc in range(KC):
                                S.add("pe", lambda e, pi=pi, kc=kc, f=f, n=n: e.matmul(
                                    psU[pi][:, 0:512], lhsT=Wu[wi][:, kc, f * 128:(f + 1) * 128],
                                    rhs=h2T[:, kc, n * 512:(n + 1) * 512], start=(kc == 0), stop=(kc == KC - 1)),
                                    reads=[("Wu", wi), "h2T"], writes=[("psU", pi)])
                            k = cn4["rk"] % 2
                            cn4["rk"] += 1
                            S.add("act", lambda e, pi=pi, k=k: e.activation(out=rl[k][:], in_=psU[pi][:, 0:512],
                                                                            func=AF.Relu),
                                  reads=[("psU", pi)], writes=[("rl", k)])
                            S.add("pool", lambda e, k=k, fcl=fcl, n=n: e.tensor_tensor(
                                out=aTv(par, fcl)[:, n * 512:(n + 1) * 512], in0=rl[k][:], in1=rl[k][:], op=ALU.mult),
                                reads=[("rl", k)], writes=[("aT", par)])

                def load_d(fb, cg):
                    wi = cn4["wd"] % 2
                    cn4["wd"] += 1
                    S.add("pool", lambda e: e.dma_start(
                        out=Wd[wi][:],
                        in_=w_down[fb * FB:(fb + 1) * FB, cg * GD:(cg + 1) * GD].rearrange("(kc p) c -> p kc c", p=128)),
                        writes=[("Wd", wi)], dma=True)
                    return wi

                def comp_d(fb, cg, wi):
                    par = fb % 2
                    for s in range(NST):
                        pi = cn4["dc"] % 3
                        cn4["dc"] += 1
                        for kc in range(FK):
                            S.add("pe", lambda e, pi=pi, kc=kc, s=s: e.matmul(
                                psD[pi][:, 0:GD], lhsT=aTv(par, kc)[:, s * 128:(s + 1) * 128], rhs=Wd[wi][:, kc, :],
                                start=(kc == 0), stop=(kc == FK - 1)),
                                reads=[("aT", par), ("Wd", wi)], writes=[("psD", pi)])
                        msl = m[:, s, cg * GD:(cg + 1) * GD]
                        if fb == 0:
                            S.add("act", lambda e, pi=pi, msl=msl: e.copy(out=msl, in_=psD[pi][:, 0:GD]),
                                  reads=[("psD", pi)], writes=[("m", s, cg)])
                        else:
                            S.add("dve", lambda e, pi=pi, msl=msl: e.tensor_tensor(out=msl, in0=psD[pi][:, 0:GD],
                                                                                 in1=msl, op=ALU.add),
                                  reads=[("psD", pi), ("m", s, cg)], writes=[("m", s, cg)])

                tasks4 = []
                for fb in range(c.NFB):
                    for u0 in range(0, FB, UW):
                        tasks4.append((load_u, comp_u, fb, u0))
                    for cg in range(ncg):
                        tasks4.append((load_d, comp_d, fb, cg))
                nxt_w = tasks4[0][0](tasks4[0][2], tasks4[0][3])
                for i, (lf_, cf_, a1, a2) in enumerate(tasks4):
                    cur_w = nxt_w
                    if i + 1 < len(tasks4):
                        t2 = tasks4[i + 1]
                        nxt_w = t2[0](t2[2], t2[3])
                    cf_(a1, a2, cur_w)
                for s in range(NST):
                    xi = s % 2
                    mk = [("m", s, cg) for cg in range(ncg)]
                    S.add("sp", lambda e, s=s, xi=xi: e.dma_start(out=xs3[xi][:], in_=x1d[s * 128:(s + 1) * 128, :]),
                          writes=[("xs3", xi)], dma=True)
                    S.add("dve", lambda e, s=s: e.scalar_tensor_tensor(
                        out=junk[:], in0=m[:, s, :], scalar=1.0, in1=m[:, s, :], op0=ALU.mult, op1=ALU.mult,
                        accum_out=st4[:, s, 0:1]), reads=mk, writes=["junk", ("st4", s)])
                    S.add("act", lambda e, s=s: e.activation(out=st4[:, s, 1:2], in_=st4[:, s, 0:1], func=AF.Sqrt,
                                                             bias=epsc[:], scale=1.0 / D),
                          reads=[("st4", s), "epsc"], writes=[("st4", s)])
                    S.add("dve", lambda e, s=s: e.reciprocal(out=st4[:, s, 2:3], in_=st4[:, s, 1:2]),
                          reads=[("st4", s)], writes=[("st4", s)])
                    S.add("dve", lambda e, s=s: e.scalar_tensor_tensor(
                        out=m[:, s, :], in0=m[:, s, :], scalar=st4[:, s, 2:3], in1=gpost2[:], op0=ALU.mult,
                        op1=ALU.mult), reads=mk + [("st4", s), "gpost2"], writes=mk)
                    S.add("dve", lambda e, s=s, xi=xi: e.tensor_tensor(out=xs3[xi][:], in0=m[:, s, :], in1=xs3[xi][:],
                                                                     op=ALU.add),
                          reads=mk + [("xs3", xi)], writes=[("xs3", xi)])
                    S.add("sp", lambda e, s=s, xi=xi: e.dma_start(out=out[s * 128:(s + 1) * 128, :], in_=xs3[xi][:]),
                          reads=[("xs3", xi)], dma=True)
                S.emit("ph4")
    return nc


def make_core_inputs(inp, core, cfg=FULL):
    c = cfg
    b, j = divmod(core, c.NCH)
    x = np.asarray(inp["x"])[b]
    CH, NCH = c.CH, c.NCH
    order = [(j - d) % NCH for d in range(NCH)]
    xctx = np.ascontiguousarray(np.concatenate([x[o * CH:(o + 1) * CH] for o in order], 0), dtype=np.float32)
    pos = np.concatenate([np.arange(o * CH, (o + 1) * CH) for o in order]).astype(np.float32)
    inv_freq = (np.float32(ROPE_THETA) ** (-np.arange(0, 32, 2, dtype=np.float32) / np.float32(32))).astype(np.float32)
    ang = (pos[:, None] * inv_freq[None, :]).astype(np.float32)
    valid = [j - d >= 0 for d in range(NCH)]
    vflag = np.array([[0.0 if v else NEG_MASK for v in valid]], np.float32)
    bpc = CH // 256
    gb = np.full((c.NST, c.NB), -1e30, np.float32)
    for qt in range(c.NST):
        bq = qt // 2
        for n in range(c.NB):
            d, i = divmod(n, bpc)
            if (d == 0 and i < bq) or (d >= 1 and valid[d]):
                gb[qt, n] = 0.0
    k = np.arange(128)
    cE = np.zeros((c.NB, c.NB * 128), np.float32)
    for n in range(c.NB):
        cE[n, n * 128:(n + 1) * 128] = 1.0
    sq = lambda a: np.ascontiguousarray(np.asarray(a)[0], dtype=np.float32)
    return {
        "xctx": xctx,
        "w_in": sq(inp["w_in"]), "w_bm": sq(inp["w_branch_moba"]), "w_bf": sq(inp["w_branch_fox"]),
        "w_out": sq(inp["w_out"]), "w_up": sq(inp["w_up"]), "w_down": sq(inp["w_down"]),
        "gvec": np.stack([sq(inp["g_mix_pre"]), sq(inp["g_mix_post"]), sq(inp["g_mlp_pre"]), sq(inp["g_mlp_post"])], 0),
        "bfg": np.asarray(inp["b_forget"], np.float32).reshape(1, c.H),
        "rope_cos": np.cos(ang).astype(np.float32), "rope_sin": np.sin(ang).astype(np.float32),
        "gbias": gb.reshape(1, -1), "vflag": vflag,
        "cident": np.eye(128, dtype=np.float32),
        "ctri": (k[:, None] <= k[None, :]).astype(np.float32),
        "cE": cE,
    }


_NC_CACHE = {}


def kernel(**inputs):
    cfg = FULL
    if "nc" not in _NC_CACHE:
        _NC_CACHE["nc"] = build(cfg)
    nc = _NC_CACHE["nc"]
    n_cores = 2 * cfg.NCH
    shared = None
    in_maps = []
    for core in range(n_cores):
        m = make_core_inputs(inputs, core, cfg)
        if shared is None:
            shared = m
        else:
            for key in m:
                if key not in ("xctx", "rope_cos", "rope_sin", "gbias", "vflag"):
                    m[key] = shared[key]
        in_maps.append(m)
    res = run_bass_kernel_spmd(nc, in_maps, core_ids=list(range(n_cores)))
    B = n_cores // cfg.NCH
    outp = np.empty((B, cfg.TCTX, cfg.D), np.float32)
    for core in range(n_cores):
        b, j = divmod(core, cfg.NCH)
        outp[b, j * cfg.CH:(j + 1) * cfg.CH] = np.asarray(res.results[core]["out"], np.float32)
    return outp
```

```python
import contextlib
import numpy as np
import concourse.bass as bass
import concourse.mybir as mybir
from concourse.bass_utils import run_bass_kernel_spmd

F32 = mybir.dt.float32
BF16 = mybir.dt.bfloat16
AF = mybir.ActivationFunctionType
ALU = mybir.AluOpType
AX = mybir.AxisListType

ENGS = ("pe", "act", "dve", "pool", "sp")
N_DMA_SEMS = 24
N_HW_SEMS = 16
OP_LIMIT = None
RMS_EPS = 1e-6
ROPE_THETA = 500000.0
NEG_MASK = -30000.0


class _Op:
    __slots__ = ("eng", "fn", "is_dma", "signal", "sigval", "dsem", "dval", "deps", "waits")


class SemState:
    def __init__(self, nc, stack):
        self.sems = {}
        for e in ("pe", "act", "dve", "pool"):
            self.sems[("eng", e)] = stack.enter_context(nc.semaphore("s_" + e))
        for i in range(N_DMA_SEMS):
            self.sems[("dma", i)] = stack.enter_context(nc.semaphore("s_dma%d" % i))
        self.val = {k: 0 for k in self.sems}
        self.dma_count = {"hw": 0, "sw": 0}


def _is_psum(k):
    n = k[0] if isinstance(k, tuple) else k
    return isinstance(n, str) and (n.startswith("ps") or n == "oacc")


class Sched:
    def __init__(self, nc, ss):
        self.nc = nc
        self.ss = ss
        self.ops = []
        self.streams = {e: [] for e in ENGS}
        self.last_w = {}
        self.readers = {}
        self.dma_last = [None] * N_DMA_SEMS
        self.dma_val = [ss.val[("dma", i)] for i in range(N_DMA_SEMS)]

    def add(self, eng, fn, reads=(), writes=(), dma=False):
        if OP_LIMIT is not None and len(self.ops) >= OP_LIMIT:
            return None
        pr = [k for k in reads if _is_psum(k)]
        if pr:
            reads = [k for k in reads if not _is_psum(k)]
            writes = list(writes) + [k for k in pr if k not in writes]
        op = _Op()
        op.eng, op.fn, op.is_dma = eng, fn, dma
        op.signal, op.sigval, op.dsem, op.dval, op.waits = False, None, None, None, None
        deps = []
        for k in reads:
            w = self.last_w.get(k)
            if w is not None:
                deps.append(w)
        for k in writes:
            w = self.last_w.get(k)
            if w is not None:
                deps.append(w)
            rd = self.readers.get(k)
            if rd:
                deps.extend(rd.values())
        if dma:
            if eng == "pool":
                s = N_HW_SEMS + self.ss.dma_count["sw"] % (N_DMA_SEMS - N_HW_SEMS)
                self.ss.dma_count["sw"] += 1
            else:
                s = self.ss.dma_count["hw"] % N_HW_SEMS
                self.ss.dma_count["hw"] += 1
            if self.dma_last[s] is not None:
                deps.append(self.dma_last[s])
            self.dma_val[s] += 16
            op.dsem, op.dval = s, self.dma_val[s]
            self.dma_last[s] = op
        op.deps = deps
        gid = len(self.ops)
        for k in reads:
            self.readers.setdefault(k, {})[("dma", gid) if dma else eng] = op
        for k in writes:
            self.last_w[k] = op
            self.readers[k] = {}
        self.streams[eng].append(op)
        self.ops.append(op)
        return op

    @staticmethod
    def _skip(op, d):
        return (not d.is_dma) and d.eng == op.eng and op.eng == "pe" and not op.is_dma

    def emit(self, name):
        nc, ss = self.nc, self.ss
        for op in self.ops:
            for d in op.deps:
                if not d.is_dma and not self._skip(op, d):
                    d.signal = True
        for e in ("pe", "act", "dve", "pool"):
            c = ss.val[("eng", e)]
            for op in self.streams[e]:
                if op.signal and not op.is_dma:
                    c += 1
                    op.sigval = c
            ss.val[("eng", e)] = c
        for i in range(N_DMA_SEMS):
            ss.val[("dma", i)] = self.dma_val[i]
        for e in ENGS:
            known = {}
            for op in self.streams[e]:
                need = {}
                for d in op.deps:
                    if d.is_dma:
                        key, val = ("dma", d.dsem), d.dval
                    elif self._skip(op, d):
                        continue
                    else:
                        key, val = ("eng", d.eng), d.sigval
                    if known.get(key, 0) >= val:
                        continue
                    if need.get(key, 0) < val:
                        need[key] = val
                known.update(need)
                op.waits = need
        sems = ss.sems
        with nc.Block() as block:
            def run(e, eng):
                for op in self.streams[e]:
                    for k, v in op.waits.items():
                        eng.wait_ge(sems[k], v)
                    ins = op.fn(eng)
                    if op.is_dma:
                        ins.then_inc(sems[("dma", op.dsem)], 16)
                    elif op.signal:
                        ins.then_inc(sems[("eng", e)], 1)

            @block.tensor
            def _(eng):
                run("pe", eng)

            @block.scalar
            def _(eng):
                run("act", eng)

            @block.vector
            def _(eng):
                run("dve", eng)

            @block.gpsimd
            def _(eng):
                run("pool", eng)

            @block.sync
            def _(eng):
                run("sp", eng)
                for i in range(N_DMA_SEMS):
                    if self.dma_val[i] > 0:
                        eng.wait_ge(sems[("dma", i)], self.dma_val[i])
                for e in ("pe", "act", "dve", "pool"):
                    if ss.val[("eng", e)] > 0:
                        eng.wait_ge(sems[("eng", e)], ss.val[("eng", e)])


class Cfg:
    def __init__(self, D=2048, H=8, NCH=4, CH=1024, DFF=8192):
        self.D, self.H, self.NCH, self.CH, self.DFF = D, H, NCH, CH, DFF
        self.KC = D // 128
        self.NST = CH // 128
        self.NT = NCH * self.NST
        self.TCTX = NCH * CH
        self.NB = self.TCTX // 256
        self.NG = CH // 512
        self.HG = min(4, H)
        self.GW = self.HG * 128
        self.HW = H * 128
        self.o_mq, self.o_mk, self.o_mv = 0, self.HW, 2 * self.HW
        self.o_fq, self.o_fk, self.o_fv = 3 * self.HW, 4 * self.HW, 5 * self.HW
        self.o_ff = 6 * self.HW
        self.o_ga = self.o_ff + H
        self.o_gb = self.o_ga + D
        self.WIN = self.o_gb + D
        self.GD = min(512, D)
        self.FC = DFF // 128
        self.FB = min(1024, DFF)
        self.NFB = DFF // self.FB
        self.UW = min(256, DFF)
        self.TOPK = 3
        assert H >= 2 and self.NB >= 8


FULL = Cfg()


def _cp(e, out, in_):
    if hasattr(e, "tensor_copy"):
        return e.tensor_copy(out=out, in_=in_)
    return e.copy(out=out, in_=in_)


def MARK(name, S):
    if MARKS is not None:
        MARKS.append((name, len(S.ops)))


MARKS = None


def build(cfg=FULL, upto=4, debug=False):
    c = cfg
    D, H, KC, NCH, CH, NST, NT, TCTX = c.D, c.H, c.KC, c.NCH, c.CH, c.NST, c.NT, c.TCTX
    NB, NG, HG, GW, HW, GD = c.NB, c.NG, c.HG, c.GW, c.HW, c.GD
    H2 = 2 * H
    NN = CH // 512
    nc = bass.Bass("TRN2", target_bir_lowering=False)
    din = lambda n, s, dt=F32: nc.dram_tensor(n, s, dt, kind="ExternalInput").ap()
    dscr = lambda n, s, dt: nc.dram_tensor(n, s, dt, kind="Internal").ap()
    xctx = din("xctx", [TCTX, D])
    w_in = din("w_in", [D, c.WIN])
    w_bm = din("w_bm", [HW, D])
    w_bf = din("w_bf", [HW, D])
    w_out = din("w_out", [D, D])
    w_up = din("w_up", [D, c.DFF])
    w_down = din("w_down", [c.DFF, D])
    gvec = din("gvec", [4, D])
    bfg = din("bfg", [1, H])
    rcos = din("rope_cos", [TCTX, 16])
    rsin = din("rope_sin", [TCTX, 16])
    gbias_d = din("gbias", [1, NST * NB])
    vflag_d = din("vflag", [1, NCH])
    cident = din("cident", [128, 128])
    ctri = din("ctri", [128, 128])
    cE = din("cE", [NB, NB * 128])
    out = nc.dram_tensor("out", [CH, D], F32, kind="ExternalOutput").ap()
    KTd = dscr("KTd", [H2, 128, TCTX], BF16)
    QTd = dscr("QTd", [H2, 128, CH], BF16)
    Vd = dscr("Vd", [NT, 128, H2 * 128], BF16)
    sgd = dscr("sgd", [2, KC, 128, CH], BF16)
    x1d = dscr("x1d", [CH, D], F32)
    if debug:
        dbg_logf = nc.dram_tensor("dbg_logf", [128, NT * H], F32, kind="ExternalOutput").ap()
        dbg_kmT = nc.dram_tensor("dbg_kmT", [128, H * NB], F32, kind="ExternalOutput").ap()
        dbg_oT = nc.dram_tensor("dbg_oT", [128, H2 * CH], F32, kind="ExternalOutput").ap()
        dbg_mg = nc.dram_tensor("dbg_mg", [128, KC * CH], F32, kind="ExternalOutput").ap()

    scale = 128.0 ** -0.5

    with contextlib.ExitStack() as top:
        ss = SemState(nc, top)
        T = lambda st, n, s, dt: st.enter_context(nc.sbuf_tensor(n, s, dt))
        P = lambda st, n, s, dt: st.enter_context(nc.psum_tensor(n, s, dt))
        ident_b = T(top, "ident_b", [128, 128], BF16)
        tri_b = T(top, "tri_b", [128, 128], BF16)
        tri_f = T(top, "tri_f", [128, 128], F32)
        ident_f = T(top, "ident_f", [128, 128], F32)
        ones_f = T(top, "ones_f", [128, 128], F32)
        nones_f = T(top, "nones_f", [128, 128], F32)
        ones_b = T(top, "ones_b", [128, 128], BF16)
        c256 = T(top, "c256", [128, 1], BF16)
        epsc = T(top, "epsc", [128, 1], F32)
        E_b = T(top, "E_b", [NB, NB * 128], BF16)
        zff = T(top, "zff", [128, NT, H], F32)
        logf = T(top, "logf", [128, NT, H], F32)
        kmT = T(top, "kmT", [128, H, NB], BF16)
        stat = T(top, "stat", [128, NT, 8], F32)
        junk = T(top, "junk", [128, D], BF16)

        with contextlib.ExitStack() as ph:
            S = Sched(nc, ss)
            g_rep = T(ph, "g_rep", [128, D], F32)
            bfg_rep = T(ph, "bfg_rep", [128, H], F32)
            cos_sb = T(ph, "cos_sb", [128, NT, 16], F32)
            sin_sb = T(ph, "sin_sb", [128, NT, 16], F32)
            xs = [T(ph, "xs%d" % i, [128, D], F32) for i in range(2)]
            hb = [T(ph, "hb%d" % i, [128, D], BF16) for i in range(3)]
            hT = [T(ph, "hT%d" % i, [128, KC, CH], BF16) for i in range(2)]
            NWB = 3
            wb = [T(ph, "wb%d" % i, [128, KC, max(GD, GW)], BF16) for i in range(NWB)]
            vst = [T(ph, "vst%d" % i, [128, NST, GW], BF16) for i in range(2)]
            kst = [T(ph, "kst%d" % i, [128, HG, CH], BF16) for i in range(2)]
            kb = [T(ph, "kb%d" % i, [128, GW], BF16) for i in range(4)]
            rt = [T(ph, "rt%d" % i, [128, HG, 16], F32) for i in range(4)]
            wff = T(ph, "wff", [128, KC, H], BF16)
            lt = T(ph, "lt", [128, NT * H], F32)
            pst = [P(ph, "pst%d" % i, [128, 8, 128], BF16) for i in range(2)]
            psmm = [P(ph, "psmm%d" % i, [128, 512], F32) for i in range(3)]
            pstk = [P(ph, "pstk%d" % i, [128, 8, 128], BF16) for i in range(2)]
            pskm = P(ph, "pskm", [128, 512], F32)

            S.add("pool", lambda e: e.dma_start(out=ident_b[:], in_=cident), writes=["ident_b"], dma=True)
            S.add("pool", lambda e: e.dma_start(out=tri_b[:], in_=ctri), writes=["tri_b"], dma=True)
            S.add("sp", lambda e: e.dma_start(out=tri_f[:], in_=ctri), writes=["tri_f"], dma=True)
            S.add("sp", lambda e: e.dma_start(out=ident_f[:], in_=cident), writes=["ident_f"], dma=True)
            S.add("pool", lambda e: e.dma_start(out=E_b[:], in_=cE), writes=["E_b"], dma=True)
            S.add("dve", lambda e: e.memset(ones_f[:], 1.0), writes=["ones_f"])
            S.add("dve", lambda e: e.memset(nones_f[:], -1.0), writes=["nones_f"])
            S.add("dve", lambda e: e.memset(ones_b[:], 1.0), writes=["ones_b"])
            S.add("dve", lambda e: e.memset(c256[:], 1.0 / 256.0), writes=["c256"])
            S.add("dve", lambda e: e.memset(epsc[:], RMS_EPS), writes=["epsc"])
            S.add("sp", lambda e: e.dma_start(out=g_rep[:], in_=gvec[0, :].partition_broadcast(128)),
                  writes=["g_rep"], dma=True)
            S.add("sp", lambda e: e.dma_start(out=bfg_rep[:], in_=bfg[0, :].partition_broadcast(128)),
                  writes=["bfg_rep"], dma=True)
            S.add("sp", lambda e: e.dma_start(out=cos_sb[:], in_=rcos.rearrange("(t p) f -> p t f", p=128)),
                  writes=["cos_sb"], dma=True)
            S.add("sp", lambda e: e.dma_start(out=sin_sb[:], in_=rsin.rearrange("(t p) f -> p t f", p=128)),
                  writes=["sin_sb"], dma=True)
            S.add("pool", lambda e: e.dma_start(
                out=wff[:], in_=w_in[:, c.o_ff:c.o_ff + H].rearrange("(kc p) c -> p kc c", p=128)),
                writes=["wff"], dma=True)

            cnt = {"mm": 0, "w": 0, "v": 0, "k": 0, "kb": 0, "tk": 0, "tk2": 0}

            def load_w(src2d, col0, width):
                i = cnt["w"] % NWB
                cnt["w"] += 1
                buf = wb[i]
                for k0 in range(0, KC, 8):
                    k1 = min(KC, k0 + 8)
                    S.add("pool", lambda e, k0=k0, k1=k1: e.dma_start(
                        out=buf[:, k0:k1, 0:width],
                        in_=src2d[k0 * 128:k1 * 128, col0:col0 + width].rearrange("(kc p) c -> p kc c", p=128)),
                        writes=[("wb", i)], dma=True)
                return buf, ("wb", i)

            def hT_stageA(d, s):
                t = d * NST + s
                xi = t % 2
                hi = t % 3
                S.add("sp", lambda e: e.dma_start(out=xs[xi][:], in_=xctx[t * 128:(t + 1) * 128, :]),
                      writes=[("xs", xi)], dma=True)
                S.add("dve", lambda e: e.scalar_tensor_tensor(
                    out=hb[hi][:], in0=xs[xi][:], scalar=1.0, in1=xs[xi][:], op0=ALU.mult, op1=ALU.mult,
                    accum_out=stat[:, t, 0:1]),
                    reads=[("xs", xi)], writes=[("hb", hi), ("stat", t)])
                S.add("act", lambda e: e.activation(out=stat[:, t, 1:2], in_=stat[:, t, 0:1], func=AF.Sqrt,
                                                    bias=epsc[:], scale=1.0 / D),
                      reads=[("stat", t), "epsc"], writes=[("stat", t)])
                S.add("dve", lambda e: e.reciprocal(out=stat[:, t, 2:3], in_=stat[:, t, 1:2]),
                      reads=[("stat", t)], writes=[("stat", t)])
                S.add("dve", lambda e: e.scalar_tensor_tensor(
                    out=hb[hi][:], in0=xs[xi][:], scalar=stat[:, t, 2:3], in1=g_rep[:], op0=ALU.mult,
                    op1=ALU.mult),
                    reads=[("xs", xi), ("stat", t), "g_rep"], writes=[("hb", hi)])

            def hT_stageB(d, s):
                hTd = hT[d % 2]
                t = d * NST + s
                hi = t % 3
                nq = min(8, KC)
                for q0 in range(0, KC, nq):
                    pi = cnt["tk"] % 2
                    cnt["tk"] += 1
                    for kk in range(nq):
                        S.add("pe", lambda e, pi=pi, kk=kk, q0=q0: e.transpose(
                            out=pst[pi][:, kk, :], in_=hb[hi][:, (q0 + kk) * 128:(q0 + kk + 1) * 128],
                            identity=ident_b[:]),
                            reads=[("hb", hi), "ident_b"], writes=[("pst", pi)])
                    S.add("act", lambda e, pi=pi, q0=q0: e.copy(
                        out=hTd[:, q0:q0 + nq, s * 128:(s + 1) * 128], in_=pst[pi][:, 0:nq, :]),
                        reads=[("pst", pi)], writes=[("hT", d % 2)])

            pendA = []
            pendB = []

            def drip(n=1):
                for _ in range(n):
                    if pendA:
                        a = pendA.pop(0)
                        hT_stageA(*a)
                        pendB.append(a)
                    if pendB and (len(pendB) > 1 or not pendA):
                        hT_stageB(*pendB.pop(0))

            def next_ps():
                i = cnt["mm"] % 3
                cnt["mm"] += 1
                return psmm[i], ("psmm", i)

            def rope_tail(s, d, mode, hbase, nh, ki):
                t = d * NST + s
                bi = s % 4
                kbt = kb[bi]
                pi = cnt["tk2"] % 2
                cnt["tk2"] += 1
                for hh in range(nh):
                    S.add("pe", lambda e, pi=pi, hh=hh: e.transpose(
                        out=pstk[pi][:, hh, :], in_=kbt[:, hh * 128:(hh + 1) * 128], identity=ident_b[:]),
                        reads=[("kb", bi), "ident_b"], writes=[("pstk", pi)])
                S.add("dve", lambda e, pi=pi: e.tensor_copy(
                    out=kst[ki][:, 0:nh, s * 128:(s + 1) * 128], in_=pstk[pi][:, 0:nh, :]),
                    reads=[("pstk", pi)], writes=[("kst", ki)])
                if mode == "k_rope" and s % 2 == 1:
                    kprev = kb[(s - 1) % 4]
                    for hh in range(nh):
                        S.add("pe", lambda e, hh=hh: e.matmul(
                            pskm[:, hh:hh + 1], lhsT=kprev[:, hh * 128:(hh + 1) * 128], rhs=c256[:],
                            start=True, stop=False),
                            reads=[("kb", (s - 1) % 4), "c256"], writes=["pskm"])
                        S.add("pe", lambda e, hh=hh: e.matmul(
                            pskm[:, hh:hh + 1], lhsT=kbt[:, hh * 128:(hh + 1) * 128], rhs=c256[:],
                            start=False, stop=True),
                            reads=[("kb", bi), "c256"], writes=["pskm"])
                    n = t // 2
                    S.add("dve", lambda e: e.tensor_copy(out=kmT[:, hbase:hbase + nh, n], in_=pskm[:, 0:nh]),
                          reads=["pskm"], writes=["kmT"])

            def tok_group(d, col0, width, mode, hbase, wbuf, wkey):
                hTd = hT[d % 2]
                nh = width // 128
                if mode == "v":
                    vi = cnt["v"] % 2
                    cnt["v"] += 1
                else:
                    ki = cnt["k"] % 2
                    cnt["k"] += 1
                for s in range(NST):
                    t = d * NST + s
                    ps, pk = next_ps()
                    for kc in range(KC):
                        S.add("pe", lambda e, ps=ps, kc=kc, s=s: e.matmul(
                            ps[:, 0:width], lhsT=hTd[:, kc, s * 128:(s + 1) * 128], rhs=wbuf[:, kc, 0:width],
                            start=(kc == 0), stop=(kc == KC - 1)),
                            reads=[("hT", d % 2), wkey], writes=[pk])
                    if mode == "v":
                        S.add("act", lambda e, ps=ps, s=s: e.copy(out=vst[vi][:, s, 0:width], in_=ps[:, 0:width]),
                              reads=[pk], writes=[("vst", vi)])
                        continue
                    bi = s % 4
                    kbt = kb[bi]
                    ps3 = ps[:, 0:width].rearrange("p (h c) -> p h c", c=128)
                    kb3 = kbt[:, 0:width].rearrange("p (h c) -> p h c", c=128)
                    cosb = cos_sb[:, t:t + 1, :].broadcast_to([128, nh, 16])
                    sinb = sin_sb[:, t:t + 1, :].broadcast_to([128, nh, 16])
                    S.add("act", lambda e, ps=ps, kbt=kbt: e.copy(out=kbt[:, 0:width], in_=ps[:, 0:width]),
                          reads=[pk], writes=[("kb", bi)])
                    x1, x2 = ps3[:, :, 0:16], ps3[:, :, 16:32]
                    r = [rt[i][:, 0:nh, :] for i in range(4)]
                    S.add("dve", lambda e, x1=x1, cosb=cosb, r=r: e.tensor_tensor(out=r[0], in0=x1, in1=cosb, op=ALU.mult),
                          reads=[pk, "cos_sb"], writes=[("rt", 0)])
                    S.add("dve", lambda e, x2=x2, sinb=sinb, r=r: e.tensor_tensor(out=r[1], in0=x2, in1=sinb, op=ALU.mult),
                          reads=[pk, "sin_sb"], writes=[("rt", 1)])
                    S.add("dve", lambda e, x2=x2, cosb=cosb, r=r: e.tensor_tensor(out=r[2], in0=x2, in1=cosb, op=ALU.mult),
                          reads=[pk, "cos_sb"], writes=[("rt", 2)])
                    S.add("dve", lambda e, x1=x1, sinb=sinb, r=r: e.tensor_tensor(out=r[3], in0=x1, in1=sinb, op=ALU.mult),
                          reads=[pk, "sin_sb"], writes=[("rt", 3)])
                    S.add("dve", lambda e, kb3=kb3, r=r: e.tensor_tensor(out=kb3[:, :, 0:16], in0=r[0], in1=r[1],
                                                                       op=ALU.subtract),
                          reads=[("rt", 0), ("rt", 1), ("kb", bi)], writes=[("kb", bi)])
                    S.add("dve", lambda e, kb3=kb3, r=r: e.tensor_tensor(out=kb3[:, :, 16:32], in0=r[2], in1=r[3],
                                                                       op=ALU.add),
                          reads=[("rt", 2), ("rt", 3), ("kb", bi)], writes=[("kb", bi)])
                    if s >= 1:
                        rope_tail(s - 1, d, mode, hbase, nh, ki)
                if mode != "v":
                    rope_tail(NST - 1, d, mode, hbase, nh, ki)
                if mode == "v":
                    S.add("sp", lambda e: e.dma_start(
                        out=Vd[d * NST:(d + 1) * NST, :, hbase * 128:hbase * 128 + width].rearrange("t p c -> p t c"),
                        in_=vst[vi][:, :, 0:width]),
                        reads=[("vst", vi)], dma=True)
                else:
                    dst = KTd[hbase:hbase + nh, :, d * CH:(d + 1) * CH] if mode == "k_rope" else QTd[hbase:hbase + nh, :, :]
                    S.add("sp", lambda e: e.dma_start(out=dst.rearrange("h p t -> p h t"), in_=kst[ki][:, 0:nh, :]),
                          reads=[("kst", ki)], dma=True)
                drip()

            def feat_group(d, col0, width, mode, hbase, wbuf, wkey, kc0=0):
                hTd = hT[d % 2]
                nh = width // 128
                for h0 in range(0, nh, HG):
                    h1 = min(nh, h0 + HG)
                    ki = cnt["k"] % 2
                    cnt["k"] += 1
                    for hh in range(h0, h1):
                        for n0 in range(0, CH, 512):
                            ps, pk = next_ps()
                            for kc in range(KC):
                                S.add("pe", lambda e, ps=ps, kc=kc, hh=hh, n0=n0: e.matmul(
                                    ps[:, 0:512], lhsT=wbuf[:, kc, hh * 128:(hh + 1) * 128],
                                    rhs=hTd[:, kc, n0:n0 + 512], start=(kc == 0), stop=(kc == KC - 1)),
                                    reads=[("hT", d % 2), wkey], writes=[pk])
                            if mode == "gate":
                                S.add("act", lambda e, ps=ps, hh=hh, n0=n0, ki=ki, h0=h0: e.activation(
                                    out=kst[ki][:, hh - h0, n0:n0 + 512], in_=ps[:, 0:512], func=AF.Sigmoid),
                                    reads=[pk], writes=[("kst", ki)])
                            else:
                                S.add("dve", lambda e, ps=ps, hh=hh, n0=n0, ki=ki, h0=h0: e.tensor_copy(
                                    out=kst[ki][:, hh - h0, n0:n0 + 512], in_=ps[:, 0:512]),
                                    reads=[pk], writes=[("kst", ki)])
                    if mode == "kT":
                        dst = KTd[hbase + h0:hbase + h1, :, d * CH:(d + 1) * CH]
                    elif mode == "qT":
                        dst = QTd[hbase + h0:hbase + h1, :, :]
                    else:
                        dst = sgd[hbase, kc0 + h0:kc0 + h1, :, :]
                    S.add("sp", lambda e, ki=ki, dst=dst, h0=h0, h1=h1: e.dma_start(
                        out=dst.rearrange("h p t -> p h t"), in_=kst[ki][:, 0:h1 - h0, :]),
                        reads=[("kst", ki)], dma=True)
                drip()

            def ff_group(d):
                hTd = hT[d % 2]
                for s in range(NST):
                    t = d * NST + s
                    ps, pk = next_ps()
                    for kc in range(KC):
                        S.add("pe", lambda e, ps=ps, kc=kc, s=s: e.matmul(
                            ps[:, 0:H], lhsT=hTd[:, kc, s * 128:(s + 1) * 128], rhs=wff[:, kc, :],
                            start=(kc == 0), stop=(kc == KC - 1)),
                            reads=[("hT", d % 2), "wff"], writes=[pk])
                    S.add("dve", lambda e, ps=ps, t=t: e.tensor_tensor(out=zff[:, t, :], in0=ps[:, 0:H], in1=bfg_rep[:],
                                                                     op=ALU.add),
                          reads=[pk, "bfg_rep"], writes=["zff"])
                drip()

            MARK("consts", S)
            for s in range(NST):
                hT_stageA(0, s)
                if s >= 1:
                    hT_stageB(0, s - 1)
            hT_stageB(0, NST - 1)
            tasks = []
            for d in range(NCH):
                if d + 1 < NCH:
                    tasks.append((None, None, lambda d=d: pendA.extend((d + 1, s) for s in range(NST))))
                for g0 in range(0, H, HG):
                    tasks.append((c.o_mk + g0 * 128, GW, lambda wb_, wk_, d=d, g0=g0: tok_group(
                        d, c.o_mk + g0 * 128, GW, "k_rope", g0, wb_, wk_)))
                for g0 in range(0, H, HG):
                    tasks.append((c.o_mv + g0 * 128, GW, lambda wb_, wk_, d=d, g0=g0: tok_group(
                        d, c.o_mv + g0 * 128, GW, "v", g0, wb_, wk_)))
                for g0 in range(0, H, HG):
                    tasks.append((c.o_fk + g0 * 128, GW, lambda wb_, wk_, d=d, g0=g0: feat_group(
                        d, c.o_fk + g0 * 128, GW, "kT", H + g0, wb_, wk_)))
                for g0 in range(0, H, HG):
                    tasks.append((c.o_fv + g0 * 128, GW, lambda wb_, wk_, d=d, g0=g0: tok_group(
                        d, c.o_fv + g0 * 128, GW, "v", H + g0, wb_, wk_)))
                tasks.append((None, None, lambda d=d: ff_group(d)))
                tasks.append((None, None, lambda: drip(2 * NST + 2)))
                if d == 0:
                    for g0 in range(0, H, HG):
                        tasks.append((c.o_mq + g0 * 128, GW, lambda wb_, wk_, g0=g0: tok_group(
                            0, c.o_mq + g0 * 128, GW, "q_rope", g0, wb_, wk_)))
                    for g0 in range(0, H, HG):
                        tasks.append((c.o_fq + g0 * 128, GW, lambda wb_, wk_, g0=g0: feat_group(
                            0, c.o_fq + g0 * 128, GW, "qT", H + g0, wb_, wk_)))
                    for c0 in range(0, D, GD):
                        tasks.append((c.o_ga + c0, GD, lambda wb_, wk_, c0=c0: feat_group(
                            0, c.o_ga + c0, GD, "gate", 0, wb_, wk_, kc0=c0 // 128)))
                    for c0 in range(0, D, GD):
                        tasks.append((c.o_gb + c0, GD, lambda wb_, wk_, c0=c0: feat_group(
                            0, c.o_gb + c0, GD, "gate", 1, wb_, wk_, kc0=c0 // 128)))
            PREF = NWB - 1
            wtasks = [i for i, tk_ in enumerate(tasks) if tk_[0] is not None]
            loaded = {}
            nxt = 0
            for i, (col0, width, fn) in enumerate(tasks):
                if col0 is None:
                    fn()
                    continue
                pos = wtasks.index(i)
                while nxt < len(wtasks) and nxt <= pos + PREF:
                    ti = wtasks[nxt]
                    loaded[ti] = load_w(w_in, tasks[ti][0], tasks[ti][1])
                    nxt += 1
                fn(*loaded.pop(i))
            MARK("proj_done", S)
            zf = zff[:].rearrange("p t h -> p (t h)")
            lf = logf[:].rearrange("p t h -> p (t h)")
            S.add("act", lambda e: e.activation(out=lt[:], in_=zf, func=AF.Exp, scale=-1.0), reads=["zff"], writes=["lt"])
            S.add("act", lambda e: e.activation(out=lt[:], in_=lt[:], func=AF.Ln, bias=1.0), reads=["lt"], writes=["lt"])
            S.add("dve", lambda e: e.tensor_scalar(out=lf, in0=lt[:], scalar1=-1.0, scalar2=None, op0=ALU.mult),
                  reads=["lt"], writes=["logf"])
            if debug:
                S.add("sp", lambda e: e.dma_start(out=dbg_logf, in_=lf), reads=["logf"], dma=True)
                S.add("pool", lambda e: e.dma_start(out=dbg_kmT, in_=kmT[:].rearrange("p h n -> p (h n)")),
                      reads=["kmT"], dma=True)
            S.emit("ph1")
        if upto < 2:
            return nc

        with contextlib.ExitStack() as mid:
            R1 = T(mid, "R1", [128, max(H2, KC), CH], BF16)
            R2 = T(mid, "R2", [128, max(KC, 16), CH], BF16)
            oT, h2T, mergedT = R1, R1, R2

            mid2 = contextlib.ExitStack()
            mid2.__enter__()
            Wbm = T(mid2, "Wbm", [128, H, D], BF16)
            Wbf = T(mid2, "Wbf", [128, H, D], BF16)
            with contextlib.ExitStack() as ph:
                S = Sched(nc, ss)
                KTb = [T(ph, "KTb%d" % i, [128, TCTX], BF16) for i in range(2)]
                Vb = [T(ph, "Vb%d" % i, [128, NT, 130], BF16) for i in range(2)]
                QTb = [T(ph, "QTb%d" % i, [128, CH], BF16) for i in range(2)]
                pT = [T(ph, "pT%d" % i, [128, 512], BF16) for i in range(4)]
                NSQ = TCTX // 512 + CH // 512
                sqb1 = [T(ph, "sqb%d" % i, [128, 512], BF16) for i in range(NSQ)]
                sqb = [sqb1, sqb1]
                nmx = T(ph, "nmx", [128, 4], F32)
                negC = T(ph, "negC", [128, H2], F32)
                gbias_rep = T(ph, "gbias_rep", [128, NST * NB], F32)
                vfl_rep = T(ph, "vfl_rep", [128, NCH], F32)
                Fsb = T(ph, "Fsb", [128, NT, H], F32)
                FrefB = T(ph, "FrefB", [128, NG, H], F32)
                biasF = T(ph, "biasF", [128, NG, NT, H], F32)
                bfh = [T(ph, "bfh%d" % i, [128, NG, NT], F32) for i in range(2)]
                gmb = T(ph, "gmb", [128, NST, NB], F32)
                m8 = T(ph, "m8", [128, NST, 8], F32)
                b1 = T(ph, "b1", [128, NST, NB], F32)
                bb = T(ph, "bb", [128, NST, NB], BF16)
                biasT = [T(ph, "biasT%d" % i, [NB, CH], BF16) for i in range(2)]
                otok = [T(ph, "otok%d" % i, [128, 4, 128], BF16) for i in range(2)]
                rc = T(ph, "rc", [128, 8], F32)
                NPS = 3
                psS = [P(ph, "psS%d" % i, [128, 512], F32) for i in range(NPS)]
                oacc_t = [[P(ph, "oacc%d_%d" % (i, k), [128, 512], F32) for k in range(2)] for i in range(2)]
                oacc = [[t[:, 0:260].rearrange("p (r c) -> p r c", c=130) for t in row] for row in oacc_t]
                psX = [P(ph, "psX%d" % i, [128, 512], F32) for i in range(1)]
                psXb = [p[:].bitcast(BF16) for p in psX]
                cn = {"x": 0, "s": 0, "p": 0, "a": 0, "sq": 0, "g": 0}

                def next_x():
                    i = cn["x"] % len(psX)
                    cn["x"] += 1
                    return i, ("psX", i)

                S.add("sp", lambda e: e.dma_start(out=gbias_rep[:], in_=gbias_d[0, :].partition_broadcast(128)),
                      writes=["gbias_rep"], dma=True)
                S.add("sp", lambda e: e.dma_start(out=vfl_rep[:], in_=vflag_d[0, :].partition_broadcast(128)),
                      writes=["vfl_rep"], dma=True)
                for i in range(2):
                    S.add("pool", lambda e, i=i: e.memset(Vb[i][:, :, 128:129], 1.0), writes=[("V", i)])

                def load_head(h, par):
                    S.add("sp", lambda e: e.dma_start(out=KTb[par][:], in_=KTd[h]), writes=[("KT", par)], dma=True)
                    S.add("sp", lambda e: e.dma_start(out=QTb[par][:], in_=QTd[h]), writes=[("QT", par)], dma=True)
                    for t0 in range(0, NT, 8):
                        S.add("sp", lambda e, t0=t0: e.dma_start(
                            out=Vb[par][:, t0:t0 + 8, 0:128],
                            in_=Vd[t0:t0 + 8, :, h * 128:(h + 1) * 128].rearrange("t p c -> p t c")),
                            writes=[("V", par)], dma=True)

                load_head(0, 0)

                xi, xk = next_x()
                psF = psX[xi][:, 0:NT * H].rearrange("p (t h) -> p t h", h=H)
                for d in range(NCH):
                    for j in range(NST):
                        t = d * NST + j
                        terms = [(tri_f, t)] + [(ones_f, d * NST + j2) for j2 in range(j)]
                        if d >= 1:
                            terms += [(nones_f, d2 * NST + j2) for d2 in range(1, d + 1) for j2 in range(NST)]
                        for i, (mm, tt) in enumerate(terms):
                            S.add("pe", lambda e, t=t, mm=mm, tt=tt, i=i, n=len(terms): e.matmul(
                                psF[:, t, :], lhsT=mm[:], rhs=logf[:, tt, :], start=(i == 0), stop=(i == n - 1)),
                                reads=["logf", "tri_f", "ones_f", "nones_f"], writes=[xk])
                S.add("dve", lambda e: e.tensor_copy(out=Fsb[:], in_=psF), reads=[xk], writes=["Fsb"])
                xi2, xk2 = next_x()
                psR = psX[xi2][:, 0:NG * H].rearrange("p (g h) -> p g h", h=H)
                for g in range(NG):
                    n = 4 * g + 2
                    for j2 in range(n):
                        S.add("pe", lambda e, g=g, j2=j2, n=n: e.matmul(
                            psR[:, g, :], lhsT=ones_f[:], rhs=logf[:, j2, :], start=(j2 == 0), stop=(j2 == n - 1)),
                            reads=["logf", "ones_f"], writes=[xk2])
                S.add("dve", lambda e: e.tensor_copy(out=FrefB[:], in_=psR), reads=[xk2], writes=["FrefB"])
                for g in range(NG):
                    S.add("dve", lambda e, g=g: e.tensor_tensor(
                        out=biasF[:, g], in0=FrefB[:, g:g + 1, :].broadcast_to([128, NT, H]), in1=Fsb[:],
                        op=ALU.subtract), reads=["FrefB", "Fsb"], writes=["biasF"])
                    S.add("dve", lambda e, g=g: e.tensor_tensor(
                        out=biasF[:, g].rearrange("p (d j) h -> p d (j h)", d=NCH),
                        in0=biasF[:, g].rearrange("p (d j) h -> p d (j h)", d=NCH),
                        in1=vfl_rep[:].unsqueeze(2).broadcast_to([128, NCH, NST * H]), op=ALU.add),
                        reads=["biasF", "vfl_rep"], writes=["biasF"])

                def head_sq(par, h):
                    tiles = [(KTb[par], ("KT", par), n0) for n0 in range(0, TCTX, 512)]
                    tiles += [(QTb[par], ("QT", par), n0) for n0 in range(0, CH, 512)]
                    for i, (buf, bkey, n0) in enumerate(tiles):
                        S.add("pool", lambda e, buf=buf, n0=n0, i=i: e.tensor_tensor(
                            out=sqb[par][i][:], in0=buf[:, n0:n0 + 512], in1=buf[:, n0:n0 + 512], op=ALU.mult),
                            reads=[bkey], writes=[("sqb", i)])

                def head_norm(par, h):
                    xi, xk = next_x()
                    ncol = NSQ * 4
                    for i in range(NSQ):
                        for cq in range(4):
                            S.add("pe", lambda e, i=i, cq=cq: e.matmul(
                                psX[xi][:, i * 4 + cq:i * 4 + cq + 1], lhsT=sqb[par][i][:, cq * 128:(cq + 1) * 128],
                                rhs=ones_b[:, 0:1], start=True, stop=True),
                                reads=[("sqb", i), "ones_b"], writes=[xk])
                    S.add("dve", lambda e: e.tensor_reduce(out=nmx[:, 0:1], in_=psX[xi][:, 0:ncol], axis=AX.X, op=ALU.max),
                          reads=[xk], writes=["nmx"])
                    xi2, xk2 = next_x()
                    S.add("pe", lambda e: e.transpose(out=psX[xi2][0:1, 0:128], in_=nmx[:, 0:1], identity=ident_f[:]),
                          reads=["nmx", "ident_f"], writes=[xk2])
                    S.add("dve", lambda e: e.tensor_reduce(out=nmx[0:1, 1:2], in_=psX[xi2][0:1, 0:128], axis=AX.X,
                                                           op=ALU.max), reads=[xk2], writes=["nmx1"])
                    xi3, xk3 = next_x()
                    S.add("pe", lambda e: e.matmul(psX[xi3][:, 0:1], lhsT=ones_f[0:1, :], rhs=nmx[0:1, 1:2],
                                                   start=True, stop=True), reads=["nmx1", "ones_f"], writes=[xk3])
                    S.add("dve", lambda e: e.tensor_scalar(out=negC[:, h:h + 1], in0=psX[xi3][:, 0:1],
                                                           scalar1=-1.02 * scale, scalar2=None, op0=ALU.mult),
                          reads=[xk3], writes=[("negC", h)])

                def moba_gate(par, hm):
                    xi, xk = next_x()
                    psg = psX[xi][:, 0:NST * NB].rearrange("p (q n) -> p q n", n=NB)
                    for qt in range(NST):
                        S.add("pe", lambda e, qt=qt: e.matmul(
                            psg[:, qt, :], lhsT=QTb[par][:, qt * 128:(qt + 1) * 128], rhs=kmT[:, hm, :],
                            start=True, stop=True), reads=[("QT", par), "kmT"], writes=[xk])
                    g3 = gbias_rep[:].rearrange("p (q n) -> p q n", n=NB)
                    S.add("dve", lambda e: e.tensor_tensor(out=gmb[:], in0=psg, in1=g3, op=ALU.add),
                          reads=[xk, "gbias_rep"], writes=["gmb"])
                    for qt in range(NST):
                        S.add("dve", lambda e, qt=qt: e.max(out=m8[:, qt, :], in_=gmb[:, qt, :]),
                              reads=["gmb"], writes=["m8"])
                    for qt in range(NST):
                        S.add("dve", lambda e, qt=qt: e.tensor_scalar(
                            out=b1[:, qt, :], in0=gmb[:, qt, :], scalar1=m8[:, qt, c.TOPK - 1:c.TOPK],
                            scalar2=-NEG_MASK, op0=ALU.is_ge, op1=ALU.mult), reads=["gmb", "m8"], writes=["b1"])
                    S.add("dve", lambda e: e.scalar_tensor_tensor(
                        out=bb[:].rearrange("p q n -> p (q n)"), in0=b1[:].rearrange("p q n -> p (q n)"),
                        scalar=NEG_MASK, in1=gbias_rep[:], op0=ALU.add, op1=ALU.min),
                        reads=["b1", "gbias_rep"], writes=["bb"])
                    for qt in range(NST):
                        S.add("dve", lambda e, qt=qt: e.memset(bb[:, qt, qt // 2:qt // 2 + 1], 0.0),
                              reads=["bb"], writes=["bb"])
                    xi2, xk2 = next_x()
                    for qt in range(NST):
                        S.add("pe", lambda e, qt=qt: e.transpose(out=psXb[xi2][0:NB, qt * 128:(qt + 1) * 128],
                                                                 in_=bb[:, qt, :], identity=ident_b[:]),
                              reads=["bb", "ident_b"], writes=[xk2])
                    S.add("dve", lambda e: e.tensor_copy(out=biasT[par][0:NB, 0:NST * 128],
                                                         in_=psXb[xi2][0:NB, 0:NST * 128]),
                          reads=[xk2], writes=[("biasT", par)])

                deferred = []

                def flush_deferred():
                    while deferred:
                        deferred.pop(0)()

                def attention(par, h, moba, mid_hook=None):
                    for g in range(NG):
                        a = cn["a"] % 2
                        cn["a"] += 1
                        tiles = [(d, j) for d in range(NCH - 1, 0, -1) for j in range(NST)]
                        tiles += [(0, j) for j in range(4 * g + 4)]

                        def qk_step(idx, d, j, g=g, a=a):
                            t = d * NST + j
                            c0 = 0 if (d > 0 or j < 4 * g) else (j - 4 * g) * 128
                            N = 512 - c0
                            q0 = g * 512 + c0
                            si = cn["s"] % NPS
                            cn["s"] += 1
                            ps, pk = psS[si], ("psS", si)
                            S.add("pe", lambda e: e.matmul(
                                ps[:, 0:N], lhsT=KTb[par][:, t * 128:(t + 1) * 128], rhs=QTb[par][:, q0:q0 + N],
                                start=True, stop=not moba), reads=[("KT", par), ("QT", par)], writes=[pk])
                            if moba:
                                n = t // 2
                                S.add("pe", lambda e: e.matmul(
                                    ps[:, 0:N], lhsT=E_b[0:NB, n * 128:(n + 1) * 128], rhs=biasT[par][0:NB, q0:q0 + N],
                                    start=False, stop=True), reads=["E_b", ("biasT", par)], writes=[pk])
                            pi = cn["p"] % 4
                            cn["p"] += 1
                            pt, ptk = pT[pi], ("pT", pi)
                            if moba:
                                bias_ap, bkey = negC[:, h:h + 1], ("negC", h)
                            else:
                                bias_ap, bkey = bfh[par][:, g, t:t + 1], ("bfh", par)
                            S.add("act", lambda e: e.activation(
                                out=pt[:, 0:N], in_=ps[:, 0:N], func=AF.Exp, bias=bias_ap, scale=scale),
                                reads=[pk, bkey], writes=[ptk])
                            if d == 0 and j >= 4 * g:
                                S.add("pool", lambda e: e.tensor_tensor(out=pt[:, 0:128], in0=pt[:, 0:128],
                                                                        in1=tri_b[:], op=ALU.mult),
                                      reads=[ptk, "tri_b"], writes=[ptk])
                            return (idx, d, j, t, c0, pt, ptk)

                        def pv_step(st, g=g, a=a):
                            idx, d, j, t, c0, pt, ptk = st
                            for qs in range(c0 // 128, 4):
                                lo = qs * 128 - c0
                                S.add("pe", lambda e, qs=qs, lo=lo, first=(idx == 0 and qs % 2 == 0),
                                      last=(d == 0 and j == 4 * g + qs): e.matmul(
                                    oacc[a][qs // 2][:, qs % 2, 0:129], lhsT=pt[:, lo:lo + 128],
                                    rhs=Vb[par][:, t, 0:129], start=first, stop=last, skip_group_check=True),
                                    reads=[ptk, ("V", par)], writes=[("oacc", a, qs // 2)])

                        pend = []
                        for idx, (d, j) in enumerate(tiles):
                            pend.append(qk_step(idx, d, j))
                            if len(pend) > 2:
                                pv_step(pend.pop(0))
                            if idx == 3:
                                flush_deferred()
                        while pend:
                            pv_step(pend.pop(0))
                        ot = otok[a]
                        for qs in range(4):
                            oa = oacc[a][qs // 2]
                            S.add("dve", lambda e, oa=oa, qs=qs, a=a: e.reciprocal(out=rc[:, a * 4 + qs:a * 4 + qs + 1],
                                                                                 in_=oa[:, qs % 2, 128:129]),
                                  reads=[("oacc", a, qs // 2)], writes=[("rc", a, qs)])
                            S.add("dve", lambda e, oa=oa, qs=qs, ot=ot, a=a: e.tensor_scalar(
                                out=ot[:, qs, :], in0=oa[:, qs % 2, 0:128], scalar1=rc[:, a * 4 + qs:a * 4 + qs + 1],
                                scalar2=None, op0=ALU.mult),
                                reads=[("oacc", a, qs // 2), ("rc", a, qs)], writes=[("otok", a)])

                        def tail(g=g, a=a, ot=ot):
                            xi, xk = next_x()
                            for qs in range(4):
                                S.add("pe", lambda e, xi=xi, qs=qs: e.transpose(
                                    out=psXb[xi][:, qs * 128:(qs + 1) * 128], in_=ot[:, qs, :], identity=ident_b[:]),
                                    reads=[("otok", a), "ident_b"], writes=[xk])
                            S.add("act", lambda e, xi=xi: e.copy(out=oT[:, h, g * 512:(g + 1) * 512],
                                                                 in_=psXb[xi][:, 0:512]),
                                  reads=[xk], writes=["oT"])
                        deferred.append(tail)
                        if g == 0 and mid_hook is not None:
                            mid_hook()
                    if NG == 1 and mid_hook is not None:
                        pass

                def prep(h):
                    par = h % 2
                    head_norm(par, h)
                    if h < H:
                        moba_gate(par, h)
                    else:
                        S.add("dve", lambda e: e.tensor_scalar(
                            out=bfh[par][:], in0=biasF[:, :, :, h - H], scalar1=negC[:, h:h + 1], scalar2=None,
                            op0=ALU.add), reads=["biasF", ("negC", h)], writes=[("bfh", par)])

                head_sq(0, 0)
                prep(0)
                for h in range(H2):
                    par = h % 2

                    def hook(h=h):
                        if h + 1 < H2:
                            prep(h + 1)
                        if h == H2 - 3:
                            for h0 in range(0, H, 4):
                                h1 = min(H, h0 + 4)
                                for (wsb, wdr, nm) in ((Wbm, w_bm, "Wbm"), (Wbf, w_bf, "Wbf")):
                                    S.add("pool", lambda e, wsb=wsb, wdr=wdr, h0=h0, h1=h1: e.dma_start(
                                        out=wsb[:, h0:h1, :],
                                        in_=wdr[h0 * 128:h1 * 128, :].rearrange("(h p) c -> p h c", p=128)),
                                        writes=[nm], dma=True)
                    if h + 1 < H2:
                        load_head(h + 1, (h + 1) % 2)
                        head_sq((h + 1) % 2, h + 1)
                    attention(par, h, h < H, mid_hook=hook)
                flush_deferred()
                if debug:
                    S.add("pool", lambda e: e.dma_start(out=dbg_oT, in_=oT[:, 0:H2, :].rearrange("p h t -> p (h t)")),
                          reads=["oT"], dma=True)
                S.emit("ph2")
            if upto < 3:
                return nc

            with contextlib.ExitStack() as ph:
                S = Sched(nc, ss)
                sga = [T(ph, "sga%d" % i, [128, CH], BF16) for i in range(2)]
                sgb = [T(ph, "sgb%d" % i, [128, CH], BF16) for i in range(2)]
                t1 = [T(ph, "t1_%d" % i, [128, 512], F32) for i in range(2)]
                t2 = [T(ph, "t2_%d" % i, [128, 512], F32) for i in range(2)]
                psm = [[P(ph, "psm%d_%d" % (i, n), [128, 512], F32) for n in range(NN)] for i in range(2)]
                psf = [[P(ph, "psf%d_%d" % (i, n), [128, 512], F32) for n in range(NN)] for i in range(2)]
                tc_ = 0
                for cc in range(KC):
                    a = cc % 2
                    S.add("sp", lambda e, a=a, cc=cc: e.dma_start(out=sga[a][:], in_=sgd[0, cc]), writes=[("sga", a)], dma=True)
                    S.add("sp", lambda e, a=a, cc=cc: e.dma_start(out=sgb[a][:], in_=sgd[1, cc]), writes=[("sgb", a)], dma=True)
                    for n in range(NN):
                        for hh in range(H):
                            S.add("pe", lambda e, a=a, n=n, hh=hh, cc=cc: e.matmul(
                                psm[a][n][:, 0:512], lhsT=Wbm[:, hh, cc * 128:(cc + 1) * 128],
                                rhs=oT[:, hh, n * 512:(n + 1) * 512], start=(hh == 0), stop=(hh == H - 1)),
                                reads=["Wbm", "oT"], writes=[("psm", a, n)])
                        for hh in range(H):
                            S.add("pe", lambda e, a=a, n=n, hh=hh, cc=cc: e.matmul(
                                psf[a][n][:, 0:512], lhsT=Wbf[:, hh, cc * 128:(cc + 1) * 128],
                                rhs=oT[:, H + hh, n * 512:(n + 1) * 512], start=(hh == 0), stop=(hh == H - 1)),
                                reads=["Wbf", "oT"], writes=[("psf", a, n)])
                        k = tc_ % 2
                        tc_ += 1
                        S.add("dve", lambda e, a=a, n=n, k=k: e.tensor_tensor(
                            out=t1[k][:], in0=psm[a][n][:, 0:512], in1=sga[a][:, n * 512:(n + 1) * 512], op=ALU.mult),
                            reads=[("psm", a, n), ("sga", a)], writes=[("t1", k)])
                        S.add("dve", lambda e, a=a, n=n, k=k: e.tensor_tensor(
                            out=t2[k][:], in0=psf[a][n][:, 0:512], in1=sgb[a][:, n * 512:(n + 1) * 512], op=ALU.mult),
                            reads=[("psf", a, n), ("sgb", a)], writes=[("t2", k)])
                        S.add("pool", lambda e, cc=cc, n=n, k=k: e.tensor_tensor(
                            out=mergedT[:, cc, n * 512:(n + 1) * 512], in0=t1[k][:], in1=t2[k][:], op=ALU.add),
                            reads=[("t1", k), ("t2", k)], writes=["mergedT"])
                if debug:
                    S.add("pool", lambda e: e.dma_start(out=dbg_mg, in_=mergedT[:, 0:KC, :].rearrange("p h t -> p (h t)")),
                          reads=["mergedT"], dma=True)
                S.emit("ph3a")
            mid2.close()

            with contextlib.ExitStack() as ph:
                S = Sched(nc, ss)
                Wo = T(ph, "Wo", [128, KC, D], BF16)
                gpost = T(ph, "gpost", [128, D], F32)
                gpre2 = T(ph, "gpre2", [128, D], F32)
                xs2 = [T(ph, "xs2_%d" % i, [128, D], F32) for i in range(2)]
                tt_ = T(ph, "tt_", [128, D], F32)
                hb2 = [T(ph, "hb2_%d" % i, [128, D], BF16) for i in range(2)]
                st3 = T(ph, "st3", [128, NST, 16], F32)
                ncg = D // GD
                psM = [P(ph, "psM%d" % i, [128, 512], F32) for i in range(6)]
                pst2 = [P(ph, "pst2_%d" % i, [128, 8, 128], BF16) for i in range(2)]
                S.add("sp", lambda e: e.dma_start(out=gpost[:], in_=gvec[1, :].partition_broadcast(128)),
                      writes=["gpost"], dma=True)
                S.add("sp", lambda e: e.dma_start(out=gpre2[:], in_=gvec[2, :].partition_broadcast(128)),
                      writes=["gpre2"], dma=True)
                for c0 in range(0, D, GD):
                    for k0 in range(0, KC, 8):
                        k1 = min(KC, k0 + 8)
                        S.add("pool", lambda e, c0=c0, k0=k0, k1=k1: e.dma_start(
                            out=Wo[:, k0:k1, c0:c0 + GD],
                            in_=w_out[k0 * 128:k1 * 128, c0:c0 + GD].rearrange("(kc p) c -> p kc c", p=128)),
                            writes=[("Wo", c0)], dma=True)
                tkc = [0]

                def h2_tail(s):
                    xi = s % 2
                    nq = min(8, KC)
                    for q0 in range(0, KC, nq):
                        pi2 = tkc[0] % 2
                        tkc[0] += 1
                        for kk in range(nq):
                            S.add("pe", lambda e, pi2=pi2, kk=kk, q0=q0: e.transpose(
                                out=pst2[pi2][:, kk, :], in_=hb2[xi][:, (q0 + kk) * 128:(q0 + kk + 1) * 128],
                                identity=ident_b[:]), reads=[("hb2", xi), "ident_b"], writes=[("pst2", pi2)])
                        S.add("act", lambda e, pi2=pi2, q0=q0: e.copy(
                            out=h2T[:, q0:q0 + nq, s * 128:(s + 1) * 128], in_=pst2[pi2][:, 0:nq, :]),
                            reads=[("pst2", pi2)], writes=["h2T"])

                mc = 0
                tk = 0
                for s in range(NST):
                    xi = s % 2
                    S.add("sp", lambda e, s=s, xi=xi: e.dma_start(out=xs2[xi][:], in_=xctx[s * 128:(s + 1) * 128, :]),
                          writes=[("xs2", xi)], dma=True)
                    pss = []
                    for cg in range(ncg):
                        pi = mc % 6
                        mc += 1
                        pss.append(pi)
                        for kc in range(KC):
                            S.add("pe", lambda e, pi=pi, kc=kc, s=s, cg=cg: e.matmul(
                                psM[pi][:, 0:GD], lhsT=mergedT[:, kc, s * 128:(s + 1) * 128],
                                rhs=Wo[:, kc, cg * GD:(cg + 1) * GD], start=(kc == 0), stop=(kc == KC - 1)),
                                reads=["mergedT", ("Wo", cg * GD)], writes=[("psM", pi)])
                        S.add("act", lambda e, pi=pi, s=s, cg=cg: e.activation(
                            out=junk[:, 0:GD], in_=psM[pi][:, 0:GD], func=AF.Square, accum_out=st3[:, s, cg:cg + 1]),
                            reads=[("psM", pi)], writes=["junk", ("st3", s)])
                    S.add("dve", lambda e, s=s: e.tensor_reduce(out=st3[:, s, 8:9], in_=st3[:, s, 0:ncg], axis=AX.X,
                                                                op=ALU.add), reads=[("st3", s)], writes=[("st3", s)])
                    S.add("act", lambda e, s=s: e.activation(out=st3[:, s, 9:10], in_=st3[:, s, 8:9], func=AF.Sqrt,
                                                             bias=epsc[:], scale=1.0 / D),
                          reads=[("st3", s), "epsc"], writes=[("st3", s)])
                    S.add("dve", lambda e, s=s: e.reciprocal(out=st3[:, s, 10:11], in_=st3[:, s, 9:10]),
                          reads=[("st3", s)], writes=[("st3", s)])
                    for cg in range(ncg):
                        pi = pss[cg]
                        S.add("dve", lambda e, pi=pi, s=s, cg=cg: e.scalar_tensor_tensor(
                            out=tt_[:, cg * GD:(cg + 1) * GD], in0=psM[pi][:, 0:GD], scalar=st3[:, s, 10:11],
                            in1=gpost[:, cg * GD:(cg + 1) * GD], op0=ALU.mult, op1=ALU.mult),
                            reads=[("psM", pi), ("st3", s), "gpost"], writes=["tt_"])
                    S.add("dve", lambda e, xi=xi: e.tensor_tensor(out=xs2[xi][:], in0=tt_[:], in1=xs2[xi][:], op=ALU.add),
                          reads=["tt_", ("xs2", xi)], writes=[("xs2", xi)])
                    S.add("sp", lambda e, s=s, xi=xi: e.dma_start(out=x1d[s * 128:(s + 1) * 128, :], in_=xs2[xi][:]),
                          reads=[("xs2", xi)], dma=True)
                    S.add("dve", lambda e, s=s, xi=xi: e.scalar_tensor_tensor(
                        out=hb2[xi][:], in0=xs2[xi][:], scalar=1.0, in1=xs2[xi][:], op0=ALU.mult, op1=ALU.mult,
                        accum_out=st3[:, s, 11:12]), reads=[("xs2", xi)], writes=[("hb2", xi), ("st3", s)])
                    S.add("act", lambda e, s=s: e.activation(out=st3[:, s, 12:13], in_=st3[:, s, 11:12], func=AF.Sqrt,
                                                             bias=epsc[:], scale=1.0 / D),
                          reads=[("st3", s), "epsc"], writes=[("st3", s)])
                    S.add("dve", lambda e, s=s: e.reciprocal(out=st3[:, s, 13:14], in_=st3[:, s, 12:13]),
                          reads=[("st3", s)], writes=[("st3", s)])
                    S.add("dve", lambda e, s=s, xi=xi: e.scalar_tensor_tensor(
                        out=hb2[xi][:], in0=xs2[xi][:], scalar=st3[:, s, 13:14], in1=gpre2[:], op0=ALU.mult,
                        op1=ALU.mult), reads=[("xs2", xi), ("st3", s), "gpre2"], writes=[("hb2", xi)])
                    if s >= 1:
                        h2_tail(s - 1)
                h2_tail(NST - 1)
                S.emit("ph3b")
            if upto < 4:
                return nc

            with contextlib.ExitStack() as ph:
                S = Sched(nc, ss)
                FB, UW = c.FB, c.UW
                FK = FB // 128
                Wu = [T(ph, "Wu%d" % i, [128, KC, UW], BF16) for i in range(2)]
                Wd = [T(ph, "Wd%d" % i, [128, FK, GD], BF16) for i in range(2)]
                m = T(ph, "m", [128, NST, D], F32)
                rl = [T(ph, "rl%d" % i, [128, 512], F32) for i in range(2)]
                gpost2 = T(ph, "gpost2", [128, D], F32)
                xs3 = [T(ph, "xs3_%d" % i, [128, D], F32) for i in range(2)]
                st4 = T(ph, "st4", [128, NST, 4], F32)
                psU = [P(ph, "psU%d" % i, [128, 512], F32) for i in range(3)]
                psD = [P(ph, "psD%d" % i, [128, 512], F32) for i in range(3)]
                aTv = lambda par, k: R2[:, par * FK + k, :]
                ncg = D // GD
                S.add("sp", lambda e: e.dma_start(out=gpost2[:], in_=gvec[3, :].partition_broadcast(128)),
                      writes=["gpost2"], dma=True)
                cn4 = {"uc": 0, "dc": 0, "wu": 0, "wd": 0, "rk": 0}

                def load_u(fb, u0):
                    col0 = fb * FB + u0
                    wi = cn4["wu"] % 2
                    cn4["wu"] += 1
                    for k0 in range(0, KC, 8):
                        k1 = min(KC, k0 + 8)
                        S.add("pool", lambda e, k0=k0, k1=k1: e.dma_start(
                            out=Wu[wi][:, k0:k1, :],
                            in_=w_up[k0 * 128:k1 * 128, col0:col0 + UW].rearrange("(kc p) c -> p kc c", p=128)),
                            writes=[("Wu", wi)], dma=True)
                    return wi

                def comp_u(fb, u0, wi):
                    par = fb % 2
                    for f in range(UW // 128):
                        fcl = (u0 + f * 128) // 128
                        for n in range(NN):
                            pi = cn4["uc"] % 3
                            cn4["uc"] += 1
                            for kc in range(KC):
                                S.add("pe", lambda e, pi=pi, kc=kc, f=f, n=n: e.matmul(
                                    psU[pi][:, 0:512], lhsT=Wu[wi][:, kc, f * 128:(f + 1) * 128],
                                    rhs=h2T[:, kc, n * 512:(n + 1) * 512], start=(kc == 0), stop=(kc == KC - 1)),
                                    reads=[("Wu", wi), "h2T"], writes=[("psU", pi)])
                            k = cn4["rk"] % 2
                            cn4["rk"] += 1
                            S.add("act", lambda e, pi=pi, k=k: e.activation(out=rl[k][:], in_=psU[pi][:, 0:512],
                                                                            func=AF.Relu),
                                  reads=[("psU", pi)], writes=[("rl", k)])
                            S.add("pool", lambda e, k=k, fcl=fcl, n=n: e.tensor_tensor(
                                out=aTv(par, fcl)[:, n * 512:(n + 1) * 512], in0=rl[k][:], in1=rl[k][:], op=ALU.mult),
                                reads=[("rl", k)], writes=[("aT", par)])

                def load_d(fb, cg):
                    wi = cn4["wd"] % 2
                    cn4["wd"] += 1
                    S.add("pool", lambda e: e.dma_start(
                        out=Wd[wi][:],
                        in_=w_down[fb * FB:(fb + 1) * FB, cg * GD:(cg + 1) * GD].rearrange("(kc p) c -> p kc c", p=128)),
                        writes=[("Wd", wi)], dma=True)
                    return wi

                def comp_d(fb, cg, wi):
                    par = fb % 2
                    for s in range(NST):
                        pi = cn4["dc"] % 3
                        cn4["dc"] += 1
                        for kc in range(FK):
                            S.add("pe", lambda e, pi=pi, kc=kc, s=s: e.matmul(
                                psD[pi][:, 0:GD], lhsT=aTv(par, kc)[:, s * 128:(s + 1) * 128], rhs=Wd[wi][:, kc, :],
                                start=(kc == 0), stop=(kc == FK - 1)),
                                reads=[("aT", par), ("Wd", wi)], writes=[("psD", pi)])
                        msl = m[:, s, cg * GD:(cg + 1) * GD]
                        if fb == 0:
                            S.add("act", lambda e, pi=pi, msl=msl: e.copy(out=msl, in_=psD[pi][:, 0:GD]),
                                  reads=[("psD", pi)], writes=[("m", s, cg)])
                        else:
                            S.add("dve", lambda e, pi=pi, msl=msl: e.tensor_tensor(out=msl, in0=psD[pi][:, 0:GD],
                                                                                 in1=msl, op=ALU.add),
                                  reads=[("psD", pi), ("m", s, cg)], writes=[("m", s, cg)])

                tasks4 = []
                for fb in range(c.NFB):
                    for u0 in range(0, FB, UW):
                        tasks4.append((load_u, comp_u, fb, u0))
                    for cg in range(ncg):
                        tasks4.append((load_d, comp_d, fb, cg))
                nxt_w = tasks4[0][0](tasks4[0][2], tasks4[0][3])
                for i, (lf_, cf_, a1, a2) in enumerate(tasks4):
                    cur_w = nxt_w
                    if i + 1 < len(tasks4):
                        t2 = tasks4[i + 1]
                        nxt_w = t2[0](t2[2], t2[3])
                    cf_(a1, a2, cur_w)
                for s in range(NST):
                    xi = s % 2
                    mk = [("m", s, cg) for cg in range(ncg)]
                    S.add("sp", lambda e, s=s, xi=xi: e.dma_start(out=xs3[xi][:], in_=x1d[s * 128:(s + 1) * 128, :]),
                          writes=[("xs3", xi)], dma=True)
                    S.add("dve", lambda e, s=s: e.scalar_tensor_tensor(
                        out=junk[:], in0=m[:, s, :], scalar=1.0, in1=m[:, s, :], op0=ALU.mult, op1=ALU.mult,
                        accum_out=st4[:, s, 0:1]), reads=mk, writes=["junk", ("st4", s)])
                    S.add("act", lambda e, s=s: e.activation(out=st4[:, s, 1:2], in_=st4[:, s, 0:1], func=AF.Sqrt,
                                                             bias=epsc[:], scale=1.0 / D),
                          reads=[("st4", s), "epsc"], writes=[("st4", s)])
                    S.add("dve", lambda e, s=s: e.reciprocal(out=st4[:, s, 2:3], in_=st4[:, s, 1:2]),
                          reads=[("st4", s)], writes=[("st4", s)])
                    S.add("dve", lambda e, s=s: e.scalar_tensor_tensor(
                        out=m[:, s, :], in0=m[:, s, :], scalar=st4[:, s, 2:3], in1=gpost2[:], op0=ALU.mult,
                        op1=ALU.mult), reads=mk + [("st4", s), "gpost2"], writes=mk)
                    S.add("dve", lambda e, s=s, xi=xi: e.tensor_tensor(out=xs3[xi][:], in0=m[:, s, :], in1=xs3[xi][:],
                                                                     op=ALU.add),
                          reads=mk + [("xs3", xi)], writes=[("xs3", xi)])
                    S.add("sp", lambda e, s=s, xi=xi: e.dma_start(out=out[s * 128:(s + 1) * 128, :], in_=xs3[xi][:]),
                          reads=[("xs3", xi)], dma=True)
                S.emit("ph4")
    return nc


def make_core_inputs(inp, core, cfg=FULL):
    c = cfg
    b, j = divmod(core, c.NCH)
    x = np.asarray(inp["x"])[b]
    CH, NCH = c.CH, c.NCH
    order = [(j - d) % NCH for d in range(NCH)]
    xctx = np.ascontiguousarray(np.concatenate([x[o * CH:(o + 1) * CH] for o in order], 0), dtype=np.float32)
    pos = np.concatenate([np.arange(o * CH, (o + 1) * CH) for o in order]).astype(np.float32)
    inv_freq = (np.float32(ROPE_THETA) ** (-np.arange(0, 32, 2, dtype=np.float32) / np.float32(32))).astype(np.float32)
    ang = (pos[:, None] * inv_freq[None, :]).astype(np.float32)
    valid = [j - d >= 0 for d in range(NCH)]
    vflag = np.array([[0.0 if v else NEG_MASK for v in valid]], np.float32)
    bpc = CH // 256
    gb = np.full((c.NST, c.NB), -1e30, np.float32)
    for qt in range(c.NST):
        bq = qt // 2
        for n in range(c.NB):
            d, i = divmod(n, bpc)
            if (d == 0 and i < bq) or (d >= 1 and valid[d]):
                gb[qt, n] = 0.0
    k = np.arange(128)
    cE = np.zeros((c.NB, c.NB * 128), np.float32)
    for n in range(c.NB):
        cE[n, n * 128:(n + 1) * 128] = 1.0
    sq = lambda a: np.ascontiguousarray(np.asarray(a)[0], dtype=np.float32)
    return {
        "xctx": xctx,
        "w_in": sq(inp["w_in"]), "w_bm": sq(inp["w_branch_moba"]), "w_bf": sq(inp["w_branch_fox"]),
        "w_out": sq(inp["w_out"]), "w_up": sq(inp["w_up"]), "w_down": sq(inp["w_down"]),
        "gvec": np.stack([sq(inp["g_mix_pre"]), sq(inp["g_mix_post"]), sq(inp["g_mlp_pre"]), sq(inp["g_mlp_post"])], 0),
        "bfg": np.asarray(inp["b_forget"], np.float32).reshape(1, c.H),
        "rope_cos": np.cos(ang).astype(np.float32), "rope_sin": np.sin(ang).astype(np.float32),
        "gbias": gb.reshape(1, -1), "vflag": vflag,
        "cident": np.eye(128, dtype=np.float32),
        "ctri": (k[:, None] <= k[None, :]).astype(np.float32),
        "cE": cE,
    }


_NC_CACHE = {}


def kernel(**inputs):
    cfg = FULL
    if "nc" not in _NC_CACHE:
        _NC_CACHE["nc"] = build(cfg)
    nc = _NC_CACHE["nc"]
    n_cores = 2 * cfg.NCH
    shared = None
    in_maps = []
    for core in range(n_cores):
        m = make_core_inputs(inputs, core, cfg)
        if shared is None:
            shared = m
        else:
            for key in m:
                if key not in ("xctx", "rope_cos", "rope_sin", "gbias", "vflag"):
                    m[key] = shared[key]
        in_maps.append(m)
    res = run_bass_kernel_spmd(nc, in_maps, core_ids=list(range(n_cores)))
    B = n_cores // cfg.NCH
    outp = np.empty((B, cfg.TCTX, cfg.D), np.float32)
    for core in range(n_cores):
        b, j = divmod(core, cfg.NCH)
        outp[b, j * cfg.CH:(j + 1) * cfg.CH] = np.asarray(res.results[core]["out"], np.float32)
    return outp
```

```python
import contextlib
import numpy as np
import concourse.bass as bass
import concourse.mybir as mybir
from concourse.bass_utils import run_bass_kernel_spmd

F32 = mybir.dt.float32
BF16 = mybir.dt.bfloat16
AF = mybir.ActivationFunctionType
ALU = mybir.AluOpType
AX = mybir.AxisListType

ENGS = ("pe", "act", "dve", "pool", "sp")
N_DMA_SEMS = 24
N_HW_SEMS = 16
OP_LIMIT = None
RMS_EPS = 1e-6
ROPE_THETA = 500000.0
NEG_MASK = -30000.0


class _Op:
    __slots__ = ("eng", "fn", "is_dma", "signal", "sigval", "dsem", "dval", "deps", "waits")


class SemState:
    def __init__(self, nc, stack):
        self.sems = {}
        for e in ("pe", "act", "dve", "pool"):
            self.sems[("eng", e)] = stack.enter_context(nc.semaphore("s_" + e))
        for i in range(N_DMA_SEMS):
            self.sems[("dma", i)] = stack.enter_context(nc.semaphore("s_dma%d" % i))
        self.val = {k: 0 for k in self.sems}
        self.dma_count = {"hw": 0, "sw": 0}


def _is_psum(k):
    n = k[0] if isinstance(k, tuple) else k
    return isinstance(n, str) and (n.startswith("ps") or n == "oacc")


class Sched:
    def __init__(self, nc, ss):
        self.nc = nc
        self.ss = ss
        self.ops = []
        self.streams = {e: [] for e in ENGS}
        self.last_w = {}
        self.readers = {}
        self.dma_last = [None] * N_DMA_SEMS
        self.dma_val = [ss.val[("dma", i)] for i in range(N_DMA_SEMS)]

    def add(self, eng, fn, reads=(), writes=(), dma=False):
        if OP_LIMIT is not None and len(self.ops) >= OP_LIMIT:
            return None
        pr = [k for k in reads if _is_psum(k)]
        if pr:
            reads = [k for k in reads if not _is_psum(k)]
            writes = list(writes) + [k for k in pr if k not in writes]
        op = _Op()
        op.eng, op.fn, op.is_dma = eng, fn, dma
        op.signal, op.sigval, op.dsem, op.dval, op.waits = False, None, None, None, None
        deps = []
        for k in reads:
            w = self.last_w.get(k)
            if w is not None:
                deps.append(w)
        for k in writes:
            w = self.last_w.get(k)
            if w is not None:
                deps.append(w)
            rd = self.readers.get(k)
            if rd:
                deps.extend(rd.values())
        if dma:
            if eng == "pool":
                s = N_HW_SEMS + self.ss.dma_count["sw"] % (N_DMA_SEMS - N_HW_SEMS)
                self.ss.dma_count["sw"] += 1
            else:
                s = self.ss.dma_count["hw"] % N_HW_SEMS
                self.ss.dma_count["hw"] += 1
            if self.dma_last[s] is not None:
                deps.append(self.dma_last[s])
            self.dma_val[s] += 16
            op.dsem, op.dval = s, self.dma_val[s]
            self.dma_last[s] = op
        op.deps = deps
        gid = len(self.ops)
        for k in reads:
            self.readers.setdefault(k, {})[("dma", gid) if dma else eng] = op
        for k in writes:
            self.last_w[k] = op
            self.readers[k] = {}
        self.streams[eng].append(op)
        self.ops.append(op)
        return op

    @staticmethod
    def _skip(op, d):
        return (not d.is_dma) and d.eng == op.eng and op.eng == "pe" and not op.is_dma

    def emit(self, name):
        nc, ss = self.nc, self.ss
        for op in self.ops:
            for d in op.deps:
                if not d.is_dma and not self._skip(op, d):
                    d.signal = True
        for e in ("pe", "act", "dve", "pool"):
            c = ss.val[("eng", e)]
            for op in self.streams[e]:
                if op.signal and not op.is_dma:
                    c += 1
                    op.sigval = c
            ss.val[("eng", e)] = c
        for i in range(N_DMA_SEMS):
            ss.val[("dma", i)] = self.dma_val[i]
        for e in ENGS:
            known = {}
            for op in self.streams[e]:
                need = {}
                for d in op.deps:
                    if d.is_dma:
                        key, val = ("dma", d.dsem), d.dval
                    elif self._skip(op, d):
                        continue
                    else:
                        key, val = ("eng", d.eng), d.sigval
                    if known.get(key, 0) >= val:
                        continue
                    if need.get(key, 0) < val:
                        need[key] = val
                known.update(need)
                op.waits = need
        sems = ss.sems
        with nc.Block() as block:
            def run(e, eng):
                for op in self.streams[e]:
                    for k, v in op.waits.items():
                        eng.wait_ge(sems[k], v)
                    ins = op.fn(eng)
                    if op.is_dma:
                        ins.then_inc(sems[("dma", op.dsem)], 16)
                    elif op.signal:
                        ins.then_inc(sems[("eng", e)], 1)

            @block.tensor
            def _(eng):
                run("pe", eng)

            @block.scalar
            def _(eng):
                run("act", eng)

            @block.vector
            def _(eng):
                run("dve", eng)

            @block.gpsimd
            def _(eng):
                run("pool", eng)

            @block.sync
            def _(eng):
                run("sp", eng)
                for i in range(N_DMA_SEMS):
                    if self.dma_val[i] > 0:
                        eng.wait_ge(sems[("dma", i)], self.dma_val[i])
                for e in ("pe", "act", "dve", "pool"):
                    if ss.val[("eng", e)] > 0:
                        eng.wait_ge(sems[("eng", e)], ss.val[("eng", e)])


class Cfg:
    def __init__(self, D=2048, H=8, NCH=4, CH=1024, DFF=8192):
        self.D, self.H, self.NCH, self.CH, self.DFF = D, H, NCH, CH, DFF
        self.KC = D // 128
        self.NST = CH // 128
        self.NT = NCH * self.NST
        self.TCTX = NCH * CH
        self.NB = self.TCTX // 256
        self.NG = CH // 512
        self.HG = min(4, H)
        self.GW = self.HG * 128
        self.HW = H * 128
        self.o_mq, self.o_mk, self.o_mv = 0, self.HW, 2 * self.HW
        self.o_fq, self.o_fk, self.o_fv = 3 * self.HW, 4 * self.HW, 5 * self.HW
        self.o_ff = 6 * self.HW
        self.o_ga = self.o_ff + H
        self.o_gb = self.o_ga + D
        self.WIN = self.o_gb + D
        self.GD = min(512, D)
        self.FC = DFF // 128
        self.FB = min(1024, DFF)
        self.NFB = DFF // self.FB
        self.UW = min(256, DFF)
        self.TOPK = 3
        assert H >= 2 and self.NB >= 8


FULL = Cfg()


def _cp(e, out, in_):
    if hasattr(e, "tensor_copy"):
        return e.tensor_copy(out=out, in_=in_)
    return e.copy(out=out, in_=in_)


def MARK(name, S):
    if MARKS is not None:
        MARKS.append((name, len(S.ops)))


MARKS = None


def build(cfg=FULL, upto=4, debug=False):
    c = cfg
    D, H, KC, NCH, CH, NST, NT, TCTX = c.D, c.H, c.KC, c.NCH, c.CH, c.NST, c.NT, c.TCTX
    NB, NG, HG, GW, HW, GD = c.NB, c.NG, c.HG, c.GW, c.HW, c.GD
    H2 = 2 * H
    NN = CH // 512
    nc = bass.Bass("TRN2", target_bir_lowering=False)
    din = lambda n, s, dt=F32: nc.dram_tensor(n, s, dt, kind="ExternalInput").ap()
    dscr = lambda n, s, dt: nc.dram_tensor(n, s, dt, kind="Internal").ap()
    xctx = din("xctx", [TCTX, D])
    w_in = din("w_in", [D, c.WIN])
    w_bm = din("w_bm", [HW, D])
    w_bf = din("w_bf", [HW, D])
    w_out = din("w_out", [D, D])
    w_up = din("w_up", [D, c.DFF])
    w_down = din("w_down", [c.DFF, D])
    gvec = din("gvec", [4, D])
    bfg = din("bfg", [1, H])
    rcos = din("rope_cos", [TCTX, 16])
    rsin = din("rope_sin", [TCTX, 16])
    gbias_d = din("gbias", [1, NST * NB])
    vflag_d = din("vflag", [1, NCH])
    cident = din("cident", [128, 128])
    ctri = din("ctri", [128, 128])
    cE = din("cE", [NB, NB * 128])
    out = nc.dram_tensor("out", [CH, D], F32, kind="ExternalOutput").ap()
    KTd = dscr("KTd", [H2, 128, TCTX], BF16)
    QTd = dscr("QTd", [H2, 128, CH], BF16)
    Vd = dscr("Vd", [NT, 128, H2 * 128], BF16)
    sgd = dscr("sgd", [2, KC, 128, CH], BF16)
    x1d = dscr("x1d", [CH, D], F32)
    if debug:
        dbg_logf = nc.dram_tensor("dbg_logf", [128, NT * H], F32, kind="ExternalOutput").ap()
        dbg_kmT = nc.dram_tensor("dbg_kmT", [128, H * NB], F32, kind="ExternalOutput").ap()
        dbg_oT = nc.dram_tensor("dbg_oT", [128, H2 * CH], F32, kind="ExternalOutput").ap()
        dbg_mg = nc.dram_tensor("dbg_mg", [128, KC * CH], F32, kind="ExternalOutput").ap()

    scale = 128.0 ** -0.5

    with contextlib.ExitStack() as top:
        ss = SemState(nc, top)
        T = lambda st, n, s, dt: st.enter_context(nc.sbuf_tensor(n, s, dt))
        P = lambda st, n, s, dt: st.enter_context(nc.psum_tensor(n, s, dt))
        ident_b = T(top, "ident_b", [128, 128], BF16)
        tri_b = T(top, "tri_b", [128, 128], BF16)
        tri_f = T(top, "tri_f", [128, 128], F32)
        ident_f = T(top, "ident_f", [128, 128], F32)
        ones_f = T(top, "ones_f", [128, 128], F32)
        nones_f = T(top, "nones_f", [128, 128], F32)
        ones_b = T(top, "ones_b", [128, 128], BF16)
        c256 = T(top, "c256", [128, 1], BF16)
        epsc = T(top, "epsc", [128, 1], F32)
        E_b = T(top, "E_b", [NB, NB * 128], BF16)
        zff = T(top, "zff", [128, NT, H], F32)
        logf = T(top, "logf", [128, NT, H], F32)
        kmT = T(top, "kmT", [128, H, NB], BF16)
        stat = T(top, "stat", [128, NT, 8], F32)
        junk = T(top, "junk", [128, D], BF16)

        with contextlib.ExitStack() as ph:
            S = Sched(nc, ss)
            g_rep = T(ph, "g_rep", [128, D], F32)
            bfg_rep = T(ph, "bfg_rep", [128, H], F32)
            cos_sb = T(ph, "cos_sb", [128, NT, 16], F32)
            sin_sb = T(ph, "sin_sb", [128, NT, 16], F32)
            xs = [T(ph, "xs%d" % i, [128, D], F32) for i in range(2)]
            hb = [T(ph, "hb%d" % i, [128, D], BF16) for i in range(3)]
            hT = [T(ph, "hT%d" % i, [128, KC, CH], BF16) for i in range(2)]
            NWB = 3
            wb = [T(ph, "wb%d" % i, [128, KC, max(GD, GW)], BF16) for i in range(NWB)]
            vst = [T(ph, "vst%d" % i, [128, NST, GW], BF16) for i in range(2)]
            kst = [T(ph, "kst%d" % i, [128, HG, CH], BF16) for i in range(2)]
            kb = [T(ph, "kb%d" % i, [128, GW], BF16) for i in range(4)]
            rt = [T(ph, "rt%d" % i, [128, HG, 16], F32) for i in range(4)]
            wff = T(ph, "wff", [128, KC, H], BF16)
            lt = T(ph, "lt", [128, NT * H], F32)
            pst = [P(ph, "pst%d" % i, [128, 8, 128], BF16) for i in range(2)]
            psmm = [P(ph, "psmm%d" % i, [128, 512], F32) for i in range(3)]
            pstk = [P(ph, "pstk%d" % i, [128, 8, 128], BF16) for i in range(2)]
            pskm = P(ph, "pskm", [128, 512], F32)

            S.add("pool", lambda e: e.dma_start(out=ident_b[:], in_=cident), writes=["ident_b"], dma=True)
            S.add("pool", lambda e: e.dma_start(out=tri_b[:], in_=ctri), writes=["tri_b"], dma=True)
            S.add("sp", lambda e: e.dma_start(out=tri_f[:], in_=ctri), writes=["tri_f"], dma=True)
            S.add("sp", lambda e: e.dma_start(out=ident_f[:], in_=cident), writes=["ident_f"], dma=True)
            S.add("pool", lambda e: e.dma_start(out=E_b[:], in_=cE), writes=["E_b"], dma=True)
            S.add("dve", lambda e: e.memset(ones_f[:], 1.0), writes=["ones_f"])
            S.add("dve", lambda e: e.memset(nones_f[:], -1.0), writes=["nones_f"])
            S.add("dve", lambda e: e.memset(ones_b[:], 1.0), writes=["ones_b"])
            S.add("dve", lambda e: e.memset(c256[:], 1.0 / 256.0), writes=["c256"])
            S.add("dve", lambda e: e.memset(epsc[:], RMS_EPS), writes=["epsc"])
            S.add("sp", lambda e: e.dma_start(out=g_rep[:], in_=gvec[0, :].partition_broadcast(128)),
                  writes=["g_rep"], dma=True)
            S.add("sp", lambda e: e.dma_start(out=bfg_rep[:], in_=bfg[0, :].partition_broadcast(128)),
                  writes=["bfg_rep"], dma=True)
            S.add("sp", lambda e: e.dma_start(out=cos_sb[:], in_=rcos.rearrange("(t p) f -> p t f", p=128)),
                  writes=["cos_sb"], dma=True)
            S.add("sp", lambda e: e.dma_start(out=sin_sb[:], in_=rsin.rearrange("(t p) f -> p t f", p=128)),
                  writes=["sin_sb"], dma=True)
            S.add("pool", lambda e: e.dma_start(
                out=wff[:], in_=w_in[:, c.o_ff:c.o_ff + H].rearrange("(kc p) c -> p kc c", p=128)),
                writes=["wff"], dma=True)

            cnt = {"mm": 0, "w": 0, "v": 0, "k": 0, "kb": 0, "tk": 0, "tk2": 0}

            def load_w(src2d, col0, width):
                i = cnt["w"] % NWB
                cnt["w"] += 1
                buf = wb[i]
                for k0 in range(0, KC, 8):
                    k1 = min(KC, k0 + 8)
                    S.add("pool", lambda e, k0=k0, k1=k1: e.dma_start(
                        out=buf[:, k0:k1, 0:width],
                        in_=src2d[k0 * 128:k1 * 128, col0:col0 + width].rearrange("(kc p) c -> p kc c", p=128)),
                        writes=[("wb", i)], dma=True)
                return buf, ("wb", i)

            def hT_stageA(d, s):
                t = d * NST + s
                xi = t % 2
                hi = t % 3
                S.add("pool", lambda e: e.dma_start(out=xs[xi][:], in_=xctx[t * 128:(t + 1) * 128, :]),
                      writes=[("xs", xi)], dma=True)
                S.add("dve", lambda e: e.scalar_tensor_tensor(
                    out=hb[hi][:], in0=xs[xi][:], scalar=1.0, in1=xs[xi][:], op0=ALU.mult, op1=ALU.mult,
                    accum_out=stat[:, t, 0:1]),
                    reads=[("xs", xi)], writes=[("hb", hi), ("stat", t)])
                S.add("act", lambda e: e.activation(out=stat[:, t, 1:2], in_=stat[:, t, 0:1], func=AF.Sqrt,
                                                    bias=epsc[:], scale=1.0 / D),
                      reads=[("stat", t), "epsc"], writes=[("stat", t)])
                S.add("dve", lambda e: e.reciprocal(out=stat[:, t, 2:3], in_=stat[:, t, 1:2]),
                      reads=[("stat", t)], writes=[("stat", t)])
                S.add("dve", lambda e: e.scalar_tensor_tensor(
                    out=hb[hi][:], in0=xs[xi][:], scalar=stat[:, t, 2:3], in1=g_rep[:], op0=ALU.mult,
                    op1=ALU.mult),
                    reads=[("xs", xi), ("stat", t), "g_rep"], writes=[("hb", hi)])

            def hT_stageB(d, s):
                hTd = hT[d % 2]
                t = d * NST + s
                hi = t % 3
                nq = min(8, KC)
                for q0 in range(0, KC, nq):
                    pi = cnt["tk"] % 2
                    cnt["tk"] += 1
                    for kk in range(nq):
                        S.add("pe", lambda e, pi=pi, kk=kk, q0=q0: e.transpose(
                            out=pst[pi][:, kk, :], in_=hb[hi][:, (q0 + kk) * 128:(q0 + kk + 1) * 128],
                            identity=ident_b[:]),
                            reads=[("hb", hi), "ident_b"], writes=[("pst", pi)])
                    S.add("act", lambda e, pi=pi, q0=q0: e.copy(
                        out=hTd[:, q0:q0 + nq, s * 128:(s + 1) * 128], in_=pst[pi][:, 0:nq, :]),
                        reads=[("pst", pi)], writes=[("hT", d % 2)])

            pendA = []
            pendB = []

            def drip(n=1):
                for _ in range(n):
                    if pendA:
                        a = pendA.pop(0)
                        hT_stageA(*a)
                        pendB.append(a)
                    if pendB and (len(pendB) > 1 or not pendA):
                        hT_stageB(*pendB.pop(0))

            def next_ps():
                i = cnt["mm"] % 3
                cnt["mm"] += 1
                return psmm[i], ("psmm", i)

            def rope_tail(s, d, mode, hbase, nh, ki):
                t = d * NST + s
                bi = s % 4
                kbt = kb[bi]
                pi = cnt["tk2"] % 2
                cnt["tk2"] += 1
                for hh in range(nh):
                    S.add("pe", lambda e, pi=pi, hh=hh: e.transpose(
                        out=pstk[pi][:, hh, :], in_=kbt[:, hh * 128:(hh + 1) * 128], identity=ident_b[:]),
                        reads=[("kb", bi), "ident_b"], writes=[("pstk", pi)])
                S.add("dve", lambda e, pi=pi: e.tensor_copy(
                    out=kst[ki][:, 0:nh, s * 128:(s + 1) * 128], in_=pstk[pi][:, 0:nh, :]),
                    reads=[("pstk", pi)], writes=[("kst", ki)])
                if mode == "k_rope" and s % 2 == 1:
                    kprev = kb[(s - 1) % 4]
                    for hh in range(nh):
                        S.add("pe", lambda e, hh=hh: e.matmul(
                            pskm[:, hh:hh + 1], lhsT=kprev[:, hh * 128:(hh + 1) * 128], rhs=c256[:],
                            start=True, stop=False),
                            reads=[("kb", (s - 1) % 4), "c256"], writes=["pskm"])
                        S.add("pe", lambda e, hh=hh: e.matmul(
                            pskm[:, hh:hh + 1], lhsT=kbt[:, hh * 128:(hh + 1) * 128], rhs=c256[:],
                            start=False, stop=True),
                            reads=[("kb", bi), "c256"], writes=["pskm"])
                    n = t // 2
                    S.add("dve", lambda e: e.tensor_copy(out=kmT[:, hbase:hbase + nh, n], in_=pskm[:, 0:nh]),
                          reads=["pskm"], writes=["kmT"])

            def tok_group(d, col0, width, mode, hbase, wbuf, wkey):
                hTd = hT[d % 2]
                nh = width // 128
                if mode == "v":
                    vi = cnt["v"] % 2
                    cnt["v"] += 1
                else:
                    ki = cnt["k"] % 2
                    cnt["k"] += 1
                for s in range(NST):
                    t = d * NST + s
                    ps, pk = next_ps()
                    for kc in range(KC):
                        S.add("pe", lambda e, ps=ps, kc=kc, s=s: e.matmul(
                            ps[:, 0:width], lhsT=hTd[:, kc, s * 128:(s + 1) * 128], rhs=wbuf[:, kc, 0:width],
                            start=(kc == 0), stop=(kc == KC - 1)),
                            reads=[("hT", d % 2), wkey], writes=[pk])
                    if mode == "v":
                        S.add("act", lambda e, ps=ps, s=s: e.copy(out=vst[vi][:, s, 0:width], in_=ps[:, 0:width]),
                              reads=[pk], writes=[("vst", vi)])
                        continue
                    bi = s % 4
                    kbt = kb[bi]
                    ps3 = ps[:, 0:width].rearrange("p (h c) -> p h c", c=128)
                    kb3 = kbt[:, 0:width].rearrange("p (h c) -> p h c", c=128)
                    cosb = cos_sb[:, t:t + 1, :].broadcast_to([128, nh, 16])
                    sinb = sin_sb[:, t:t + 1, :].broadcast_to([128, nh, 16])
                    S.add("act", lambda e, ps=ps, kbt=kbt: e.copy(out=kbt[:, 0:width], in_=ps[:, 0:width]),
                          reads=[pk], writes=[("kb", bi)])
                    x1, x2 = ps3[:, :, 0:16], ps3[:, :, 16:32]
                    r = [rt[i][:, 0:nh, :] for i in range(4)]
                    S.add("dve", lambda e, x1=x1, cosb=cosb, r=r: e.tensor_tensor(out=r[0], in0=x1, in1=cosb, op=ALU.mult),
                          reads=[pk, "cos_sb"], writes=[("rt", 0)])
                    S.add("dve", lambda e, x2=x2, sinb=sinb, r=r: e.tensor_tensor(out=r[1], in0=x2, in1=sinb, op=ALU.mult),
                          reads=[pk, "sin_sb"], writes=[("rt", 1)])
                    S.add("dve", lambda e, x2=x2, cosb=cosb, r=r: e.tensor_tensor(out=r[2], in0=x2, in1=cosb, op=ALU.mult),
                          reads=[pk, "cos_sb"], writes=[("rt", 2)])
                    S.add("dve", lambda e, x1=x1, sinb=sinb, r=r: e.tensor_tensor(out=r[3], in0=x1, in1=sinb, op=ALU.mult),
                          reads=[pk, "sin_sb"], writes=[("rt", 3)])
                    S.add("dve", lambda e, kb3=kb3, r=r: e.tensor_tensor(out=kb3[:, :, 0:16], in0=r[0], in1=r[1],
                                                                       op=ALU.subtract),
                          reads=[("rt", 0), ("rt", 1), ("kb", bi)], writes=[("kb", bi)])
                    S.add("dve", lambda e, kb3=kb3, r=r: e.tensor_tensor(out=kb3[:, :, 16:32], in0=r[2], in1=r[3],
                                                                       op=ALU.add),
                          reads=[("rt", 2), ("rt", 3), ("kb", bi)], writes=[("kb", bi)])
                    if s >= 1:
                        rope_tail(s - 1, d, mode, hbase, nh, ki)
                if mode != "v":
                    rope_tail(NST - 1, d, mode, hbase, nh, ki)
                if mode == "v":
                    S.add("sp", lambda e: e.dma_start(
                        out=Vd[d * NST:(d + 1) * NST, :, hbase * 128:hbase * 128 + width].rearrange("t p c -> p t c"),
                        in_=vst[vi][:, :, 0:width]),
                        reads=[("vst", vi)], dma=True)
                else:
                    dst = KTd[hbase:hbase + nh, :, d * CH:(d + 1) * CH] if mode == "k_rope" else QTd[hbase:hbase + nh, :, :]
                    S.add("sp", lambda e: e.dma_start(out=dst.rearrange("h p t -> p h t"), in_=kst[ki][:, 0:nh, :]),
                          reads=[("kst", ki)], dma=True)
                drip()

            def feat_group(d, col0, width, mode, hbase, wbuf, wkey, kc0=0):
                hTd = hT[d % 2]
                nh = width // 128
                for h0 in range(0, nh, HG):
                    h1 = min(nh, h0 + HG)
                    ki = cnt["k"] % 2
                    cnt["k"] += 1
                    for hh in range(h0, h1):
                        for n0 in range(0, CH, 512):
                            ps, pk = next_ps()
                            for kc in range(KC):
                                S.add("pe", lambda e, ps=ps, kc=kc, hh=hh, n0=n0: e.matmul(
                                    ps[:, 0:512], lhsT=wbuf[:, kc, hh * 128:(hh + 1) * 128],
                                    rhs=hTd[:, kc, n0:n0 + 512], start=(kc == 0), stop=(kc == KC - 1)),
                                    reads=[("hT", d % 2), wkey], writes=[pk])
                            if mode == "gate":
                                S.add("act", lambda e, ps=ps, hh=hh, n0=n0, ki=ki, h0=h0: e.activation(
                                    out=kst[ki][:, hh - h0, n0:n0 + 512], in_=ps[:, 0:512], func=AF.Sigmoid),
                                    reads=[pk], writes=[("kst", ki)])
                            else:
                                S.add("dve", lambda e, ps=ps, hh=hh, n0=n0, ki=ki, h0=h0: e.tensor_copy(
                                    out=kst[ki][:, hh - h0, n0:n0 + 512], in_=ps[:, 0:512]),
                                    reads=[pk], writes=[("kst", ki)])
                    if mode == "kT":
                        dst = KTd[hbase + h0:hbase + h1, :, d * CH:(d + 1) * CH]
                    elif mode == "qT":
                        dst = QTd[hbase + h0:hbase + h1, :, :]
                    else:
                        dst = sgd[hbase, kc0 + h0:kc0 + h1, :, :]
                    S.add("sp", lambda e, ki=ki, dst=dst, h0=h0, h1=h1: e.dma_start(
                        out=dst.rearrange("h p t -> p h t"), in_=kst[ki][:, 0:h1 - h0, :]),
                        reads=[("kst", ki)], dma=True)
                drip()

            def ff_group(d):
                hTd = hT[d % 2]
                for s in range(NST):
                    t = d * NST + s
                    ps, pk = next_ps()
                    for kc in range(KC):
                        S.add("pe", lambda e, ps=ps, kc=kc, s=s: e.matmul(
                            ps[:, 0:H], lhsT=hTd[:, kc, s * 128:(s + 1) * 128], rhs=wff[:, kc, :],
                            start=(kc == 0), stop=(kc == KC - 1)),
                            reads=[("hT", d % 2), "wff"], writes=[pk])
                    S.add("dve", lambda e, ps=ps, t=t: e.tensor_tensor(out=zff[:, t, :], in0=ps[:, 0:H], in1=bfg_rep[:],
                                                                     op=ALU.add),
                          reads=[pk, "bfg_rep"], writes=["zff"])
                drip()

            MARK("consts", S)
            for s in range(NST):
                hT_stageA(0, s)
                if s >= 1:
                    hT_stageB(0, s - 1)
            hT_stageB(0, NST - 1)
            tasks = []
            for d in range(NCH):
                if d + 1 < NCH:
                    tasks.append((None, None, lambda d=d: pendA.extend((d + 1, s) for s in range(NST))))
                for g0 in range(0, H, HG):
                    tasks.append((c.o_mk + g0 * 128, GW, lambda wb_, wk_, d=d, g0=g0: tok_group(
                        d, c.o_mk + g0 * 128, GW, "k_rope", g0, wb_, wk_)))
                for g0 in range(0, H, HG):
                    tasks.append((c.o_mv + g0 * 128, GW, lambda wb_, wk_, d=d, g0=g0: tok_group(
                        d, c.o_mv + g0 * 128, GW, "v", g0, wb_, wk_)))
                for g0 in range(0, H, HG):
                    tasks.append((c.o_fk + g0 * 128, GW, lambda wb_, wk_, d=d, g0=g0: feat_group(
                        d, c.o_fk + g0 * 128, GW, "kT", H + g0, wb_, wk_)))
                for g0 in range(0, H, HG):
                    tasks.append((c.o_fv + g0 * 128, GW, lambda wb_, wk_, d=d, g0=g0: tok_group(
                        d, c.o_fv + g0 * 128, GW, "v", H + g0, wb_, wk_)))
                tasks.append((None, None, lambda d=d: ff_group(d)))
                tasks.append((None, None, lambda: drip(2 * NST + 2)))
                if d == 0:
                    for g0 in range(0, H, HG):
                        tasks.append((c.o_mq + g0 * 128, GW, lambda wb_, wk_, g0=g0: tok_group(
                            0, c.o_mq + g0 * 128, GW, "q_rope", g0, wb_, wk_)))
                    for g0 in range(0, H, HG):
                        tasks.append((c.o_fq + g0 * 128, GW, lambda wb_, wk_, g0=g0: feat_group(
                            0, c.o_fq + g0 * 128, GW, "qT", H + g0, wb_, wk_)))
                    for c0 in range(0, D, GD):
                        tasks.append((c.o_ga + c0, GD, lambda wb_, wk_, c0=c0: feat_group(
                            0, c.o_ga + c0, GD, "gate", 0, wb_, wk_, kc0=c0 // 128)))
                    for c0 in range(0, D, GD):
                        tasks.append((c.o_gb + c0, GD, lambda wb_, wk_, c0=c0: feat_group(
                            0, c.o_gb + c0, GD, "gate", 1, wb_, wk_, kc0=c0 // 128)))
            PREF = NWB - 1
            wtasks = [i for i, tk_ in enumerate(tasks) if tk_[0] is not None]
            loaded = {}
            nxt = 0
            for i, (col0, width, fn) in enumerate(tasks):
                if col0 is None:
                    fn()
                    continue
                pos = wtasks.index(i)
                while nxt < len(wtasks) and nxt <= pos + PREF:
                    ti = wtasks[nxt]
                    loaded[ti] = load_w(w_in, tasks[ti][0], tasks[ti][1])
                    nxt += 1
                fn(*loaded.pop(i))
            MARK("proj_done", S)
            zf = zff[:].rearrange("p t h -> p (t h)")
            lf = logf[:].rearrange("p t h -> p (t h)")
            S.add("act", lambda e: e.activation(out=lt[:], in_=zf, func=AF.Exp, scale=-1.0), reads=["zff"], writes=["lt"])
            S.add("act", lambda e: e.activation(out=lt[:], in_=lt[:], func=AF.Ln, bias=1.0), reads=["lt"], writes=["lt"])
            S.add("dve", lambda e: e.tensor_scalar(out=lf, in0=lt[:], scalar1=-1.0, scalar2=None, op0=ALU.mult),
                  reads=["lt"], writes=["logf"])
            if debug:
                S.add("sp", lambda e: e.dma_start(out=dbg_logf, in_=lf), reads=["logf"], dma=True)
                S.add("pool", lambda e: e.dma_start(out=dbg_kmT, in_=kmT[:].rearrange("p h n -> p (h n)")),
                      reads=["kmT"], dma=True)
            S.emit("ph1")
        if upto < 2:
            return nc

        with contextlib.ExitStack() as mid:
            R1 = T(mid, "R1", [128, max(H2, KC), CH], BF16)
            R2 = T(mid, "R2", [128, max(KC, 16), CH], BF16)
            oT, h2T, mergedT = R1, R1, R2

            mid2 = contextlib.ExitStack()
            mid2.__enter__()
            Wbm = T(mid2, "Wbm", [128, H, D], BF16)
            Wbf = T(mid2, "Wbf", [128, H, D], BF16)
            with contextlib.ExitStack() as ph:
                S = Sched(nc, ss)
                KTb = [T(ph, "KTb%d" % i, [128, TCTX], BF16) for i in range(2)]
                Vb = [T(ph, "Vb%d" % i, [128, NT, 130], BF16) for i in range(2)]
                QTb = [T(ph, "QTb%d" % i, [128, CH], BF16) for i in range(2)]
                pT = [T(ph, "pT%d" % i, [128, 512], BF16) for i in range(4)]
                NSQ = TCTX // 512 + CH // 512
                sqb1 = [T(ph, "sqb%d" % i, [128, 512], BF16) for i in range(NSQ)]
                sqb = [sqb1, sqb1]
                nmx = T(ph, "nmx", [128, 4], F32)
                negC = T(ph, "negC", [128, H2], F32)
                gbias_rep = T(ph, "gbias_rep", [128, NST * NB], F32)
                vfl_rep = T(ph, "vfl_rep", [128, NCH], F32)
                Fsb = T(ph, "Fsb", [128, NT, H], F32)
                FrefB = T(ph, "FrefB", [128, NG, H], F32)
                biasF = T(ph, "biasF", [128, NG, NT, H], F32)
                bfh = [T(ph, "bfh%d" % i, [128, NG, NT], F32) for i in range(2)]
                gmb = T(ph, "gmb", [128, NST, NB], F32)
                m8 = T(ph, "m8", [128, NST, 8], F32)
                b1 = T(ph, "b1", [128, NST, NB], F32)
                bb = T(ph, "bb", [128, NST, NB], BF16)
                biasT = [T(ph, "biasT%d" % i, [NB, CH], BF16) for i in range(2)]
                otok = [T(ph, "otok%d" % i, [128, 4, 128], BF16) for i in range(2)]
                rc = T(ph, "rc", [128, 8], F32)
                NPS = 3
                psS = [P(ph, "psS%d" % i, [128, 512], F32) for i in range(NPS)]
                oacc_t = [[P(ph, "oacc%d_%d" % (i, k), [128, 512], F32) for k in range(2)] for i in range(2)]
                oacc = [[t[:, 0:260].rearrange("p (r c) -> p r c", c=130) for t in row] for row in oacc_t]
                psX = [P(ph, "psX%d" % i, [128, 512], F32) for i in range(1)]
                psXb = [p[:].bitcast(BF16) for p in psX]
                cn = {"x": 0, "s": 0, "p": 0, "a": 0, "sq": 0, "g": 0}

                def next_x():
                    i = cn["x"] % len(psX)
                    cn["x"] += 1
                    return i, ("psX", i)

                S.add("sp", lambda e: e.dma_start(out=gbias_rep[:], in_=gbias_d[0, :].partition_broadcast(128)),
                      writes=["gbias_rep"], dma=True)
                S.add("sp", lambda e: e.dma_start(out=vfl_rep[:], in_=vflag_d[0, :].partition_broadcast(128)),
                      writes=["vfl_rep"], dma=True)
                for i in range(2):
                    S.add("pool", lambda e, i=i: e.memset(Vb[i][:, :, 128:129], 1.0), writes=[("V", i)])

                def load_head(h, par):
                    S.add("sp", lambda e: e.dma_start(out=KTb[par][:], in_=KTd[h]), writes=[("KT", par)], dma=True)
                    S.add("sp", lambda e: e.dma_start(out=QTb[par][:], in_=QTd[h]), writes=[("QT", par)], dma=True)
                    for t0 in range(0, NT, 8):
                        S.add("sp", lambda e, t0=t0: e.dma_start(
                            out=Vb[par][:, t0:t0 + 8, 0:128],
                            in_=Vd[t0:t0 + 8, :, h * 128:(h + 1) * 128].rearrange("t p c -> p t c")),
                            writes=[("V", par)], dma=True)

                load_head(0, 0)

                xi, xk = next_x()
                psF = psX[xi][:, 0:NT * H].rearrange("p (t h) -> p t h", h=H)
                for d in range(NCH):
                    for j in range(NST):
                        t = d * NST + j
                        terms = [(tri_f, t)] + [(ones_f, d * NST + j2) for j2 in range(j)]
                        if d >= 1:
                            terms += [(nones_f, d2 * NST + j2) for d2 in range(1, d + 1) for j2 in range(NST)]
                        for i, (mm, tt) in enumerate(terms):
                            S.add("pe", lambda e, t=t, mm=mm, tt=tt, i=i, n=len(terms): e.matmul(
                                psF[:, t, :], lhsT=mm[:], rhs=logf[:, tt, :], start=(i == 0), stop=(i == n - 1)),
                                reads=["logf", "tri_f", "ones_f", "nones_f"], writes=[xk])
                S.add("dve", lambda e: e.tensor_copy(out=Fsb[:], in_=psF), reads=[xk], writes=["Fsb"])
                xi2, xk2 = next_x()
                psR = psX[xi2][:, 0:NG * H].rearrange("p (g h) -> p g h", h=H)
                for g in range(NG):
                    n = 4 * g + 2
                    for j2 in range(n):
                        S.add("pe", lambda e, g=g, j2=j2, n=n: e.matmul(
                            psR[:, g, :], lhsT=ones_f[:], rhs=logf[:, j2, :], start=(j2 == 0), stop=(j2 == n - 1)),
                            reads=["logf", "ones_f"], writes=[xk2])
                S.add("dve", lambda e: e.tensor_copy(out=FrefB[:], in_=psR), reads=[xk2], writes=["FrefB"])
                for g in range(NG):
                    S.add("dve", lambda e, g=g: e.tensor_tensor(
                        out=biasF[:, g], in0=FrefB[:, g:g + 1, :].broadcast_to([128, NT, H]), in1=Fsb[:],
                        op=ALU.subtract), reads=["FrefB", "Fsb"], writes=["biasF"])
                    S.add("dve", lambda e, g=g: e.tensor_tensor(
                        out=biasF[:, g].rearrange("p (d j) h -> p d (j h)", d=NCH),
                        in0=biasF[:, g].rearrange("p (d j) h -> p d (j h)", d=NCH),
                        in1=vfl_rep[:].unsqueeze(2).broadcast_to([128, NCH, NST * H]), op=ALU.add),
                        reads=["biasF", "vfl_rep"], writes=["biasF"])

                def head_sq(par, h):
                    tiles = [(KTb[par], ("KT", par), n0) for n0 in range(0, TCTX, 512)]
                    tiles += [(QTb[par], ("QT", par), n0) for n0 in range(0, CH, 512)]
                    for i, (buf, bkey, n0) in enumerate(tiles):
                        S.add("pool", lambda e, buf=buf, n0=n0, i=i: e.tensor_tensor(
                            out=sqb[par][i][:], in0=buf[:, n0:n0 + 512], in1=buf[:, n0:n0 + 512], op=ALU.mult),
                            reads=[bkey], writes=[("sqb", i)])

                def head_norm(par, h):
                    xi, xk = next_x()
                    ncol = NSQ * 4
                    for i in range(NSQ):
                        for cq in range(4):
                            S.add("pe", lambda e, i=i, cq=cq: e.matmul(
                                psX[xi][:, i * 4 + cq:i * 4 + cq + 1], lhsT=sqb[par][i][:, cq * 128:(cq + 1) * 128],
                                rhs=ones_b[:, 0:1], start=True, stop=True),
                                reads=[("sqb", i), "ones_b"], writes=[xk])
                    S.add("dve", lambda e: e.tensor_reduce(out=nmx[:, 0:1], in_=psX[xi][:, 0:ncol], axis=AX.X, op=ALU.max),
                          reads=[xk], writes=["nmx"])
                    xi2, xk2 = next_x()
                    S.add("pe", lambda e: e.transpose(out=psX[xi2][0:1, 0:128], in_=nmx[:, 0:1], identity=ident_f[:]),
                          reads=["nmx", "ident_f"], writes=[xk2])
                    S.add("dve", lambda e: e.tensor_reduce(out=nmx[0:1, 1:2], in_=psX[xi2][0:1, 0:128], axis=AX.X,
                                                           op=ALU.max), reads=[xk2], writes=["nmx1"])
                    xi3, xk3 = next_x()
                    S.add("pe", lambda e: e.matmul(psX[xi3][:, 0:1], lhsT=ones_f[0:1, :], rhs=nmx[0:1, 1:2],
                                                   start=True, stop=True), reads=["nmx1", "ones_f"], writes=[xk3])
                    S.add("dve", lambda e: e.tensor_scalar(out=negC[:, h:h + 1], in0=psX[xi3][:, 0:1],
                                                           scalar1=-1.02 * scale, scalar2=None, op0=ALU.mult),
                          reads=[xk3], writes=[("negC", h)])

                def moba_gate(par, hm):
                    xi, xk = next_x()
                    psg = psX[xi][:, 0:NST * NB].rearrange("p (q n) -> p q n", n=NB)
                    for qt in range(NST):
                        S.add("pe", lambda e, qt=qt: e.matmul(
                            psg[:, qt, :], lhsT=QTb[par][:, qt * 128:(qt + 1) * 128], rhs=kmT[:, hm, :],
                            start=True, stop=True), reads=[("QT", par), "kmT"], writes=[xk])
                    g3 = gbias_rep[:].rearrange("p (q n) -> p q n", n=NB)
                    S.add("dve", lambda e: e.tensor_tensor(out=gmb[:], in0=psg, in1=g3, op=ALU.add),
                          reads=[xk, "gbias_rep"], writes=["gmb"])
                    for qt in range(NST):
                        S.add("dve", lambda e, qt=qt: e.max(out=m8[:, qt, :], in_=gmb[:, qt, :]),
                              reads=["gmb"], writes=["m8"])
                    for qt in range(NST):
                        S.add("dve", lambda e, qt=qt: e.tensor_scalar(
                            out=b1[:, qt, :], in0=gmb[:, qt, :], scalar1=m8[:, qt, c.TOPK - 1:c.TOPK],
                            scalar2=-NEG_MASK, op0=ALU.is_ge, op1=ALU.mult), reads=["gmb", "m8"], writes=["b1"])
                    S.add("dve", lambda e: e.scalar_tensor_tensor(
                        out=bb[:].rearrange("p q n -> p (q n)"), in0=b1[:].rearrange("p q n -> p (q n)"),
                        scalar=NEG_MASK, in1=gbias_rep[:], op0=ALU.add, op1=ALU.min),
                        reads=["b1", "gbias_rep"], writes=["bb"])
                    for qt in range(NST):
                        S.add("dve", lambda e, qt=qt: e.memset(bb[:, qt, qt // 2:qt // 2 + 1], 0.0),
                              reads=["bb"], writes=["bb"])
                    xi2, xk2 = next_x()
                    for qt in range(NST):
                        S.add("pe", lambda e, qt=qt: e.transpose(out=psXb[xi2][0:NB, qt * 128:(qt + 1) * 128],
                                                                 in_=bb[:, qt, :], identity=ident_b[:]),
                              reads=["bb", "ident_b"], writes=[xk2])
                    S.add("dve", lambda e: e.tensor_copy(out=biasT[par][0:NB, 0:NST * 128],
                                                         in_=psXb[xi2][0:NB, 0:NST * 128]),
                          reads=[xk2], writes=[("biasT", par)])

                deferred = []

                def flush_deferred():
                    while deferred:
                        deferred.pop(0)()

                def attention(par, h, moba, mid_hook=None):
                    for g in range(NG):
                        a = cn["a"] % 2
                        cn["a"] += 1
                        tiles = [(d, j) for d in range(NCH - 1, 0, -1) for j in range(NST)]
                        tiles += [(0, j) for j in range(4 * g + 4)]

                        def qk_step(idx, d, j, g=g, a=a):
                            t = d * NST + j
                            c0 = 0 if (d > 0 or j < 4 * g) else (j - 4 * g) * 128
                            N = 512 - c0
                            q0 = g * 512 + c0
                            si = cn["s"] % NPS
                            cn["s"] += 1
                            ps, pk = psS[si], ("psS", si)
                            S.add("pe", lambda e: e.matmul(
                                ps[:, 0:N], lhsT=KTb[par][:, t * 128:(t + 1) * 128], rhs=QTb[par][:, q0:q0 + N],
                                start=True, stop=not moba), reads=[("KT", par), ("QT", par)], writes=[pk])
                            if moba:
                                n = t // 2
                                S.add("pe", lambda e: e.matmul(
                                    ps[:, 0:N], lhsT=E_b[0:NB, n * 128:(n + 1) * 128], rhs=biasT[par][0:NB, q0:q0 + N],
                                    start=False, stop=True), reads=["E_b", ("biasT", par)], writes=[pk])
                            pi = cn["p"] % 4
                            cn["p"] += 1
                            pt, ptk = pT[pi], ("pT", pi)
                            if moba:
                                bias_ap, bkey = negC[:, h:h + 1], ("negC", h)
                            else:
                                bias_ap, bkey = bfh[par][:, g, t:t + 1], ("bfh", par)
                            S.add("act", lambda e: e.activation(
                                out=pt[:, 0:N], in_=ps[:, 0:N], func=AF.Exp, bias=bias_ap, scale=scale),
                                reads=[pk, bkey], writes=[ptk])
                            if d == 0 and j >= 4 * g:
                                S.add("pool", lambda e: e.tensor_tensor(out=pt[:, 0:128], in0=pt[:, 0:128],
                                                                        in1=tri_b[:], op=ALU.mult),
                                      reads=[ptk, "tri_b"], writes=[ptk])
                            return (idx, d, j, t, c0, pt, ptk)

                        def pv_step(st, g=g, a=a):
                            idx, d, j, t, c0, pt, ptk = st
                            for qs in range(c0 // 128, 4):
                                lo = qs * 128 - c0
                                S.add("pe", lambda e, qs=qs, lo=lo, first=(idx == 0 and qs % 2 == 0),
                                      last=(d == 0 and j == 4 * g + qs): e.matmul(
                                    oacc[a][qs // 2][:, qs % 2, 0:129], lhsT=pt[:, lo:lo + 128],
                                    rhs=Vb[par][:, t, 0:129], start=first, stop=last, skip_group_check=True),
                                    reads=[ptk, ("V", par)], writes=[("oacc", a, qs // 2)])

                        pend = []
                        for idx, (d, j) in enumerate(tiles):
                            pend.append(qk_step(idx, d, j))
                            if len(pend) > 2:
                                pv_step(pend.pop(0))
                            if idx == 3:
                                flush_deferred()
                        while pend:
                            pv_step(pend.pop(0))
                        ot = otok[a]
                        for qs in range(4):
                            oa = oacc[a][qs // 2]
                            S.add("dve", lambda e, oa=oa, qs=qs, a=a: e.reciprocal(out=rc[:, a * 4 + qs:a * 4 + qs + 1],
                                                                                 in_=oa[:, qs % 2, 128:129]),
                                  reads=[("oacc", a, qs // 2)], writes=[("rc", a, qs)])
                            S.add("dve", lambda e, oa=oa, qs=qs, ot=ot, a=a: e.tensor_scalar(
                                out=ot[:, qs, :], in0=oa[:, qs % 2, 0:128], scalar1=rc[:, a * 4 + qs:a * 4 + qs + 1],
                                scalar2=None, op0=ALU.mult),
                                reads=[("oacc", a, qs // 2), ("rc", a, qs)], writes=[("otok", a)])

                        def tail(g=g, a=a, ot=ot):
                            xi, xk = next_x()
                            for qs in range(4):
                                S.add("pe", lambda e, xi=xi, qs=qs: e.transpose(
                                    out=psXb[xi][:, qs * 128:(qs + 1) * 128], in_=ot[:, qs, :], identity=ident_b[:]),
                                    reads=[("otok", a), "ident_b"], writes=[xk])
                            S.add("act", lambda e, xi=xi: e.copy(out=oT[:, h, g * 512:(g + 1) * 512],
                                                                 in_=psXb[xi][:, 0:512]),
                                  reads=[xk], writes=["oT"])
                        deferred.append(tail)
                        if g == 0 and mid_hook is not None:
                            mid_hook()
                    if NG == 1 and mid_hook is not None:
                        pass

                def prep(h):
                    par = h % 2
                    head_norm(par, h)
                    if h < H:
                        moba_gate(par, h)
                    else:
                        S.add("dve", lambda e: e.tensor_scalar(
                            out=bfh[par][:], in0=biasF[:, :, :, h - H], scalar1=negC[:, h:h + 1], scalar2=None,
                            op0=ALU.add), reads=["biasF", ("negC", h)], writes=[("bfh", par)])

                head_sq(0, 0)
                prep(0)
                for h in range(H2):
                    par = h % 2

                    def hook(h=h):
                        if h + 1 < H2:
                            prep(h + 1)
                        if h == H2 - 3:
                            for h0 in range(0, H, 4):
                                h1 = min(H, h0 + 4)
                                for (wsb, wdr, nm) in ((Wbm, w_bm, "Wbm"), (Wbf, w_bf, "Wbf")):
                                    S.add("pool", lambda e, wsb=wsb, wdr=wdr, h0=h0, h1=h1: e.dma_start(
                                        out=wsb[:, h0:h1, :],
                                        in_=wdr[h0 * 128:h1 * 128, :].rearrange("(h p) c -> p h c", p=128)),
                                        writes=[nm], dma=True)
                    if h + 1 < H2:
                        load_head(h + 1, (h + 1) % 2)
                        head_sq((h + 1) % 2, h + 1)
                    attention(par, h, h < H, mid_hook=hook)
                flush_deferred()
                if debug:
                    S.add("pool", lambda e: e.dma_start(out=dbg_oT, in_=oT[:, 0:H2, :].rearrange("p h t -> p (h t)")),
                          reads=["oT"], dma=True)
                S.emit("ph2")
            if upto < 3:
                return nc

            with contextlib.ExitStack() as ph:
                S = Sched(nc, ss)
                sga = [T(ph, "sga%d" % i, [128, CH], BF16) for i in range(2)]
                sgb = [T(ph, "sgb%d" % i, [128, CH], BF16) for i in range(2)]
                t1 = [T(ph, "t1_%d" % i, [128, 512], F32) for i in range(2)]
                t2 = [T(ph, "t2_%d" % i, [128, 512], F32) for i in range(2)]
                psm = [[P(ph, "psm%d_%d" % (i, n), [128, 512], F32) for n in range(NN)] for i in range(2)]
                psf = [[P(ph, "psf%d_%d" % (i, n), [128, 512], F32) for n in range(NN)] for i in range(2)]
                tc_ = 0
                for cc in range(KC):
                    a = cc % 2
                    S.add("sp", lambda e, a=a, cc=cc: e.dma_start(out=sga[a][:], in_=sgd[0, cc]), writes=[("sga", a)], dma=True)
                    S.add("sp", lambda e, a=a, cc=cc: e.dma_start(out=sgb[a][:], in_=sgd[1, cc]), writes=[("sgb", a)], dma=True)
                    for n in range(NN):
                        for hh in range(H):
                            S.add("pe", lambda e, a=a, n=n, hh=hh, cc=cc: e.matmul(
                                psm[a][n][:, 0:512], lhsT=Wbm[:, hh, cc * 128:(cc + 1) * 128],
                                rhs=oT[:, hh, n * 512:(n + 1) * 512], start=(hh == 0), stop=(hh == H - 1)),
                                reads=["Wbm", "oT"], writes=[("psm", a, n)])
                        for hh in range(H):
                            S.add("pe", lambda e, a=a, n=n, hh=hh, cc=cc: e.matmul(
                                psf[a][n][:, 0:512], lhsT=Wbf[:, hh, cc * 128:(cc + 1) * 128],
                                rhs=oT[:, H + hh, n * 512:(n + 1) * 512], start=(hh == 0), stop=(hh == H - 1)),
                                reads=["Wbf", "oT"], writes=[("psf", a, n)])
                        k = tc_ % 2
                        tc_ += 1
                        S.add("dve", lambda e, a=a, n=n, k=k: e.tensor_tensor(
                            out=t1[k][:], in0=psm[a][n][:, 0:512], in1=sga[a][:, n * 512:(n + 1) * 512], op=ALU.mult),
                            reads=[("psm", a, n), ("sga", a)], writes=[("t1", k)])
                        S.add("dve", lambda e, a=a, n=n, k=k: e.tensor_tensor(
                            out=t2[k][:], in0=psf[a][n][:, 0:512], in1=sgb[a][:, n * 512:(n + 1) * 512], op=ALU.mult),
                            reads=[("psf", a, n), ("sgb", a)], writes=[("t2", k)])
                        S.add("pool", lambda e, cc=cc, n=n, k=k: e.tensor_tensor(
                            out=mergedT[:, cc, n * 512:(n + 1) * 512], in0=t1[k][:], in1=t2[k][:], op=ALU.add),
                            reads=[("t1", k), ("t2", k)], writes=["mergedT"])
                if debug:
                    S.add("pool", lambda e: e.dma_start(out=dbg_mg, in_=mergedT[:, 0:KC, :].rearrange("p h t -> p (h t)")),
                          reads=["mergedT"], dma=True)
                S.emit("ph3a")
            mid2.close()

            with contextlib.ExitStack() as ph:
                S = Sched(nc, ss)
                Wo = T(ph, "Wo", [128, KC, D], BF16)
                gpost = T(ph, "gpost", [128, D], F32)
                gpre2 = T(ph, "gpre2", [128, D], F32)
                xs2 = [T(ph, "xs2_%d" % i, [128, D], F32) for i in range(2)]
                tt_ = T(ph, "tt_", [128, D], F32)
                hb2 = [T(ph, "hb2_%d" % i, [128, D], BF16) for i in range(2)]
                st3 = T(ph, "st3", [128, NST, 16], F32)
                ncg = D // GD
                psM = [P(ph, "psM%d" % i, [128, 512], F32) for i in range(6)]
                pst2 = [P(ph, "pst2_%d" % i, [128, 8, 128], BF16) for i in range(2)]
                S.add("sp", lambda e: e.dma_start(out=gpost[:], in_=gvec[1, :].partition_broadcast(128)),
                      writes=["gpost"], dma=True)
                S.add("sp", lambda e: e.dma_start(out=gpre2[:], in_=gvec[2, :].partition_broadcast(128)),
                      writes=["gpre2"], dma=True)
                for c0 in range(0, D, GD):
                    for k0 in range(0, KC, 8):
                        k1 = min(KC, k0 + 8)
                        S.add("pool", lambda e, c0=c0, k0=k0, k1=k1: e.dma_start(
                            out=Wo[:, k0:k1, c0:c0 + GD],
                            in_=w_out[k0 * 128:k1 * 128, c0:c0 + GD].rearrange("(kc p) c -> p kc c", p=128)),
                            writes=[("Wo", c0)], dma=True)
                tkc = [0]

                def h2_tail(s):
                    xi = s % 2
                    nq = min(8, KC)
                    for q0 in range(0, KC, nq):
                        pi2 = tkc[0] % 2
                        tkc[0] += 1
                        for kk in range(nq):
                            S.add("pe", lambda e, pi2=pi2, kk=kk, q0=q0: e.transpose(
                                out=pst2[pi2][:, kk, :], in_=hb2[xi][:, (q0 + kk) * 128:(q0 + kk + 1) * 128],
                                identity=ident_b[:]), reads=[("hb2", xi), "ident_b"], writes=[("pst2", pi2)])
                        S.add("act", lambda e, pi2=pi2, q0=q0: e.copy(
                            out=h2T[:, q0:q0 + nq, s * 128:(s + 1) * 128], in_=pst2[pi2][:, 0:nq, :]),
                            reads=[("pst2", pi2)], writes=["h2T"])

                mc = 0
                tk = 0
                for s in range(NST):
                    xi = s % 2
                    S.add("pool", lambda e, s=s, xi=xi: e.dma_start(out=xs2[xi][:], in_=xctx[s * 128:(s + 1) * 128, :]),
                          writes=[("xs2", xi)], dma=True)
                    pss = []
                    for cg in range(ncg):
                        pi = mc % 6
                        mc += 1
                        pss.append(pi)
                        for kc in range(KC):
                            S.add("pe", lambda e, pi=pi, kc=kc, s=s, cg=cg: e.matmul(
                                psM[pi][:, 0:GD], lhsT=mergedT[:, kc, s * 128:(s + 1) * 128],
                                rhs=Wo[:, kc, cg * GD:(cg + 1) * GD], start=(kc == 0), stop=(kc == KC - 1)),
                                reads=["mergedT", ("Wo", cg * GD)], writes=[("psM", pi)])
                        S.add("act", lambda e, pi=pi, s=s, cg=cg: e.activation(
                            out=junk[:, 0:GD], in_=psM[pi][:, 0:GD], func=AF.Square, accum_out=st3[:, s, cg:cg + 1]),
                            reads=[("psM", pi)], writes=["junk", ("st3", s)])
                    S.add("dve", lambda e, s=s: e.tensor_reduce(out=st3[:, s, 8:9], in_=st3[:, s, 0:ncg], axis=AX.X,
                                                                op=ALU.add), reads=[("st3", s)], writes=[("st3", s)])
                    S.add("act", lambda e, s=s: e.activation(out=st3[:, s, 9:10], in_=st3[:, s, 8:9], func=AF.Sqrt,
                                                             bias=epsc[:], scale=1.0 / D),
                          reads=[("st3", s), "epsc"], writes=[("st3", s)])
                    S.add("dve", lambda e, s=s: e.reciprocal(out=st3[:, s, 10:11], in_=st3[:, s, 9:10]),
                          reads=[("st3", s)], writes=[("st3", s)])
                    for cg in range(ncg):
                        pi = pss[cg]
                        S.add("dve", lambda e, pi=pi, s=s, cg=cg: e.scalar_tensor_tensor(
                            out=tt_[:, cg * GD:(cg + 1) * GD], in0=psM[pi][:, 0:GD], scalar=st3[:, s, 10:11],
                            in1=gpost[:, cg * GD:(cg + 1) * GD], op0=ALU.mult, op1=ALU.mult),
                            reads=[("psM", pi), ("st3", s), "gpost"], writes=["tt_"])
                    S.add("dve", lambda e, xi=xi: e.tensor_tensor(out=xs2[xi][:], in0=tt_[:], in1=xs2[xi][:], op=ALU.add),
                          reads=["tt_", ("xs2", xi)], writes=[("xs2", xi)])
                    S.add("sp", lambda e, s=s, xi=xi: e.dma_start(out=x1d[s * 128:(s + 1) * 128, :], in_=xs2[xi][:]),
                          reads=[("xs2", xi)], dma=True)
                    S.add("dve", lambda e, s=s, xi=xi: e.scalar_tensor_tensor(
                        out=hb2[xi][:], in0=xs2[xi][:], scalar=1.0, in1=xs2[xi][:], op0=ALU.mult, op1=ALU.mult,
                        accum_out=st3[:, s, 11:12]), reads=[("xs2", xi)], writes=[("hb2", xi), ("st3", s)])
                    S.add("act", lambda e, s=s: e.activation(out=st3[:, s, 12:13], in_=st3[:, s, 11:12], func=AF.Sqrt,
                                                             bias=epsc[:], scale=1.0 / D),
                          reads=[("st3", s), "epsc"], writes=[("st3", s)])
                    S.add("dve", lambda e, s=s: e.reciprocal(out=st3[:, s, 13:14], in_=st3[:, s, 12:13]),
                          reads=[("st3", s)], writes=[("st3", s)])
                    S.add("dve", lambda e, s=s, xi=xi: e.scalar_tensor_tensor(
                        out=hb2[xi][:], in0=xs2[xi][:], scalar=st3[:, s, 13:14], in1=gpre2[:], op0=ALU.mult,
                        op1=ALU.mult), reads=[("xs2", xi), ("st3", s), "gpre2"], writes=[("hb2", xi)])
                    if s >= 1:
                        h2_tail(s - 1)
                h2_tail(NST - 1)
                S.emit("ph3b")
            if upto < 4:
                return nc

            with contextlib.ExitStack() as ph:
                S = Sched(nc, ss)
                FB, UW = c.FB, c.UW
                FK = FB // 128
                Wu = [T(ph, "Wu%d" % i, [128, KC, UW], BF16) for i in range(2)]
                Wd = [T(ph, "Wd%d" % i, [128, FK, GD], BF16) for i in range(2)]
                m = T(ph, "m", [128, NST, D], F32)
                rl = [T(ph, "rl%d" % i, [128, 512], F32) for i in range(2)]
                gpost2 = T(ph, "gpost2", [128, D], F32)
                xs3 = [T(ph, "xs3_%d" % i, [128, D], F32) for i in range(2)]
                st4 = T(ph, "st4", [128, NST, 4], F32)
                psU = [P(ph, "psU%d" % i, [128, 512], F32) for i in range(3)]
                psD = [P(ph, "psD%d" % i, [128, 512], F32) for i in range(3)]
                aTv = lambda par, k: R2[:, par * FK + k, :]
                ncg = D // GD
                S.add("sp", lambda e: e.dma_start(out=gpost2[:], in_=gvec[3, :].partition_broadcast(128)),
                      writes=["gpost2"], dma=True)
                cn4 = {"uc": 0, "dc": 0, "wu": 0, "wd": 0, "rk": 0}

                def load_u(fb, u0):
                    col0 = fb * FB + u0
                    wi = cn4["wu"] % 2
                    cn4["wu"] += 1
                    for k0 in range(0, KC, 8):
                        k1 = min(KC, k0 + 8)
                        S.add("pool", lambda e, k0=k0, k1=k1: e.dma_start(
                            out=Wu[wi][:, k0:k1, :],
                            in_=w_up[k0 * 128:k1 * 128, col0:col0 + UW].rearrange("(kc p) c -> p kc c", p=128)),
                            writes=[("Wu", wi)], dma=True)
                    return wi

                def comp_u(fb, u0, wi):
                    par = fb % 2
                    for f in range(UW // 128):
                        fcl = (u0 + f * 128) // 128
                        for n in range(NN):
                            pi = cn4["uc"] % 3
                            cn4["uc"] += 1
                            for kc in range(KC):
                                S.add("pe", lambda e, pi=pi, kc=kc, f=f, n=n: e.matmul(
                                    psU[pi][:, 0:512], lhsT=Wu[wi][:, kc, f * 128:(f + 1) * 128],
                                    rhs=h2T[:, kc, n * 512:(n + 1) * 512], start=(kc == 0), stop=(kc == KC - 1)),
                                    reads=[("Wu", wi), "h2T"], writes=[("psU", pi)])
                            k = cn4["rk"] % 2
                            cn4["rk"] += 1
                            S.add("act", lambda e, pi=pi, k=k: e.activation(out=rl[k][:], in_=psU[pi][:, 0:512],
                                                                            func=AF.Relu),
                                  reads=[("psU", pi)], writes=[("rl", k)])
                            S.add("pool", lambda e, k=k, fcl=fcl, n=n: e.tensor_tensor(
                                out=aTv(par, fcl)[:, n * 512:(n + 1) * 512], in0=rl[k][:], in1=rl[k][:], op=ALU.mult),
                                reads=[("rl", k)], writes=[("aT", par)])

                def load_d(fb, cg):
                    wi = cn4["wd"] % 2
                    cn4["wd"] += 1
                    S.add("pool", lambda e: e.dma_start(
                        out=Wd[wi][:],
                        in_=w_down[fb * FB:(fb + 1) * FB, cg * GD:(cg + 1) * GD].rearrange("(kc p) c -> p kc c", p=128)),
                        writes=[("Wd", wi)], dma=True)
                    return wi

                def comp_d(fb, cg, wi):
                    par = fb % 2
                    for s in range(NST):
                        pi = cn4["dc"] % 3
                        cn4["dc"] += 1
                        for kc in range(FK):
                            S.add("pe", lambda e, pi=pi, kc=kc, s=s: e.matmul(
                                psD[pi][:, 0:GD], lhsT=aTv(par, kc)[:, s * 128:(s + 1) * 128], rhs=Wd[wi][:, kc, :],
                                start=(kc == 0), stop=(kc == FK - 1)),
                                reads=[("aT", par), ("Wd", wi)], writes=[("psD", pi)])
                        msl = m[:, s, cg * GD:(cg + 1) * GD]
                        if fb == 0:
                            S.add("act", lambda e, pi=pi, msl=msl: e.copy(out=msl, in_=psD[pi][:, 0:GD]),
                                  reads=[("psD", pi)], writes=[("m", s, cg)])
                        else:
                            S.add("dve", lambda e, pi=pi, msl=msl: e.tensor_tensor(out=msl, in0=psD[pi][:, 0:GD],
                                                                                 in1=msl, op=ALU.add),
                                  reads=[("psD", pi), ("m", s, cg)], writes=[("m", s, cg)])

                tasks4 = []
                for fb in range(c.NFB):
                    for u0 in range(0, FB, UW):
                        tasks4.append((load_u, comp_u, fb, u0))
                    for cg in range(ncg):
                        tasks4.append((load_d, comp_d, fb, cg))
                nxt_w = tasks4[0][0](tasks4[0][2], tasks4[0][3])
                for i, (lf_, cf_, a1, a2) in enumerate(tasks4):
                    cur_w = nxt_w
                    if i + 1 < len(tasks4):
                        t2 = tasks4[i + 1]
                        nxt_w = t2[0](t2[2], t2[3])
                    cf_(a1, a2, cur_w)
                for s in range(NST):
                    xi = s % 2
                    mk = [("m", s, cg) for cg in range(ncg)]
                    S.add("pool", lambda e, s=s, xi=xi: e.dma_start(out=xs3[xi][:], in_=x1d[s * 128:(s + 1) * 128, :]),
                          writes=[("xs3", xi)], dma=True)
                    S.add("dve", lambda e, s=s: e.scalar_tensor_tensor(
                        out=junk[:], in0=m[:, s, :], scalar=1.0, in1=m[:, s, :], op0=ALU.mult, op1=ALU.mult,
                        accum_out=st4[:, s, 0:1]), reads=mk, writes=["junk", ("st4", s)])
                    S.add("act", lambda e, s=s: e.activation(out=st4[:, s, 1:2], in_=st4[:, s, 0:1], func=AF.Sqrt,
                                                             bias=epsc[:], scale=1.0 / D),
                          reads=[("st4", s), "epsc"], writes=[("st4", s)])
                    S.add("dve", lambda e, s=s: e.reciprocal(out=st4[:, s, 2:3], in_=st4[:, s, 1:2]),
                          reads=[("st4", s)], writes=[("st4", s)])
                    S.add("dve", lambda e, s=s: e.scalar_tensor_tensor(
                        out=m[:, s, :], in0=m[:, s, :], scalar=st4[:, s, 2:3], in1=gpost2[:], op0=ALU.mult,
                        op1=ALU.mult), reads=mk + [("st4", s), "gpost2"], writes=mk)
                    S.add("dve", lambda e, s=s, xi=xi: e.tensor_tensor(out=xs3[xi][:], in0=m[:, s, :], in1=xs3[xi][:],
                                                                     op=ALU.add),
                          reads=mk + [("xs3", xi)], writes=[("xs3", xi)])
                    S.add("sp", lambda e, s=s, xi=xi: e.dma_start(out=out[s * 128:(s + 1) * 128, :], in_=xs3[xi][:]),
                          reads=[("xs3", xi)], dma=True)
                S.emit("ph4")
    return nc


def make_core_inputs(inp, core, cfg=FULL):
    c = cfg
    b, j = divmod(core, c.NCH)
    x = np.asarray(inp["x"])[b]
    CH, NCH = c.CH, c.NCH
    order = [(j - d) % NCH for d in range(NCH)]
    xctx = np.ascontiguousarray(np.concatenate([x[o * CH:(o + 1) * CH] for o in order], 0), dtype=np.float32)
    pos = np.concatenate([np.arange(o * CH, (o + 1) * CH) for o in order]).astype(np.float32)
    inv_freq = (np.float32(ROPE_THETA) ** (-np.arange(0, 32, 2, dtype=np.float32) / np.float32(32))).astype(np.float32)
    ang = (pos[:, None] * inv_freq[None, :]).astype(np.float32)
    valid = [j - d >= 0 for d in range(NCH)]
    vflag = np.array([[0.0 if v else NEG_MASK for v in valid]], np.float32)
    bpc = CH // 256
    gb = np.full((c.NST, c.NB), -1e30, np.float32)
    for qt in range(c.NST):
        bq = qt // 2
        for n in range(c.NB):
            d, i = divmod(n, bpc)
            if (d == 0 and i < bq) or (d >= 1 and valid[d]):
                gb[qt, n] = 0.0
    k = np.arange(128)
    cE = np.zeros((c.NB, c.NB * 128), np.float32)
    for n in range(c.NB):
        cE[n, n * 128:(n + 1) * 128] = 1.0
    sq = lambda a: np.ascontiguousarray(np.asarray(a)[0], dtype=np.float32)
    return {
        "xctx": xctx,
        "w_in": sq(inp["w_in"]), "w_bm": sq(inp["w_branch_moba"]), "w_bf": sq(inp["w_branch_fox"]),
        "w_out": sq(inp["w_out"]), "w_up": sq(inp["w_up"]), "w_down": sq(inp["w_down"]),
        "gvec": np.stack([sq(inp["g_mix_pre"]), sq(inp["g_mix_post"]), sq(inp["g_mlp_pre"]), sq(inp["g_mlp_post"])], 0),
        "bfg": np.asarray(inp["b_forget"], np.float32).reshape(1, c.H),
        "rope_cos": np.cos(ang).astype(np.float32), "rope_sin": np.sin(ang).astype(np.float32),
        "gbias": gb.reshape(1, -1), "vflag": vflag,
        "cident": np.eye(128, dtype=np.float32),
        "ctri": (k[:, None] <= k[None, :]).astype(np.float32),
        "cE": cE,
    }


_NC_CACHE = {}


def kernel(**inputs):
    cfg = FULL
    if "nc" not in _NC_CACHE:
        _NC_CACHE["nc"] = build(cfg)
    nc = _NC_CACHE["nc"]
    n_cores = 2 * cfg.NCH
    shared = None
    in_maps = []
    for core in range(n_cores):
        m = make_core_inputs(inputs, core, cfg)
        if shared is None:
            shared = m
        else:
            for key in m:
                if key not in ("xctx", "rope_cos", "rope_sin", "gbias", "vflag"):
                    m[key] = shared[key]
        in_maps.append(m)
    res = run_bass_kernel_spmd(nc, in_maps, core_ids=list(range(n_cores)))
    B = n_cores // cfg.NCH
    outp = np.empty((B, cfg.TCTX, cfg.D), np.float32)
    for core in range(n_cores):
        b, j = divmod(core, cfg.NCH)
        outp[b, j * cfg.CH:(j + 1) * cfg.CH] = np.asarray(res.results[core]["out"], np.float32)
    return outp
```

```python
import contextlib
import numpy as np
import concourse.bass as bass
import concourse.mybir as mybir
from concourse.bass_utils import run_bass_kernel_spmd

F32 = mybir.dt.float32
BF16 = mybir.dt.bfloat16
AF = mybir.ActivationFunctionType
ALU = mybir.AluOpType
AX = mybir.AxisListType

ENGS = ("pe", "act", "dve", "pool", "sp")
N_DMA_SEMS = 24
N_HW_SEMS = 16
OP_LIMIT = None
RMS_EPS = 1e-6
ROPE_THETA = 500000.0
NEG_MASK = -30000.0


class _Op:
    __slots__ = ("eng", "fn", "is_dma", "signal", "sigval", "dsem", "dval", "deps", "waits")


class SemState:
    def __init__(self, nc, stack):
        self.sems = {}
        for e in ("pe", "act", "dve", "pool"):
            self.sems[("eng", e)] = stack.enter_context(nc.semaphore("s_" + e))
        for i in range(N_DMA_SEMS):
            self.sems[("dma", i)] = stack.enter_context(nc.semaphore("s_dma%d" % i))
        self.val = {k: 0 for k in self.sems}
        self.dma_count = {"hw": 0, "sw": 0}


def _is_psum(k):
    n = k[0] if isinstance(k, tuple) else k
    return isinstance(n, str) and (n.startswith("ps") or n == "oacc")


class Sched:
    def __init__(self, nc, ss):
        self.nc = nc
        self.ss = ss
        self.ops = []
        self.streams = {e: [] for e in ENGS}
        self.last_w = {}
        self.readers = {}
        self.dma_last = [None] * N_DMA_SEMS
        self.dma_val = [ss.val[("dma", i)] for i in range(N_DMA_SEMS)]

    def add(self, eng, fn, reads=(), writes=(), dma=False):
        if OP_LIMIT is not None and len(self.ops) >= OP_LIMIT:
            return None
        pr = [k for k in reads if _is_psum(k)]
        if pr:
            reads = [k for k in reads if not _is_psum(k)]
            writes = list(writes) + [k for k in pr if k not in writes]
        op = _Op()
        op.eng, op.fn, op.is_dma = eng, fn, dma
        op.signal, op.sigval, op.dsem, op.dval, op.waits = False, None, None, None, None
        deps = []
        for k in reads:
            w = self.last_w.get(k)
            if w is not None:
                deps.append(w)
        for k in writes:
            w = self.last_w.get(k)
            if w is not None:
                deps.append(w)
            rd = self.readers.get(k)
            if rd:
                deps.extend(rd.values())
        if dma:
            if eng == "pool":
                s = N_HW_SEMS + self.ss.dma_count["sw"] % (N_DMA_SEMS - N_HW_SEMS)
                self.ss.dma_count["sw"] += 1
            else:
                s = self.ss.dma_count["hw"] % N_HW_SEMS
                self.ss.dma_count["hw"] += 1
            if self.dma_last[s] is not None:
                deps.append(self.dma_last[s])
            self.dma_val[s] += 16
            op.dsem, op.dval = s, self.dma_val[s]
            self.dma_last[s] = op
        op.deps = deps
        gid = len(self.ops)
        for k in reads:
            self.readers.setdefault(k, {})[("dma", gid) if dma else eng] = op
        for k in writes:
            self.last_w[k] = op
            self.readers[k] = {}
        self.streams[eng].append(op)
        self.ops.append(op)
        return op

    @staticmethod
    def _skip(op, d):
        return (not d.is_dma) and d.eng == op.eng and op.eng == "pe" and not op.is_dma

    def emit(self, name):
        nc, ss = self.nc, self.ss
        for op in self.ops:
            for d in op.deps:
                if not d.is_dma and not self._skip(op, d):
                    d.signal = True
        for e in ("pe", "act", "dve", "pool"):
            c = ss.val[("eng", e)]
            for op in self.streams[e]:
                if op.signal and not op.is_dma:
                    c += 1
                    op.sigval = c
            ss.val[("eng", e)] = c
        for i in range(N_DMA_SEMS):
            ss.val[("dma", i)] = self.dma_val[i]
        for e in ENGS:
            known = {}
            for op in self.streams[e]:
                need = {}
                for d in op.deps:
                    if d.is_dma:
                        key, val = ("dma", d.dsem), d.dval
                    elif self._skip(op, d):
                        continue
                    else:
                        key, val = ("eng", d.eng), d.sigval
                    if known.get(key, 0) >= val:
                        continue
                    if need.get(key, 0) < val:
                        need[key] = val
                known.update(need)
                op.waits = need
        sems = ss.sems
        with nc.Block() as block:
            def run(e, eng):
                for op in self.streams[e]:
                    for k, v in op.waits.items():
                        eng.wait_ge(sems[k], v)
                    ins = op.fn(eng)
                    if op.is_dma:
                        ins.then_inc(sems[("dma", op.dsem)], 16)
                    elif op.signal:
                        ins.then_inc(sems[("eng", e)], 1)

            @block.tensor
            def _(eng):
                run("pe", eng)

            @block.scalar
            def _(eng):
                run("act", eng)

            @block.vector
            def _(eng):
                run("dve", eng)

            @block.gpsimd
            def _(eng):
                run("pool", eng)

            @block.sync
            def _(eng):
                run("sp", eng)
                for i in range(N_DMA_SEMS):
                    if self.dma_val[i] > 0:
                        eng.wait_ge(sems[("dma", i)], self.dma_val[i])
                for e in ("pe", "act", "dve", "pool"):
                    if ss.val[("eng", e)] > 0:
                        eng.wait_ge(sems[("eng", e)], ss.val[("eng", e)])


class Cfg:
    def __init__(self, D=2048, H=8, NCH=4, CH=1024, DFF=8192):
        self.D, self.H, self.NCH, self.CH, self.DFF = D, H, NCH, CH, DFF
        self.KC = D // 128
        self.NST = CH // 128
        self.NT = NCH * self.NST
        self.TCTX = NCH * CH
        self.NB = self.TCTX // 256
        self.NG = CH // 512
        self.HG = min(4, H)
        self.GW = self.HG * 128
        self.HW = H * 128
        self.o_mq, self.o_mk, self.o_mv = 0, self.HW, 2 * self.HW
        self.o_fq, self.o_fk, self.o_fv = 3 * self.HW, 4 * self.HW, 5 * self.HW
        self.o_ff = 6 * self.HW
        self.o_ga = self.o_ff + H
        self.o_gb = self.o_ga + D
        self.WIN = self.o_gb + D
        self.GD = min(512, D)
        self.FC = DFF // 128
        self.FB = min(1024, DFF)
        self.NFB = DFF // self.FB
        self.UW = min(256, DFF)
        self.TOPK = 3
        assert H >= 2 and self.NB >= 8


FULL = Cfg()


def _cp(e, out, in_):
    if hasattr(e, "tensor_copy"):
        return e.tensor_copy(out=out, in_=in_)
    return e.copy(out=out, in_=in_)


def MARK(name, S):
    if MARKS is not None:
        MARKS.append((name, len(S.ops)))


MARKS = None


def build(cfg=FULL, upto=4, debug=False):
    c = cfg
    D, H, KC, NCH, CH, NST, NT, TCTX = c.D, c.H, c.KC, c.NCH, c.CH, c.NST, c.NT, c.TCTX
    NB, NG, HG, GW, HW, GD = c.NB, c.NG, c.HG, c.GW, c.HW, c.GD
    H2 = 2 * H
    NN = CH // 512
    nc = bass.Bass("TRN2", target_bir_lowering=False)
    din = lambda n, s, dt=F32: nc.dram_tensor(n, s, dt, kind="ExternalInput").ap()
    dscr = lambda n, s, dt: nc.dram_tensor(n, s, dt, kind="Internal").ap()
    xctx = din("xctx", [TCTX, D])
    w_in = din("w_in", [D, c.WIN])
    w_bm = din("w_bm", [HW, D])
    w_bf = din("w_bf", [HW, D])
    w_out = din("w_out", [D, D])
    w_up = din("w_up", [D, c.DFF])
    w_down = din("w_down", [c.DFF, D])
    gvec = din("gvec", [4, D])
    bfg = din("bfg", [1, H])
    rcos = din("rope_cos", [TCTX, 16])
    rsin = din("rope_sin", [TCTX, 16])
    gbias_d = din("gbias", [1, NST * NB])
    vflag_d = din("vflag", [1, NCH])
    cident = din("cident", [128, 128])
    ctri = din("ctri", [128, 128])
    cE = din("cE", [NB, NB * 128])
    out = nc.dram_tensor("out", [CH, D], F32, kind="ExternalOutput").ap()
    KTd = dscr("KTd", [H2, 128, TCTX], BF16)
    QTd = dscr("QTd", [H2, 128, CH], BF16)
    Vd = dscr("Vd", [NT, 128, H2 * 128], BF16)
    sgd = dscr("sgd", [2, KC, 128, CH], BF16)
    x1d = dscr("x1d", [CH, D], F32)
    if debug:
        dbg_logf = nc.dram_tensor("dbg_logf", [128, NT * H], F32, kind="ExternalOutput").ap()
        dbg_kmT = nc.dram_tensor("dbg_kmT", [128, H * NB], F32, kind="ExternalOutput").ap()
        dbg_oT = nc.dram_tensor("dbg_oT", [128, H2 * CH], F32, kind="ExternalOutput").ap()
        dbg_mg = nc.dram_tensor("dbg_mg", [128, KC * CH], F32, kind="ExternalOutput").ap()

    scale = 128.0 ** -0.5

    with contextlib.ExitStack() as top:
        ss = SemState(nc, top)
        T = lambda st, n, s, dt: st.enter_context(nc.sbuf_tensor(n, s, dt))
        P = lambda st, n, s, dt: st.enter_context(nc.psum_tensor(n, s, dt))
        ident_b = T(top, "ident_b", [128, 128], BF16)
        tri_b = T(top, "tri_b", [128, 128], BF16)
        tri_f = T(top, "tri_f", [128, 128], F32)
        ident_f = T(top, "ident_f", [128, 128], F32)
        ones_f = T(top, "ones_f", [128, 128], F32)
        nones_f = T(top, "nones_f", [128, 128], F32)
        ones_b = T(top, "ones_b", [128, 128], BF16)
        c256 = T(top, "c256", [128, 1], BF16)
        epsc = T(top, "epsc", [128, 1], F32)
        E_b = T(top, "E_b", [NB, NB * 128], BF16)
        zff = T(top, "zff", [128, NT, H], F32)
        logf = T(top, "logf", [128, NT, H], F32)
        kmT = T(top, "kmT", [128, H, NB], BF16)
        stat = T(top, "stat", [128, NT, 8], F32)
        junk = T(top, "junk", [128, D], BF16)

        with contextlib.ExitStack() as ph:
            S = Sched(nc, ss)
            g_rep = T(ph, "g_rep", [128, D], F32)
            bfg_rep = T(ph, "bfg_rep", [128, H], F32)
            cos_sb = T(ph, "cos_sb", [128, NT, 16], F32)
            sin_sb = T(ph, "sin_sb", [128, NT, 16], F32)
            xs = [T(ph, "xs%d" % i, [128, D], F32) for i in range(2)]
            hb = [T(ph, "hb%d" % i, [128, D], BF16) for i in range(3)]
            hT = [T(ph, "hT%d" % i, [128, KC, CH], BF16) for i in range(2)]
            NWB = 3
            wb = [T(ph, "wb%d" % i, [128, KC, max(GD, GW)], BF16) for i in range(NWB)]
            vst = [T(ph, "vst%d" % i, [128, NST, GW], BF16) for i in range(2)]
            kst = [T(ph, "kst%d" % i, [128, HG, CH], BF16) for i in range(2)]
            kb = [T(ph, "kb%d" % i, [128, GW], BF16) for i in range(4)]
            rt = [T(ph, "rt%d" % i, [128, HG, 16], F32) for i in range(4)]
            wff = T(ph, "wff", [128, KC, H], BF16)
            lt = T(ph, "lt", [128, NT * H], F32)
            pst = [P(ph, "pst%d" % i, [128, 8, 128], BF16) for i in range(2)]
            psmm = [P(ph, "psmm%d" % i, [128, 512], F32) for i in range(3)]
            pstk = [P(ph, "pstk%d" % i, [128, 8, 128], BF16) for i in range(2)]
            pskm = P(ph, "pskm", [128, 512], F32)

            S.add("pool", lambda e: e.dma_start(out=ident_b[:], in_=cident), writes=["ident_b"], dma=True)
            S.add("pool", lambda e: e.dma_start(out=tri_b[:], in_=ctri), writes=["tri_b"], dma=True)
            S.add("sp", lambda e: e.dma_start(out=tri_f[:], in_=ctri), writes=["tri_f"], dma=True)
            S.add("sp", lambda e: e.dma_start(out=ident_f[:], in_=cident), writes=["ident_f"], dma=True)
            S.add("pool", lambda e: e.dma_start(out=E_b[:], in_=cE), writes=["E_b"], dma=True)
            S.add("dve", lambda e: e.memset(ones_f[:], 1.0), writes=["ones_f"])
            S.add("dve", lambda e: e.memset(nones_f[:], -1.0), writes=["nones_f"])
            S.add("dve", lambda e: e.memset(ones_b[:], 1.0), writes=["ones_b"])
            S.add("dve", lambda e: e.memset(c256[:], 1.0 / 256.0), writes=["c256"])
            S.add("dve", lambda e: e.memset(epsc[:], RMS_EPS), writes=["epsc"])
            S.add("sp", lambda e: e.dma_start(out=g_rep[:], in_=gvec[0, :].partition_broadcast(128)),
                  writes=["g_rep"], dma=True)
            S.add("sp", lambda e: e.dma_start(out=bfg_rep[:], in_=bfg[0, :].partition_broadcast(128)),
                  writes=["bfg_rep"], dma=True)
            S.add("sp", lambda e: e.dma_start(out=cos_sb[:], in_=rcos.rearrange("(t p) f -> p t f", p=128)),
                  writes=["cos_sb"], dma=True)
            S.add("sp", lambda e: e.dma_start(out=sin_sb[:], in_=rsin.rearrange("(t p) f -> p t f", p=128)),
                  writes=["sin_sb"], dma=True)
            S.add("pool", lambda e: e.dma_start(
                out=wff[:], in_=w_in[:, c.o_ff:c.o_ff + H].rearrange("(kc p) c -> p kc c", p=128)),
                writes=["wff"], dma=True)

            cnt = {"mm": 0, "w": 0, "v": 0, "k": 0, "kb": 0, "tk": 0, "tk2": 0}

            def load_w(src2d, col0, width):
                i = cnt["w"] % NWB
                cnt["w"] += 1
                buf = wb[i]
                for k0 in range(0, KC, 8):
                    k1 = min(KC, k0 + 8)
                    S.add("pool", lambda e, k0=k0, k1=k1: e.dma_start(
                        out=buf[:, k0:k1, 0:width],
                        in_=src2d[k0 * 128:k1 * 128, col0:col0 + width].rearrange("(kc p) c -> p kc c", p=128)),
                        writes=[("wb", i)], dma=True)
                return buf, ("wb", i)

            def hT_stageA(d, s):
                t = d * NST + s
                xi = t % 2
                hi = t % 3
                S.add("pool", lambda e: e.dma_start(out=xs[xi][:], in_=xctx[t * 128:(t + 1) * 128, :]),
                      writes=[("xs", xi)], dma=True)
                S.add("dve", lambda e: e.scalar_tensor_tensor(
                    out=hb[hi][:], in0=xs[xi][:], scalar=1.0, in1=xs[xi][:], op0=ALU.mult, op1=ALU.mult,
                    accum_out=stat[:, t, 0:1]),
                    reads=[("xs", xi)], writes=[("hb", hi), ("stat", t)])
                S.add("act", lambda e: e.activation(out=stat[:, t, 1:2], in_=stat[:, t, 0:1], func=AF.Sqrt,
                                                    bias=epsc[:], scale=1.0 / D),
                      reads=[("stat", t), "epsc"], writes=[("stat", t)])
                S.add("dve", lambda e: e.reciprocal(out=stat[:, t, 2:3], in_=stat[:, t, 1:2]),
                      reads=[("stat", t)], writes=[("stat", t)])
                S.add("dve", lambda e: e.scalar_tensor_tensor(
                    out=hb[hi][:], in0=xs[xi][:], scalar=stat[:, t, 2:3], in1=g_rep[:], op0=ALU.mult,
                    op1=ALU.mult),
                    reads=[("xs", xi), ("stat", t), "g_rep"], writes=[("hb", hi)])

            def hT_stageB(d, s):
                hTd = hT[d % 2]
                t = d * NST + s
                hi = t % 3
                nq = min(8, KC)
                for q0 in range(0, KC, nq):
                    pi = cnt["tk"] % 2
                    cnt["tk"] += 1
                    for kk in range(nq):
                        S.add("pe", lambda e, pi=pi, kk=kk, q0=q0: e.transpose(
                            out=pst[pi][:, kk, :], in_=hb[hi][:, (q0 + kk) * 128:(q0 + kk + 1) * 128],
                            identity=ident_b[:]),
                            reads=[("hb", hi), "ident_b"], writes=[("pst", pi)])
                    S.add("act", lambda e, pi=pi, q0=q0: e.copy(
                        out=hTd[:, q0:q0 + nq, s * 128:(s + 1) * 128], in_=pst[pi][:, 0:nq, :]),
                        reads=[("pst", pi)], writes=[("hT", d % 2)])

            pendA = []
            pendB = []

            def drip(n=1):
                for _ in range(n):
                    if pendA:
                        a = pendA.pop(0)
                        hT_stageA(*a)
                        pendB.append(a)
                    if pendB and (len(pendB) > 1 or not pendA):
                        hT_stageB(*pendB.pop(0))

            def next_ps():
                i = cnt["mm"] % 3
                cnt["mm"] += 1
                return psmm[i], ("psmm", i)

            def rope_tail(s, d, mode, hbase, nh, ki):
                t = d * NST + s
                bi = s % 4
                kbt = kb[bi]
                pi = cnt["tk2"] % 2
                cnt["tk2"] += 1
                for hh in range(nh):
                    S.add("pe", lambda e, pi=pi, hh=hh: e.transpose(
                        out=pstk[pi][:, hh, :], in_=kbt[:, hh * 128:(hh + 1) * 128], identity=ident_b[:]),
                        reads=[("kb", bi), "ident_b"], writes=[("pstk", pi)])
                S.add("dve", lambda e, pi=pi: e.tensor_copy(
                    out=kst[ki][:, 0:nh, s * 128:(s + 1) * 128], in_=pstk[pi][:, 0:nh, :]),
                    reads=[("pstk", pi)], writes=[("kst", ki)])
                if mode == "k_rope" and s % 2 == 1:
                    kprev = kb[(s - 1) % 4]
                    for hh in range(nh):
                        S.add("pe", lambda e, hh=hh: e.matmul(
                            pskm[:, hh:hh + 1], lhsT=kprev[:, hh * 128:(hh + 1) * 128], rhs=c256[:],
                            start=True, stop=False),
                            reads=[("kb", (s - 1) % 4), "c256"], writes=["pskm"])
                        S.add("pe", lambda e, hh=hh: e.matmul(
                            pskm[:, hh:hh + 1], lhsT=kbt[:, hh * 128:(hh + 1) * 128], rhs=c256[:],
                            start=False, stop=True),
                            reads=[("kb", bi), "c256"], writes=["pskm"])
                    n = t // 2
                    S.add("dve", lambda e: e.tensor_copy(out=kmT[:, hbase:hbase + nh, n], in_=pskm[:, 0:nh]),
                          reads=["pskm"], writes=["kmT"])

            def tok_group(d, col0, width, mode, hbase, wbuf, wkey):
                hTd = hT[d % 2]
                nh = width // 128
                if mode == "v":
                    vi = cnt["v"] % 2
                    cnt["v"] += 1
                else:
                    ki = cnt["k"] % 2
                    cnt["k"] += 1
                for s in range(NST):
                    t = d * NST + s
                    ps, pk = next_ps()
                    for kc in range(KC):
                        S.add("pe", lambda e, ps=ps, kc=kc, s=s: e.matmul(
                            ps[:, 0:width], lhsT=hTd[:, kc, s * 128:(s + 1) * 128], rhs=wbuf[:, kc, 0:width],
                            start=(kc == 0), stop=(kc == KC - 1)),
                            reads=[("hT", d % 2), wkey], writes=[pk])
                    if mode == "v":
                        S.add("act", lambda e, ps=ps, s=s: e.copy(out=vst[vi][:, s, 0:width], in_=ps[:, 0:width]),
                              reads=[pk], writes=[("vst", vi)])
                        continue
                    bi = s % 4
                    kbt = kb[bi]
                    ps3 = ps[:, 0:width].rearrange("p (h c) -> p h c", c=128)
                    kb3 = kbt[:, 0:width].rearrange("p (h c) -> p h c", c=128)
                    cosb = cos_sb[:, t:t + 1, :].broadcast_to([128, nh, 16])
                    sinb = sin_sb[:, t:t + 1, :].broadcast_to([128, nh, 16])
                    S.add("act", lambda e, ps=ps, kbt=kbt: e.copy(out=kbt[:, 0:width], in_=ps[:, 0:width]),
                          reads=[pk], writes=[("kb", bi)])
                    x1, x2 = ps3[:, :, 0:16], ps3[:, :, 16:32]
                    r = [rt[i][:, 0:nh, :] for i in range(4)]
                    S.add("dve", lambda e, x1=x1, cosb=cosb, r=r: e.tensor_tensor(out=r[0], in0=x1, in1=cosb, op=ALU.mult),
                          reads=[pk, "cos_sb"], writes=[("rt", 0)])
                    S.add("dve", lambda e, x2=x2, sinb=sinb, r=r: e.tensor_tensor(out=r[1], in0=x2, in1=sinb, op=ALU.mult),
                          reads=[pk, "sin_sb"], writes=[("rt", 1)])
                    S.add("dve", lambda e, x2=x2, cosb=cosb, r=r: e.tensor_tensor(out=r[2], in0=x2, in1=cosb, op=ALU.mult),
                          reads=[pk, "cos_sb"], writes=[("rt", 2)])
                    S.add("dve", lambda e, x1=x1, sinb=sinb, r=r: e.tensor_tensor(out=r[3], in0=x1, in1=sinb, op=ALU.mult),
                          reads=[pk, "sin_sb"], writes=[("rt", 3)])
                    S.add("dve", lambda e, kb3=kb3, r=r: e.tensor_tensor(out=kb3[:, :, 0:16], in0=r[0], in1=r[1],
                                                                       op=ALU.subtract),
                          reads=[("rt", 0), ("rt", 1), ("kb", bi)], writes=[("kb", bi)])
                    S.add("dve", lambda e, kb3=kb3, r=r: e.tensor_tensor(out=kb3[:, :, 16:32], in0=r[2], in1=r[3],
                                                                       op=ALU.add),
                          reads=[("rt", 2), ("rt", 3), ("kb", bi)], writes=[("kb", bi)])
                    if s >= 1:
                        rope_tail(s - 1, d, mode, hbase, nh, ki)
                if mode != "v":
                    rope_tail(NST - 1, d, mode, hbase, nh, ki)
                if mode == "v":
                    S.add("sp", lambda e: e.dma_start(
                        out=Vd[d * NST:(d + 1) * NST, :, hbase * 128:hbase * 128 + width].rearrange("t p c -> p t c"),
                        in_=vst[vi][:, :, 0:width]),
                        reads=[("vst", vi)], dma=True)
                else:
                    dst = KTd[hbase:hbase + nh, :, d * CH:(d + 1) * CH] if mode == "k_rope" else QTd[hbase:hbase + nh, :, :]
                    S.add("sp", lambda e: e.dma_start(out=dst.rearrange("h p t -> p h t"), in_=kst[ki][:, 0:nh, :]),
                          reads=[("kst", ki)], dma=True)
                drip()

            def feat_group(d, col0, width, mode, hbase, wbuf, wkey, kc0=0):
                hTd = hT[d % 2]
                nh = width // 128
                for h0 in range(0, nh, HG):
                    h1 = min(nh, h0 + HG)
                    ki = cnt["k"] % 2
                    cnt["k"] += 1
                    for hh in range(h0, h1):
                        for n0 in range(0, CH, 512):
                            ps, pk = next_ps()
                            for kc in range(KC):
                                S.add("pe", lambda e, ps=ps, kc=kc, hh=hh, n0=n0: e.matmul(
                                    ps[:, 0:512], lhsT=wbuf[:, kc, hh * 128:(hh + 1) * 128],
                                    rhs=hTd[:, kc, n0:n0 + 512], start=(kc == 0), stop=(kc == KC - 1)),
                                    reads=[("hT", d % 2), wkey], writes=[pk])
                            if mode == "gate":
                                S.add("act", lambda e, ps=ps, hh=hh, n0=n0, ki=ki, h0=h0: e.activation(
                                    out=kst[ki][:, hh - h0, n0:n0 + 512], in_=ps[:, 0:512], func=AF.Sigmoid),
                                    reads=[pk], writes=[("kst", ki)])
                            else:
                                S.add("dve", lambda e, ps=ps, hh=hh, n0=n0, ki=ki, h0=h0: e.tensor_copy(
                                    out=kst[ki][:, hh - h0, n0:n0 + 512], in_=ps[:, 0:512]),
                                    reads=[pk], writes=[("kst", ki)])
                    if mode == "kT":
                        dst = KTd[hbase + h0:hbase + h1, :, d * CH:(d + 1) * CH]
                    elif mode == "qT":
                        dst = QTd[hbase + h0:hbase + h1, :, :]
                    else:
                        dst = sgd[hbase, kc0 + h0:kc0 + h1, :, :]
                    S.add("sp", lambda e, ki=ki, dst=dst, h0=h0, h1=h1: e.dma_start(
                        out=dst.rearrange("h p t -> p h t"), in_=kst[ki][:, 0:h1 - h0, :]),
                        reads=[("kst", ki)], dma=True)
                drip()

            def ff_group(d):
                hTd = hT[d % 2]
                for s in range(NST):
                    t = d * NST + s
                    ps, pk = next_ps()
                    for kc in range(KC):
                        S.add("pe", lambda e, ps=ps, kc=kc, s=s: e.matmul(
                            ps[:, 0:H], lhsT=hTd[:, kc, s * 128:(s + 1) * 128], rhs=wff[:, kc, :],
                            start=(kc == 0), stop=(kc == KC - 1)),
                            reads=[("hT", d % 2), "wff"], writes=[pk])
                    S.add("dve", lambda e, ps=ps, t=t: e.tensor_tensor(out=zff[:, t, :], in0=ps[:, 0:H], in1=bfg_rep[:],
                                                                     op=ALU.add),
                          reads=[pk, "bfg_rep"], writes=["zff"])
                drip()

            MARK("consts", S)
            for s in range(NST):
                hT_stageA(0, s)
                if s >= 1:
                    hT_stageB(0, s - 1)
            hT_stageB(0, NST - 1)
            tasks = []
            for d in range(NCH):
                if d + 1 < NCH:
                    tasks.append((None, None, lambda d=d: pendA.extend((d + 1, s) for s in range(NST))))
                for g0 in range(0, H, HG):
                    tasks.append((c.o_mk + g0 * 128, GW, lambda wb_, wk_, d=d, g0=g0: tok_group(
                        d, c.o_mk + g0 * 128, GW, "k_rope", g0, wb_, wk_)))
                for g0 in range(0, H, HG):
                    tasks.append((c.o_mv + g0 * 128, GW, lambda wb_, wk_, d=d, g0=g0: tok_group(
                        d, c.o_mv + g0 * 128, GW, "v", g0, wb_, wk_)))
                for g0 in range(0, H, HG):
                    tasks.append((c.o_fk + g0 * 128, GW, lambda wb_, wk_, d=d, g0=g0: feat_group(
                        d, c.o_fk + g0 * 128, GW, "kT", H + g0, wb_, wk_)))
                for g0 in range(0, H, HG):
                    tasks.append((c.o_fv + g0 * 128, GW, lambda wb_, wk_, d=d, g0=g0: tok_group(
                        d, c.o_fv + g0 * 128, GW, "v", H + g0, wb_, wk_)))
                tasks.append((None, None, lambda d=d: ff_group(d)))
                tasks.append((None, None, lambda: drip(2 * NST + 2)))
                if d == 0:
                    for g0 in range(0, H, HG):
                        tasks.append((c.o_mq + g0 * 128, GW, lambda wb_, wk_, g0=g0: tok_group(
                            0, c.o_mq + g0 * 128, GW, "q_rope", g0, wb_, wk_)))
                    for g0 in range(0, H, HG):
                        tasks.append((c.o_fq + g0 * 128, GW, lambda wb_, wk_, g0=g0: feat_group(
                            0, c.o_fq + g0 * 128, GW, "qT", H + g0, wb_, wk_)))
                    for c0 in range(0, D, GD):
                        tasks.append((c.o_ga + c0, GD, lambda wb_, wk_, c0=c0: feat_group(
                            0, c.o_ga + c0, GD, "gate", 0, wb_, wk_, kc0=c0 // 128)))
                    for c0 in range(0, D, GD):
                        tasks.append((c.o_gb + c0, GD, lambda wb_, wk_, c0=c0: feat_group(
                            0, c.o_gb + c0, GD, "gate", 1, wb_, wk_, kc0=c0 // 128)))
            PREF = NWB - 1
            wtasks = [i for i, tk_ in enumerate(tasks) if tk_[0] is not None]
            loaded = {}
            nxt = 0
            for i, (col0, width, fn) in enumerate(tasks):
                if col0 is None:
                    fn()
                    continue
                pos = wtasks.index(i)
                while nxt < len(wtasks) and nxt <= pos + PREF:
                    ti = wtasks[nxt]
                    loaded[ti] = load_w(w_in, tasks[ti][0], tasks[ti][1])
                    nxt += 1
                fn(*loaded.pop(i))
            MARK("proj_done", S)
            zf = zff[:].rearrange("p t h -> p (t h)")
            lf = logf[:].rearrange("p t h -> p (t h)")
            S.add("act", lambda e: e.activation(out=lt[:], in_=zf, func=AF.Exp, scale=-1.0), reads=["zff"], writes=["lt"])
            S.add("act", lambda e: e.activation(out=lt[:], in_=lt[:], func=AF.Ln, bias=1.0), reads=["lt"], writes=["lt"])
            S.add("dve", lambda e: e.tensor_scalar(out=lf, in0=lt[:], scalar1=-1.0, scalar2=None, op0=ALU.mult),
                  reads=["lt"], writes=["logf"])
            if debug:
                S.add("sp", lambda e: e.dma_start(out=dbg_logf, in_=lf), reads=["logf"], dma=True)
                S.add("pool", lambda e: e.dma_start(out=dbg_kmT, in_=kmT[:].rearrange("p h n -> p (h n)")),
                      reads=["kmT"], dma=True)
            S.emit("ph1")
        if upto < 2:
            return nc

        with contextlib.ExitStack() as mid:
            R1 = T(mid, "R1", [128, max(H2, KC), CH], BF16)
            R2 = T(mid, "R2", [128, max(KC, 16), CH], BF16)
            oT, h2T, mergedT = R1, R1, R2

            mid2 = contextlib.ExitStack()
            mid2.__enter__()
            Wbm = T(mid2, "Wbm", [128, H, D], BF16)
            Wbf = T(mid2, "Wbf", [128, H, D], BF16)
            with contextlib.ExitStack() as ph:
                S = Sched(nc, ss)
                KTb = [T(ph, "KTb%d" % i, [128, TCTX], BF16) for i in range(2)]
                Vb = [T(ph, "Vb%d" % i, [128, NT, 130], BF16) for i in range(2)]
                QTb = [T(ph, "QTb%d" % i, [128, CH], BF16) for i in range(2)]
                pT = [T(ph, "pT%d" % i, [128, 512], BF16) for i in range(4)]
                NSQ = TCTX // 512 + CH // 512
                sqb1 = [T(ph, "sqb%d" % i, [128, 512], BF16) for i in range(NSQ)]
                sqb = [sqb1, sqb1]
                nmx = T(ph, "nmx", [128, 4], F32)
                negC = T(ph, "negC", [128, H2], F32)
                gbias_rep = T(ph, "gbias_rep", [128, NST * NB], F32)
                vfl_rep = T(ph, "vfl_rep", [128, NCH], F32)
                Fsb = T(ph, "Fsb", [128, NT, H], F32)
                totB = T(ph, "totB", [128, NT, H], F32)
                offF = T(ph, "offF", [128, NT, H], F32)
                Tch = T(ph, "Tch", [128, NCH, H], F32)
                FrefB = T(ph, "FrefB", [128, NG, H], F32)
                biasF = T(ph, "biasF", [128, NG, NT, H], F32)
                bfh = [T(ph, "bfh%d" % i, [128, NG, NT], F32) for i in range(2)]
                gmb = T(ph, "gmb", [128, NST, NB], F32)
                m8 = T(ph, "m8", [128, NST, 8], F32)
                b1 = T(ph, "b1", [128, NST, NB], F32)
                bb = T(ph, "bb", [128, NST, NB], BF16)
                biasT = [T(ph, "biasT%d" % i, [NB, CH], BF16) for i in range(2)]
                rcp1 = T(ph, "rcp", [128, 512], F32)
                rcp = [rcp1, rcp1]
                NPS = 3
                psS = [P(ph, "psS%d" % i, [128, 512], F32) for i in range(NPS)]
                oacc_t = [[P(ph, "oacc%d_%d" % (i, k), [128, 512], F32) for k in range(2)] for i in range(2)]
                oacc = [[t[:, 0:260].rearrange("p (r c) -> p r c", c=130) for t in row] for row in oacc_t]
                psX = [P(ph, "psX%d" % i, [128, 512], F32) for i in range(1)]
                psXb = [p[:].bitcast(BF16) for p in psX]
                cn = {"x": 0, "s": 0, "p": 0, "a": 0, "sq": 0, "g": 0}

                def next_x():
                    i = cn["x"] % len(psX)
                    cn["x"] += 1
                    return i, ("psX", i)

                S.add("sp", lambda e: e.dma_start(out=gbias_rep[:], in_=gbias_d[0, :].partition_broadcast(128)),
                      writes=["gbias_rep"], dma=True)
                S.add("sp", lambda e: e.dma_start(out=vfl_rep[:], in_=vflag_d[0, :].partition_broadcast(128)),
                      writes=["vfl_rep"], dma=True)
                for i in range(2):
                    S.add("pool", lambda e, i=i: e.memset(Vb[i][:, :, 128:129], 1.0), writes=[("V", i)])

                def load_head(h, par):
                    S.add("sp", lambda e: e.dma_start(out=KTb[par][:], in_=KTd[h]), writes=[("KT", par)], dma=True)
                    S.add("sp", lambda e: e.dma_start(out=QTb[par][:], in_=QTd[h]), writes=[("QT", par)], dma=True)
                    for t0 in range(0, NT, 8):
                        S.add("sp", lambda e, t0=t0: e.dma_start(
                            out=Vb[par][:, t0:t0 + 8, 0:128],
                            in_=Vd[t0:t0 + 8, :, h * 128:(h + 1) * 128].rearrange("t p c -> p t c")),
                            writes=[("V", par)], dma=True)

                load_head(0, 0)

                lf_all = logf[:].rearrange("p t h -> p (t h)")
                S.add("pe", lambda e: e.matmul(psS[0][:, 0:NT * H], lhsT=tri_f[:], rhs=lf_all, start=True, stop=True),
                      reads=["logf", "tri_f"], writes=[("psS", 0)])
                S.add("pe", lambda e: e.matmul(psS[1][:, 0:NT * H], lhsT=ones_f[:], rhs=lf_all, start=True, stop=True),
                      reads=["logf", "ones_f"], writes=[("psS", 1)])
                S.add("dve", lambda e: e.tensor_copy(out=Fsb[:].rearrange("p t h -> p (t h)"), in_=psS[0][:, 0:NT * H]),
                      reads=[("psS", 0)], writes=["Fsb"])
                S.add("dve", lambda e: e.tensor_copy(out=totB[:].rearrange("p t h -> p (t h)"), in_=psS[1][:, 0:NT * H]),
                      reads=[("psS", 1)], writes=["totB"])
                for d in range(NCH):
                    S.add("dve", lambda e, d=d: e.tensor_reduce(
                        out=Tch[:, d, :], in_=totB[:, d * NST:(d + 1) * NST, :].rearrange("p j h -> p h j"),
                        axis=AX.X, op=ALU.add), reads=["totB"], writes=["Tch"])
                S.add("dve", lambda e: e.memset(offF[:, 0, :], 0.0), writes=["offF"])
                for d in range(1, NCH):
                    prev_t = 0 if d == 1 else (d - 1) * NST
                    S.add("dve", lambda e, d=d, prev_t=prev_t: e.tensor_tensor(
                        out=offF[:, d * NST, :], in0=offF[:, prev_t, :], in1=Tch[:, d, :], op=ALU.subtract),
                        reads=["offF", "Tch"], writes=["offF"])
                for d in range(NCH):
                    for j in range(1, NST):
                        t = d * NST + j
                        S.add("dve", lambda e, t=t: e.tensor_tensor(
                            out=offF[:, t, :], in0=offF[:, t - 1, :], in1=totB[:, t - 1, :], op=ALU.add),
                            reads=["offF", "totB"], writes=["offF"])
                S.add("dve", lambda e: e.tensor_tensor(out=Fsb[:], in0=Fsb[:], in1=offF[:], op=ALU.add),
                      reads=["Fsb", "offF"], writes=["Fsb"])
                for g in range(NG):
                    S.add("dve", lambda e, g=g: e.tensor_copy(out=FrefB[:, g, :], in_=offF[:, 4 * g + 2, :]),
                          reads=["offF"], writes=["FrefB"])
                for g in range(NG):
                    S.add("dve", lambda e, g=g: e.tensor_tensor(
                        out=biasF[:, g], in0=FrefB[:, g:g + 1, :].broadcast_to([128, NT, H]), in1=Fsb[:],
                        op=ALU.subtract), reads=["FrefB", "Fsb"], writes=["biasF"])
                    S.add("dve", lambda e, g=g: e.tensor_tensor(
                        out=biasF[:, g].rearrange("p (d j) h -> p d (j h)", d=NCH),
                        in0=biasF[:, g].rearrange("p (d j) h -> p d (j h)", d=NCH),
                        in1=vfl_rep[:].unsqueeze(2).broadcast_to([128, NCH, NST * H]), op=ALU.add),
                        reads=["biasF", "vfl_rep"], writes=["biasF"])

                def head_sq(par, h):
                    tiles = [(KTb[par], ("KT", par), n0) for n0 in range(0, TCTX, 512)]
                    tiles += [(QTb[par], ("QT", par), n0) for n0 in range(0, CH, 512)]
                    for i, (buf, bkey, n0) in enumerate(tiles):
                        S.add("pool", lambda e, buf=buf, n0=n0, i=i: e.tensor_tensor(
                            out=sqb[par][i][:], in0=buf[:, n0:n0 + 512], in1=buf[:, n0:n0 + 512], op=ALU.mult),
                            reads=[bkey], writes=[("sqb", i)])

                def head_norm(par, h):
                    xi, xk = next_x()
                    ncol = NSQ * 4
                    for i in range(NSQ):
                        for cq in range(4):
                            S.add("pe", lambda e, i=i, cq=cq: e.matmul(
                                psX[xi][:, i * 4 + cq:i * 4 + cq + 1], lhsT=sqb[par][i][:, cq * 128:(cq + 1) * 128],
                                rhs=ones_b[:, 0:1], start=True, stop=True),
                                reads=[("sqb", i), "ones_b"], writes=[xk])
                    S.add("dve", lambda e: e.tensor_reduce(out=nmx[:, 0:1], in_=psX[xi][:, 0:ncol], axis=AX.X, op=ALU.max),
                          reads=[xk], writes=["nmx"])
                    xi2, xk2 = next_x()
                    S.add("pe", lambda e: e.transpose(out=psX[xi2][0:1, 0:128], in_=nmx[:, 0:1], identity=ident_f[:]),
                          reads=["nmx", "ident_f"], writes=[xk2])
                    S.add("dve", lambda e: e.tensor_reduce(out=nmx[0:1, 1:2], in_=psX[xi2][0:1, 0:128], axis=AX.X,
                                                           op=ALU.max), reads=[xk2], writes=["nmx1"])
                    xi3, xk3 = next_x()
                    S.add("pe", lambda e: e.matmul(psX[xi3][:, 0:1], lhsT=ones_f[0:1, :], rhs=nmx[0:1, 1:2],
                                                   start=True, stop=True), reads=["nmx1", "ones_f"], writes=[xk3])
                    S.add("dve", lambda e: e.tensor_scalar(out=negC[:, h:h + 1], in0=psX[xi3][:, 0:1],
                                                           scalar1=-1.02 * scale, scalar2=None, op0=ALU.mult),
                          reads=[xk3], writes=[("negC", h)])

                def moba_gate(par, hm):
                    xi, xk = next_x()
                    psg = psX[xi][:, 0:NST * NB].rearrange("p (q n) -> p q n", n=NB)
                    for qt in range(NST):
                        S.add("pe", lambda e, qt=qt: e.matmul(
                            psg[:, qt, :], lhsT=QTb[par][:, qt * 128:(qt + 1) * 128], rhs=kmT[:, hm, :],
                            start=True, stop=True), reads=[("QT", par), "kmT"], writes=[xk])
                    g3 = gbias_rep[:].rearrange("p (q n) -> p q n", n=NB)
                    S.add("dve", lambda e: e.tensor_tensor(out=gmb[:], in0=psg, in1=g3, op=ALU.add),
                          reads=[xk, "gbias_rep"], writes=["gmb"])
                    for qt in range(NST):
                        S.add("dve", lambda e, qt=qt: e.max(out=m8[:, qt, :], in_=gmb[:, qt, :]),
                              reads=["gmb"], writes=["m8"])
                    for qt in range(NST):
                        S.add("dve", lambda e, qt=qt: e.tensor_scalar(
                            out=b1[:, qt, :], in0=gmb[:, qt, :], scalar1=m8[:, qt, c.TOPK - 1:c.TOPK],
                            scalar2=-NEG_MASK, op0=ALU.is_ge, op1=ALU.mult), reads=["gmb", "m8"], writes=["b1"])
                    S.add("dve", lambda e: e.scalar_tensor_tensor(
                        out=bb[:].rearrange("p q n -> p (q n)"), in0=b1[:].rearrange("p q n -> p (q n)"),
                        scalar=NEG_MASK, in1=gbias_rep[:], op0=ALU.add, op1=ALU.min),
                        reads=["b1", "gbias_rep"], writes=["bb"])
                    for qt in range(NST):
                        S.add("dve", lambda e, qt=qt: e.memset(bb[:, qt, qt // 2:qt // 2 + 1], 0.0),
                              reads=["bb"], writes=["bb"])
                    xi2, xk2 = next_x()
                    for qt in range(NST):
                        S.add("pe", lambda e, qt=qt: e.transpose(out=psXb[xi2][0:NB, qt * 128:(qt + 1) * 128],
                                                                 in_=bb[:, qt, :], identity=ident_b[:]),
                              reads=["bb", "ident_b"], writes=[xk2])
                    S.add("dve", lambda e: e.tensor_copy(out=biasT[par][0:NB, 0:NST * 128],
                                                         in_=psXb[xi2][0:NB, 0:NST * 128]),
                          reads=[xk2], writes=[("biasT", par)])

                deferred = []

                def flush_deferred():
                    while deferred:
                        deferred.pop(0)()

                def attention(par, h, moba, mid_hook=None):
                    for g in range(NG):
                        a = cn["a"] % 2
                        cn["a"] += 1
                        tiles = [(d, j) for d in range(NCH - 1, 0, -1) for j in range(NST)]
                        tiles += [(0, j) for j in range(4 * g + 4)]

                        def qk_step(idx, d, j, g=g, a=a):
                            t = d * NST + j
                            c0 = 0 if (d > 0 or j < 4 * g) else (j - 4 * g) * 128
                            N = 512 - c0
                            q0 = g * 512 + c0
                            si = cn["s"] % NPS
                            cn["s"] += 1
                            ps, pk = psS[si], ("psS", si)
                            S.add("pe", lambda e: e.matmul(
                                ps[:, 0:N], lhsT=KTb[par][:, t * 128:(t + 1) * 128], rhs=QTb[par][:, q0:q0 + N],
                                start=True, stop=not moba), reads=[("KT", par), ("QT", par)], writes=[pk])
                            if moba:
                                n = t // 2
                                S.add("pe", lambda e: e.matmul(
                                    ps[:, 0:N], lhsT=E_b[0:NB, n * 128:(n + 1) * 128], rhs=biasT[par][0:NB, q0:q0 + N],
                                    start=False, stop=True), reads=["E_b", ("biasT", par)], writes=[pk])
                            pi = cn["p"] % 4
                            cn["p"] += 1
                            pt, ptk = pT[pi], ("pT", pi)
                            if moba:
                                bias_ap, bkey = negC[:, h:h + 1], ("negC", h)
                            else:
                                bias_ap, bkey = bfh[par][:, g, t:t + 1], ("bfh", par)
                            S.add("act", lambda e: e.activation(
                                out=pt[:, 0:N], in_=ps[:, 0:N], func=AF.Exp, bias=bias_ap, scale=scale),
                                reads=[pk, bkey], writes=[ptk])
                            if d == 0 and j >= 4 * g:
                                S.add("pool", lambda e: e.tensor_tensor(out=pt[:, 0:128], in0=pt[:, 0:128],
                                                                        in1=tri_b[:], op=ALU.mult),
                                      reads=[ptk, "tri_b"], writes=[ptk])
                            return (idx, d, j, t, c0, pt, ptk)

                        def pv_step(st, g=g, a=a, ntiles=len(tiles)):
                            idx, d, j, t, c0, pt, ptk = st
                            N = 512 - c0
                            first, last = (idx == 0), (idx == ntiles - 1)
                            S.add("pe", lambda e: e.matmul(
                                oacc_t[a][0][:, c0:512], lhsT=Vb[par][:, t, 0:128], rhs=pt[:, 0:N],
                                start=first, stop=last), reads=[ptk, ("V", par)], writes=[("oacc", a, 0)])
                            S.add("pe", lambda e: e.matmul(
                                oacc_t[a][1][:, c0:512], lhsT=ones_b[:], rhs=pt[:, 0:N],
                                start=first, stop=last), reads=[ptk, "ones_b"], writes=[("oacc", a, 1)])

                        pend = []
                        for idx, (d, j) in enumerate(tiles):
                            pend.append(qk_step(idx, d, j))
                            if len(pend) > 2:
                                pv_step(pend.pop(0))
                            if idx == 3:
                                flush_deferred()
                        while pend:
                            pv_step(pend.pop(0))
                        S.add("dve", lambda e, a=a: e.reciprocal(out=rcp[a][:], in_=oacc_t[a][1][:, 0:512]),
                              reads=[("oacc", a, 1)], writes=["rcp"])
                        S.add("dve", lambda e, a=a, g=g: e.tensor_tensor(
                            out=oT[:, h, g * 512:(g + 1) * 512], in0=oacc_t[a][0][:, 0:512], in1=rcp[a][:], op=ALU.mult),
                            reads=[("oacc", a, 0), "rcp"], writes=["oT"])
                        if g == 0 and mid_hook is not None:
                            mid_hook()
                    if NG == 1 and mid_hook is not None:
                        pass

                def prep(h):
                    par = h % 2
                    head_norm(par, h)
                    if h < H:
                        moba_gate(par, h)
                    else:
                        S.add("dve", lambda e: e.tensor_scalar(
                            out=bfh[par][:], in0=biasF[:, :, :, h - H], scalar1=negC[:, h:h + 1], scalar2=None,
                            op0=ALU.add), reads=["biasF", ("negC", h)], writes=[("bfh", par)])

                head_sq(0, 0)
                prep(0)
                for h in range(H2):
                    par = h % 2

                    def hook(h=h):
                        if h + 1 < H2:
                            prep(h + 1)
                        if h == H2 - 3:
                            for h0 in range(0, H, 4):
                                h1 = min(H, h0 + 4)
                                for (wsb, wdr, nm) in ((Wbm, w_bm, "Wbm"), (Wbf, w_bf, "Wbf")):
                                    S.add("pool", lambda e, wsb=wsb, wdr=wdr, h0=h0, h1=h1: e.dma_start(
                                        out=wsb[:, h0:h1, :],
                                        in_=wdr[h0 * 128:h1 * 128, :].rearrange("(h p) c -> p h c", p=128)),
                                        writes=[nm], dma=True)
                    if h + 1 < H2:
                        load_head(h + 1, (h + 1) % 2)
                        head_sq((h + 1) % 2, h + 1)
                    attention(par, h, h < H, mid_hook=hook)
                flush_deferred()
                if debug:
                    S.add("pool", lambda e: e.dma_start(out=dbg_oT, in_=oT[:, 0:H2, :].rearrange("p h t -> p (h t)")),
                          reads=["oT"], dma=True)
                S.emit("ph2")
            if upto < 3:
                return nc

            with contextlib.ExitStack() as ph:
                S = Sched(nc, ss)
                sga = [T(ph, "sga%d" % i, [128, CH], BF16) for i in range(2)]
                sgb = [T(ph, "sgb%d" % i, [128, CH], BF16) for i in range(2)]
                t1 = [T(ph, "t1_%d" % i, [128, 512], F32) for i in range(2)]
                t2 = [T(ph, "t2_%d" % i, [128, 512], F32) for i in range(2)]
                psm = [[P(ph, "psm%d_%d" % (i, n), [128, 512], F32) for n in range(NN)] for i in range(2)]
                psf = [[P(ph, "psf%d_%d" % (i, n), [128, 512], F32) for n in range(NN)] for i in range(2)]
                tc_ = 0
                for cc in range(KC):
                    a = cc % 2
                    S.add("sp", lambda e, a=a, cc=cc: e.dma_start(out=sga[a][:], in_=sgd[0, cc]), writes=[("sga", a)], dma=True)
                    S.add("sp", lambda e, a=a, cc=cc: e.dma_start(out=sgb[a][:], in_=sgd[1, cc]), writes=[("sgb", a)], dma=True)
                    for n in range(NN):
                        for hh in range(H):
                            S.add("pe", lambda e, a=a, n=n, hh=hh, cc=cc: e.matmul(
                                psm[a][n][:, 0:512], lhsT=Wbm[:, hh, cc * 128:(cc + 1) * 128],
                                rhs=oT[:, hh, n * 512:(n + 1) * 512], start=(hh == 0), stop=(hh == H - 1)),
                                reads=["Wbm", "oT"], writes=[("psm", a, n)])
                        for hh in range(H):
                            S.add("pe", lambda e, a=a, n=n, hh=hh, cc=cc: e.matmul(
                                psf[a][n][:, 0:512], lhsT=Wbf[:, hh, cc * 128:(cc + 1) * 128],
                                rhs=oT[:, H + hh, n * 512:(n + 1) * 512], start=(hh == 0), stop=(hh == H - 1)),
                                reads=["Wbf", "oT"], writes=[("psf", a, n)])
                        k = tc_ % 2
                        tc_ += 1
                        S.add("dve", lambda e, a=a, n=n, k=k: e.tensor_tensor(
                            out=t1[k][:], in0=psm[a][n][:, 0:512], in1=sga[a][:, n * 512:(n + 1) * 512], op=ALU.mult),
                            reads=[("psm", a, n), ("sga", a)], writes=[("t1", k)])
                        S.add("dve", lambda e, a=a, n=n, k=k: e.tensor_tensor(
                            out=t2[k][:], in0=psf[a][n][:, 0:512], in1=sgb[a][:, n * 512:(n + 1) * 512], op=ALU.mult),
                            reads=[("psf", a, n), ("sgb", a)], writes=[("t2", k)])
                        S.add("pool", lambda e, cc=cc, n=n, k=k: e.tensor_tensor(
                            out=mergedT[:, cc, n * 512:(n + 1) * 512], in0=t1[k][:], in1=t2[k][:], op=ALU.add),
                            reads=[("t1", k), ("t2", k)], writes=["mergedT"])
                if debug:
                    S.add("pool", lambda e: e.dma_start(out=dbg_mg, in_=mergedT[:, 0:KC, :].rearrange("p h t -> p (h t)")),
                          reads=["mergedT"], dma=True)
                S.emit("ph3a")
            mid2.close()

            with contextlib.ExitStack() as ph:
                S = Sched(nc, ss)
                Wo = T(ph, "Wo", [128, KC, D], BF16)
                gpost = T(ph, "gpost", [128, D], F32)
                gpre2 = T(ph, "gpre2", [128, D], F32)
                xs2 = [T(ph, "xs2_%d" % i, [128, D], F32) for i in range(2)]
                tt_ = T(ph, "tt_", [128, D], F32)
                hb2 = [T(ph, "hb2_%d" % i, [128, D], BF16) for i in range(2)]
                st3 = T(ph, "st3", [128, NST, 16], F32)
                ncg = D // GD
                psM = [P(ph, "psM%d" % i, [128, 512], F32) for i in range(6)]
                pst2 = [P(ph, "pst2_%d" % i, [128, 8, 128], BF16) for i in range(2)]
                S.add("sp", lambda e: e.dma_start(out=gpost[:], in_=gvec[1, :].partition_broadcast(128)),
                      writes=["gpost"], dma=True)
                S.add("sp", lambda e: e.dma_start(out=gpre2[:], in_=gvec[2, :].partition_broadcast(128)),
                      writes=["gpre2"], dma=True)
                for c0 in range(0, D, GD):
                    for k0 in range(0, KC, 8):
                        k1 = min(KC, k0 + 8)
                        S.add("pool", lambda e, c0=c0, k0=k0, k1=k1: e.dma_start(
                            out=Wo[:, k0:k1, c0:c0 + GD],
                            in_=w_out[k0 * 128:k1 * 128, c0:c0 + GD].rearrange("(kc p) c -> p kc c", p=128)),
                            writes=[("Wo", c0)], dma=True)
                tkc = [0]

                def h2_tail(s):
                    xi = s % 2
                    nq = min(8, KC)
                    for q0 in range(0, KC, nq):
                        pi2 = tkc[0] % 2
                        tkc[0] += 1
                        for kk in range(nq):
                            S.add("pe", lambda e, pi2=pi2, kk=kk, q0=q0: e.transpose(
                                out=pst2[pi2][:, kk, :], in_=hb2[xi][:, (q0 + kk) * 128:(q0 + kk + 1) * 128],
                                identity=ident_b[:]), reads=[("hb2", xi), "ident_b"], writes=[("pst2", pi2)])
                        S.add("act", lambda e, pi2=pi2, q0=q0: e.copy(
                            out=h2T[:, q0:q0 + nq, s * 128:(s + 1) * 128], in_=pst2[pi2][:, 0:nq, :]),
                            reads=[("pst2", pi2)], writes=["h2T"])

                mc = 0
                tk = 0
                for s in range(NST):
                    xi = s % 2
                    S.add("pool", lambda e, s=s, xi=xi: e.dma_start(out=xs2[xi][:], in_=xctx[s * 128:(s + 1) * 128, :]),
                          writes=[("xs2", xi)], dma=True)
                    pss = []
                    for cg in range(ncg):
                        pi = mc % 6
                        mc += 1
                        pss.append(pi)
                        for kc in range(KC):
                            S.add("pe", lambda e, pi=pi, kc=kc, s=s, cg=cg: e.matmul(
                                psM[pi][:, 0:GD], lhsT=mergedT[:, kc, s * 128:(s + 1) * 128],
                                rhs=Wo[:, kc, cg * GD:(cg + 1) * GD], start=(kc == 0), stop=(kc == KC - 1)),
                                reads=["mergedT", ("Wo", cg * GD)], writes=[("psM", pi)])
                        S.add("act", lambda e, pi=pi, s=s, cg=cg: e.activation(
                            out=junk[:, 0:GD], in_=psM[pi][:, 0:GD], func=AF.Square, accum_out=st3[:, s, cg:cg + 1]),
                            reads=[("psM", pi)], writes=["junk", ("st3", s)])
                    S.add("dve", lambda e, s=s: e.tensor_reduce(out=st3[:, s, 8:9], in_=st3[:, s, 0:ncg], axis=AX.X,
                                                                op=ALU.add), reads=[("st3", s)], writes=[("st3", s)])
                    S.add("act", lambda e, s=s: e.activation(out=st3[:, s, 9:10], in_=st3[:, s, 8:9], func=AF.Sqrt,
                                                             bias=epsc[:], scale=1.0 / D),
                          reads=[("st3", s), "epsc"], writes=[("st3", s)])
                    S.add("dve", lambda e, s=s: e.reciprocal(out=st3[:, s, 10:11], in_=st3[:, s, 9:10]),
                          reads=[("st3", s)], writes=[("st3", s)])
                    for cg in range(ncg):
                        pi = pss[cg]
                        S.add("dve", lambda e, pi=pi, s=s, cg=cg: e.scalar_tensor_tensor(
                            out=tt_[:, cg * GD:(cg + 1) * GD], in0=psM[pi][:, 0:GD], scalar=st3[:, s, 10:11],
                            in1=gpost[:, cg * GD:(cg + 1) * GD], op0=ALU.mult, op1=ALU.mult),
                            reads=[("psM", pi), ("st3", s), "gpost"], writes=["tt_"])
                    S.add("dve", lambda e, xi=xi: e.tensor_tensor(out=xs2[xi][:], in0=tt_[:], in1=xs2[xi][:], op=ALU.add),
                          reads=["tt_", ("xs2", xi)], writes=[("xs2", xi)])
                    S.add("sp", lambda e, s=s, xi=xi: e.dma_start(out=x1d[s * 128:(s + 1) * 128, :], in_=xs2[xi][:]),
                          reads=[("xs2", xi)], dma=True)
                    S.add("dve", lambda e, s=s, xi=xi: e.scalar_tensor_tensor(
                        out=hb2[xi][:], in0=xs2[xi][:], scalar=1.0, in1=xs2[xi][:], op0=ALU.mult, op1=ALU.mult,
                        accum_out=st3[:, s, 11:12]), reads=[("xs2", xi)], writes=[("hb2", xi), ("st3", s)])
                    S.add("act", lambda e, s=s: e.activation(out=st3[:, s, 12:13], in_=st3[:, s, 11:12], func=AF.Sqrt,
                                                             bias=epsc[:], scale=1.0 / D),
                          reads=[("st3", s), "epsc"], writes=[("st3", s)])
                    S.add("dve", lambda e, s=s: e.reciprocal(out=st3[:, s, 13:14], in_=st3[:, s, 12:13]),
                          reads=[("st3", s)], writes=[("st3", s)])
                    S.add("dve", lambda e, s=s, xi=xi: e.scalar_tensor_tensor(
                        out=hb2[xi][:], in0=xs2[xi][:], scalar=st3[:, s, 13:14], in1=gpre2[:], op0=ALU.mult,
                        op1=ALU.mult), reads=[("xs2", xi), ("st3", s), "gpre2"], writes=[("hb2", xi)])
                    if s >= 1:
                        h2_tail(s - 1)
                h2_tail(NST - 1)
                S.emit("ph3b")
            if upto < 4:
                return nc

            with contextlib.ExitStack() as ph:
                S = Sched(nc, ss)
                FB, UW = c.FB, c.UW
                FK = FB // 128
                Wu = [T(ph, "Wu%d" % i, [128, KC, UW], BF16) for i in range(2)]
                Wd = [T(ph, "Wd%d" % i, [128, FK, GD], BF16) for i in range(2)]
                m = T(ph, "m", [128, NST, D], F32)
                rl = [T(ph, "rl%d" % i, [128, 512], F32) for i in range(2)]
                gpost2 = T(ph, "gpost2", [128, D], F32)
                xs3 = [T(ph, "xs3_%d" % i, [128, D], F32) for i in range(2)]
                st4 = T(ph, "st4", [128, NST, 4], F32)
                psU = [P(ph, "psU%d" % i, [128, 512], F32) for i in range(3)]
                psD = [P(ph, "psD%d" % i, [128, 512], F32) for i in range(3)]
                aTv = lambda par, k: R2[:, par * FK + k, :]
                ncg = D // GD
                S.add("sp", lambda e: e.dma_start(out=gpost2[:], in_=gvec[3, :].partition_broadcast(128)),
                      writes=["gpost2"], dma=True)
                cn4 = {"uc": 0, "dc": 0, "wu": 0, "wd": 0, "rk": 0}

                def load_u(fb, u0):
                    col0 = fb * FB + u0
                    wi = cn4["wu"] % 2
                    cn4["wu"] += 1
                    for k0 in range(0, KC, 8):
                        k1 = min(KC, k0 + 8)
                        S.add("pool", lambda e, k0=k0, k1=k1: e.dma_start(
                            out=Wu[wi][:, k0:k1, :],
                            in_=w_up[k0 * 128:k1 * 128, col0:col0 + UW].rearrange("(kc p) c -> p kc c", p=128)),
                            writes=[("Wu", wi)], dma=True)
                    return wi

                def comp_u(fb, u0, wi):
                    par = fb % 2
                    for f in range(UW // 128):
                        fcl = (u0 + f * 128) // 128
                        for n in range(NN):
                            pi = cn4["uc"] % 3
                            cn4["uc"] += 1
                            for kc in range(KC):
                                S.add("pe", lambda e, pi=pi, kc=kc, f=f, n=n: e.matmul(
                                    psU[pi][:, 0:512], lhsT=Wu[wi][:, kc, f * 128:(f + 1) * 128],
                                    rhs=h2T[:, kc, n * 512:(n + 1) * 512], start=(kc == 0), stop=(kc == KC - 1)),
                                    reads=[("Wu", wi), "h2T"], writes=[("psU", pi)])
                            k = cn4["rk"] % 2
                            cn4["rk"] += 1
                            S.add("act", lambda e, pi=pi, k=k: e.activation(out=rl[k][:], in_=psU[pi][:, 0:512],
                                                                            func=AF.Relu),
                                  reads=[("psU", pi)], writes=[("rl", k)])
                            S.add("pool", lambda e, k=k, fcl=fcl, n=n: e.tensor_tensor(
                                out=aTv(par, fcl)[:, n * 512:(n + 1) * 512], in0=rl[k][:], in1=rl[k][:], op=ALU.mult),
                                reads=[("rl", k)], writes=[("aT", par)])

                def load_d(fb, cg):
                    wi = cn4["wd"] % 2
                    cn4["wd"] += 1
                    S.add("pool", lambda e: e.dma_start(
                        out=Wd[wi][:],
                        in_=w_down[fb * FB:(fb + 1) * FB, cg * GD:(cg + 1) * GD].rearrange("(kc p) c -> p kc c", p=128)),
                        writes=[("Wd", wi)], dma=True)
                    return wi

                def comp_d(fb, cg, wi):
                    par = fb % 2
                    for s in range(NST):
                        pi = cn4["dc"] % 3
                        cn4["dc"] += 1
                        for kc in range(FK):
                            S.add("pe", lambda e, pi=pi, kc=kc, s=s: e.matmul(
                                psD[pi][:, 0:GD], lhsT=aTv(par, kc)[:, s * 128:(s + 1) * 128], rhs=Wd[wi][:, kc, :],
                                start=(kc == 0), stop=(kc == FK - 1)),
                                reads=[("aT", par), ("Wd", wi)], writes=[("psD", pi)])
                        msl = m[:, s, cg * GD:(cg + 1) * GD]
                        if fb == 0:
                            S.add("act", lambda e, pi=pi, msl=msl: e.copy(out=msl, in_=psD[pi][:, 0:GD]),
                                  reads=[("psD", pi)], writes=[("m", s, cg)])
                        else:
                            S.add("dve", lambda e, pi=pi, msl=msl: e.tensor_tensor(out=msl, in0=psD[pi][:, 0:GD],
                                                                                 in1=msl, op=ALU.add),
                                  reads=[("psD", pi), ("m", s, cg)], writes=[("m", s, cg)])

                tasks4 = []
                for fb in range(c.NFB):
                    for u0 in range(0, FB, UW):
                        tasks4.append((load_u, comp_u, fb, u0))
                    for cg in range(ncg):
                        tasks4.append((load_d, comp_d, fb, cg))
                nxt_w = tasks4[0][0](tasks4[0][2], tasks4[0][3])
                for i, (lf_, cf_, a1, a2) in enumerate(tasks4):
                    cur_w = nxt_w
                    if i + 1 < len(tasks4):
                        t2 = tasks4[i + 1]
                        nxt_w = t2[0](t2[2], t2[3])
                    cf_(a1, a2, cur_w)
                for s in range(NST):
                    xi = s % 2
                    mk = [("m", s, cg) for cg in range(ncg)]
                    S.add("pool", lambda e, s=s, xi=xi: e.dma_start(out=xs3[xi][:], in_=x1d[s * 128:(s + 1) * 128, :]),
                          writes=[("xs3", xi)], dma=True)
                    S.add("dve", lambda e, s=s: e.scalar_tensor_tensor(
                        out=junk[:], in0=m[:, s, :], scalar=1.0, in1=m[:, s, :], op0=ALU.mult, op1=ALU.mult,
                        accum_out=st4[:, s, 0:1]), reads=mk, writes=["junk", ("st4", s)])
                    S.add("act", lambda e, s=s: e.activation(out=st4[:, s, 1:2], in_=st4[:, s, 0:1], func=AF.Sqrt,
                                                             bias=epsc[:], scale=1.0 / D),
                          reads=[("st4", s), "epsc"], writes=[("st4", s)])
                    S.add("dve", lambda e, s=s: e.reciprocal(out=st4[:, s, 2:3], in_=st4[:, s, 1:2]),
                          reads=[("st4", s)], writes=[("st4", s)])
                    S.add("dve", lambda e, s=s: e.scalar_tensor_tensor(
                        out=m[:, s, :], in0=m[:, s, :], scalar=st4[:, s, 2:3], in1=gpost2[:], op0=ALU.mult,
                        op1=ALU.mult), reads=mk + [("st4", s), "gpost2"], writes=mk)
                    S.add("dve", lambda e, s=s, xi=xi: e.tensor_tensor(out=xs3[xi][:], in0=m[:, s, :], in1=xs3[xi][:],
                                                                     op=ALU.add),
                          reads=mk + [("xs3", xi)], writes=[("xs3", xi)])
                    S.add("sp", lambda e, s=s, xi=xi: e.dma_start(out=out[s * 128:(s + 1) * 128, :], in_=xs3[xi][:]),
                          reads=[("xs3", xi)], dma=True)
                S.emit("ph4")
    return nc


def make_core_inputs(inp, core, cfg=FULL):
    c = cfg
    b, j = divmod(core, c.NCH)
    x = np.asarray(inp["x"])[b]
    CH, NCH = c.CH, c.NCH
    order = [(j - d) % NCH for d in range(NCH)]
    xctx = np.ascontiguousarray(np.concatenate([x[o * CH:(o + 1) * CH] for o in order], 0), dtype=np.float32)
    pos = np.concatenate([np.arange(o * CH, (o + 1) * CH) for o in order]).astype(np.float32)
    inv_freq = (np.float32(ROPE_THETA) ** (-np.arange(0, 32, 2, dtype=np.float32) / np.float32(32))).astype(np.float32)
    ang = (pos[:, None] * inv_freq[None, :]).astype(np.float32)
    valid = [j - d >= 0 for d in range(NCH)]
    vflag = np.array([[0.0 if v else NEG_MASK for v in valid]], np.float32)
    bpc = CH // 256
    gb = np.full((c.NST, c.NB), -1e30, np.float32)
    for qt in range(c.NST):
        bq = qt // 2
        for n in range(c.NB):
            d, i = divmod(n, bpc)
            if (d == 0 and i < bq) or (d >= 1 and valid[d]):
                gb[qt, n] = 0.0
    k = np.arange(128)
    cE = np.zeros((c.NB, c.NB * 128), np.float32)
    for n in range(c.NB):
        cE[n, n * 128:(n + 1) * 128] = 1.0
    sq = lambda a: np.ascontiguousarray(np.asarray(a)[0], dtype=np.float32)
    return {
        "xctx": xctx,
        "w_in": sq(inp["w_in"]), "w_bm": sq(inp["w_branch_moba"]), "w_bf": sq(inp["w_branch_fox"]),
        "w_out": sq(inp["w_out"]), "w_up": sq(inp["w_up"]), "w_down": sq(inp["w_down"]),
        "gvec": np.stack([sq(inp["g_mix_pre"]), sq(inp["g_mix_post"]), sq(inp["g_mlp_pre"]), sq(inp["g_mlp_post"])], 0),
        "bfg": np.asarray(inp["b_forget"], np.float32).reshape(1, c.H),
        "rope_cos": np.cos(ang).astype(np.float32), "rope_sin": np.sin(ang).astype(np.float32),
        "gbias": gb.reshape(1, -1), "vflag": vflag,
        "cident": np.eye(128, dtype=np.float32),
        "ctri": (k[:, None] <= k[None, :]).astype(np.float32),
        "cE": cE,
    }


_NC_CACHE = {}


def kernel(**inputs):
    cfg = FULL
    if "nc" not in _NC_CACHE:
        _NC_CACHE["nc"] = build(cfg)
    nc = _NC_CACHE["nc"]
    n_cores = 2 * cfg.NCH
    shared = None
    in_maps = []
    for core in range(n_cores):
        m = make_core_inputs(inputs, core, cfg)
        if shared is None:
            shared = m
        else:
            for key in m:
                if key not in ("xctx", "rope_cos", "rope_sin", "gbias", "vflag"):
                    m[key] = shared[key]
        in_maps.append(m)
    res = run_bass_kernel_spmd(nc, in_maps, core_ids=list(range(n_cores)))
    B = n_cores // cfg.NCH
    outp = np.empty((B, cfg.TCTX, cfg.D), np.float32)
    for core in range(n_cores):
        b, j = divmod(core, cfg.NCH)
        outp[b, j * cfg.CH:(j + 1) * cfg.CH] = np.asarray(res.results[core]["out"], np.float32)
    return outp
```

```python
import contextlib
import numpy as np
import concourse.bass as bass
import concourse.mybir as mybir
from concourse.bass_utils import run_bass_kernel_spmd

F32 = mybir.dt.float32
BF16 = mybir.dt.bfloat16
AF = mybir.ActivationFunctionType
ALU = mybir.AluOpType
AX = mybir.AxisListType

ENGS = ("pe", "act", "dve", "pool", "sp")
N_DMA_SEMS = 24
N_HW_SEMS = 16
OP_LIMIT = None
RMS_EPS = 1e-6
ROPE_THETA = 500000.0
NEG_MASK = -30000.0


class _Op:
    __slots__ = ("eng", "fn", "is_dma", "signal", "sigval", "dsem", "dval", "deps", "waits")


class SemState:
    def __init__(self, nc, stack):
        self.sems = {}
        for e in ("pe", "act", "dve", "pool"):
            self.sems[("eng", e)] = stack.enter_context(nc.semaphore("s_" + e))
        for i in range(N_DMA_SEMS):
            self.sems[("dma", i)] = stack.enter_context(nc.semaphore("s_dma%d" % i))
        self.val = {k: 0 for k in self.sems}
        self.dma_count = {"hw": 0, "sw": 0}


def _is_psum(k):
    n = k[0] if isinstance(k, tuple) else k
    return isinstance(n, str) and (n.startswith("ps") or n == "oacc")


class Sched:
    def __init__(self, nc, ss):
        self.nc = nc
        self.ss = ss
        self.ops = []
        self.streams = {e: [] for e in ENGS}
        self.last_w = {}
        self.readers = {}
        self.dma_last = [None] * N_DMA_SEMS
        self.dma_val = [ss.val[("dma", i)] for i in range(N_DMA_SEMS)]

    def add(self, eng, fn, reads=(), writes=(), dma=False):
        if OP_LIMIT is not None and len(self.ops) >= OP_LIMIT:
            return None
        pr = [k for k in reads if _is_psum(k)]
        if pr:
            reads = [k for k in reads if not _is_psum(k)]
            writes = list(writes) + [k for k in pr if k not in writes]
        op = _Op()
        op.eng, op.fn, op.is_dma = eng, fn, dma
        op.signal, op.sigval, op.dsem, op.dval, op.waits = False, None, None, None, None
        deps = []
        for k in reads:
            w = self.last_w.get(k)
            if w is not None:
                deps.append(w)
        for k in writes:
            w = self.last_w.get(k)
            if w is not None:
                deps.append(w)
            rd = self.readers.get(k)
            if rd:
                deps.extend(rd.values())
        if dma:
            if eng == "pool":
                s = N_HW_SEMS + self.ss.dma_count["sw"] % (N_DMA_SEMS - N_HW_SEMS)
                self.ss.dma_count["sw"] += 1
            else:
                s = self.ss.dma_count["hw"] % N_HW_SEMS
                self.ss.dma_count["hw"] += 1
            if self.dma_last[s] is not None:
                deps.append(self.dma_last[s])
            self.dma_val[s] += 16
            op.dsem, op.dval = s, self.dma_val[s]
            self.dma_last[s] = op
        op.deps = deps
        gid = len(self.ops)
        for k in reads:
            self.readers.setdefault(k, {})[("dma", gid) if dma else eng] = op
        for k in writes:
            self.last_w[k] = op
            self.readers[k] = {}
        self.streams[eng].append(op)
        self.ops.append(op)
        return op

    @staticmethod
    def _skip(op, d):
        return (not d.is_dma) and d.eng == op.eng and op.eng == "pe" and not op.is_dma

    def emit(self, name):
        nc, ss = self.nc, self.ss
        for op in self.ops:
            for d in op.deps:
                if not d.is_dma and not self._skip(op, d):
                    d.signal = True
        for e in ("pe", "act", "dve", "pool"):
            c = ss.val[("eng", e)]
            for op in self.streams[e]:
                if op.signal and not op.is_dma:
                    c += 1
                    op.sigval = c
            ss.val[("eng", e)] = c
        for i in range(N_DMA_SEMS):
            ss.val[("dma", i)] = self.dma_val[i]
        for e in ENGS:
            known = {}
            for op in self.streams[e]:
                need = {}
                for d in op.deps:
                    if d.is_dma:
                        key, val = ("dma", d.dsem), d.dval
                    elif self._skip(op, d):
                        continue
                    else:
                        key, val = ("eng", d.eng), d.sigval
                    if known.get(key, 0) >= val:
                        continue
                    if need.get(key, 0) < val:
                        need[key] = val
                known.update(need)
                op.waits = need
        sems = ss.sems
        with nc.Block() as block:
            def run(e, eng):
                for op in self.streams[e]:
                    for k, v in op.waits.items():
                        eng.wait_ge(sems[k], v)
                    ins = op.fn(eng)
                    if op.is_dma:
                        ins.then_inc(sems[("dma", op.dsem)], 16)
                    elif op.signal:
                        ins.then_inc(sems[("eng", e)], 1)

            @block.tensor
            def _(eng):
                run("pe", eng)

            @block.scalar
            def _(eng):
                run("act", eng)

            @block.vector
            def _(eng):
                run("dve", eng)

            @block.gpsimd
            def _(eng):
                run("pool", eng)

            @block.sync
            def _(eng):
                run("sp", eng)
                for i in range(N_DMA_SEMS):
                    if self.dma_val[i] > 0:
                        eng.wait_ge(sems[("dma", i)], self.dma_val[i])
                for e in ("pe", "act", "dve", "pool"):
                    if ss.val[("eng", e)] > 0:
                        eng.wait_ge(sems[("eng", e)], ss.val[("eng", e)])


class Cfg:
    def __init__(self, D=2048, H=8, NCH=4, CH=1024, DFF=8192):
        self.D, self.H, self.NCH, self.CH, self.DFF = D, H, NCH, CH, DFF
        self.KC = D // 128
        self.NST = CH // 128
        self.NT = NCH * self.NST
        self.TCTX = NCH * CH
        self.NB = self.TCTX // 256
        self.NG = CH // 512
        self.HG = min(4, H)
        self.GW = self.HG * 128
        self.HW = H * 128
        self.o_mq, self.o_mk, self.o_mv = 0, self.HW, 2 * self.HW
        self.o_fq, self.o_fk, self.o_fv = 3 * self.HW, 4 * self.HW, 5 * self.HW
        self.o_ff = 6 * self.HW
        self.o_ga = self.o_ff + H
        self.o_gb = self.o_ga + D
        self.WIN = self.o_gb + D
        self.GD = min(512, D)
        self.FC = DFF // 128
        self.FB = min(1024, DFF)
        self.NFB = DFF // self.FB
        self.UW = min(256, DFF)
        self.TOPK = 3
        assert H >= 2 and self.NB >= 8


FULL = Cfg()


def _cp(e, out, in_):
    if hasattr(e, "tensor_copy"):
        return e.tensor_copy(out=out, in_=in_)
    return e.copy(out=out, in_=in_)


def MARK(name, S):
    if MARKS is not None:
        MARKS.append((name, len(S.ops)))


MARKS = None


def build(cfg=FULL, upto=4, debug=False):
    c = cfg
    D, H, KC, NCH, CH, NST, NT, TCTX = c.D, c.H, c.KC, c.NCH, c.CH, c.NST, c.NT, c.TCTX
    NB, NG, HG, GW, HW, GD = c.NB, c.NG, c.HG, c.GW, c.HW, c.GD
    H2 = 2 * H
    NN = CH // 512
    nc = bass.Bass("TRN2", target_bir_lowering=False)
    din = lambda n, s, dt=F32: nc.dram_tensor(n, s, dt, kind="ExternalInput").ap()
    dscr = lambda n, s, dt: nc.dram_tensor(n, s, dt, kind="Internal").ap()
    xctx = din("xctx", [TCTX, D])
    w_in = din("w_in", [D, c.WIN])
    w_bm = din("w_bm", [HW, D])
    w_bf = din("w_bf", [HW, D])
    w_out = din("w_out", [D, D])
    w_up = din("w_up", [D, c.DFF])
    w_down = din("w_down", [c.DFF, D])
    gvec = din("gvec", [4, D])
    bfg = din("bfg", [1, H])
    rcos = din("rope_cos", [TCTX, 16])
    rsin = din("rope_sin", [TCTX, 16])
    gbias_d = din("gbias", [1, NST * NB])
    vflag_d = din("vflag", [1, NCH])
    cident = din("cident", [128, 128])
    ctri = din("ctri", [128, 128])
    cE = din("cE", [NB, NB * 128])
    out = nc.dram_tensor("out", [CH, D], F32, kind="ExternalOutput").ap()
    KTd = dscr("KTd", [H2, 128, TCTX], BF16)
    QTd = dscr("QTd", [H2, 128, CH], BF16)
    Vd = dscr("Vd", [NT, 128, H2 * 128], BF16)
    sgd = dscr("sgd", [2, KC, 128, CH], BF16)
    x1d = dscr("x1d", [CH, D], F32)
    if debug:
        dbg_logf = nc.dram_tensor("dbg_logf", [128, NT * H], F32, kind="ExternalOutput").ap()
        dbg_kmT = nc.dram_tensor("dbg_kmT", [128, H * NB], F32, kind="ExternalOutput").ap()
        dbg_oT = nc.dram_tensor("dbg_oT", [128, H2 * CH], F32, kind="ExternalOutput").ap()
        dbg_mg = nc.dram_tensor("dbg_mg", [128, KC * CH], F32, kind="ExternalOutput").ap()

    scale = 128.0 ** -0.5

    with contextlib.ExitStack() as top:
        ss = SemState(nc, top)
        T = lambda st, n, s, dt: st.enter_context(nc.sbuf_tensor(n, s, dt))
        P = lambda st, n, s, dt: st.enter_context(nc.psum_tensor(n, s, dt))
        ident_b = T(top, "ident_b", [128, 128], BF16)
        tri_b = T(top, "tri_b", [128, 128], BF16)
        tri_f = T(top, "tri_f", [128, 128], F32)
        ident_f = T(top, "ident_f", [128, 128], F32)
        ones_f = T(top, "ones_f", [128, 128], F32)
        nones_f = T(top, "nones_f", [128, 128], F32)
        ones_b = T(top, "ones_b", [128, 128], BF16)
        c256 = T(top, "c256", [128, 1], BF16)
        epsc = T(top, "epsc", [128, 1], F32)
        E_b = T(top, "E_b", [NB, NB * 128], BF16)
        zff = T(top, "zff", [128, NT, H], F32)
        logf = T(top, "logf", [128, NT, H], F32)
        kmT = T(top, "kmT", [128, H, NB], BF16)
        stat = T(top, "stat", [128, NT, 8], F32)
        junk = T(top, "junk", [128, D], BF16)

        with contextlib.ExitStack() as ph:
            S = Sched(nc, ss)
            g_rep = T(ph, "g_rep", [128, D], F32)
            bfg_rep = T(ph, "bfg_rep", [128, H], F32)
            cos_sb = T(ph, "cos_sb", [128, NT, 16], F32)
            sin_sb = T(ph, "sin_sb", [128, NT, 16], F32)
            xs = [T(ph, "xs%d" % i, [128, D], F32) for i in range(2)]
            hb = [T(ph, "hb%d" % i, [128, D], BF16) for i in range(3)]
            hT = [T(ph, "hT%d" % i, [128, KC, CH], BF16) for i in range(2)]
            NWB = 3
            wb = [T(ph, "wb%d" % i, [128, KC, max(GD, GW)], BF16) for i in range(NWB)]
            vst = [T(ph, "vst%d" % i, [128, NST, GW], BF16) for i in range(2)]
            kst = [T(ph, "kst%d" % i, [128, HG, CH], BF16) for i in range(2)]
            kb = [T(ph, "kb%d" % i, [128, GW], BF16) for i in range(4)]
            rt = [T(ph, "rt%d" % i, [128, HG, 16], F32) for i in range(4)]
            wff = T(ph, "wff", [128, KC, H], BF16)
            lt = T(ph, "lt", [128, NT * H], F32)
            pst = [P(ph, "pst%d" % i, [128, 8, 128], BF16) for i in range(2)]
            psmm = [P(ph, "psmm%d" % i, [128, 512], F32) for i in range(3)]
            pstk = [P(ph, "pstk%d" % i, [128, 8, 128], BF16) for i in range(2)]
            pskm = P(ph, "pskm", [128, 512], F32)

            S.add("pool", lambda e: e.dma_start(out=ident_b[:], in_=cident), writes=["ident_b"], dma=True)
            S.add("pool", lambda e: e.dma_start(out=tri_b[:], in_=ctri), writes=["tri_b"], dma=True)
            S.add("sp", lambda e: e.dma_start(out=tri_f[:], in_=ctri), writes=["tri_f"], dma=True)
            S.add("sp", lambda e: e.dma_start(out=ident_f[:], in_=cident), writes=["ident_f"], dma=True)
            S.add("pool", lambda e: e.dma_start(out=E_b[:], in_=cE), writes=["E_b"], dma=True)
            S.add("dve", lambda e: e.memset(ones_f[:], 1.0), writes=["ones_f"])
            S.add("dve", lambda e: e.memset(nones_f[:], -1.0), writes=["nones_f"])
            S.add("dve", lambda e: e.memset(ones_b[:], 1.0), writes=["ones_b"])
            S.add("dve", lambda e: e.memset(c256[:], 1.0 / 256.0), writes=["c256"])
            S.add("dve", lambda e: e.memset(epsc[:], RMS_EPS), writes=["epsc"])
            S.add("sp", lambda e: e.dma_start(out=g_rep[:], in_=gvec[0, :].partition_broadcast(128)),
                  writes=["g_rep"], dma=True)
            S.add("sp", lambda e: e.dma_start(out=bfg_rep[:], in_=bfg[0, :].partition_broadcast(128)),
                  writes=["bfg_rep"], dma=True)
            S.add("sp", lambda e: e.dma_start(out=cos_sb[:], in_=rcos.rearrange("(t p) f -> p t f", p=128)),
                  writes=["cos_sb"], dma=True)
            S.add("sp", lambda e: e.dma_start(out=sin_sb[:], in_=rsin.rearrange("(t p) f -> p t f", p=128)),
                  writes=["sin_sb"], dma=True)
            S.add("pool", lambda e: e.dma_start(
                out=wff[:], in_=w_in[:, c.o_ff:c.o_ff + H].rearrange("(kc p) c -> p kc c", p=128)),
                writes=["wff"], dma=True)

            cnt = {"mm": 0, "w": 0, "v": 0, "k": 0, "kb": 0, "tk": 0, "tk2": 0}

            def load_w(src2d, col0, width):
                i = cnt["w"] % NWB
                cnt["w"] += 1
                buf = wb[i]
                for k0 in range(0, KC, 8):
                    k1 = min(KC, k0 + 8)
                    S.add("pool", lambda e, k0=k0, k1=k1: e.dma_start(
                        out=buf[:, k0:k1, 0:width],
                        in_=src2d[k0 * 128:k1 * 128, col0:col0 + width].rearrange("(kc p) c -> p kc c", p=128)),
                        writes=[("wb", i)], dma=True)
                return buf, ("wb", i)

            def hT_stageA(d, s):
                t = d * NST + s
                xi = t % 2
                hi = t % 3
                S.add("pool", lambda e: e.dma_start(out=xs[xi][:], in_=xctx[t * 128:(t + 1) * 128, :]),
                      writes=[("xs", xi)], dma=True)
                S.add("dve", lambda e: e.scalar_tensor_tensor(
                    out=hb[hi][:], in0=xs[xi][:], scalar=1.0, in1=xs[xi][:], op0=ALU.mult, op1=ALU.mult,
                    accum_out=stat[:, t, 0:1]),
                    reads=[("xs", xi)], writes=[("hb", hi), ("stat", t)])
                S.add("act", lambda e: e.activation(out=stat[:, t, 1:2], in_=stat[:, t, 0:1], func=AF.Sqrt,
                                                    bias=epsc[:], scale=1.0 / D),
                      reads=[("stat", t), "epsc"], writes=[("stat", t)])
                S.add("dve", lambda e: e.reciprocal(out=stat[:, t, 2:3], in_=stat[:, t, 1:2]),
                      reads=[("stat", t)], writes=[("stat", t)])
                S.add("dve", lambda e: e.scalar_tensor_tensor(
                    out=hb[hi][:], in0=xs[xi][:], scalar=stat[:, t, 2:3], in1=g_rep[:], op0=ALU.mult,
                    op1=ALU.mult),
                    reads=[("xs", xi), ("stat", t), "g_rep"], writes=[("hb", hi)])

            def hT_stageB(d, s):
                hTd = hT[d % 2]
                t = d * NST + s
                hi = t % 3
                nq = min(8, KC)
                for q0 in range(0, KC, nq):
                    pi = cnt["tk"] % 2
                    cnt["tk"] += 1
                    for kk in range(nq):
                        S.add("pe", lambda e, pi=pi, kk=kk, q0=q0: e.transpose(
                            out=pst[pi][:, kk, :], in_=hb[hi][:, (q0 + kk) * 128:(q0 + kk + 1) * 128],
                            identity=ident_b[:]),
                            reads=[("hb", hi), "ident_b"], writes=[("pst", pi)])
                    S.add("act", lambda e, pi=pi, q0=q0: e.copy(
                        out=hTd[:, q0:q0 + nq, s * 128:(s + 1) * 128], in_=pst[pi][:, 0:nq, :]),
                        reads=[("pst", pi)], writes=[("hT", d % 2)])

            pendA = []
            pendB = []

            def drip(n=1):
                for _ in range(n):
                    if pendA:
                        a = pendA.pop(0)
                        hT_stageA(*a)
                        pendB.append(a)
                    if pendB and (len(pendB) > 1 or not pendA):
                        hT_stageB(*pendB.pop(0))

            def next_ps():
                i = cnt["mm"] % 3
                cnt["mm"] += 1
                return psmm[i], ("psmm", i)

            def rope_tail(s, d, mode, hbase, nh, ki):
                t = d * NST + s
                bi = s % 4
                kbt = kb[bi]
                pi = cnt["tk2"] % 2
                cnt["tk2"] += 1
                for hh in range(nh):
                    S.add("pe", lambda e, pi=pi, hh=hh: e.transpose(
                        out=pstk[pi][:, hh, :], in_=kbt[:, hh * 128:(hh + 1) * 128], identity=ident_b[:]),
                        reads=[("kb", bi), "ident_b"], writes=[("pstk", pi)])
                S.add("dve", lambda e, pi=pi: e.tensor_copy(
                    out=kst[ki][:, 0:nh, s * 128:(s + 1) * 128], in_=pstk[pi][:, 0:nh, :]),
                    reads=[("pstk", pi)], writes=[("kst", ki)])
                if mode == "k_rope" and s % 2 == 1:
                    kprev = kb[(s - 1) % 4]
                    for hh in range(nh):
                        S.add("pe", lambda e, hh=hh: e.matmul(
                            pskm[:, hh:hh + 1], lhsT=kprev[:, hh * 128:(hh + 1) * 128], rhs=c256[:],
                            start=True, stop=False),
                            reads=[("kb", (s - 1) % 4), "c256"], writes=["pskm"])
                        S.add("pe", lambda e, hh=hh: e.matmul(
                            pskm[:, hh:hh + 1], lhsT=kbt[:, hh * 128:(hh + 1) * 128], rhs=c256[:],
                            start=False, stop=True),
                            reads=[("kb", bi), "c256"], writes=["pskm"])
                    n = t // 2
                    S.add("dve", lambda e: e.tensor_copy(out=kmT[:, hbase:hbase + nh, n], in_=pskm[:, 0:nh]),
                          reads=["pskm"], writes=["kmT"])

            def tok_group(d, col0, width, mode, hbase, wbuf, wkey):
                hTd = hT[d % 2]
                nh = width // 128
                if mode == "v":
                    vi = cnt["v"] % 2
                    cnt["v"] += 1
                else:
                    ki = cnt["k"] % 2
                    cnt["k"] += 1
                for s in range(NST):
                    t = d * NST + s
                    ps, pk = next_ps()
                    for kc in range(KC):
                        S.add("pe", lambda e, ps=ps, kc=kc, s=s: e.matmul(
                            ps[:, 0:width], lhsT=hTd[:, kc, s * 128:(s + 1) * 128], rhs=wbuf[:, kc, 0:width],
                            start=(kc == 0), stop=(kc == KC - 1)),
                            reads=[("hT", d % 2), wkey], writes=[pk])
                    if mode == "v":
                        S.add("act", lambda e, ps=ps, s=s: e.copy(out=vst[vi][:, s, 0:width], in_=ps[:, 0:width]),
                              reads=[pk], writes=[("vst", vi)])
                        continue
                    bi = s % 4
                    kbt = kb[bi]
                    ps3 = ps[:, 0:width].rearrange("p (h c) -> p h c", c=128)
                    kb3 = kbt[:, 0:width].rearrange("p (h c) -> p h c", c=128)
                    cosb = cos_sb[:, t:t + 1, :].broadcast_to([128, nh, 16])
                    sinb = sin_sb[:, t:t + 1, :].broadcast_to([128, nh, 16])
                    S.add("act", lambda e, ps=ps, kbt=kbt: e.copy(out=kbt[:, 0:width], in_=ps[:, 0:width]),
                          reads=[pk], writes=[("kb", bi)])
                    x1, x2 = ps3[:, :, 0:16], ps3[:, :, 16:32]
                    r = [rt[i][:, 0:nh, :] for i in range(4)]
                    S.add("dve", lambda e, x1=x1, cosb=cosb, r=r: e.tensor_tensor(out=r[0], in0=x1, in1=cosb, op=ALU.mult),
                          reads=[pk, "cos_sb"], writes=[("rt", 0)])
                    S.add("dve", lambda e, x2=x2, sinb=sinb, r=r: e.tensor_tensor(out=r[1], in0=x2, in1=sinb, op=ALU.mult),
                          reads=[pk, "sin_sb"], writes=[("rt", 1)])
                    S.add("dve", lambda e, x2=x2, cosb=cosb, r=r: e.tensor_tensor(out=r[2], in0=x2, in1=cosb, op=ALU.mult),
                          reads=[pk, "cos_sb"], writes=[("rt", 2)])
                    S.add("dve", lambda e, x1=x1, sinb=sinb, r=r: e.tensor_tensor(out=r[3], in0=x1, in1=sinb, op=ALU.mult),
                          reads=[pk, "sin_sb"], writes=[("rt", 3)])
                    S.add("dve", lambda e, kb3=kb3, r=r: e.tensor_tensor(out=kb3[:, :, 0:16], in0=r[0], in1=r[1],
                                                                       op=ALU.subtract),
                          reads=[("rt", 0), ("rt", 1), ("kb", bi)], writes=[("kb", bi)])
                    S.add("dve", lambda e, kb3=kb3, r=r: e.tensor_tensor(out=kb3[:, :, 16:32], in0=r[2], in1=r[3],
                                                                       op=ALU.add),
                          reads=[("rt", 2), ("rt", 3), ("kb", bi)], writes=[("kb", bi)])
                    if s >= 1:
                        rope_tail(s - 1, d, mode, hbase, nh, ki)
                if mode != "v":
                    rope_tail(NST - 1, d, mode, hbase, nh, ki)
                if mode == "v":
                    S.add("sp", lambda e: e.dma_start(
                        out=Vd[d * NST:(d + 1) * NST, :, hbase * 128:hbase * 128 + width].rearrange("t p c -> p t c"),
                        in_=vst[vi][:, :, 0:width]),
                        reads=[("vst", vi)], dma=True)
                else:
                    dst = KTd[hbase:hbase + nh, :, d * CH:(d + 1) * CH] if mode == "k_rope" else QTd[hbase:hbase + nh, :, :]
                    S.add("sp", lambda e: e.dma_start(out=dst.rearrange("h p t -> p h t"), in_=kst[ki][:, 0:nh, :]),
                          reads=[("kst", ki)], dma=True)
                drip()

            def feat_group(d, col0, width, mode, hbase, wbuf, wkey, kc0=0):
                hTd = hT[d % 2]
                nh = width // 128
                for h0 in range(0, nh, HG):
                    h1 = min(nh, h0 + HG)
                    ki = cnt["k"] % 2
                    cnt["k"] += 1
                    for hh in range(h0, h1):
                        for n0 in range(0, CH, 512):
                            ps, pk = next_ps()
                            for kc in range(KC):
                                S.add("pe", lambda e, ps=ps, kc=kc, hh=hh, n0=n0: e.matmul(
                                    ps[:, 0:512], lhsT=wbuf[:, kc, hh * 128:(hh + 1) * 128],
                                    rhs=hTd[:, kc, n0:n0 + 512], start=(kc == 0), stop=(kc == KC - 1)),
                                    reads=[("hT", d % 2), wkey], writes=[pk])
                            if mode == "gate":
                                S.add("act", lambda e, ps=ps, hh=hh, n0=n0, ki=ki, h0=h0: e.activation(
                                    out=kst[ki][:, hh - h0, n0:n0 + 512], in_=ps[:, 0:512], func=AF.Sigmoid),
                                    reads=[pk], writes=[("kst", ki)])
                            else:
                                S.add("dve", lambda e, ps=ps, hh=hh, n0=n0, ki=ki, h0=h0: e.tensor_copy(
                                    out=kst[ki][:, hh - h0, n0:n0 + 512], in_=ps[:, 0:512]),
                                    reads=[pk], writes=[("kst", ki)])
                    if mode == "kT":
                        dst = KTd[hbase + h0:hbase + h1, :, d * CH:(d + 1) * CH]
                    elif mode == "qT":
                        dst = QTd[hbase + h0:hbase + h1, :, :]
                    else:
                        dst = sgd[hbase, kc0 + h0:kc0 + h1, :, :]
                    S.add("sp", lambda e, ki=ki, dst=dst, h0=h0, h1=h1: e.dma_start(
                        out=dst.rearrange("h p t -> p h t"), in_=kst[ki][:, 0:h1 - h0, :]),
                        reads=[("kst", ki)], dma=True)
                drip()

            def ff_group(d):
                hTd = hT[d % 2]
                for s in range(NST):
                    t = d * NST + s
                    ps, pk = next_ps()
                    for kc in range(KC):
                        S.add("pe", lambda e, ps=ps, kc=kc, s=s: e.matmul(
                            ps[:, 0:H], lhsT=hTd[:, kc, s * 128:(s + 1) * 128], rhs=wff[:, kc, :],
                            start=(kc == 0), stop=(kc == KC - 1)),
                            reads=[("hT", d % 2), "wff"], writes=[pk])
                    S.add("dve", lambda e, ps=ps, t=t: e.tensor_tensor(out=zff[:, t, :], in0=ps[:, 0:H], in1=bfg_rep[:],
                                                                     op=ALU.add),
                          reads=[pk, "bfg_rep"], writes=["zff"])
                drip()

            MARK("consts", S)
            for s in range(NST):
                hT_stageA(0, s)
                if s >= 1:
                    hT_stageB(0, s - 1)
            hT_stageB(0, NST - 1)
            tasks = []
            for d in range(NCH):
                if d + 1 < NCH:
                    tasks.append((None, None, lambda d=d: pendA.extend((d + 1, s) for s in range(NST))))
                for g0 in range(0, H, HG):
                    tasks.append((c.o_mk + g0 * 128, GW, lambda wb_, wk_, d=d, g0=g0: tok_group(
                        d, c.o_mk + g0 * 128, GW, "k_rope", g0, wb_, wk_)))
                for g0 in range(0, H, HG):
                    tasks.append((c.o_mv + g0 * 128, GW, lambda wb_, wk_, d=d, g0=g0: tok_group(
                        d, c.o_mv + g0 * 128, GW, "v", g0, wb_, wk_)))
                for g0 in range(0, H, HG):
                    tasks.append((c.o_fk + g0 * 128, GW, lambda wb_, wk_, d=d, g0=g0: feat_group(
                        d, c.o_fk + g0 * 128, GW, "kT", H + g0, wb_, wk_)))
                for g0 in range(0, H, HG):
                    tasks.append((c.o_fv + g0 * 128, GW, lambda wb_, wk_, d=d, g0=g0: tok_group(
                        d, c.o_fv + g0 * 128, GW, "v", H + g0, wb_, wk_)))
                tasks.append((None, None, lambda d=d: ff_group(d)))
                tasks.append((None, None, lambda: drip(2 * NST + 2)))
                if d == 0:
                    for g0 in range(0, H, HG):
                        tasks.append((c.o_mq + g0 * 128, GW, lambda wb_, wk_, g0=g0: tok_group(
                            0, c.o_mq + g0 * 128, GW, "q_rope", g0, wb_, wk_)))
                    for g0 in range(0, H, HG):
                        tasks.append((c.o_fq + g0 * 128, GW, lambda wb_, wk_, g0=g0: feat_group(
                            0, c.o_fq + g0 * 128, GW, "qT", H + g0, wb_, wk_)))
                    for c0 in range(0, D, GD):
                        tasks.append((c.o_ga + c0, GD, lambda wb_, wk_, c0=c0: feat_group(
                            0, c.o_ga + c0, GD, "gate", 0, wb_, wk_, kc0=c0 // 128)))
                    for c0 in range(0, D, GD):
                        tasks.append((c.o_gb + c0, GD, lambda wb_, wk_, c0=c0: feat_group(
                            0, c.o_gb + c0, GD, "gate", 1, wb_, wk_, kc0=c0 // 128)))
            PREF = NWB - 1
            wtasks = [i for i, tk_ in enumerate(tasks) if tk_[0] is not None]
            loaded = {}
            nxt = 0
            for i, (col0, width, fn) in enumerate(tasks):
                if col0 is None:
                    fn()
                    continue
                pos = wtasks.index(i)
                while nxt < len(wtasks) and nxt <= pos + PREF:
                    ti = wtasks[nxt]
                    loaded[ti] = load_w(w_in, tasks[ti][0], tasks[ti][1])
                    nxt += 1
                fn(*loaded.pop(i))
            MARK("proj_done", S)
            zf = zff[:].rearrange("p t h -> p (t h)")
            lf = logf[:].rearrange("p t h -> p (t h)")
            S.add("act", lambda e: e.activation(out=lt[:], in_=zf, func=AF.Exp, scale=-1.0), reads=["zff"], writes=["lt"])
            S.add("act", lambda e: e.activation(out=lt[:], in_=lt[:], func=AF.Ln, bias=1.0), reads=["lt"], writes=["lt"])
            S.add("dve", lambda e: e.tensor_scalar(out=lf, in0=lt[:], scalar1=-1.0, scalar2=None, op0=ALU.mult),
                  reads=["lt"], writes=["logf"])
            if debug:
                S.add("sp", lambda e: e.dma_start(out=dbg_logf, in_=lf), reads=["logf"], dma=True)
                S.add("pool", lambda e: e.dma_start(out=dbg_kmT, in_=kmT[:].rearrange("p h n -> p (h n)")),
                      reads=["kmT"], dma=True)
            S.emit("ph1")
        if upto < 2:
            return nc

        with contextlib.ExitStack() as mid:
            R1 = T(mid, "R1", [128, max(H2, KC), CH], BF16)
            R2 = T(mid, "R2", [128, max(KC, 16), CH], BF16)
            oT, h2T, mergedT = R1, R1, R2

            mid2 = contextlib.ExitStack()
            mid2.__enter__()
            Wbm = T(mid2, "Wbm", [128, H, D], BF16)
            Wbf = T(mid2, "Wbf", [128, H, D], BF16)
            with contextlib.ExitStack() as ph:
                S = Sched(nc, ss)
                KTb = [T(ph, "KTb%d" % i, [128, TCTX], BF16) for i in range(2)]
                Vb = [T(ph, "Vb%d" % i, [128, NT, 130], BF16) for i in range(2)]
                QTb = [T(ph, "QTb%d" % i, [128, CH], BF16) for i in range(2)]
                pT = [T(ph, "pT%d" % i, [128, 512], BF16) for i in range(4)]
                NSQ = TCTX // 512 + CH // 512
                sqb1 = [T(ph, "sqb%d" % i, [128, 512], BF16) for i in range(NSQ)]
                sqb = [sqb1, sqb1]
                nmx = T(ph, "nmx", [128, 4], F32)
                negC = T(ph, "negC", [128, H2], F32)
                gbias_rep = T(ph, "gbias_rep", [128, NST * NB], F32)
                vfl_rep = T(ph, "vfl_rep", [128, NCH], F32)
                Fsb = T(ph, "Fsb", [128, NT, H], F32)
                totB = T(ph, "totB", [128, NT, H], F32)
                offF = T(ph, "offF", [128, NT, H], F32)
                Tch = T(ph, "Tch", [128, NCH, H], F32)
                FrefB = T(ph, "FrefB", [128, NG, H], F32)
                biasF = T(ph, "biasF", [128, NG, NT, H], F32)
                bfh = [T(ph, "bfh%d" % i, [128, NG, NT], F32) for i in range(2)]
                gmb = T(ph, "gmb", [128, NST, NB], F32)
                m8 = T(ph, "m8", [128, NST, 8], F32)
                b1 = T(ph, "b1", [128, NST, NB], F32)
                bb = T(ph, "bb", [128, NST, NB], BF16)
                biasT = [T(ph, "biasT%d" % i, [NB, CH], BF16) for i in range(2)]
                rcp1 = T(ph, "rcp", [128, 512], F32)
                rcp = [rcp1, rcp1]
                NPS = 3
                psS = [P(ph, "psS%d" % i, [128, 512], F32) for i in range(NPS)]
                oacc_t = [[P(ph, "oacc%d_%d" % (i, k), [128, 512], F32) for k in range(2)] for i in range(2)]
                oacc = [[t[:, 0:260].rearrange("p (r c) -> p r c", c=130) for t in row] for row in oacc_t]
                psX = [P(ph, "psX%d" % i, [128, 512], F32) for i in range(1)]
                psXb = [p[:].bitcast(BF16) for p in psX]
                cn = {"x": 0, "s": 0, "p": 0, "a": 0, "sq": 0, "g": 0}

                def next_x():
                    i = cn["x"] % len(psX)
                    cn["x"] += 1
                    return i, ("psX", i)

                S.add("sp", lambda e: e.dma_start(out=gbias_rep[:], in_=gbias_d[0, :].partition_broadcast(128)),
                      writes=["gbias_rep"], dma=True)
                S.add("sp", lambda e: e.dma_start(out=vfl_rep[:], in_=vflag_d[0, :].partition_broadcast(128)),
                      writes=["vfl_rep"], dma=True)
                for i in range(2):
                    S.add("pool", lambda e, i=i: e.memset(Vb[i][:, :, 128:129], 1.0), writes=[("V", i)])

                def load_head(h, par):
                    S.add("sp", lambda e: e.dma_start(out=KTb[par][:], in_=KTd[h]), writes=[("KT", par)], dma=True)
                    S.add("sp", lambda e: e.dma_start(out=QTb[par][:], in_=QTd[h]), writes=[("QT", par)], dma=True)
                    for t0 in range(0, NT, 8):
                        S.add("sp", lambda e, t0=t0: e.dma_start(
                            out=Vb[par][:, t0:t0 + 8, 0:128],
                            in_=Vd[t0:t0 + 8, :, h * 128:(h + 1) * 128].rearrange("t p c -> p t c")),
                            writes=[("V", par)], dma=True)

                load_head(0, 0)

                lf_all = logf[:].rearrange("p t h -> p (t h)")
                S.add("pe", lambda e: e.matmul(psS[0][:, 0:NT * H], lhsT=tri_f[:], rhs=lf_all, start=True, stop=True),
                      reads=["logf", "tri_f"], writes=[("psS", 0)])
                S.add("pe", lambda e: e.matmul(psS[1][:, 0:NT * H], lhsT=ones_f[:], rhs=lf_all, start=True, stop=True),
                      reads=["logf", "ones_f"], writes=[("psS", 1)])
                S.add("dve", lambda e: e.tensor_copy(out=Fsb[:].rearrange("p t h -> p (t h)"), in_=psS[0][:, 0:NT * H]),
                      reads=[("psS", 0)], writes=["Fsb"])
                S.add("dve", lambda e: e.tensor_copy(out=totB[:].rearrange("p t h -> p (t h)"), in_=psS[1][:, 0:NT * H]),
                      reads=[("psS", 1)], writes=["totB"])
                for d in range(NCH):
                    S.add("dve", lambda e, d=d: e.tensor_reduce(
                        out=Tch[:, d, :], in_=totB[:, d * NST:(d + 1) * NST, :].rearrange("p j h -> p h j"),
                        axis=AX.X, op=ALU.add), reads=["totB"], writes=["Tch"])
                S.add("dve", lambda e: e.memset(offF[:, 0, :], 0.0), writes=["offF"])
                for d in range(1, NCH):
                    prev_t = 0 if d == 1 else (d - 1) * NST
                    S.add("dve", lambda e, d=d, prev_t=prev_t: e.tensor_tensor(
                        out=offF[:, d * NST, :], in0=offF[:, prev_t, :], in1=Tch[:, d, :], op=ALU.subtract),
                        reads=["offF", "Tch"], writes=["offF"])
                for d in range(NCH):
                    for j in range(1, NST):
                        t = d * NST + j
                        S.add("dve", lambda e, t=t: e.tensor_tensor(
                            out=offF[:, t, :], in0=offF[:, t - 1, :], in1=totB[:, t - 1, :], op=ALU.add),
                            reads=["offF", "totB"], writes=["offF"])
                S.add("dve", lambda e: e.tensor_tensor(out=Fsb[:], in0=Fsb[:], in1=offF[:], op=ALU.add),
                      reads=["Fsb", "offF"], writes=["Fsb"])
                for g in range(NG):
                    S.add("dve", lambda e, g=g: e.tensor_copy(out=FrefB[:, g, :], in_=offF[:, 4 * g + 2, :]),
                          reads=["offF"], writes=["FrefB"])
                for g in range(NG):
                    S.add("dve", lambda e, g=g: e.tensor_tensor(
                        out=biasF[:, g], in0=FrefB[:, g:g + 1, :].broadcast_to([128, NT, H]), in1=Fsb[:],
                        op=ALU.subtract), reads=["FrefB", "Fsb"], writes=["biasF"])
                    S.add("dve", lambda e, g=g: e.tensor_tensor(
                        out=biasF[:, g].rearrange("p (d j) h -> p d (j h)", d=NCH),
                        in0=biasF[:, g].rearrange("p (d j) h -> p d (j h)", d=NCH),
                        in1=vfl_rep[:].unsqueeze(2).broadcast_to([128, NCH, NST * H]), op=ALU.add),
                        reads=["biasF", "vfl_rep"], writes=["biasF"])

                def head_sq(par, h):
                    tiles = [(KTb[par], ("KT", par), n0) for n0 in range(0, TCTX, 512)]
                    tiles += [(QTb[par], ("QT", par), n0) for n0 in range(0, CH, 512)]
                    for i, (buf, bkey, n0) in enumerate(tiles):
                        S.add("pool", lambda e, buf=buf, n0=n0, i=i: e.tensor_tensor(
                            out=sqb[par][i][:], in0=buf[:, n0:n0 + 512], in1=buf[:, n0:n0 + 512], op=ALU.mult),
                            reads=[bkey], writes=[("sqb", i)])

                def prep_stages(h):
                    par = h % 2
                    st = []

                    def s1():
                        xi, xk = next_x()
                        ncol = NSQ * 4
                        for i in range(NSQ):
                            for cq in range(4):
                                S.add("pe", lambda e, i=i, cq=cq: e.matmul(
                                    psX[xi][:, i * 4 + cq:i * 4 + cq + 1], lhsT=sqb[par][i][:, cq * 128:(cq + 1) * 128],
                                    rhs=ones_b[:, 0:1], start=True, stop=True),
                                    reads=[("sqb", i), "ones_b"], writes=[xk])
                        S.add("dve", lambda e: e.tensor_reduce(out=nmx[:, 0:1], in_=psX[xi][:, 0:ncol], axis=AX.X,
                                                               op=ALU.max), reads=[xk], writes=["nmx"])

                    def s2():
                        xi2, xk2 = next_x()
                        S.add("pe", lambda e: e.transpose(out=psX[xi2][0:1, 0:128], in_=nmx[:, 0:1], identity=ident_f[:]),
                              reads=["nmx", "ident_f"], writes=[xk2])
                        S.add("dve", lambda e: e.tensor_reduce(out=nmx[0:1, 1:2], in_=psX[xi2][0:1, 0:128], axis=AX.X,
                                                               op=ALU.max), reads=[xk2], writes=["nmx1"])

                    def s3():
                        xi3, xk3 = next_x()
                        S.add("pe", lambda e: e.matmul(psX[xi3][:, 0:1], lhsT=ones_f[0:1, :], rhs=nmx[0:1, 1:2],
                                                       start=True, stop=True), reads=["nmx1", "ones_f"], writes=[xk3])
                        S.add("dve", lambda e: e.tensor_scalar(out=negC[:, h:h + 1], in0=psX[xi3][:, 0:1],
                                                               scalar1=-1.02 * scale, scalar2=None, op0=ALU.mult),
                              reads=[xk3], writes=[("negC", h)])
                        if h >= H:
                            S.add("dve", lambda e: e.tensor_scalar(
                                out=bfh[par][:], in0=biasF[:, :, :, h - H], scalar1=negC[:, h:h + 1], scalar2=None,
                                op0=ALU.add), reads=["biasF", ("negC", h)], writes=[("bfh", par)])

                    st += [s1, s2, s3]
                    if h < H:
                        hm = h

                        def s4():
                            xi, xk = next_x()
                            psg = psX[xi][:, 0:NST * NB].rearrange("p (q n) -> p q n", n=NB)
                            for qt in range(NST):
                                S.add("pe", lambda e, qt=qt: e.matmul(
                                    psg[:, qt, :], lhsT=QTb[par][:, qt * 128:(qt + 1) * 128], rhs=kmT[:, hm, :],
                                    start=True, stop=True), reads=[("QT", par), "kmT"], writes=[xk])
                            g3 = gbias_rep[:].rearrange("p (q n) -> p q n", n=NB)
                            S.add("dve", lambda e: e.tensor_tensor(out=gmb[:], in0=psg, in1=g3, op=ALU.add),
                                  reads=[xk, "gbias_rep"], writes=["gmb"])
                            for qt in range(NST):
                                S.add("dve", lambda e, qt=qt: e.max(out=m8[:, qt, :], in_=gmb[:, qt, :]),
                                      reads=["gmb"], writes=["m8"])
                            for qt in range(NST):
                                S.add("dve", lambda e, qt=qt: e.tensor_scalar(
                                    out=b1[:, qt, :], in0=gmb[:, qt, :], scalar1=m8[:, qt, c.TOPK - 1:c.TOPK],
                                    scalar2=-NEG_MASK, op0=ALU.is_ge, op1=ALU.mult), reads=["gmb", "m8"], writes=["b1"])
                            S.add("dve", lambda e: e.scalar_tensor_tensor(
                                out=bb[:].rearrange("p q n -> p (q n)"), in0=b1[:].rearrange("p q n -> p (q n)"),
                                scalar=NEG_MASK, in1=gbias_rep[:], op0=ALU.add, op1=ALU.min),
                                reads=["b1", "gbias_rep"], writes=["bb"])
                            for qt in range(NST):
                                S.add("dve", lambda e, qt=qt: e.memset(bb[:, qt, qt // 2:qt // 2 + 1], 0.0),
                                      reads=["bb"], writes=["bb"])

                        def s5():
                            xi2, xk2 = next_x()
                            for qt in range(NST):
                                S.add("pe", lambda e, qt=qt: e.transpose(out=psXb[xi2][0:NB, qt * 128:(qt + 1) * 128],
                                                                         in_=bb[:, qt, :], identity=ident_b[:]),
                                      reads=["bb", "ident_b"], writes=[xk2])
                            S.add("dve", lambda e: e.tensor_copy(out=biasT[par][0:NB, 0:NST * 128],
                                                                 in_=psXb[xi2][0:NB, 0:NST * 128]),
                                  reads=[xk2], writes=[("biasT", par)])
                        st += [s4, s5]
                    return st

                staged = []

                def tick():
                    for it in staged:
                        it[0] -= 1
                    while staged and staged[0][0] <= 0:
                        staged.pop(0)[1]()

                def flush_staged():
                    while staged:
                        staged.pop(0)[1]()

                deferred = []

                def flush_deferred():
                    while deferred:
                        deferred.pop(0)()

                def attention(par, h, moba, mid_hook=None):
                    for g in range(NG):
                        a = cn["a"] % 2
                        cn["a"] += 1
                        tiles = [(d, j) for d in range(NCH - 1, 0, -1) for j in range(NST)]
                        tiles += [(0, j) for j in range(4 * g + 4)]

                        def qk_step(idx, d, j, g=g, a=a):
                            t = d * NST + j
                            c0 = 0 if (d > 0 or j < 4 * g) else (j - 4 * g) * 128
                            N = 512 - c0
                            q0 = g * 512 + c0
                            si = cn["s"] % NPS
                            cn["s"] += 1
                            ps, pk = psS[si], ("psS", si)
                            S.add("pe", lambda e: e.matmul(
                                ps[:, 0:N], lhsT=KTb[par][:, t * 128:(t + 1) * 128], rhs=QTb[par][:, q0:q0 + N],
                                start=True, stop=not moba), reads=[("KT", par), ("QT", par)], writes=[pk])
                            if moba:
                                n = t // 2
                                S.add("pe", lambda e: e.matmul(
                                    ps[:, 0:N], lhsT=E_b[0:NB, n * 128:(n + 1) * 128], rhs=biasT[par][0:NB, q0:q0 + N],
                                    start=False, stop=True), reads=["E_b", ("biasT", par)], writes=[pk])
                            pi = cn["p"] % 4
                            cn["p"] += 1
                            pt, ptk = pT[pi], ("pT", pi)
                            if moba:
                                bias_ap, bkey = negC[:, h:h + 1], ("negC", h)
                            else:
                                bias_ap, bkey = bfh[par][:, g, t:t + 1], ("bfh", par)
                            S.add("act", lambda e: e.activation(
                                out=pt[:, 0:N], in_=ps[:, 0:N], func=AF.Exp, bias=bias_ap, scale=scale),
                                reads=[pk, bkey], writes=[ptk])
                            if d == 0 and j >= 4 * g:
                                S.add("pool", lambda e: e.tensor_tensor(out=pt[:, 0:128], in0=pt[:, 0:128],
                                                                        in1=tri_b[:], op=ALU.mult),
                                      reads=[ptk, "tri_b"], writes=[ptk])
                            return (idx, d, j, t, c0, pt, ptk)

                        def pv_step(st, g=g, a=a, ntiles=len(tiles)):
                            idx, d, j, t, c0, pt, ptk = st
                            N = 512 - c0
                            first, last = (idx == 0), (idx == ntiles - 1)
                            S.add("pe", lambda e: e.matmul(
                                oacc_t[a][0][:, c0:512], lhsT=Vb[par][:, t, 0:128], rhs=pt[:, 0:N],
                                start=first, stop=last), reads=[ptk, ("V", par)], writes=[("oacc", a, 0)])
                            S.add("pe", lambda e: e.matmul(
                                oacc_t[a][1][:, c0:512], lhsT=ones_b[:], rhs=pt[:, 0:N],
                                start=first, stop=last), reads=[ptk, "ones_b"], writes=[("oacc", a, 1)])

                        pend = []
                        for idx, (d, j) in enumerate(tiles):
                            pend.append(qk_step(idx, d, j))
                            if len(pend) > 2:
                                pv_step(pend.pop(0))
                            tick()
                        while pend:
                            pv_step(pend.pop(0))
                        S.add("dve", lambda e, a=a: e.reciprocal(out=rcp[a][:], in_=oacc_t[a][1][:, 0:512]),
                              reads=[("oacc", a, 1)], writes=["rcp"])
                        S.add("dve", lambda e, a=a, g=g: e.tensor_tensor(
                            out=oT[:, h, g * 512:(g + 1) * 512], in0=oacc_t[a][0][:, 0:512], in1=rcp[a][:], op=ALU.mult),
                            reads=[("oacc", a, 0), "rcp"], writes=["oT"])
                        if g == 0 and mid_hook is not None:
                            mid_hook()
                    flush_staged()

                def prep(h, spread=4):
                    for k, fn in enumerate(prep_stages(h)):
                        staged.append([1 + k * spread, fn])

                head_sq(0, 0)
                prep(0)
                flush_staged()
                for h in range(H2):
                    par = h % 2

                    def hook(h=h):
                        if h + 1 < H2:
                            prep(h + 1)
                        if h == H2 - 3:
                            for h0 in range(0, H, 4):
                                h1 = min(H, h0 + 4)
                                for (wsb, wdr, nm) in ((Wbm, w_bm, "Wbm"), (Wbf, w_bf, "Wbf")):
                                    S.add("pool", lambda e, wsb=wsb, wdr=wdr, h0=h0, h1=h1: e.dma_start(
                                        out=wsb[:, h0:h1, :],
                                        in_=wdr[h0 * 128:h1 * 128, :].rearrange("(h p) c -> p h c", p=128)),
                                        writes=[nm], dma=True)
                    if h + 1 < H2:
                        load_head(h + 1, (h + 1) % 2)
                        head_sq((h + 1) % 2, h + 1)
                    attention(par, h, h < H, mid_hook=hook)
                flush_deferred()
                if debug:
                    S.add("pool", lambda e: e.dma_start(out=dbg_oT, in_=oT[:, 0:H2, :].rearrange("p h t -> p (h t)")),
                          reads=["oT"], dma=True)
                S.emit("ph2")
            if upto < 3:
                return nc

            with contextlib.ExitStack() as ph:
                S = Sched(nc, ss)
                sga = [T(ph, "sga%d" % i, [128, CH], BF16) for i in range(2)]
                sgb = [T(ph, "sgb%d" % i, [128, CH], BF16) for i in range(2)]
                t1 = [T(ph, "t1_%d" % i, [128, 512], F32) for i in range(2)]
                t2 = [T(ph, "t2_%d" % i, [128, 512], F32) for i in range(2)]
                psm = [[P(ph, "psm%d_%d" % (i, n), [128, 512], F32) for n in range(NN)] for i in range(2)]
                psf = [[P(ph, "psf%d_%d" % (i, n), [128, 512], F32) for n in range(NN)] for i in range(2)]
                tc_ = 0
                for cc in range(KC):
                    a = cc % 2
                    S.add("sp", lambda e, a=a, cc=cc: e.dma_start(out=sga[a][:], in_=sgd[0, cc]), writes=[("sga", a)], dma=True)
                    S.add("sp", lambda e, a=a, cc=cc: e.dma_start(out=sgb[a][:], in_=sgd[1, cc]), writes=[("sgb", a)], dma=True)
                    for n in range(NN):
                        for hh in range(H):
                            S.add("pe", lambda e, a=a, n=n, hh=hh, cc=cc: e.matmul(
                                psm[a][n][:, 0:512], lhsT=Wbm[:, hh, cc * 128:(cc + 1) * 128],
                                rhs=oT[:, hh, n * 512:(n + 1) * 512], start=(hh == 0), stop=(hh == H - 1)),
                                reads=["Wbm", "oT"], writes=[("psm", a, n)])
                        for hh in range(H):
                            S.add("pe", lambda e, a=a, n=n, hh=hh, cc=cc: e.matmul(
                                psf[a][n][:, 0:512], lhsT=Wbf[:, hh, cc * 128:(cc + 1) * 128],
                                rhs=oT[:, H + hh, n * 512:(n + 1) * 512], start=(hh == 0), stop=(hh == H - 1)),
                                reads=["Wbf", "oT"], writes=[("psf", a, n)])
                        k = tc_ % 2
                        tc_ += 1
                        S.add("dve", lambda e, a=a, n=n, k=k: e.tensor_tensor(
                            out=t1[k][:], in0=psm[a][n][:, 0:512], in1=sga[a][:, n * 512:(n + 1) * 512], op=ALU.mult),
                            reads=[("psm", a, n), ("sga", a)], writes=[("t1", k)])
                        S.add("dve", lambda e, a=a, n=n, k=k: e.tensor_tensor(
                            out=t2[k][:], in0=psf[a][n][:, 0:512], in1=sgb[a][:, n * 512:(n + 1) * 512], op=ALU.mult),
                            reads=[("psf", a, n), ("sgb", a)], writes=[("t2", k)])
                        S.add("pool", lambda e, cc=cc, n=n, k=k: e.tensor_tensor(
                            out=mergedT[:, cc, n * 512:(n + 1) * 512], in0=t1[k][:], in1=t2[k][:], op=ALU.add),
                            reads=[("t1", k), ("t2", k)], writes=["mergedT"])
                if debug:
                    S.add("pool", lambda e: e.dma_start(out=dbg_mg, in_=mergedT[:, 0:KC, :].rearrange("p h t -> p (h t)")),
                          reads=["mergedT"], dma=True)
                S.emit("ph3a")
            mid2.close()

            with contextlib.ExitStack() as ph:
                S = Sched(nc, ss)
                Wo = T(ph, "Wo", [128, KC, D], BF16)
                gpost = T(ph, "gpost", [128, D], F32)
                gpre2 = T(ph, "gpre2", [128, D], F32)
                xs2 = [T(ph, "xs2_%d" % i, [128, D], F32) for i in range(2)]
                tt_ = T(ph, "tt_", [128, D], F32)
                hb2 = [T(ph, "hb2_%d" % i, [128, D], BF16) for i in range(2)]
                st3 = T(ph, "st3", [128, NST, 16], F32)
                ncg = D // GD
                psM = [P(ph, "psM%d" % i, [128, 512], F32) for i in range(6)]
                pst2 = [P(ph, "pst2_%d" % i, [128, 8, 128], BF16) for i in range(2)]
                S.add("sp", lambda e: e.dma_start(out=gpost[:], in_=gvec[1, :].partition_broadcast(128)),
                      writes=["gpost"], dma=True)
                S.add("sp", lambda e: e.dma_start(out=gpre2[:], in_=gvec[2, :].partition_broadcast(128)),
                      writes=["gpre2"], dma=True)
                for c0 in range(0, D, GD):
                    for k0 in range(0, KC, 8):
                        k1 = min(KC, k0 + 8)
                        S.add("pool", lambda e, c0=c0, k0=k0, k1=k1: e.dma_start(
                            out=Wo[:, k0:k1, c0:c0 + GD],
                            in_=w_out[k0 * 128:k1 * 128, c0:c0 + GD].rearrange("(kc p) c -> p kc c", p=128)),
                            writes=[("Wo", c0)], dma=True)
                tkc = [0]

                def h2_tail(s):
                    xi = s % 2
                    nq = min(8, KC)
                    for q0 in range(0, KC, nq):
                        pi2 = tkc[0] % 2
                        tkc[0] += 1
                        for kk in range(nq):
                            S.add("pe", lambda e, pi2=pi2, kk=kk, q0=q0: e.transpose(
                                out=pst2[pi2][:, kk, :], in_=hb2[xi][:, (q0 + kk) * 128:(q0 + kk + 1) * 128],
                                identity=ident_b[:]), reads=[("hb2", xi), "ident_b"], writes=[("pst2", pi2)])
                        S.add("act", lambda e, pi2=pi2, q0=q0: e.copy(
                            out=h2T[:, q0:q0 + nq, s * 128:(s + 1) * 128], in_=pst2[pi2][:, 0:nq, :]),
                            reads=[("pst2", pi2)], writes=["h2T"])

                mc = 0
                tk = 0
                for s in range(NST):
                    xi = s % 2
                    S.add("pool", lambda e, s=s, xi=xi: e.dma_start(out=xs2[xi][:], in_=xctx[s * 128:(s + 1) * 128, :]),
                          writes=[("xs2", xi)], dma=True)
                    pss = []
                    for cg in range(ncg):
                        pi = mc % 6
                        mc += 1
                        pss.append(pi)
                        for kc in range(KC):
                            S.add("pe", lambda e, pi=pi, kc=kc, s=s, cg=cg: e.matmul(
                                psM[pi][:, 0:GD], lhsT=mergedT[:, kc, s * 128:(s + 1) * 128],
                                rhs=Wo[:, kc, cg * GD:(cg + 1) * GD], start=(kc == 0), stop=(kc == KC - 1)),
                                reads=["mergedT", ("Wo", cg * GD)], writes=[("psM", pi)])
                        S.add("act", lambda e, pi=pi, s=s, cg=cg: e.activation(
                            out=junk[:, 0:GD], in_=psM[pi][:, 0:GD], func=AF.Square, accum_out=st3[:, s, cg:cg + 1]),
                            reads=[("psM", pi)], writes=["junk", ("st3", s)])
                    S.add("dve", lambda e, s=s: e.tensor_reduce(out=st3[:, s, 8:9], in_=st3[:, s, 0:ncg], axis=AX.X,
                                                                op=ALU.add), reads=[("st3", s)], writes=[("st3", s)])
                    S.add("act", lambda e, s=s: e.activation(out=st3[:, s, 9:10], in_=st3[:, s, 8:9], func=AF.Sqrt,
                                                             bias=epsc[:], scale=1.0 / D),
                          reads=[("st3", s), "epsc"], writes=[("st3", s)])
                    S.add("dve", lambda e, s=s: e.reciprocal(out=st3[:, s, 10:11], in_=st3[:, s, 9:10]),
                          reads=[("st3", s)], writes=[("st3", s)])
                    for cg in range(ncg):
                        pi = pss[cg]
                        S.add("dve", lambda e, pi=pi, s=s, cg=cg: e.scalar_tensor_tensor(
                            out=tt_[:, cg * GD:(cg + 1) * GD], in0=psM[pi][:, 0:GD], scalar=st3[:, s, 10:11],
                            in1=gpost[:, cg * GD:(cg + 1) * GD], op0=ALU.mult, op1=ALU.mult),
                            reads=[("psM", pi), ("st3", s), "gpost"], writes=["tt_"])
                    S.add("dve", lambda e, xi=xi: e.tensor_tensor(out=xs2[xi][:], in0=tt_[:], in1=xs2[xi][:], op=ALU.add),
                          reads=["tt_", ("xs2", xi)], writes=[("xs2", xi)])
                    S.add("sp", lambda e, s=s, xi=xi: e.dma_start(out=x1d[s * 128:(s + 1) * 128, :], in_=xs2[xi][:]),
                          reads=[("xs2", xi)], dma=True)
                    S.add("dve", lambda e, s=s, xi=xi: e.scalar_tensor_tensor(
                        out=hb2[xi][:], in0=xs2[xi][:], scalar=1.0, in1=xs2[xi][:], op0=ALU.mult, op1=ALU.mult,
                        accum_out=st3[:, s, 11:12]), reads=[("xs2", xi)], writes=[("hb2", xi), ("st3", s)])
                    S.add("act", lambda e, s=s: e.activation(out=st3[:, s, 12:13], in_=st3[:, s, 11:12], func=AF.Sqrt,
                                                             bias=epsc[:], scale=1.0 / D),
                          reads=[("st3", s), "epsc"], writes=[("st3", s)])
                    S.add("dve", lambda e, s=s: e.reciprocal(out=st3[:, s, 13:14], in_=st3[:, s, 12:13]),
                          reads=[("st3", s)], writes=[("st3", s)])
                    S.add("dve", lambda e, s=s, xi=xi: e.scalar_tensor_tensor(
                        out=hb2[xi][:], in0=xs2[xi][:], scalar=st3[:, s, 13:14], in1=gpre2[:], op0=ALU.mult,
                        op1=ALU.mult), reads=[("xs2", xi), ("st3", s), "gpre2"], writes=[("hb2", xi)])
                    if s >= 1:
                        h2_tail(s - 1)
                h2_tail(NST - 1)
                S.emit("ph3b")
            if upto < 4:
                return nc

            with contextlib.ExitStack() as ph:
                S = Sched(nc, ss)
                FB, UW = c.FB, c.UW
                FK = FB // 128
                Wu = [T(ph, "Wu%d" % i, [128, KC, UW], BF16) for i in range(2)]
                Wd = [T(ph, "Wd%d" % i, [128, FK, GD], BF16) for i in range(2)]
                m = T(ph, "m", [128, NST, D], F32)
                rl = [T(ph, "rl%d" % i, [128, 512], F32) for i in range(2)]
                gpost2 = T(ph, "gpost2", [128, D], F32)
                xs3 = [T(ph, "xs3_%d" % i, [128, D], F32) for i in range(2)]
                st4 = T(ph, "st4", [128, NST, 4], F32)
                psU = [P(ph, "psU%d" % i, [128, 512], F32) for i in range(3)]
                psD = [P(ph, "psD%d" % i, [128, 512], F32) for i in range(3)]
                aTv = lambda par, k: R2[:, par * FK + k, :]
                ncg = D // GD
                S.add("sp", lambda e: e.dma_start(out=gpost2[:], in_=gvec[3, :].partition_broadcast(128)),
                      writes=["gpost2"], dma=True)
                cn4 = {"uc": 0, "dc": 0, "wu": 0, "wd": 0, "rk": 0}

                def load_u(fb, u0):
                    col0 = fb * FB + u0
                    wi = cn4["wu"] % 2
                    cn4["wu"] += 1
                    for k0 in range(0, KC, 8):
                        k1 = min(KC, k0 + 8)
                        S.add("pool", lambda e, k0=k0, k1=k1: e.dma_start(
                            out=Wu[wi][:, k0:k1, :],
                            in_=w_up[k0 * 128:k1 * 128, col0:col0 + UW].rearrange("(kc p) c -> p kc c", p=128)),
                            writes=[("Wu", wi)], dma=True)
                    return wi

                def comp_u(fb, u0, wi):
                    par = fb % 2
                    for f in range(UW // 128):
                        fcl = (u0 + f * 128) // 128
                        for n in range(NN):
                            pi = cn4["uc"] % 3
                            cn4["uc"] += 1
                            for kc in range(KC):
                                S.add("pe", lambda e, pi=pi, kc=kc, f=f, n=n: e.matmul(
                                    psU[pi][:, 0:512], lhsT=Wu[wi][:, kc, f * 128:(f + 1) * 128],
                                    rhs=h2T[:, kc, n * 512:(n + 1) * 512], start=(kc == 0), stop=(kc == KC - 1)),
                                    reads=[("Wu", wi), "h2T"], writes=[("psU", pi)])
                            k = cn4["rk"] % 2
                            cn4["rk"] += 1
                            S.add("act", lambda e, pi=pi, k=k: e.activation(out=rl[k][:], in_=psU[pi][:, 0:512],
                                                                            func=AF.Relu),
                                  reads=[("psU", pi)], writes=[("rl", k)])
                            S.add("pool", lambda e, k=k, fcl=fcl, n=n: e.tensor_tensor(
                                out=aTv(par, fcl)[:, n * 512:(n + 1) * 512], in0=rl[k][:], in1=rl[k][:], op=ALU.mult),
                                reads=[("rl", k)], writes=[("aT", par)])

                def load_d(fb, cg):
                    wi = cn4["wd"] % 2
                    cn4["wd"] += 1
                    S.add("pool", lambda e: e.dma_start(
                        out=Wd[wi][:],
                        in_=w_down[fb * FB:(fb + 1) * FB, cg * GD:(cg + 1) * GD].rearrange("(kc p) c -> p kc c", p=128)),
                        writes=[("Wd", wi)], dma=True)
                    return wi

                def comp_d(fb, cg, wi):
                    par = fb % 2
                    for s in range(NST):
                        pi = cn4["dc"] % 3
                        cn4["dc"] += 1
                        for kc in range(FK):
                            S.add("pe", lambda e, pi=pi, kc=kc, s=s: e.matmul(
                                psD[pi][:, 0:GD], lhsT=aTv(par, kc)[:, s * 128:(s + 1) * 128], rhs=Wd[wi][:, kc, :],
                                start=(kc == 0), stop=(kc == FK - 1)),
                                reads=[("aT", par), ("Wd", wi)], writes=[("psD", pi)])
                        msl = m[:, s, cg * GD:(cg + 1) * GD]
                        if fb == 0:
                            S.add("act", lambda e, pi=pi, msl=msl: e.copy(out=msl, in_=psD[pi][:, 0:GD]),
                                  reads=[("psD", pi)], writes=[("m", s, cg)])
                        else:
                            S.add("dve", lambda e, pi=pi, msl=msl: e.tensor_tensor(out=msl, in0=psD[pi][:, 0:GD],
                                                                                 in1=msl, op=ALU.add),
                                  reads=[("psD", pi), ("m", s, cg)], writes=[("m", s, cg)])

                tasks4 = []
                for fb in range(c.NFB):
                    for u0 in range(0, FB, UW):
                        tasks4.append((load_u, comp_u, fb, u0))
                    for cg in range(ncg):
                        tasks4.append((load_d, comp_d, fb, cg))
                nxt_w = tasks4[0][0](tasks4[0][2], tasks4[0][3])
                for i, (lf_, cf_, a1, a2) in enumerate(tasks4):
                    cur_w = nxt_w
                    if i + 1 < len(tasks4):
                        t2 = tasks4[i + 1]
                        nxt_w = t2[0](t2[2], t2[3])
                    cf_(a1, a2, cur_w)
                for s in range(NST):
                    xi = s % 2
                    mk = [("m", s, cg) for cg in range(ncg)]
                    S.add("pool", lambda e, s=s, xi=xi: e.dma_start(out=xs3[xi][:], in_=x1d[s * 128:(s + 1) * 128, :]),
                          writes=[("xs3", xi)], dma=True)
                    S.add("dve", lambda e, s=s: e.scalar_tensor_tensor(
                        out=junk[:], in0=m[:, s, :], scalar=1.0, in1=m[:, s, :], op0=ALU.mult, op1=ALU.mult,
                        accum_out=st4[:, s, 0:1]), reads=mk, writes=["junk", ("st4", s)])
                    S.add("act", lambda e, s=s: e.activation(out=st4[:, s, 1:2], in_=st4[:, s, 0:1], func=AF.Sqrt,
                                                             bias=epsc[:], scale=1.0 / D),
                          reads=[("st4", s), "epsc"], writes=[("st4", s)])
                    S.add("dve", lambda e, s=s: e.reciprocal(out=st4[:, s, 2:3], in_=st4[:, s, 1:2]),
                          reads=[("st4", s)], writes=[("st4", s)])
                    S.add("dve", lambda e, s=s: e.scalar_tensor_tensor(
                        out=m[:, s, :], in0=m[:, s, :], scalar=st4[:, s, 2:3], in1=gpost2[:], op0=ALU.mult,
                        op1=ALU.mult), reads=mk + [("st4", s), "gpost2"], writes=mk)
                    S.add("dve", lambda e, s=s, xi=xi: e.tensor_tensor(out=xs3[xi][:], in0=m[:, s, :], in1=xs3[xi][:],
                                                                     op=ALU.add),
                          reads=mk + [("xs3", xi)], writes=[("xs3", xi)])
                    S.add("sp", lambda e, s=s, xi=xi: e.dma_start(out=out[s * 128:(s + 1) * 128, :], in_=xs3[xi][:]),
                          reads=[("xs3", xi)], dma=True)
                S.emit("ph4")
    return nc


def make_core_inputs(inp, core, cfg=FULL):
    c = cfg
    b, j = divmod(core, c.NCH)
    x = np.asarray(inp["x"])[b]
    CH, NCH = c.CH, c.NCH
    order = [(j - d) % NCH for d in range(NCH)]
    xctx = np.ascontiguousarray(np.concatenate([x[o * CH:(o + 1) * CH] for o in order], 0), dtype=np.float32)
    pos = np.concatenate([np.arange(o * CH, (o + 1) * CH) for o in order]).astype(np.float32)
    inv_freq = (np.float32(ROPE_THETA) ** (-np.arange(0, 32, 2, dtype=np.float32) / np.float32(32))).astype(np.float32)
    ang = (pos[:, None] * inv_freq[None, :]).astype(np.float32)
    valid = [j - d >= 0 for d in range(NCH)]
    vflag = np.array([[0.0 if v else NEG_MASK for v in valid]], np.float32)
    bpc = CH // 256
    gb = np.full((c.NST, c.NB), -1e30, np.float32)
    for qt in range(c.NST):
        bq = qt // 2
        for n in range(c.NB):
            d, i = divmod(n, bpc)
            if (d == 0 and i < bq) or (d >= 1 and valid[d]):
                gb[qt, n] = 0.0
    k = np.arange(128)
    cE = np.zeros((c.NB, c.NB * 128), np.float32)
    for n in range(c.NB):
        cE[n, n * 128:(n + 1) * 128] = 1.0
    sq = lambda a: np.ascontiguousarray(np.asarray(a)[0], dtype=np.float32)
    return {
        "xctx": xctx,
        "w_in": sq(inp["w_in"]), "w_bm": sq(inp["w_branch_moba"]), "w_bf": sq(inp["w_branch_fox"]),
        "w_out": sq(inp["w_out"]), "w_up": sq(inp["w_up"]), "w_down": sq(inp["w_down"]),
        "gvec": np.stack([sq(inp["g_mix_pre"]), sq(inp["g_mix_post"]), sq(inp["g_mlp_pre"]), sq(inp["g_mlp_post"])], 0),
        "bfg": np.asarray(inp["b_forget"], np.float32).reshape(1, c.H),
        "rope_cos": np.cos(ang).astype(np.float32), "rope_sin": np.sin(ang).astype(np.float32),
        "gbias": gb.reshape(1, -1), "vflag": vflag,
        "cident": np.eye(128, dtype=np.float32),
        "ctri": (k[:, None] <= k[None, :]).astype(np.float32),
        "cE": cE,
    }


_NC_CACHE = {}


def kernel(**inputs):
    cfg = FULL
    if "nc" not in _NC_CACHE:
        _NC_CACHE["nc"] = build(cfg)
    nc = _NC_CACHE["nc"]
    n_cores = 2 * cfg.NCH
    shared = None
    in_maps = []
    for core in range(n_cores):
        m = make_core_inputs(inputs, core, cfg)
        if shared is None:
            shared = m
        else:
            for key in m:
                if key not in ("xctx", "rope_cos", "rope_sin", "gbias", "vflag"):
                    m[key] = shared[key]
        in_maps.append(m)
    res = run_bass_kernel_spmd(nc, in_maps, core_ids=list(range(n_cores)))
    B = n_cores // cfg.NCH
    outp = np.empty((B, cfg.TCTX, cfg.D), np.float32)
    for core in range(n_cores):
        b, j = divmod(core, cfg.NCH)
        outp[b, j * cfg.CH:(j + 1) * cfg.CH] = np.asarray(res.results[core]["out"], np.float32)
    return outp
```

```python
import contextlib
import numpy as np
import concourse.bass as bass
import concourse.mybir as mybir
from concourse.bass_utils import run_bass_kernel_spmd

F32 = mybir.dt.float32
BF16 = mybir.dt.bfloat16
AF = mybir.ActivationFunctionType
ALU = mybir.AluOpType
AX = mybir.AxisListType

ENGS = ("pe", "act", "dve", "pool", "sp")
N_DMA_SEMS = 24
N_HW_SEMS = 16
OP_LIMIT = None
RMS_EPS = 1e-6
ROPE_THETA = 500000.0
NEG_MASK = -30000.0


class _Op:
    __slots__ = ("eng", "fn", "is_dma", "signal", "sigval", "dsem", "dval", "deps", "waits")


class SemState:
    def __init__(self, nc, stack):
        self.sems = {}
        for e in ("pe", "act", "dve", "pool"):
            self.sems[("eng", e)] = stack.enter_context(nc.semaphore("s_" + e))
        for i in range(N_DMA_SEMS):
            self.sems[("dma", i)] = stack.enter_context(nc.semaphore("s_dma%d" % i))
        self.val = {k: 0 for k in self.sems}
        self.dma_count = {"hw": 0, "sw": 0}


def _is_psum(k):
    n = k[0] if isinstance(k, tuple) else k
    return isinstance(n, str) and (n.startswith("ps") or n == "oacc")


class Sched:
    def __init__(self, nc, ss):
        self.nc = nc
        self.ss = ss
        self.ops = []
        self.streams = {e: [] for e in ENGS}
        self.last_w = {}
        self.readers = {}
        self.dma_last = [None] * N_DMA_SEMS
        self.dma_val = [ss.val[("dma", i)] for i in range(N_DMA_SEMS)]

    def add(self, eng, fn, reads=(), writes=(), dma=False):
        if OP_LIMIT is not None and len(self.ops) >= OP_LIMIT:
            return None
        pr = [k for k in reads if _is_psum(k)]
        if pr:
            reads = [k for k in reads if not _is_psum(k)]
            writes = list(writes) + [k for k in pr if k not in writes]
        op = _Op()
        op.eng, op.fn, op.is_dma = eng, fn, dma
        op.signal, op.sigval, op.dsem, op.dval, op.waits = False, None, None, None, None
        deps = []
        for k in reads:
            w = self.last_w.get(k)
            if w is not None:
                deps.append(w)
        for k in writes:
            w = self.last_w.get(k)
            if w is not None:
                deps.append(w)
            rd = self.readers.get(k)
            if rd:
                deps.extend(rd.values())
        if dma:
            if eng == "pool":
                s = N_HW_SEMS + self.ss.dma_count["sw"] % (N_DMA_SEMS - N_HW_SEMS)
                self.ss.dma_count["sw"] += 1
            else:
                s = self.ss.dma_count["hw"] % N_HW_SEMS
                self.ss.dma_count["hw"] += 1
            if self.dma_last[s] is not None:
                deps.append(self.dma_last[s])
            self.dma_val[s] += 16
            op.dsem, op.dval = s, self.dma_val[s]
            self.dma_last[s] = op
        op.deps = deps
        gid = len(self.ops)
        for k in reads:
            self.readers.setdefault(k, {})[("dma", gid) if dma else eng] = op
        for k in writes:
            self.last_w[k] = op
            self.readers[k] = {}
        self.streams[eng].append(op)
        self.ops.append(op)
        return op

    @staticmethod
    def _skip(op, d):
        return (not d.is_dma) and d.eng == op.eng and op.eng == "pe" and not op.is_dma

    def emit(self, name):
        nc, ss = self.nc, self.ss
        for op in self.ops:
            for d in op.deps:
                if not d.is_dma and not self._skip(op, d):
                    d.signal = True
        for e in ("pe", "act", "dve", "pool"):
            c = ss.val[("eng", e)]
            for op in self.streams[e]:
                if op.signal and not op.is_dma:
                    c += 1
                    op.sigval = c
            ss.val[("eng", e)] = c
        for i in range(N_DMA_SEMS):
            ss.val[("dma", i)] = self.dma_val[i]
        for e in ENGS:
            known = {}
            for op in self.streams[e]:
                need = {}
                for d in op.deps:
                    if d.is_dma:
                        key, val = ("dma", d.dsem), d.dval
                    elif self._skip(op, d):
                        continue
                    else:
                        key, val = ("eng", d.eng), d.sigval
                    if known.get(key, 0) >= val:
                        continue
                    if need.get(key, 0) < val:
                        need[key] = val
                known.update(need)
                op.waits = need
        sems = ss.sems
        with nc.Block() as block:
            def run(e, eng):
                for op in self.streams[e]:
                    for k, v in op.waits.items():
                        eng.wait_ge(sems[k], v)
                    ins = op.fn(eng)
                    if op.is_dma:
                        ins.then_inc(sems[("dma", op.dsem)], 16)
                    elif op.signal:
                        ins.then_inc(sems[("eng", e)], 1)

            @block.tensor
            def _(eng):
                run("pe", eng)

            @block.scalar
            def _(eng):
                run("act", eng)

            @block.vector
            def _(eng):
                run("dve", eng)

            @block.gpsimd
            def _(eng):
                run("pool", eng)

            @block.sync
            def _(eng):
                run("sp", eng)
                for i in range(N_DMA_SEMS):
                    if self.dma_val[i] > 0:
                        eng.wait_ge(sems[("dma", i)], self.dma_val[i])
                for e in ("pe", "act", "dve", "pool"):
                    if ss.val[("eng", e)] > 0:
                        eng.wait_ge(sems[("eng", e)], ss.val[("eng", e)])


class Cfg:
    def __init__(self, D=2048, H=8, NCH=4, CH=1024, DFF=8192):
        self.D, self.H, self.NCH, self.CH, self.DFF = D, H, NCH, CH, DFF
        self.KC = D // 128
        self.NST = CH // 128
        self.NT = NCH * self.NST
        self.TCTX = NCH * CH
        self.NB = self.TCTX // 256
        self.NG = CH // 512
        self.HG = min(4, H)
        self.GW = self.HG * 128
        self.HW = H * 128
        self.o_mq, self.o_mk, self.o_mv = 0, self.HW, 2 * self.HW
        self.o_fq, self.o_fk, self.o_fv = 3 * self.HW, 4 * self.HW, 5 * self.HW
        self.o_ff = 6 * self.HW
        self.o_ga = self.o_ff + H
        self.o_gb = self.o_ga + D
        self.WIN = self.o_gb + D
        self.GD = min(512, D)
        self.FC = DFF // 128
        self.FB = min(1024, DFF)
        self.NFB = DFF // self.FB
        self.UW = min(256, DFF)
        self.TOPK = 3
        assert H >= 2 and self.NB >= 8


FULL = Cfg()


def _cp(e, out, in_):
    if hasattr(e, "tensor_copy"):
        return e.tensor_copy(out=out, in_=in_)
    return e.copy(out=out, in_=in_)


def MARK(name, S):
    if MARKS is not None:
        MARKS.append((name, len(S.ops)))


MARKS = None


def build(cfg=FULL, upto=4, debug=False):
    c = cfg
    D, H, KC, NCH, CH, NST, NT, TCTX = c.D, c.H, c.KC, c.NCH, c.CH, c.NST, c.NT, c.TCTX
    NB, NG, HG, GW, HW, GD = c.NB, c.NG, c.HG, c.GW, c.HW, c.GD
    H2 = 2 * H
    NN = CH // 512
    nc = bass.Bass("TRN2", target_bir_lowering=False)
    din = lambda n, s, dt=F32: nc.dram_tensor(n, s, dt, kind="ExternalInput").ap()
    dscr = lambda n, s, dt: nc.dram_tensor(n, s, dt, kind="Internal").ap()
    xctx = din("xctx", [TCTX, D])
    w_in = din("w_in", [D, c.WIN])
    w_bm = din("w_bm", [HW, D])
    w_bf = din("w_bf", [HW, D])
    w_out = din("w_out", [D, D])
    w_up = din("w_up", [D, c.DFF])
    w_down = din("w_down", [c.DFF, D])
    gvec = din("gvec", [4, D])
    bfg = din("bfg", [1, H])
    rcos = din("rope_cos", [TCTX, 16])
    rsin = din("rope_sin", [TCTX, 16])
    gbias_d = din("gbias", [1, NST * NB])
    vflag_d = din("vflag", [1, NCH])
    cident = din("cident", [128, 128])
    ctri = din("ctri", [128, 128])
    cE = din("cE", [NB, NB * 128])
    out = nc.dram_tensor("out", [CH, D], F32, kind="ExternalOutput").ap()
    KTd = dscr("KTd", [H2, 128, TCTX], BF16)
    QTd = dscr("QTd", [H2, 128, CH], BF16)
    Vd = dscr("Vd", [NT, 128, H2 * 128], BF16)
    sgd = dscr("sgd", [2, KC, 128, CH], BF16)
    x1d = dscr("x1d", [CH, D], F32)
    if debug:
        dbg_logf = nc.dram_tensor("dbg_logf", [128, NT * H], F32, kind="ExternalOutput").ap()
        dbg_kmT = nc.dram_tensor("dbg_kmT", [128, H * NB], F32, kind="ExternalOutput").ap()
        dbg_oT = nc.dram_tensor("dbg_oT", [128, H2 * CH], F32, kind="ExternalOutput").ap()
        dbg_mg = nc.dram_tensor("dbg_mg", [128, KC * CH], F32, kind="ExternalOutput").ap()

    scale = 128.0 ** -0.5

    with contextlib.ExitStack() as top:
        ss = SemState(nc, top)
        T = lambda st, n, s, dt: st.enter_context(nc.sbuf_tensor(n, s, dt))
        P = lambda st, n, s, dt: st.enter_context(nc.psum_tensor(n, s, dt))
        ident_b = T(top, "ident_b", [128, 128], BF16)
        tri_b = T(top, "tri_b", [128, 128], BF16)
        tri_f = T(top, "tri_f", [128, 128], F32)
        ident_f = T(top, "ident_f", [128, 128], F32)
        ones_f = T(top, "ones_f", [128, 128], F32)
        nones_f = T(top, "nones_f", [128, 128], F32)
        ones_b = T(top, "ones_b", [128, 128], BF16)
        c256 = T(top, "c256", [128, 1], BF16)
        epsc = T(top, "epsc", [128, 1], F32)
        E_b = T(top, "E_b", [NB, NB * 128], BF16)
        zff = T(top, "zff", [128, NT, H], F32)
        logf = T(top, "logf", [128, NT, H], F32)
        kmT = T(top, "kmT", [128, H, NB], BF16)
        stat = T(top, "stat", [128, NT, 8], F32)
        junk = T(top, "junk", [128, D], BF16)

        with contextlib.ExitStack() as ph:
            S = Sched(nc, ss)
            g_rep = T(ph, "g_rep", [128, D], F32)
            bfg_rep = T(ph, "bfg_rep", [128, H], F32)
            cos_sb = T(ph, "cos_sb", [128, NT, 16], F32)
            sin_sb = T(ph, "sin_sb", [128, NT, 16], F32)
            xs = [T(ph, "xs%d" % i, [128, D], F32) for i in range(2)]
            hb = [T(ph, "hb%d" % i, [128, D], BF16) for i in range(3)]
            hT = [T(ph, "hT%d" % i, [128, KC, CH], BF16) for i in range(2)]
            NWB = 3
            wb = [T(ph, "wb%d" % i, [128, KC, max(GD, GW)], BF16) for i in range(NWB)]
            vst = [T(ph, "vst%d" % i, [128, NST, GW], BF16) for i in range(2)]
            kst = [T(ph, "kst%d" % i, [128, HG, CH], BF16) for i in range(2)]
            kb = [T(ph, "kb%d" % i, [128, GW], BF16) for i in range(4)]
            rt = [T(ph, "rt%d" % i, [128, HG, 16], F32) for i in range(4)]
            wff = T(ph, "wff", [128, KC, H], BF16)
            lt = T(ph, "lt", [128, NT * H], F32)
            pst = [P(ph, "pst%d" % i, [128, 8, 128], BF16) for i in range(2)]
            psmm = [P(ph, "psmm%d" % i, [128, 512], F32) for i in range(3)]
            pstk = [P(ph, "pstk%d" % i, [128, 8, 128], BF16) for i in range(2)]
            pskm = P(ph, "pskm", [128, 512], F32)

            S.add("pool", lambda e: e.dma_start(out=ident_b[:], in_=cident), writes=["ident_b"], dma=True)
            S.add("pool", lambda e: e.dma_start(out=tri_b[:], in_=ctri), writes=["tri_b"], dma=True)
            S.add("sp", lambda e: e.dma_start(out=tri_f[:], in_=ctri), writes=["tri_f"], dma=True)
            S.add("sp", lambda e: e.dma_start(out=ident_f[:], in_=cident), writes=["ident_f"], dma=True)
            S.add("pool", lambda e: e.dma_start(out=E_b[:], in_=cE), writes=["E_b"], dma=True)
            S.add("dve", lambda e: e.memset(ones_f[:], 1.0), writes=["ones_f"])
            S.add("dve", lambda e: e.memset(nones_f[:], -1.0), writes=["nones_f"])
            S.add("dve", lambda e: e.memset(ones_b[:], 1.0), writes=["ones_b"])
            S.add("dve", lambda e: e.memset(c256[:], 1.0 / 256.0), writes=["c256"])
            S.add("dve", lambda e: e.memset(epsc[:], RMS_EPS), writes=["epsc"])
            S.add("sp", lambda e: e.dma_start(out=g_rep[:], in_=gvec[0, :].partition_broadcast(128)),
                  writes=["g_rep"], dma=True)
            S.add("sp", lambda e: e.dma_start(out=bfg_rep[:], in_=bfg[0, :].partition_broadcast(128)),
                  writes=["bfg_rep"], dma=True)
            S.add("sp", lambda e: e.dma_start(out=cos_sb[:], in_=rcos.rearrange("(t p) f -> p t f", p=128)),
                  writes=["cos_sb"], dma=True)
            S.add("sp", lambda e: e.dma_start(out=sin_sb[:], in_=rsin.rearrange("(t p) f -> p t f", p=128)),
                  writes=["sin_sb"], dma=True)
            S.add("pool", lambda e: e.dma_start(
                out=wff[:], in_=w_in[:, c.o_ff:c.o_ff + H].rearrange("(kc p) c -> p kc c", p=128)),
                writes=["wff"], dma=True)

            cnt = {"mm": 0, "w": 0, "v": 0, "k": 0, "kb": 0, "tk": 0, "tk2": 0}

            def load_w(src2d, col0, width):
                i = cnt["w"] % NWB
                cnt["w"] += 1
                buf = wb[i]
                for k0 in range(0, KC, 8):
                    k1 = min(KC, k0 + 8)
                    S.add("pool", lambda e, k0=k0, k1=k1: e.dma_start(
                        out=buf[:, k0:k1, 0:width],
                        in_=src2d[k0 * 128:k1 * 128, col0:col0 + width].rearrange("(kc p) c -> p kc c", p=128)),
                        writes=[("wb", i)], dma=True)
                return buf, ("wb", i)

            def hT_stageA(d, s):
                t = d * NST + s
                xi = t % 2
                hi = t % 3
                S.add("pool", lambda e: e.dma_start(out=xs[xi][:], in_=xctx[t * 128:(t + 1) * 128, :]),
                      writes=[("xs", xi)], dma=True)
                S.add("dve", lambda e: e.scalar_tensor_tensor(
                    out=hb[hi][:], in0=xs[xi][:], scalar=1.0, in1=xs[xi][:], op0=ALU.mult, op1=ALU.mult,
                    accum_out=stat[:, t, 0:1]),
                    reads=[("xs", xi)], writes=[("hb", hi), ("stat", t)])
                S.add("act", lambda e: e.activation(out=stat[:, t, 1:2], in_=stat[:, t, 0:1], func=AF.Sqrt,
                                                    bias=epsc[:], scale=1.0 / D),
                      reads=[("stat", t), "epsc"], writes=[("stat", t)])
                S.add("dve", lambda e: e.reciprocal(out=stat[:, t, 2:3], in_=stat[:, t, 1:2]),
                      reads=[("stat", t)], writes=[("stat", t)])
                S.add("dve", lambda e: e.scalar_tensor_tensor(
                    out=hb[hi][:], in0=xs[xi][:], scalar=stat[:, t, 2:3], in1=g_rep[:], op0=ALU.mult,
                    op1=ALU.mult),
                    reads=[("xs", xi), ("stat", t), "g_rep"], writes=[("hb", hi)])

            def hT_stageB(d, s):
                hTd = hT[d % 2]
                t = d * NST + s
                hi = t % 3
                nq = min(8, KC)
                for q0 in range(0, KC, nq):
                    pi = cnt["tk"] % 2
                    cnt["tk"] += 1
                    for kk in range(nq):
                        S.add("pe", lambda e, pi=pi, kk=kk, q0=q0: e.transpose(
                            out=pst[pi][:, kk, :], in_=hb[hi][:, (q0 + kk) * 128:(q0 + kk + 1) * 128],
                            identity=ident_b[:]),
                            reads=[("hb", hi), "ident_b"], writes=[("pst", pi)])
                    S.add("act", lambda e, pi=pi, q0=q0: e.copy(
                        out=hTd[:, q0:q0 + nq, s * 128:(s + 1) * 128], in_=pst[pi][:, 0:nq, :]),
                        reads=[("pst", pi)], writes=[("hT", d % 2)])

            pendA = []
            pendB = []

            def drip(n=1):
                for _ in range(n):
                    if pendA:
                        a = pendA.pop(0)
                        hT_stageA(*a)
                        pendB.append(a)
                    if pendB and (len(pendB) > 1 or not pendA):
                        hT_stageB(*pendB.pop(0))

            def next_ps():
                i = cnt["mm"] % 3
                cnt["mm"] += 1
                return psmm[i], ("psmm", i)

            def rope_tail(s, d, mode, hbase, nh, ki):
                t = d * NST + s
                bi = s % 4
                kbt = kb[bi]
                pi = cnt["tk2"] % 2
                cnt["tk2"] += 1
                for hh in range(nh):
                    S.add("pe", lambda e, pi=pi, hh=hh: e.transpose(
                        out=pstk[pi][:, hh, :], in_=kbt[:, hh * 128:(hh + 1) * 128], identity=ident_b[:]),
                        reads=[("kb", bi), "ident_b"], writes=[("pstk", pi)])
                S.add("dve", lambda e, pi=pi: e.tensor_copy(
                    out=kst[ki][:, 0:nh, s * 128:(s + 1) * 128], in_=pstk[pi][:, 0:nh, :]),
                    reads=[("pstk", pi)], writes=[("kst", ki)])
                if mode == "k_rope" and s % 2 == 1:
                    kprev = kb[(s - 1) % 4]
                    for hh in range(nh):
                        S.add("pe", lambda e, hh=hh: e.matmul(
                            pskm[:, hh:hh + 1], lhsT=kprev[:, hh * 128:(hh + 1) * 128], rhs=c256[:],
                            start=True, stop=False),
                            reads=[("kb", (s - 1) % 4), "c256"], writes=["pskm"])
                        S.add("pe", lambda e, hh=hh: e.matmul(
                            pskm[:, hh:hh + 1], lhsT=kbt[:, hh * 128:(hh + 1) * 128], rhs=c256[:],
                            start=False, stop=True),
                            reads=[("kb", bi), "c256"], writes=["pskm"])
                    n = t // 2
                    S.add("dve", lambda e: e.tensor_copy(out=kmT[:, hbase:hbase + nh, n], in_=pskm[:, 0:nh]),
                          reads=["pskm"], writes=["kmT"])

            def tok_group(d, col0, width, mode, hbase, wbuf, wkey):
                hTd = hT[d % 2]
                nh = width // 128
                if mode == "v":
                    vi = cnt["v"] % 2
                    cnt["v"] += 1
                else:
                    ki = cnt["k"] % 2
                    cnt["k"] += 1
                for s in range(NST):
                    t = d * NST + s
                    ps, pk = next_ps()
                    for kc in range(KC):
                        S.add("pe", lambda e, ps=ps, kc=kc, s=s: e.matmul(
                            ps[:, 0:width], lhsT=hTd[:, kc, s * 128:(s + 1) * 128], rhs=wbuf[:, kc, 0:width],
                            start=(kc == 0), stop=(kc == KC - 1)),
                            reads=[("hT", d % 2), wkey], writes=[pk])
                    if mode == "v":
                        S.add("act", lambda e, ps=ps, s=s: e.copy(out=vst[vi][:, s, 0:width], in_=ps[:, 0:width]),
                              reads=[pk], writes=[("vst", vi)])
                        continue
                    bi = s % 4
                    kbt = kb[bi]
                    ps3 = ps[:, 0:width].rearrange("p (h c) -> p h c", c=128)
                    kb3 = kbt[:, 0:width].rearrange("p (h c) -> p h c", c=128)
                    cosb = cos_sb[:, t:t + 1, :].broadcast_to([128, nh, 16])
                    sinb = sin_sb[:, t:t + 1, :].broadcast_to([128, nh, 16])
                    S.add("act", lambda e, ps=ps, kbt=kbt: e.copy(out=kbt[:, 0:width], in_=ps[:, 0:width]),
                          reads=[pk], writes=[("kb", bi)])
                    x1, x2 = ps3[:, :, 0:16], ps3[:, :, 16:32]
                    r = [rt[i][:, 0:nh, :] for i in range(4)]
                    S.add("dve", lambda e, x1=x1, cosb=cosb, r=r: e.tensor_tensor(out=r[0], in0=x1, in1=cosb, op=ALU.mult),
                          reads=[pk, "cos_sb"], writes=[("rt", 0)])
                    S.add("dve", lambda e, x2=x2, sinb=sinb, r=r: e.tensor_tensor(out=r[1], in0=x2, in1=sinb, op=ALU.mult),
                          reads=[pk, "sin_sb"], writes=[("rt", 1)])
                    S.add("dve", lambda e, x2=x2, cosb=cosb, r=r: e.tensor_tensor(out=r[2], in0=x2, in1=cosb, op=ALU.mult),
                          reads=[pk, "cos_sb"], writes=[("rt", 2)])
                    S.add("dve", lambda e, x1=x1, sinb=sinb, r=r: e.tensor_tensor(out=r[3], in0=x1, in1=sinb, op=ALU.mult),
                          reads=[pk, "sin_sb"], writes=[("rt", 3)])
                    S.add("dve", lambda e, kb3=kb3, r=r: e.tensor_tensor(out=kb3[:, :, 0:16], in0=r[0], in1=r[1],
                                                                       op=ALU.subtract),
                          reads=[("rt", 0), ("rt", 1), ("kb", bi)], writes=[("kb", bi)])
                    S.add("dve", lambda e, kb3=kb3, r=r: e.tensor_tensor(out=kb3[:, :, 16:32], in0=r[2], in1=r[3],
                                                                       op=ALU.add),
                          reads=[("rt", 2), ("rt", 3), ("kb", bi)], writes=[("kb", bi)])
                    if s >= 1:
                        rope_tail(s - 1, d, mode, hbase, nh, ki)
                if mode != "v":
                    rope_tail(NST - 1, d, mode, hbase, nh, ki)
                if mode == "v":
                    S.add("sp", lambda e: e.dma_start(
                        out=Vd[d * NST:(d + 1) * NST, :, hbase * 128:hbase * 128 + width].rearrange("t p c -> p t c"),
                        in_=vst[vi][:, :, 0:width]),
                        reads=[("vst", vi)], dma=True)
                else:
                    dst = KTd[hbase:hbase + nh, :, d * CH:(d + 1) * CH] if mode == "k_rope" else QTd[hbase:hbase + nh, :, :]
                    S.add("sp", lambda e: e.dma_start(out=dst.rearrange("h p t -> p h t"), in_=kst[ki][:, 0:nh, :]),
                          reads=[("kst", ki)], dma=True)
                drip()

            def feat_group(d, col0, width, mode, hbase, wbuf, wkey, kc0=0):
                hTd = hT[d % 2]
                nh = width // 128
                for h0 in range(0, nh, HG):
                    h1 = min(nh, h0 + HG)
                    ki = cnt["k"] % 2
                    cnt["k"] += 1
                    for hh in range(h0, h1):
                        for n0 in range(0, CH, 512):
                            ps, pk = next_ps()
                            for kc in range(KC):
                                S.add("pe", lambda e, ps=ps, kc=kc, hh=hh, n0=n0: e.matmul(
                                    ps[:, 0:512], lhsT=wbuf[:, kc, hh * 128:(hh + 1) * 128],
                                    rhs=hTd[:, kc, n0:n0 + 512], start=(kc == 0), stop=(kc == KC - 1)),
                                    reads=[("hT", d % 2), wkey], writes=[pk])
                            if mode == "gate":
                                S.add("act", lambda e, ps=ps, hh=hh, n0=n0, ki=ki, h0=h0: e.activation(
                                    out=kst[ki][:, hh - h0, n0:n0 + 512], in_=ps[:, 0:512], func=AF.Sigmoid),
                                    reads=[pk], writes=[("kst", ki)])
                            else:
                                S.add("dve", lambda e, ps=ps, hh=hh, n0=n0, ki=ki, h0=h0: e.tensor_copy(
                                    out=kst[ki][:, hh - h0, n0:n0 + 512], in_=ps[:, 0:512]),
                                    reads=[pk], writes=[("kst", ki)])
                    if mode == "kT":
                        dst = KTd[hbase + h0:hbase + h1, :, d * CH:(d + 1) * CH]
                    elif mode == "qT":
                        dst = QTd[hbase + h0:hbase + h1, :, :]
                    else:
                        dst = sgd[hbase, kc0 + h0:kc0 + h1, :, :]
                    S.add("sp", lambda e, ki=ki, dst=dst, h0=h0, h1=h1: e.dma_start(
                        out=dst.rearrange("h p t -> p h t"), in_=kst[ki][:, 0:h1 - h0, :]),
                        reads=[("kst", ki)], dma=True)
                drip()

            def ff_group(d):
                hTd = hT[d % 2]
                for s in range(NST):
                    t = d * NST + s
                    ps, pk = next_ps()
                    for kc in range(KC):
                        S.add("pe", lambda e, ps=ps, kc=kc, s=s: e.matmul(
                            ps[:, 0:H], lhsT=hTd[:, kc, s * 128:(s + 1) * 128], rhs=wff[:, kc, :],
                            start=(kc == 0), stop=(kc == KC - 1)),
                            reads=[("hT", d % 2), "wff"], writes=[pk])
                    S.add("dve", lambda e, ps=ps, t=t: e.tensor_tensor(out=zff[:, t, :], in0=ps[:, 0:H], in1=bfg_rep[:],
                                                                     op=ALU.add),
                          reads=[pk, "bfg_rep"], writes=["zff"])
                drip()

            MARK("consts", S)
            for s in range(NST):
                hT_stageA(0, s)
                if s >= 1:
                    hT_stageB(0, s - 1)
            hT_stageB(0, NST - 1)
            tasks = []
            for d in range(NCH):
                if d + 1 < NCH:
                    tasks.append((None, None, lambda d=d: pendA.extend((d + 1, s) for s in range(NST))))
                for g0 in range(0, H, HG):
                    tasks.append((c.o_mk + g0 * 128, GW, lambda wb_, wk_, d=d, g0=g0: tok_group(
                        d, c.o_mk + g0 * 128, GW, "k_rope", g0, wb_, wk_)))
                for g0 in range(0, H, HG):
                    tasks.append((c.o_mv + g0 * 128, GW, lambda wb_, wk_, d=d, g0=g0: tok_group(
                        d, c.o_mv + g0 * 128, GW, "v", g0, wb_, wk_)))
                for g0 in range(0, H, HG):
                    tasks.append((c.o_fk + g0 * 128, GW, lambda wb_, wk_, d=d, g0=g0: feat_group(
                        d, c.o_fk + g0 * 128, GW, "kT", H + g0, wb_, wk_)))
                for g0 in range(0, H, HG):
                    tasks.append((c.o_fv + g0 * 128, GW, lambda wb_, wk_, d=d, g0=g0: tok_group(
                        d, c.o_fv + g0 * 128, GW, "v", H + g0, wb_, wk_)))
                tasks.append((None, None, lambda d=d: ff_group(d)))
                tasks.append((None, None, lambda: drip(2 * NST + 2)))
                if d == 0:
                    for g0 in range(0, H, HG):
                        tasks.append((c.o_mq + g0 * 128, GW, lambda wb_, wk_, g0=g0: tok_group(
                            0, c.o_mq + g0 * 128, GW, "q_rope", g0, wb_, wk_)))
                    for g0 in range(0, H, HG):
                        tasks.append((c.o_fq + g0 * 128, GW, lambda wb_, wk_, g0=g0: feat_group(
                            0, c.o_fq + g0 * 128, GW, "qT", H + g0, wb_, wk_)))
                    for c0 in range(0, D, GD):
                        tasks.append((c.o_ga + c0, GD, lambda wb_, wk_, c0=c0: feat_group(
                            0, c.o_ga + c0, GD, "gate", 0, wb_, wk_, kc0=c0 // 128)))
                    for c0 in range(0, D, GD):
                        tasks.append((c.o_gb + c0, GD, lambda wb_, wk_, c0=c0: feat_group(
                            0, c.o_gb + c0, GD, "gate", 1, wb_, wk_, kc0=c0 // 128)))
            PREF = NWB - 1
            wtasks = [i for i, tk_ in enumerate(tasks) if tk_[0] is not None]
            loaded = {}
            nxt = 0
            for i, (col0, width, fn) in enumerate(tasks):
                if col0 is None:
                    fn()
                    continue
                pos = wtasks.index(i)
                while nxt < len(wtasks) and nxt <= pos + PREF:
                    ti = wtasks[nxt]
                    loaded[ti] = load_w(w_in, tasks[ti][0], tasks[ti][1])
                    nxt += 1
                fn(*loaded.pop(i))
            MARK("proj_done", S)
            zf = zff[:].rearrange("p t h -> p (t h)")
            lf = logf[:].rearrange("p t h -> p (t h)")
            S.add("act", lambda e: e.activation(out=lt[:], in_=zf, func=AF.Exp, scale=-1.0), reads=["zff"], writes=["lt"])
            S.add("act", lambda e: e.activation(out=lt[:], in_=lt[:], func=AF.Ln, bias=1.0), reads=["lt"], writes=["lt"])
            S.add("dve", lambda e: e.tensor_scalar(out=lf, in0=lt[:], scalar1=-1.0, scalar2=None, op0=ALU.mult),
                  reads=["lt"], writes=["logf"])
            if debug:
                S.add("sp", lambda e: e.dma_start(out=dbg_logf, in_=lf), reads=["logf"], dma=True)
                S.add("pool", lambda e: e.dma_start(out=dbg_kmT, in_=kmT[:].rearrange("p h n -> p (h n)")),
                      reads=["kmT"], dma=True)
            S.emit("ph1")
        if upto < 2:
            return nc

        with contextlib.ExitStack() as mid:
            R1 = T(mid, "R1", [128, max(H2, KC), CH], BF16)
            R2 = T(mid, "R2", [128, max(KC, 16), CH], BF16)
            oT, h2T, mergedT = R1, R1, R2

            mid2 = contextlib.ExitStack()
            mid2.__enter__()
            Wbm = T(mid2, "Wbm", [128, H, D], BF16)
            Wbf = T(mid2, "Wbf", [128, H, D], BF16)
            with contextlib.ExitStack() as ph:
                S = Sched(nc, ss)
                KTb = [T(ph, "KTb%d" % i, [128, TCTX], BF16) for i in range(2)]
                Vb = [T(ph, "Vb%d" % i, [128, NT, 130], BF16) for i in range(2)]
                QTb = [T(ph, "QTb%d" % i, [128, CH], BF16) for i in range(2)]
                pT = [T(ph, "pT%d" % i, [128, 512], BF16) for i in range(4)]
                NSQ = TCTX // 512 + CH // 512
                sqb1 = [T(ph, "sqb%d" % i, [128, 512], BF16) for i in range(NSQ)]
                sqb = [sqb1, sqb1]
                nmx = T(ph, "nmx", [128, 4], F32)
                negC = T(ph, "negC", [128, H2], F32)
                gbias_rep = T(ph, "gbias_rep", [128, NST * NB], F32)
                vfl_rep = T(ph, "vfl_rep", [128, NCH], F32)
                Fsb = T(ph, "Fsb", [128, NT, H], F32)
                totB = T(ph, "totB", [128, NT, H], F32)
                offF = T(ph, "offF", [128, NT, H], F32)
                Tch = T(ph, "Tch", [128, NCH, H], F32)
                FrefB = T(ph, "FrefB", [128, NG, H], F32)
                biasF = T(ph, "biasF", [128, NG, NT, H], F32)
                bfh = [T(ph, "bfh%d" % i, [128, NG, NT], F32) for i in range(2)]
                gmb = T(ph, "gmb", [128, NST, NB], F32)
                m8 = T(ph, "m8", [128, NST, 8], F32)
                b1 = T(ph, "b1", [128, NST, NB], F32)
                bb = T(ph, "bb", [128, NST, NB], BF16)
                biasT = [T(ph, "biasT%d" % i, [NB, CH], BF16) for i in range(2)]
                rcp1 = T(ph, "rcp", [128, 512], F32)
                rcp = [rcp1, rcp1]
                NPS = 3
                psS = [P(ph, "psS%d" % i, [128, 512], F32) for i in range(NPS)]
                oacc_t = [[P(ph, "oacc%d_%d" % (i, k), [128, 512], F32) for k in range(2)] for i in range(2)]
                oacc = [[t[:, 0:260].rearrange("p (r c) -> p r c", c=130) for t in row] for row in oacc_t]
                psX = [P(ph, "psX%d" % i, [128, 512], F32) for i in range(1)]
                psXb = [p[:].bitcast(BF16) for p in psX]
                cn = {"x": 0, "s": 0, "p": 0, "a": 0, "sq": 0, "g": 0}

                def next_x():
                    i = cn["x"] % len(psX)
                    cn["x"] += 1
                    return i, ("psX", i)

                S.add("sp", lambda e: e.dma_start(out=gbias_rep[:], in_=gbias_d[0, :].partition_broadcast(128)),
                      writes=["gbias_rep"], dma=True)
                S.add("sp", lambda e: e.dma_start(out=vfl_rep[:], in_=vflag_d[0, :].partition_broadcast(128)),
                      writes=["vfl_rep"], dma=True)
                for i in range(2):
                    S.add("pool", lambda e, i=i: e.memset(Vb[i][:, :, 128:129], 1.0), writes=[("V", i)])

                def load_head(h, par):
                    S.add("sp", lambda e: e.dma_start(out=KTb[par][:], in_=KTd[h]), writes=[("KT", par)], dma=True)
                    S.add("sp", lambda e: e.dma_start(out=QTb[par][:], in_=QTd[h]), writes=[("QT", par)], dma=True)
                    for t0 in range(0, NT, 8):
                        S.add("sp", lambda e, t0=t0: e.dma_start(
                            out=Vb[par][:, t0:t0 + 8, 0:128],
                            in_=Vd[t0:t0 + 8, :, h * 128:(h + 1) * 128].rearrange("t p c -> p t c")),
                            writes=[("V", par)], dma=True)

                load_head(0, 0)

                lf_all = logf[:].rearrange("p t h -> p (t h)")
                S.add("pe", lambda e: e.matmul(psS[0][:, 0:NT * H], lhsT=tri_f[:], rhs=lf_all, start=True, stop=True),
                      reads=["logf", "tri_f"], writes=[("psS", 0)])
                S.add("pe", lambda e: e.matmul(psS[1][:, 0:NT * H], lhsT=ones_f[:], rhs=lf_all, start=True, stop=True),
                      reads=["logf", "ones_f"], writes=[("psS", 1)])
                S.add("dve", lambda e: e.tensor_copy(out=Fsb[:].rearrange("p t h -> p (t h)"), in_=psS[0][:, 0:NT * H]),
                      reads=[("psS", 0)], writes=["Fsb"])
                S.add("dve", lambda e: e.tensor_copy(out=totB[:].rearrange("p t h -> p (t h)"), in_=psS[1][:, 0:NT * H]),
                      reads=[("psS", 1)], writes=["totB"])
                for d in range(NCH):
                    S.add("dve", lambda e, d=d: e.tensor_reduce(
                        out=Tch[:, d, :], in_=totB[:, d * NST:(d + 1) * NST, :].rearrange("p j h -> p h j"),
                        axis=AX.X, op=ALU.add), reads=["totB"], writes=["Tch"])
                S.add("dve", lambda e: e.memset(offF[:, 0, :], 0.0), writes=["offF"])
                for d in range(1, NCH):
                    prev_t = 0 if d == 1 else (d - 1) * NST
                    S.add("dve", lambda e, d=d, prev_t=prev_t: e.tensor_tensor(
                        out=offF[:, d * NST, :], in0=offF[:, prev_t, :], in1=Tch[:, d, :], op=ALU.subtract),
                        reads=["offF", "Tch"], writes=["offF"])
                for d in range(NCH):
                    for j in range(1, NST):
                        t = d * NST + j
                        S.add("dve", lambda e, t=t: e.tensor_tensor(
                            out=offF[:, t, :], in0=offF[:, t - 1, :], in1=totB[:, t - 1, :], op=ALU.add),
                            reads=["offF", "totB"], writes=["offF"])
                S.add("dve", lambda e: e.tensor_tensor(out=Fsb[:], in0=Fsb[:], in1=offF[:], op=ALU.add),
                      reads=["Fsb", "offF"], writes=["Fsb"])
                for g in range(NG):
                    S.add("dve", lambda e, g=g: e.tensor_copy(out=FrefB[:, g, :], in_=offF[:, 4 * g + 2, :]),
                          reads=["offF"], writes=["FrefB"])
                for g in range(NG):
                    S.add("dve", lambda e, g=g: e.tensor_tensor(
                        out=biasF[:, g], in0=FrefB[:, g:g + 1, :].broadcast_to([128, NT, H]), in1=Fsb[:],
                        op=ALU.subtract), reads=["FrefB", "Fsb"], writes=["biasF"])
                    S.add("dve", lambda e, g=g: e.tensor_tensor(
                        out=biasF[:, g].rearrange("p (d j) h -> p d (j h)", d=NCH),
                        in0=biasF[:, g].rearrange("p (d j) h -> p d (j h)", d=NCH),
                        in1=vfl_rep[:].unsqueeze(2).broadcast_to([128, NCH, NST * H]), op=ALU.add),
                        reads=["biasF", "vfl_rep"], writes=["biasF"])

                def head_sq(par, h):
                    tiles = [(KTb[par], ("KT", par), n0) for n0 in range(0, TCTX, 512)]
                    tiles += [(QTb[par], ("QT", par), n0) for n0 in range(0, CH, 512)]
                    for i, (buf, bkey, n0) in enumerate(tiles):
                        S.add("pool", lambda e, buf=buf, n0=n0, i=i: e.tensor_tensor(
                            out=sqb[par][i][:], in0=buf[:, n0:n0 + 512], in1=buf[:, n0:n0 + 512], op=ALU.mult),
                            reads=[bkey], writes=[("sqb", i)])

                def prep_stages(h):
                    par = h % 2
                    st = []

                    def s1():
                        xi, xk = next_x()
                        ncol = NSQ * 4
                        for i in range(NSQ):
                            for cq in range(4):
                                S.add("pe", lambda e, i=i, cq=cq: e.matmul(
                                    psX[xi][:, i * 4 + cq:i * 4 + cq + 1], lhsT=sqb[par][i][:, cq * 128:(cq + 1) * 128],
                                    rhs=ones_b[:, 0:1], start=True, stop=True),
                                    reads=[("sqb", i), "ones_b"], writes=[xk])
                        S.add("dve", lambda e: e.tensor_reduce(out=nmx[:, 0:1], in_=psX[xi][:, 0:ncol], axis=AX.X,
                                                               op=ALU.max), reads=[xk], writes=["nmx"])

                    def s2():
                        xi2, xk2 = next_x()
                        S.add("pe", lambda e: e.transpose(out=psX[xi2][0:1, 0:128], in_=nmx[:, 0:1], identity=ident_f[:]),
                              reads=["nmx", "ident_f"], writes=[xk2])
                        S.add("dve", lambda e: e.tensor_reduce(out=nmx[0:1, 1:2], in_=psX[xi2][0:1, 0:128], axis=AX.X,
                                                               op=ALU.max), reads=[xk2], writes=["nmx1"])

                    def s3():
                        xi3, xk3 = next_x()
                        S.add("pe", lambda e: e.matmul(psX[xi3][:, 0:1], lhsT=ones_f[0:1, :], rhs=nmx[0:1, 1:2],
                                                       start=True, stop=True), reads=["nmx1", "ones_f"], writes=[xk3])
                        S.add("dve", lambda e: e.tensor_scalar(out=negC[:, h:h + 1], in0=psX[xi3][:, 0:1],
                                                               scalar1=-1.02 * scale, scalar2=None, op0=ALU.mult),
                              reads=[xk3], writes=[("negC", h)])
                        if h >= H:
                            S.add("dve", lambda e: e.tensor_scalar(
                                out=bfh[par][:], in0=biasF[:, :, :, h - H], scalar1=negC[:, h:h + 1], scalar2=None,
                                op0=ALU.add), reads=["biasF", ("negC", h)], writes=[("bfh", par)])

                    st += [s1, s2, s3]
                    if h < H:
                        hm = h

                        def s4():
                            xi, xk = next_x()
                            psg = psX[xi][:, 0:NST * NB].rearrange("p (q n) -> p q n", n=NB)
                            for qt in range(NST):
                                S.add("pe", lambda e, qt=qt: e.matmul(
                                    psg[:, qt, :], lhsT=QTb[par][:, qt * 128:(qt + 1) * 128], rhs=kmT[:, hm, :],
                                    start=True, stop=True), reads=[("QT", par), "kmT"], writes=[xk])
                            g3 = gbias_rep[:].rearrange("p (q n) -> p q n", n=NB)
                            S.add("dve", lambda e: e.tensor_tensor(out=gmb[:], in0=psg, in1=g3, op=ALU.add),
                                  reads=[xk, "gbias_rep"], writes=["gmb"])
                            for qt in range(NST):
                                S.add("dve", lambda e, qt=qt: e.max(out=m8[:, qt, :], in_=gmb[:, qt, :]),
                                      reads=["gmb"], writes=["m8"])
                            for qt in range(NST):
                                S.add("dve", lambda e, qt=qt: e.tensor_scalar(
                                    out=b1[:, qt, :], in0=gmb[:, qt, :], scalar1=m8[:, qt, c.TOPK - 1:c.TOPK],
                                    scalar2=-NEG_MASK, op0=ALU.is_ge, op1=ALU.mult), reads=["gmb", "m8"], writes=["b1"])
                            S.add("dve", lambda e: e.scalar_tensor_tensor(
                                out=bb[:].rearrange("p q n -> p (q n)"), in0=b1[:].rearrange("p q n -> p (q n)"),
                                scalar=NEG_MASK, in1=gbias_rep[:], op0=ALU.add, op1=ALU.min),
                                reads=["b1", "gbias_rep"], writes=["bb"])
                            for qt in range(NST):
                                S.add("dve", lambda e, qt=qt: e.memset(bb[:, qt, qt // 2:qt // 2 + 1], 0.0),
                                      reads=["bb"], writes=["bb"])

                        def s5():
                            xi2, xk2 = next_x()
                            for qt in range(NST):
                                S.add("pe", lambda e, qt=qt: e.transpose(out=psXb[xi2][0:NB, qt * 128:(qt + 1) * 128],
                                                                         in_=bb[:, qt, :], identity=ident_b[:]),
                                      reads=["bb", "ident_b"], writes=[xk2])
                            S.add("dve", lambda e: e.tensor_copy(out=biasT[par][0:NB, 0:NST * 128],
                                                                 in_=psXb[xi2][0:NB, 0:NST * 128]),
                                  reads=[xk2], writes=[("biasT", par)])
                        st += [s4, s5]
                    return st

                staged = []

                def tick():
                    for it in staged:
                        it[0] -= 1
                    while staged and staged[0][0] <= 0:
                        staged.pop(0)[1]()

                def flush_staged():
                    while staged:
                        staged.pop(0)[1]()

                deferred = []

                def flush_deferred():
                    while deferred:
                        deferred.pop(0)()

                def attention(par, h, moba, mid_hook=None):
                    for g in range(NG):
                        a = cn["a"] % 2
                        cn["a"] += 1
                        tiles = [(d, j) for d in range(NCH - 1, 0, -1) for j in range(NST)]
                        tiles += [(0, j) for j in range(4 * g + 4)]

                        def qk_step(idx, d, j, g=g, a=a):
                            t = d * NST + j
                            c0 = 0 if (d > 0 or j < 4 * g) else (j - 4 * g) * 128
                            N = 512 - c0
                            q0 = g * 512 + c0
                            si = cn["s"] % NPS
                            cn["s"] += 1
                            ps, pk = psS[si], ("psS", si)
                            S.add("pe", lambda e: e.matmul(
                                ps[:, 0:N], lhsT=KTb[par][:, t * 128:(t + 1) * 128], rhs=QTb[par][:, q0:q0 + N],
                                start=True, stop=not moba), reads=[("KT", par), ("QT", par)], writes=[pk])
                            if moba:
                                n = t // 2
                                S.add("pe", lambda e: e.matmul(
                                    ps[:, 0:N], lhsT=E_b[0:NB, n * 128:(n + 1) * 128], rhs=biasT[par][0:NB, q0:q0 + N],
                                    start=False, stop=True), reads=["E_b", ("biasT", par)], writes=[pk])
                            pi = cn["p"] % 4
                            cn["p"] += 1
                            pt, ptk = pT[pi], ("pT", pi)
                            if moba:
                                bias_ap, bkey = negC[:, h:h + 1], ("negC", h)
                            else:
                                bias_ap, bkey = bfh[par][:, g, t:t + 1], ("bfh", par)
                            S.add("act", lambda e: e.activation(
                                out=pt[:, 0:N], in_=ps[:, 0:N], func=AF.Exp, bias=bias_ap, scale=scale),
                                reads=[pk, bkey], writes=[ptk])
                            if d == 0 and j >= 4 * g:
                                S.add("pool", lambda e: e.tensor_tensor(out=pt[:, 0:128], in0=pt[:, 0:128],
                                                                        in1=tri_b[:], op=ALU.mult),
                                      reads=[ptk, "tri_b"], writes=[ptk])
                            return (idx, d, j, t, c0, pt, ptk)

                        def pv_step(st, g=g, a=a, ntiles=len(tiles)):
                            idx, d, j, t, c0, pt, ptk = st
                            N = 512 - c0
                            first, last = (idx == 0), (idx == ntiles - 1)
                            S.add("pe", lambda e: e.matmul(
                                oacc_t[a][0][:, c0:512], lhsT=Vb[par][:, t, 0:128], rhs=pt[:, 0:N],
                                start=first, stop=last), reads=[ptk, ("V", par)], writes=[("oacc", a, 0)])
                            S.add("pe", lambda e: e.matmul(
                                oacc_t[a][1][:, c0:512], lhsT=ones_b[:], rhs=pt[:, 0:N],
                                start=first, stop=last), reads=[ptk, "ones_b"], writes=[("oacc", a, 1)])

                        pend = []
                        for idx, (d, j) in enumerate(tiles):
                            pend.append(qk_step(idx, d, j))
                            if len(pend) > 2:
                                pv_step(pend.pop(0))
                            tick()
                        while pend:
                            pv_step(pend.pop(0))
                        S.add("dve", lambda e, a=a: e.reciprocal(out=rcp[a][:], in_=oacc_t[a][1][:, 0:512]),
                              reads=[("oacc", a, 1)], writes=["rcp"])
                        S.add("dve", lambda e, a=a, g=g: e.tensor_tensor(
                            out=oT[:, h, g * 512:(g + 1) * 512], in0=oacc_t[a][0][:, 0:512], in1=rcp[a][:], op=ALU.mult),
                            reads=[("oacc", a, 0), "rcp"], writes=["oT"])
                        if g == 0 and mid_hook is not None:
                            mid_hook()
                    flush_staged()

                def prep(h, spread=4):
                    for k, fn in enumerate(prep_stages(h)):
                        staged.append([1 + k * spread, fn])

                head_sq(0, 0)
                prep(0)
                flush_staged()
                for h in range(H2):
                    par = h % 2

                    def hook(h=h):
                        if h + 1 < H2:
                            prep(h + 1)
                        if h == H2 - 3:
                            for h0 in range(0, H, 4):
                                h1 = min(H, h0 + 4)
                                for (wsb, wdr, nm) in ((Wbm, w_bm, "Wbm"), (Wbf, w_bf, "Wbf")):
                                    S.add("pool", lambda e, wsb=wsb, wdr=wdr, h0=h0, h1=h1: e.dma_start(
                                        out=wsb[:, h0:h1, :],
                                        in_=wdr[h0 * 128:h1 * 128, :].rearrange("(h p) c -> p h c", p=128)),
                                        writes=[nm], dma=True)
                    if h + 1 < H2:
                        load_head(h + 1, (h + 1) % 2)
                        head_sq((h + 1) % 2, h + 1)
                    attention(par, h, h < H, mid_hook=hook)
                flush_deferred()
                if debug:
                    S.add("pool", lambda e: e.dma_start(out=dbg_oT, in_=oT[:, 0:H2, :].rearrange("p h t -> p (h t)")),
                          reads=["oT"], dma=True)
                S.emit("ph2")
            if upto < 3:
                return nc

            with contextlib.ExitStack() as ph:
                S = Sched(nc, ss)
                sga = [T(ph, "sga%d" % i, [128, CH], BF16) for i in range(2)]
                sgb = [T(ph, "sgb%d" % i, [128, CH], BF16) for i in range(2)]
                t1 = [T(ph, "t1_%d" % i, [128, 512], F32) for i in range(2)]
                t2 = [T(ph, "t2_%d" % i, [128, 512], F32) for i in range(2)]
                psm = [[P(ph, "psm%d_%d" % (i, n), [128, 512], F32) for n in range(NN)] for i in range(2)]
                psf = [[P(ph, "psf%d_%d" % (i, n), [128, 512], F32) for n in range(NN)] for i in range(2)]
                tc_ = 0
                for cc in range(KC):
                    a = cc % 2
                    S.add("sp", lambda e, a=a, cc=cc: e.dma_start(out=sga[a][:], in_=sgd[0, cc]), writes=[("sga", a)], dma=True)
                    S.add("sp", lambda e, a=a, cc=cc: e.dma_start(out=sgb[a][:], in_=sgd[1, cc]), writes=[("sgb", a)], dma=True)
                    for n in range(NN):
                        for hh in range(H):
                            S.add("pe", lambda e, a=a, n=n, hh=hh, cc=cc: e.matmul(
                                psm[a][n][:, 0:512], lhsT=Wbm[:, hh, cc * 128:(cc + 1) * 128],
                                rhs=oT[:, hh, n * 512:(n + 1) * 512], start=(hh == 0), stop=(hh == H - 1)),
                                reads=["Wbm", "oT"], writes=[("psm", a, n)])
                        for hh in range(H):
                            S.add("pe", lambda e, a=a, n=n, hh=hh, cc=cc: e.matmul(
                                psf[a][n][:, 0:512], lhsT=Wbf[:, hh, cc * 128:(cc + 1) * 128],
                                rhs=oT[:, H + hh, n * 512:(n + 1) * 512], start=(hh == 0), stop=(hh == H - 1)),
                                reads=["Wbf", "oT"], writes=[("psf", a, n)])
                        k = tc_ % 2
                        tc_ += 1
                        S.add("dve", lambda e, a=a, n=n, k=k: e.tensor_tensor(
                            out=t1[k][:], in0=psm[a][n][:, 0:512], in1=sga[a][:, n * 512:(n + 1) * 512], op=ALU.mult),
                            reads=[("psm", a, n), ("sga", a)], writes=[("t1", k)])
                        S.add("dve", lambda e, a=a, n=n, k=k: e.tensor_tensor(
                            out=t2[k][:], in0=psf[a][n][:, 0:512], in1=sgb[a][:, n * 512:(n + 1) * 512], op=ALU.mult),
                            reads=[("psf", a, n), ("sgb", a)], writes=[("t2", k)])
                        S.add("pool", lambda e, cc=cc, n=n, k=k: e.tensor_tensor(
                            out=mergedT[:, cc, n * 512:(n + 1) * 512], in0=t1[k][:], in1=t2[k][:], op=ALU.add),
                            reads=[("t1", k), ("t2", k)], writes=["mergedT"])
                if debug:
                    S.add("pool", lambda e: e.dma_start(out=dbg_mg, in_=mergedT[:, 0:KC, :].rearrange("p h t -> p (h t)")),
                          reads=["mergedT"], dma=True)
                S.emit("ph3a")
            mid2.close()
            mid3 = contextlib.ExitStack()
            mid3.__enter__()
            Wu = [T(mid3, "Wu0", [128, KC, c.UW], BF16)]
            NPRE = 1

            with contextlib.ExitStack() as ph:
                S = Sched(nc, ss)
                Wo = T(ph, "Wo", [128, KC, D], BF16)
                gpost = T(ph, "gpost", [128, D], F32)
                gpre2 = T(ph, "gpre2", [128, D], F32)
                xs2 = [T(ph, "xs2_%d" % i, [128, D], F32) for i in range(2)]
                tt2 = [T(ph, "tt_%d" % i, [128, D], F32) for i in range(2)]
                hb2 = [T(ph, "hb2_%d" % i, [128, D], BF16) for i in range(2)]
                st3 = T(ph, "st3", [128, NST, 16], F32)
                ncg = D // GD
                psM = [P(ph, "psM%d" % i, [128, 512], F32) for i in range(6)]
                pst2 = [P(ph, "pst2_%d" % i, [128, 8, 128], BF16) for i in range(2)]
                S.add("sp", lambda e: e.dma_start(out=gpost[:], in_=gvec[1, :].partition_broadcast(128)),
                      writes=["gpost"], dma=True)
                S.add("sp", lambda e: e.dma_start(out=gpre2[:], in_=gvec[2, :].partition_broadcast(128)),
                      writes=["gpre2"], dma=True)
                for c0 in range(0, D, GD):
                    for k0 in range(0, KC, 8):
                        k1 = min(KC, k0 + 8)
                        S.add("pool", lambda e, c0=c0, k0=k0, k1=k1: e.dma_start(
                            out=Wo[:, k0:k1, c0:c0 + GD],
                            in_=w_out[k0 * 128:k1 * 128, c0:c0 + GD].rearrange("(kc p) c -> p kc c", p=128)),
                            writes=[("Wo", c0)], dma=True)
                tkc = [0]

                def h2_tail(s):
                    xi = s % 2
                    nq = min(8, KC)
                    for q0 in range(0, KC, nq):
                        pi2 = tkc[0] % 2
                        tkc[0] += 1
                        for kk in range(nq):
                            S.add("pe", lambda e, pi2=pi2, kk=kk, q0=q0: e.transpose(
                                out=pst2[pi2][:, kk, :], in_=hb2[xi][:, (q0 + kk) * 128:(q0 + kk + 1) * 128],
                                identity=ident_b[:]), reads=[("hb2", xi), "ident_b"], writes=[("pst2", pi2)])
                        S.add("act", lambda e, pi2=pi2, q0=q0: e.copy(
                            out=h2T[:, q0:q0 + nq, s * 128:(s + 1) * 128], in_=pst2[pi2][:, 0:nq, :]),
                            reads=[("pst2", pi2)], writes=["h2T"])

                mc = 0
                tk = 0
                for s in range(NST):
                    xi = s % 2
                    S.add("pool", lambda e, s=s, xi=xi: e.dma_start(out=xs2[xi][:], in_=xctx[s * 128:(s + 1) * 128, :]),
                          writes=[("xs2", xi)], dma=True)
                    pss = []
                    for cg in range(ncg):
                        pi = mc % 6
                        mc += 1
                        pss.append(pi)
                        for kc in range(KC):
                            S.add("pe", lambda e, pi=pi, kc=kc, s=s, cg=cg: e.matmul(
                                psM[pi][:, 0:GD], lhsT=mergedT[:, kc, s * 128:(s + 1) * 128],
                                rhs=Wo[:, kc, cg * GD:(cg + 1) * GD], start=(kc == 0), stop=(kc == KC - 1)),
                                reads=["mergedT", ("Wo", cg * GD)], writes=[("psM", pi)])
                        S.add("act", lambda e, pi=pi, s=s, cg=cg: e.activation(
                            out=junk[:, 0:GD], in_=psM[pi][:, 0:GD], func=AF.Square, accum_out=st3[:, s, cg:cg + 1]),
                            reads=[("psM", pi)], writes=["junk", ("st3", s)])
                        S.add("dve", lambda e, pi=pi, s=s, cg=cg: e.tensor_copy(
                            out=tt2[s % 2][:, cg * GD:(cg + 1) * GD], in_=psM[pi][:, 0:GD]),
                            reads=[("psM", pi)], writes=[("tt", s % 2)])
                    S.add("dve", lambda e, s=s: e.tensor_reduce(out=st3[:, s, 8:9], in_=st3[:, s, 0:ncg], axis=AX.X,
                                                                op=ALU.add), reads=[("st3", s)], writes=[("st3", s)])
                    S.add("act", lambda e, s=s: e.activation(out=st3[:, s, 9:10], in_=st3[:, s, 8:9], func=AF.Sqrt,
                                                             bias=epsc[:], scale=1.0 / D),
                          reads=[("st3", s), "epsc"], writes=[("st3", s)])
                    S.add("dve", lambda e, s=s: e.reciprocal(out=st3[:, s, 10:11], in_=st3[:, s, 9:10]),
                          reads=[("st3", s)], writes=[("st3", s)])
                    S.add("dve", lambda e, s=s: e.scalar_tensor_tensor(
                        out=tt2[s % 2][:], in0=tt2[s % 2][:], scalar=st3[:, s, 10:11], in1=gpost[:],
                        op0=ALU.mult, op1=ALU.mult),
                        reads=[("tt", s % 2), ("st3", s), "gpost"], writes=[("tt", s % 2)])
                    S.add("dve", lambda e, xi=xi, s=s: e.tensor_tensor(out=xs2[xi][:], in0=tt2[s % 2][:], in1=xs2[xi][:],
                                                                     op=ALU.add),
                          reads=[("tt", s % 2), ("xs2", xi)], writes=[("xs2", xi)])
                    S.add("sp", lambda e, s=s, xi=xi: e.dma_start(out=x1d[s * 128:(s + 1) * 128, :], in_=xs2[xi][:]),
                          reads=[("xs2", xi)], dma=True)
                    S.add("dve", lambda e, s=s, xi=xi: e.scalar_tensor_tensor(
                        out=hb2[xi][:], in0=xs2[xi][:], scalar=1.0, in1=xs2[xi][:], op0=ALU.mult, op1=ALU.mult,
                        accum_out=st3[:, s, 11:12]), reads=[("xs2", xi)], writes=[("hb2", xi), ("st3", s)])
                    S.add("act", lambda e, s=s: e.activation(out=st3[:, s, 12:13], in_=st3[:, s, 11:12], func=AF.Sqrt,
                                                             bias=epsc[:], scale=1.0 / D),
                          reads=[("st3", s), "epsc"], writes=[("st3", s)])
                    S.add("dve", lambda e, s=s: e.reciprocal(out=st3[:, s, 13:14], in_=st3[:, s, 12:13]),
                          reads=[("st3", s)], writes=[("st3", s)])
                    S.add("dve", lambda e, s=s, xi=xi: e.scalar_tensor_tensor(
                        out=hb2[xi][:], in0=xs2[xi][:], scalar=st3[:, s, 13:14], in1=gpre2[:], op0=ALU.mult,
                        op1=ALU.mult), reads=[("xs2", xi), ("st3", s), "gpre2"], writes=[("hb2", xi)])
                    if s >= 1:
                        h2_tail(s - 1)
                h2_tail(NST - 1)
                for gi in range(NPRE):
                    for k0 in range(0, KC, 8):
                        k1 = min(KC, k0 + 8)
                        S.add("pool", lambda e, gi=gi, k0=k0, k1=k1: e.dma_start(
                            out=Wu[gi][:, k0:k1, :],
                            in_=w_up[k0 * 128:k1 * 128, gi * c.UW:(gi + 1) * c.UW].rearrange("(kc p) c -> p kc c", p=128)),
                            writes=[("Wu", gi)], dma=True)
                S.emit("ph3b")
            if upto < 4:
                return nc

            with contextlib.ExitStack() as ph:
                S = Sched(nc, ss)
                FB, UW = c.FB, c.UW
                FK = FB // 128
                Wd = [T(ph, "Wd%d" % i, [128, FK, GD], BF16) for i in range(2)]
                m = T(ph, "m", [128, NST, D], F32)
                rl = [T(ph, "rl%d" % i, [128, 512], F32) for i in range(2)]
                gpost2 = T(ph, "gpost2", [128, D], F32)
                xs3 = [T(ph, "xs3_%d" % i, [128, D], F32) for i in range(2)]
                st4 = T(ph, "st4", [128, NST, 4], F32)
                psU = [P(ph, "psU%d" % i, [128, 512], F32) for i in range(3)]
                psD = [P(ph, "psD%d" % i, [128, 512], F32) for i in range(3)]
                aTv = lambda par, k: R2[:, par * FK + k, :]
                ncg = D // GD
                S.add("sp", lambda e: e.dma_start(out=gpost2[:], in_=gvec[3, :].partition_broadcast(128)),
                      writes=["gpost2"], dma=True)
                Wu.append(T(ph, "Wu1", [128, KC, UW], BF16))
                cn4 = {"uc": 0, "dc": 0, "wu": 0, "wd": 0, "rk": 0, "pre": NPRE}

                def load_u(fb, u0):
                    col0 = fb * FB + u0
                    wi = cn4["wu"] % 2
                    cn4["wu"] += 1
                    if cn4["pre"] > 0:
                        cn4["pre"] -= 1
                        return wi
                    for k0 in range(0, KC, 8):
                        k1 = min(KC, k0 + 8)
                        S.add("pool", lambda e, k0=k0, k1=k1: e.dma_start(
                            out=Wu[wi][:, k0:k1, :],
                            in_=w_up[k0 * 128:k1 * 128, col0:col0 + UW].rearrange("(kc p) c -> p kc c", p=128)),
                            writes=[("Wu", wi)], dma=True)
                    return wi

                def comp_u(fb, u0, wi):
                    par = fb % 2
                    for f in range(UW // 128):
                        fcl = (u0 + f * 128) // 128
                        for n in range(NN):
                            pi = cn4["uc"] % 3
                            cn4["uc"] += 1
                            for kc in range(KC):
                                S.add("pe", lambda e, pi=pi, kc=kc, f=f, n=n: e.matmul(
                                    psU[pi][:, 0:512], lhsT=Wu[wi][:, kc, f * 128:(f + 1) * 128],
                                    rhs=h2T[:, kc, n * 512:(n + 1) * 512], start=(kc == 0), stop=(kc == KC - 1)),
                                    reads=[("Wu", wi), "h2T"], writes=[("psU", pi)])
                            k = cn4["rk"] % 2
                            cn4["rk"] += 1
                            S.add("act", lambda e, pi=pi, k=k: e.activation(out=rl[k][:], in_=psU[pi][:, 0:512],
                                                                            func=AF.Relu),
                                  reads=[("psU", pi)], writes=[("rl", k)])
                            S.add("pool", lambda e, k=k, fcl=fcl, n=n: e.tensor_tensor(
                                out=aTv(par, fcl)[:, n * 512:(n + 1) * 512], in0=rl[k][:], in1=rl[k][:], op=ALU.mult),
                                reads=[("rl", k)], writes=[("aT", par)])

                def load_d(fb, cg):
                    wi = cn4["wd"] % 2
                    cn4["wd"] += 1
                    S.add("pool", lambda e: e.dma_start(
                        out=Wd[wi][:],
                        in_=w_down[fb * FB:(fb + 1) * FB, cg * GD:(cg + 1) * GD].rearrange("(kc p) c -> p kc c", p=128)),
                        writes=[("Wd", wi)], dma=True)
                    return wi

                def comp_d(fb, cg, wi):
                    par = fb % 2
                    for s in range(NST):
                        pi = cn4["dc"] % 3
                        cn4["dc"] += 1
                        for kc in range(FK):
                            S.add("pe", lambda e, pi=pi, kc=kc, s=s: e.matmul(
                                psD[pi][:, 0:GD], lhsT=aTv(par, kc)[:, s * 128:(s + 1) * 128], rhs=Wd[wi][:, kc, :],
                                start=(kc == 0), stop=(kc == FK - 1)),
                                reads=[("aT", par), ("Wd", wi)], writes=[("psD", pi)])
                        msl = m[:, s, cg * GD:(cg + 1) * GD]
                        if fb == 0:
                            S.add("act", lambda e, pi=pi, msl=msl: e.copy(out=msl, in_=psD[pi][:, 0:GD]),
                                  reads=[("psD", pi)], writes=[("m", s, cg)])
                        else:
                            S.add("dve", lambda e, pi=pi, msl=msl: e.tensor_tensor(out=msl, in0=psD[pi][:, 0:GD],
                                                                                 in1=msl, op=ALU.add),
                                  reads=[("psD", pi), ("m", s, cg)], writes=[("m", s, cg)])

                tasks4 = []
                for fb in range(c.NFB):
                    for u0 in range(0, FB, UW):
                        tasks4.append((load_u, comp_u, fb, u0))
                    for cg in range(ncg):
                        tasks4.append((load_d, comp_d, fb, cg))
                nxt_w = tasks4[0][0](tasks4[0][2], tasks4[0][3])
                for i, (lf_, cf_, a1, a2) in enumerate(tasks4):
                    cur_w = nxt_w
                    if i + 1 < len(tasks4):
                        t2 = tasks4[i + 1]
                        nxt_w = t2[0](t2[2], t2[3])
                    cf_(a1, a2, cur_w)
                for s in range(NST):
                    xi = s % 2
                    mk = [("m", s, cg) for cg in range(ncg)]
                    S.add("pool", lambda e, s=s, xi=xi: e.dma_start(out=xs3[xi][:], in_=x1d[s * 128:(s + 1) * 128, :]),
                          writes=[("xs3", xi)], dma=True)
                    S.add("dve", lambda e, s=s: e.scalar_tensor_tensor(
                        out=junk[:], in0=m[:, s, :], scalar=1.0, in1=m[:, s, :], op0=ALU.mult, op1=ALU.mult,
                        accum_out=st4[:, s, 0:1]), reads=mk, writes=["junk", ("st4", s)])
                    S.add("act", lambda e, s=s: e.activation(out=st4[:, s, 1:2], in_=st4[:, s, 0:1], func=AF.Sqrt,
                                                             bias=epsc[:], scale=1.0 / D),
                          reads=[("st4", s), "epsc"], writes=[("st4", s)])
                    S.add("dve", lambda e, s=s: e.reciprocal(out=st4[:, s, 2:3], in_=st4[:, s, 1:2]),
                          reads=[("st4", s)], writes=[("st4", s)])
                    S.add("dve", lambda e, s=s: e.scalar_tensor_tensor(
                        out=m[:, s, :], in0=m[:, s, :], scalar=st4[:, s, 2:3], in1=gpost2[:], op0=ALU.mult,
                        op1=ALU.mult), reads=mk + [("st4", s), "gpost2"], writes=mk)
                    S.add("dve", lambda e, s=s, xi=xi: e.tensor_tensor(out=xs3[xi][:], in0=m[:, s, :], in1=xs3[xi][:],
                                                                     op=ALU.add),
                          reads=mk + [("xs3", xi)], writes=[("xs3", xi)])
                    S.add("sp", lambda e, s=s, xi=xi: e.dma_start(out=out[s * 128:(s + 1) * 128, :], in_=xs3[xi][:]),
                          reads=[("xs3", xi)], dma=True)
                S.emit("ph4")
            mid3.close()
    return nc


def make_core_inputs(inp, core, cfg=FULL):
    c = cfg
    b, j = divmod(core, c.NCH)
    x = np.asarray(inp["x"])[b]
    CH, NCH = c.CH, c.NCH
    order = [(j - d) % NCH for d in range(NCH)]
    xctx = np.ascontiguousarray(np.concatenate([x[o * CH:(o + 1) * CH] for o in order], 0), dtype=np.float32)
    pos = np.concatenate([np.arange(o * CH, (o + 1) * CH) for o in order]).astype(np.float32)
    inv_freq = (np.float32(ROPE_THETA) ** (-np.arange(0, 32, 2, dtype=np.float32) / np.float32(32))).astype(np.float32)
    ang = (pos[:, None] * inv_freq[None, :]).astype(np.float32)
    valid = [j - d >= 0 for d in range(NCH)]
    vflag = np.array([[0.0 if v else NEG_MASK for v in valid]], np.float32)
    bpc = CH // 256
    gb = np.full((c.NST, c.NB), -1e30, np.float32)
    for qt in range(c.NST):
        bq = qt // 2
        for n in range(c.NB):
            d, i = divmod(n, bpc)
            if (d == 0 and i < bq) or (d >= 1 and valid[d]):
                gb[qt, n] = 0.0
    k = np.arange(128)
    cE = np.zeros((c.NB, c.NB * 128), np.float32)
    for n in range(c.NB):
        cE[n, n * 128:(n + 1) * 128] = 1.0
    sq = lambda a: np.ascontiguousarray(np.asarray(a)[0], dtype=np.float32)
    return {
        "xctx": xctx,
        "w_in": sq(inp["w_in"]), "w_bm": sq(inp["w_branch_moba"]), "w_bf": sq(inp["w_branch_fox"]),
        "w_out": sq(inp["w_out"]), "w_up": sq(inp["w_up"]), "w_down": sq(inp["w_down"]),
        "gvec": np.stack([sq(inp["g_mix_pre"]), sq(inp["g_mix_post"]), sq(inp["g_mlp_pre"]), sq(inp["g_mlp_post"])], 0),
        "bfg": np.asarray(inp["b_forget"], np.float32).reshape(1, c.H),
        "rope_cos": np.cos(ang).astype(np.float32), "rope_sin": np.sin(ang).astype(np.float32),
        "gbias": gb.reshape(1, -1), "vflag": vflag,
        "cident": np.eye(128, dtype=np.float32),
        "ctri": (k[:, None] <= k[None, :]).astype(np.float32),
        "cE": cE,
    }


_NC_CACHE = {}


def kernel(**inputs):
    cfg = FULL
    if "nc" not in _NC_CACHE:
        _NC_CACHE["nc"] = build(cfg)
    nc = _NC_CACHE["nc"]
    n_cores = 2 * cfg.NCH
    shared = None
    in_maps = []
    for core in range(n_cores):
        m = make_core_inputs(inputs, core, cfg)
        if shared is None:
            shared = m
        else:
            for key in m:
                if key not in ("xctx", "rope_cos", "rope_sin", "gbias", "vflag"):
                    m[key] = shared[key]
        in_maps.append(m)
    res = run_bass_kernel_spmd(nc, in_maps, core_ids=list(range(n_cores)))
    B = n_cores // cfg.NCH
    outp = np.empty((B, cfg.TCTX, cfg.D), np.float32)
    for core in range(n_cores):
        b, j = divmod(core, cfg.NCH)
        outp[b, j * cfg.CH:(j + 1) * cfg.CH] = np.asarray(res.results[core]["out"], np.float32)
    return outp
```
